# Optimizing a Trainium2 kernel written in Bass

```python
import math
import jax, jax.numpy as jnp
from jax import lax
import numpy as np

D_MODEL = 1024
BATCH = 16
SEQ = 4096
DEPTH = 2

GRID_W = 64
CTX_LEN = 256
N_AB = (DEPTH + 1) // 2
N_C = DEPTH // 2
F32 = jnp.float32
EPS = 1e-6
HY_D = 768
HY_ORDER = 2
HY_EMB = 33
HY_BANDS = (HY_EMB - 1) // 2
HY_FILT = 64
HY_CONV = 3
HY_DECAY_SHORT = 0.3
HY_DECAY_LONG = 1.5
HY_TARGET = 1e-2
S5_D = 256
S5_GROUP = 16
S5_GROUPS = S5_D // S5_GROUP
S5_STATE = 64
S5_DT_MIN = 1e-3
S5_DT_MAX = 1e-1
C_HEADS = 8
C_HEAD_DIM = 128
C_D = C_HEADS * C_HEAD_DIM
C_CHUNK = 64
MOE_GROUPS = 4
MOE_EPG = 8
MOE_TOPK = 2
MOE_HIDDEN = 256

kernel_name = 'hyena_s5_hgrn2_hmoe_prefix_trunk'


def rmsnorm(x, g):
    x32 = x.astype(F32)
    y = x32 * lax.rsqrt(jnp.mean(x32 * x32, axis=-1, keepdims=True) + EPS)
    return (y * g).astype(x.dtype)


def short_conv(u, w, b):
    y = lax.conv_general_dilated(
        u, w.astype(u.dtype)[:, None, :], window_strides=(1,), padding=((1, 1),),
        dimension_numbers=('NWC', 'WIO', 'NWC'), feature_group_count=u.shape[-1])
    return y + b


def hyena_filter_fft(L, fw1, fb1, ff1, fw2, fb2, ff2, fw3):
    pos = jnp.arange(L, dtype=F32)
    t = pos / max(L - 1, 1)
    w = 2.0 * math.pi * pos / L
    bands = jnp.linspace(1e-4, HY_BANDS - 1, HY_BANDS, dtype=F32)
    ang = w[:, None] * bands[None, :]
    z = jnp.concatenate([t[:, None], jnp.cos(ang), -jnp.sin(ang)], axis=-1)
    hdn = jnp.sin(ff1 * (z @ fw1 + fb1))
    hdn = jnp.sin(ff2 * (hdn @ fw2 + fb2))
    h = (hdn @ fw3).astype(F32).reshape(L, HY_ORDER, 2, HY_D)
    deltas = jnp.abs(jnp.linspace(math.log(HY_TARGET) / HY_DECAY_LONG,
                                  math.log(HY_TARGET) / HY_DECAY_SHORT, HY_D, dtype=F32))
    h = h * jnp.exp(-t[:, None] * deltas)[:, None, None, :]
    k = jnp.concatenate([h[:, :, 0], jnp.zeros((1, HY_ORDER, HY_D), F32), h[:0:-1, :, 1]], axis=0)
    k = k * lax.rsqrt(jnp.sum(k * k, axis=0, keepdims=True))
    return jnp.fft.rfft(k, axis=0)


def fft_conv(z, kf, bias):
    L = z.shape[1]
    zf = jnp.fft.rfft(z, n=2 * L, axis=1)
    return jnp.fft.irfft(zf * kf, n=2 * L, axis=1)[:, :L] + z * bias


def hyena_mix(u3, filt, conv_w, conv_b, bias):
    kf = hyena_filter_fft(u3.shape[1], *filt)
    uc = short_conv(u3, conv_w, conv_b).astype(F32)
    x1, x2, v = jnp.split(uc, 3, axis=-1)
    z = x1 * fft_conv(v, kf[:, 0], bias[0])
    return x2 * fft_conv(z, kf[:, 1], bias[1])


def ssm_binop(e_i, e_j):
    a_i, b_i = e_i
    a_j, b_j = e_j
    return a_j * a_i, a_j * b_i + b_j


def s5_discretise(lam_re, lam_im, log_step, b_re, b_im):
    lam = lax.complex(lam_re.astype(F32), lam_im.astype(F32))
    dt = jnp.exp(log_step.astype(F32))[..., None]
    lam_bar = jnp.exp(lam * dt)
    b_bar = ((lam_bar - 1.0) / lam)[..., None] * lax.complex(b_re.astype(F32), b_im.astype(F32))
    return lam_bar, b_bar


def s5_scan(u, lam_bar, b_bar, s0, reverse):
    bu = jnp.einsum('blgn,gpn->blgp', u.astype(jnp.complex64), b_bar)
    if s0 is not None:
        bu = bu.at[:, -1 if reverse else 0].add(lam_bar * s0)
    a = jnp.broadcast_to(lam_bar, (1, u.shape[1]) + lam_bar.shape)
    return lax.associative_scan(ssm_binop, (a, bu), reverse=reverse, axis=1)[1]


def s5_readout(u, st_f, st_b, c_mat, d, glu_w, glu_b):
    Bsz, L = u.shape[:2]
    y = jnp.real(jnp.einsum('blgp,gnp->blgn', st_f, c_mat[0]) + jnp.einsum('blgp,gnp->blgn', st_b, c_mat[1]))
    y = jax.nn.gelu(y.reshape(Bsz, L, S5_D) + d * u.reshape(Bsz, L, S5_D))
    a, g = jnp.split(y @ glu_w + glu_b, 2, axis=-1)
    return a * jax.nn.sigmoid(g)


def ab_mixer(h_lat, h_ctx, w_in, w_out, filt, conv_w, conv_b, hy_bias, s5p, ctx_out):
    lam_re, lam_im, log_step, b_re, b_im, c_re, c_im, d, glu_w, glu_b = s5p
    lam_bar, b_bar = s5_discretise(lam_re, lam_im, log_step, b_re, b_im)
    c_mat = lax.complex(c_re.astype(F32), c_im.astype(F32))

    def s5_in(p):
        return p[..., -S5_D:].astype(F32).reshape(p.shape[0], p.shape[1], S5_GROUPS, S5_GROUP)

    def mix(p, u, st_f, st_b):
        y = jnp.concatenate([hyena_mix(p[..., :3 * HY_D], filt, conv_w, conv_b, hy_bias),
                             s5_readout(u, st_f, st_b, c_mat, d, glu_w, glu_b)], axis=-1)
        return y.astype(p.dtype) @ w_out

    p_ctx = h_ctx @ (w_in if ctx_out else w_in[:, 3 * HY_D:])
    u_ctx = s5_in(p_ctx)
    st_cf = s5_scan(u_ctx, lam_bar[0], b_bar[0], None, False)
    st_cb = s5_scan(u_ctx, lam_bar[1], b_bar[1], None, True)
    p_lat = h_lat @ w_in
    u_lat = s5_in(p_lat)
    st_lf = s5_scan(u_lat, lam_bar[0], b_bar[0], st_cf[:, -1], False)
    st_lb = s5_scan(u_lat, lam_bar[1], b_bar[1], st_cb[:, 0], True)
    y_lat = mix(p_lat, u_lat, st_lf, st_lb)
    y_ctx = mix(p_ctx, u_ctx, st_cf, st_cb) if ctx_out else None
    return y_lat, y_ctx


def gla_chunk_scan(q, k, v, logf, s0):
    with_output = q is not None
    Bsz, H, L, _ = k.shape
    n = L // C_CHUNK

    def chunks(t):
        return t.reshape(Bsz, H, n, C_CHUNK, t.shape[-1]).transpose(2, 0, 1, 3, 4)

    xs = {'k': chunks(k), 'v': chunks(v), 'g': chunks(logf)}
    if with_output:
        xs['q'] = chunks(q)
    lower = jnp.tril(jnp.ones((C_CHUNK, C_CHUNK), bool))[:, :, None]

    def step(S, xc):
        b = jnp.cumsum(xc['g'], axis=2)
        b_end = b[:, :, -1:, :]
        S_new = (jnp.exp(b_end[:, :, 0, :])[..., None] * S
                 + jnp.einsum('bhsd,bhse->bhde', xc['k'] * jnp.exp(b_end - b), xc['v']))
        if not with_output:
            return S_new, None
        qc = xc['q']
        rel = jnp.where(lower, b[:, :, :, None, :] - b[:, :, None, :, :], -jnp.inf)
        att = jnp.einsum('bhtd,bhsd,bhtsd->bhts', qc, xc['k'], jnp.exp(rel))
        o = (jnp.einsum('bhtd,bhde->bhte', qc * jnp.exp(b), S)
             + jnp.einsum('bhts,bhse->bhte', att, xc['v']))
        return S_new, o

    S_fin, o = lax.scan(step, s0, xs)
    if with_output:
        o = o.transpose(1, 2, 0, 3, 4).reshape(Bsz, H, L, -1)
    return o, S_fin


def bidir_gla(q, v, f_fwd, f_bwd, s0_fwd, s0_bwd):
    def flip(t):
        return None if t is None else jnp.flip(t, axis=2)
    o_f, s_f = gla_chunk_scan(q, 1.0 - f_fwd, v, jnp.log(f_fwd), s0_fwd)
    o_b, s_b = gla_chunk_scan(flip(q), flip(1.0 - f_bwd), flip(v), flip(jnp.log(f_bwd)), s0_bwd)
    o = None if q is None else o_f + flip(o_b)
    return o, s_f, s_b


def hgrn2_mixer(h_lat, h_ctx, w_in, w_out, lb, norm_g, ctx_out):
    def heads(t):
        return t.astype(F32).reshape(t.shape[0], t.shape[1], C_HEADS, C_HEAD_DIM).transpose(0, 2, 1, 3)

    def recur_inputs(p):
        i, z_f, z_b = jnp.split(p, 3, axis=-1)
        f_f = lb[0] + (1.0 - lb[0]) * jax.nn.sigmoid(z_f.astype(F32))
        f_b = lb[1] + (1.0 - lb[1]) * jax.nn.sigmoid(z_b.astype(F32))
        return heads(i), heads(f_f), heads(f_b)

    def readout(o, g):
        o = o * lax.rsqrt(jnp.mean(o * o, axis=-1, keepdims=True) + EPS)
        o = o.transpose(0, 2, 1, 3).reshape(g.shape[0], g.shape[1], C_D) * norm_g
        return (o * jax.nn.sigmoid(g.astype(F32))).astype(g.dtype) @ w_out

    zeros = jnp.zeros((h_ctx.shape[0], C_HEADS, C_HEAD_DIM, C_HEAD_DIM), F32)
    if ctx_out:
        p_ctx = h_ctx @ w_in
        q_c = heads(jax.nn.silu(p_ctx[..., :C_D]))
    else:
        p_ctx = h_ctx @ w_in[:, 2 * C_D:]
        q_c = None
    v_c, f_fc, f_bc = recur_inputs(p_ctx[..., -3 * C_D:])
    o_c, s_f, s_b = bidir_gla(q_c, v_c, f_fc, f_bc, zeros, zeros)
    p_lat = h_lat @ w_in
    v_l, f_fl, f_bl = recur_inputs(p_lat[..., 2 * C_D:])
    o_l, _, _ = bidir_gla(heads(jax.nn.silu(p_lat[..., :C_D])), v_l, f_fl, f_bl, s_f, s_b)
    y_lat = readout(o_l, p_lat[..., C_D:2 * C_D])
    y_ctx = readout(o_c, p_ctx[..., C_D:2 * C_D]) if ctx_out else None
    return y_lat, y_ctx


def hier_moe(h, wg, bg, we, be, w_gate, w_up, w_down):
    g_prob = jax.nn.softmax((h @ wg + bg).astype(F32), axis=-1)
    p_top, g_idx = lax.top_k(g_prob, 1)
    e_logits = (h @ we + be).astype(F32).reshape(h.shape[0], h.shape[1], MOE_GROUPS, MOE_EPG)
    e_in = jnp.take_along_axis(e_logits, g_idx[..., None], axis=-2)[..., 0, :]
    e_val, e_idx = lax.top_k(e_in, MOE_TOPK)
    e_w = jax.nn.softmax(e_val, axis=-1) * p_top
    within = jnp.einsum('blk,blke->ble', e_w, jax.nn.one_hot(e_idx, MOE_EPG, dtype=F32))
    gate = jax.nn.one_hot(g_idx[..., 0], MOE_GROUPS, dtype=F32)[..., :, None] * within[..., None, :]
    out = jnp.zeros(h.shape, F32)
    for g in range(MOE_GROUPS):
        a = jnp.einsum('bld,edh->bleh', h, w_gate[g])
        u = jnp.einsum('bld,edh->bleh', h, w_up[g])
        out = out + jnp.einsum('bleh,ehd,ble->bld', jax.nn.silu(a) * u, w_down[g], gate[..., g, :])
    return out.astype(h.dtype)


def setup_inputs(seed: int = 0) -> dict:
    key = jax.random.key(seed)
    keys = iter(jax.random.split(key, 64))

    def nrm(shape, scale):
        return jax.random.normal(next(keys), shape, F32) * scale

    D = D_MODEL
    s5_shape = (N_AB, 2, S5_GROUPS, S5_STATE)
    n_idx = jnp.arange(S5_STATE, dtype=F32)
    return {
        'x': nrm((BATCH, SEQ, D), 1.0),
        'c': nrm((BATCH, D), 1.0),
        'ctx': nrm((BATCH, CTX_LEN, D), 1.0),
        'c_ctx': nrm((D,), 1.0),
        'mod_w': nrm((DEPTH, D, 6 * D), 0.5 * D ** -0.5),
        'mod_b': nrm((DEPTH, 6 * D), 0.02),
        'norm1_g': 1.0 + nrm((DEPTH, D), 0.02),
        'norm2_g': 1.0 + nrm((DEPTH, D), 0.02),
        'final_g': 1.0 + nrm((D,), 0.02),
        'ab_w_in': nrm((N_AB, D, 3 * HY_D + S5_D), D ** -0.5),
        'ab_w_out': nrm((N_AB, HY_D + S5_D, D), (HY_D + S5_D) ** -0.5),
        'hy_conv_w': nrm((N_AB, HY_CONV, 3 * HY_D), HY_CONV ** -0.5),
        'hy_conv_b': nrm((N_AB, 3 * HY_D), 0.02),
        'hy_fw1': nrm((N_AB, HY_EMB, HY_FILT), HY_EMB ** -0.5),
        'hy_fb1': nrm((N_AB, HY_FILT), 0.02),
        'hy_ff1': 1.0 + nrm((N_AB, HY_FILT), 0.02),
        'hy_fw2': nrm((N_AB, HY_FILT, HY_FILT), HY_FILT ** -0.5),
        'hy_fb2': nrm((N_AB, HY_FILT), 0.02),
        'hy_ff2': 1.0 + nrm((N_AB, HY_FILT), 0.02),
        'hy_fw3': nrm((N_AB, HY_FILT, HY_ORDER * 2 * HY_D), HY_FILT ** -0.5),
        'hy_bias': nrm((N_AB, HY_ORDER, HY_D), 0.5),
        's5_lam_re': -0.5 + nrm(s5_shape, 0.01),
        's5_lam_im': math.pi * n_idx + nrm(s5_shape, 0.01),
        's5_log_step': jax.random.uniform(next(keys), (N_AB, 2, S5_GROUPS), F32,
                                          math.log(S5_DT_MIN), math.log(S5_DT_MAX)),
        's5_b_re': nrm(s5_shape + (S5_GROUP,), (2 * S5_GROUP) ** -0.5),
        's5_b_im': nrm(s5_shape + (S5_GROUP,), (2 * S5_GROUP) ** -0.5),
        's5_c_re': nrm((N_AB, 2, S5_GROUPS, S5_GROUP, S5_STATE), S5_STATE ** -0.5),
        's5_c_im': nrm((N_AB, 2, S5_GROUPS, S5_GROUP, S5_STATE), S5_STATE ** -0.5),
        's5_d': nrm((N_AB, S5_D), 1.0),
        's5_glu_w': nrm((N_AB, S5_D, 2 * S5_D), S5_D ** -0.5),
        's5_glu_b': nrm((N_AB, 2 * S5_D), 0.02),
        'c_w_in': nrm((N_C, D, 5 * C_D), D ** -0.5),
        'c_w_out': nrm((N_C, C_D, D), C_D ** -0.5),
        'c_lower_bounds': nrm((2, DEPTH, C_D), 0.1),
        'c_norm_g': 1.0 + nrm((N_C, C_D), 0.02),
        'moe_wg': nrm((DEPTH, D, MOE_GROUPS), D ** -0.5),
        'moe_bg': nrm((DEPTH, MOE_GROUPS), 0.01),
        'moe_we': nrm((DEPTH, D, MOE_GROUPS * MOE_EPG), D ** -0.5),
        'moe_be': nrm((DEPTH, MOE_GROUPS * MOE_EPG), 0.01),
        'moe_w_gate': nrm((DEPTH, MOE_GROUPS, MOE_EPG, D, MOE_HIDDEN), D ** -0.5),
        'moe_w_up': nrm((DEPTH, MOE_GROUPS, MOE_EPG, D, MOE_HIDDEN), D ** -0.5),
        'moe_w_down': nrm((DEPTH, MOE_GROUPS, MOE_EPG, MOE_HIDDEN, D), MOE_HIDDEN ** -0.5),
    }


def reference(x, c, ctx, c_ctx, mod_w, mod_b, norm1_g, norm2_g, final_g,
              ab_w_in, ab_w_out, hy_conv_w, hy_conv_b, hy_fw1, hy_fb1, hy_ff1,
              hy_fw2, hy_fb2, hy_ff2, hy_fw3, hy_bias,
              s5_lam_re, s5_lam_im, s5_log_step, s5_b_re, s5_b_im, s5_c_re, s5_c_im,
              s5_d, s5_glu_w, s5_glu_b,
              c_w_in, c_w_out, c_lower_bounds, c_norm_g,
              moe_wg, moe_bg, moe_we, moe_be, moe_w_gate, moe_w_up, moe_w_down):
    silu_c = jax.nn.silu(c)
    silu_cc = jax.nn.silu(c_ctx)
    sm = jax.nn.softmax(c_lower_bounds.astype(F32), axis=1)
    lower_bounds = jnp.cumsum(sm, axis=1) - sm[:, :1]
    for l in range(DEPTH):
        j = l // 2
        ctx_out = l < DEPTH - 1
        n_mod = 6 if ctx_out else 2
        m = jnp.split((silu_c @ mod_w[l] + mod_b[l])[:, None, :], 6, axis=-1)
        mc = jnp.split(silu_cc @ mod_w[l][:, :n_mod * D_MODEL] + mod_b[l][:n_mod * D_MODEL], n_mod)
        h = rmsnorm(x, norm1_g[l]) * (1.0 + m[1]) + m[0]
        hc = rmsnorm(ctx, norm1_g[l]) * (1.0 + mc[1]) + mc[0]
        if l % 2 == 0:
            filt = (hy_fw1[j], hy_fb1[j], hy_ff1[j], hy_fw2[j], hy_fb2[j], hy_ff2[j], hy_fw3[j])
            s5p = (s5_lam_re[j], s5_lam_im[j], s5_log_step[j], s5_b_re[j], s5_b_im[j],
                   s5_c_re[j], s5_c_im[j], s5_d[j], s5_glu_w[j], s5_glu_b[j])
            y, yc = ab_mixer(h, hc, ab_w_in[j], ab_w_out[j], filt, hy_conv_w[j], hy_conv_b[j],
                             hy_bias[j], s5p, ctx_out)
        else:
            y, yc = hgrn2_mixer(h, hc, c_w_in[j], c_w_out[j], lower_bounds[:, l], c_norm_g[j], ctx_out)
        moe_p = (moe_wg[l], moe_bg[l], moe_we[l], moe_be[l], moe_w_gate[l], moe_w_up[l], moe_w_down[l])
        x = x + m[2] * y
        x = x + m[5] * hier_moe(rmsnorm(x, norm2_g[l]) * (1.0 + m[4]) + m[3], *moe_p)
        if ctx_out:
            ctx = ctx + mc[2] * yc
            ctx = ctx + mc[5] * hier_moe(rmsnorm(ctx, norm2_g[l]) * (1.0 + mc[4]) + mc[3], *moe_p)
    return rmsnorm(x, final_g)
```

```python
import contextlib
import math
import numpy as np
import concourse.bass as bass
import concourse.mybir as mybir
from concourse.bass_utils import run_bass_kernel_spmd

F32 = mybir.dt.float32
BF16 = mybir.dt.bfloat16
ALU = mybir.AluOpType
AF = mybir.ActivationFunctionType
AX = mybir.AxisListType
AP = bass.AP

NCORES = 8
NB = 2
L = 4096
LC = 256
D = 1024
HY = 768
S5D = 256
EPS = 1e-6
MAGIC = 12582912.0
TWO_PI = 2.0 * math.pi


class Buf:
    __slots__ = ("w", "r", "name")

    def __init__(self, name):
        self.name = name
        self.w = None
        self.r = []


class Tile(Buf):
    __slots__ = ("t",)

    def __init__(self, name, t):
        Buf.__init__(self, name)
        self.t = t

    def __getitem__(self, key):
        return self.t[key]


class DT(Buf):
    __slots__ = ("h", "a", "regions")

    def __init__(self, name, h):
        Buf.__init__(self, name)
        self.h = h
        self.a = h.ap()
        self.regions = {}

    def __getitem__(self, key):
        return self.a[key]

    def reg(self, key):
        b = self.regions.get(key)
        if b is None:
            b = Buf(f"{self.name}:{key}")
            self.regions[key] = b
        return b


class KB:
    ENG = ("pe", "act", "dve", "pool", "sp")

    def __init__(self, nc):
        self.nc = nc
        self.es = contextlib.ExitStack()
        self.eng = {"pe": nc.tensor, "act": nc.scalar, "dve": nc.vector, "pool": nc.gpsimd, "sp": nc.sync}
        self.sems = {}
        self.sem_list = []
        self.cnt = {}
        self.cur = {}
        self.spare = {e: [] for e in self.ENG}
        nsem = {"pe": 9, "act": 3, "dve": 12, "pool": 3, "sp": 1}
        for e in self.ENG:
            for i in range(nsem[e]):
                s = self.es.enter_context(nc.semaphore(f"s_{e}{i}"))
                self.spare[e].append(self._reg_sem(s))
            self.cur[e] = self.spare[e].pop(0)
        self.dring = {}
        for e in ("sp", "pool", "act"):
            ring = []
            for i in range(12):
                s = self.es.enter_context(nc.semaphore(f"d_{e}{i}"))
                ring.append(self._reg_sem(s))
            self.dring[e] = [ring, 0]
        self.waited = {e: {} for e in self.ENG}
        self.bufs = {}
        self.ninstr = 0

    def _reg_sem(self, s):
        self.sem_list.append(s)
        self.cnt[len(self.sem_list) - 1] = 0
        return len(self.sem_list) - 1

    def tile(self, stack, name, shape, dt=F32):
        self.ninstr += 1
        name = f"t{self.ninstr}_" + name
        t = stack.enter_context(self.nc.sbuf_tensor(name, list(shape), dt))
        tl = Tile(name, t)
        self.bufs[name] = tl
        return tl

    def ptile(self, stack, name, shape, dt=F32):
        name = "t_" + name
        t = stack.enter_context(self.nc.psum_tensor(name, list(shape), dt))
        tl = Tile(name, t)
        self.bufs[name] = tl
        return tl

    def dram(self, name, shape, dt=F32, kind="Internal"):
        h = self.nc.dram_tensor(name, list(shape), dt, kind=kind)
        d = DT(name, h)
        self.bufs[name] = d
        return d

    def buf_of(self, ap):
        return self.bufs[ap.tensor.name]

    def _wait(self, e, tok):
        si, val = tok
        if self.waited[e].get(si, 0) >= val:
            return
        self.eng[e].wait_ge(self.sem_list[si], val)
        self.waited[e][si] = val

    def _sync(self, e, rb, wb):
        own = self.cur[e]
        for b in rb:
            if b.w is not None and not (e == "pe" and b.w[0] == own):
                self._wait(e, b.w)
        for b in wb:
            if b.w is not None and not (e == "pe" and b.w[0] == own):
                self._wait(e, b.w)
            for t in b.r:
                if not (e == "pe" and t[0] == own):
                    self._wait(e, t)

    def _mark(self, tok, rb, wb):
        for b in rb:
            b.r.append(tok)
            if len(b.r) > 24:
                best = {}
                for t in b.r:
                    if best.get(t[0], 0) < t[1]:
                        best[t[0]] = t[1]
                b.r = list(best.items())
        for b in wb:
            b.w = tok
            b.r = []

    def op(self, e, fn, outs, ins, rb=None, wb=None):
        if rb is None:
            rb = [self.buf_of(a) for a in ins if isinstance(a, AP)]
        if wb is None:
            wb = [self.buf_of(a) for a in outs if isinstance(a, AP)]
        self._sync(e, rb, wb)
        ins_ = fn()
        si = self.cur[e]
        self.cnt[si] += 1
        ins_.then_inc(self.sem_list[si], 1)
        tok = (si, self.cnt[si])
        self._mark(tok, rb, wb)
        self.ninstr += 1
        return tok

    def dma(self, e, out, in_, rb=None, wb=None, **kw):
        if rb is None:
            rb = [self.buf_of(in_)]
        if wb is None:
            wb = [self.buf_of(out)]
        self._sync(e, rb, wb)
        ring, pos = self.dring[e]
        si = ring[pos % len(ring)]
        self.dring[e][1] = pos + 1
        self._wait(e, (si, self.cnt[si]))
        self.cnt[si] += 16
        self.eng[e].dma_start(out=out, in_=in_, **kw).then_inc(self.sem_list[si], 16)
        tok = (si, self.cnt[si])
        self._mark(tok, rb, wb)
        self.ninstr += 1
        return tok

    def barrier(self):
        toks = [(si, c) for si, c in self.cnt.items() if c > 0]
        for e in self.ENG:
            for t in toks:
                if t[0] == self.cur[e]:
                    continue
                self._wait(e, t)
        for e in self.ENG:
            if self.cnt[self.cur[e]] > 16000 and self.spare[e]:
                self.cur[e] = self.spare[e].pop(0)

    def mm(self, out, lhsT, rhs, start=True, stop=True):
        return self.op("pe", lambda: self.nc.tensor.matmul(out, lhsT, rhs, start=start, stop=stop), [out], [lhsT, rhs])

    def act(self, out, in_, func, bias=0.0, scale=1.0, accum_out=None, e="act"):
        outs = [out] + ([accum_out] if accum_out is not None else [])
        ins = [in_] + [a for a in (bias, scale) if isinstance(a, AP)]
        kw = {}
        if accum_out is not None:
            kw["accum_out"] = accum_out
        return self.op("act", lambda: self.nc.scalar.activation(out, in_, func, bias=bias, scale=scale, **kw), outs, ins)

    def copy(self, e, out, in_):
        if e == "act":
            return self.op("act", lambda: self.nc.scalar.copy(out, in_), [out], [in_])
        return self.op(e, lambda: self.eng[e].tensor_copy(out, in_), [out], [in_])

    def tt(self, e, out, a, b, op):
        return self.op(e, lambda: self.eng[e].tensor_tensor(out, a, b, op), [out], [a, b])

    def ts(self, e, out, a, s1, s2, op0, op1=None, accum_out=None):
        outs = [out] + ([accum_out] if accum_out is not None else [])
        ins = [a] + [s for s in (s1, s2) if isinstance(s, AP)]
        if op1 is None:
            return self.op(e, lambda: self.eng[e].tensor_single_scalar(out, a, s1, op0), outs, ins)
        kw = {}
        if accum_out is not None:
            kw["accum_out"] = accum_out
        return self.op(e, lambda: self.eng[e].tensor_scalar(out, a, s1, s2, op0, op1, **kw), outs, ins)

    def stt(self, e, out, a, s, b, op0, op1):
        ins = [a, b] + ([s] if isinstance(s, AP) else [])
        return self.op(e, lambda: self.eng[e].scalar_tensor_tensor(out, a, s, b, op0, op1), [out], ins)

    def memset(self, e, out, val):
        return self.op(e, lambda: self.eng[e].memset(out, val), [out], [])

    def recip(self, out, in_):
        return self.op("dve", lambda: self.nc.vector.reciprocal(out, in_), [out], [in_])

    def scan(self, e, out, d0, d1, init, op0, op1):
        ins = [d0, d1] + ([init] if isinstance(init, AP) else [])
        return self.op(e, lambda: self.eng[e].tensor_tensor_scan(out, d0, d1, init, op0, op1), [out], ins)


def host_consts():
    c = {}
    c["ident"] = np.eye(128, dtype=np.float32)
    c["antiI"] = np.eye(128, dtype=np.float32)[::-1].copy()
    c["ones"] = np.ones((128, 128), np.float32)
    for nm, Lf in (("lat", L), ("ctx", LC)):
        pos = np.arange(Lf, dtype=np.float32)
        t = pos / np.float32(max(Lf - 1, 1))
        w = np.float32(2.0 * math.pi) * pos / np.float32(Lf)
        bands = np.linspace(1e-4, 15, 16, dtype=np.float32)
        ang = w[:, None] * bands[None, :]
        z = np.concatenate([t[:, None], np.cos(ang), -np.sin(ang)], axis=-1).astype(np.float32)
        c["zT_" + nm] = np.ascontiguousarray(z.T)
        c["trow_" + nm] = np.ascontiguousarray(np.broadcast_to(t[None, :], (128, Lf))).astype(np.float32)
    deltas = np.abs(np.linspace(math.log(1e-2) / 1.5, math.log(1e-2) / 0.3, HY, dtype=np.float32))
    c["hy_ndelta"] = (-deltas).reshape(HY, 1).astype(np.float32)
    cm = np.ones((128, 512), np.float32)
    cm[:, ::64] = 0.0
    c["cmask"] = cm
    c["triu"] = np.triu(np.ones((64, 64), np.float32))
    c["tril"] = np.tril(np.ones((64, 64), np.float32))
    c["iota1"] = np.ascontiguousarray(np.broadcast_to(np.arange(1, 257, dtype=np.float32)[None, :], (128, 256)))
    return c


CONST_SHAPES = {"ident": [128, 128], "antiI": [128, 128], "ones": [128, 128], "zT_lat": [33, L], "zT_ctx": [33, LC],
                "trow_lat": [128, L], "trow_ctx": [128, LC], "hy_ndelta": [HY, 1], "iota1": [128, 256], "cmask": [128, 512], "triu": [64, 64], "tril": [64, 64]}

WEIGHT_SHAPES = {
    "mod_w": [2, 1024, 6144], "mod_b": [2, 6144], "norm1_g": [2, 1024], "norm2_g": [2, 1024], "final_g": [1024],
    "ab_w_in": [1, 1024, 2560], "ab_w_out": [1, 1024, 1024], "hy_conv_w": [1, 3, 2304], "hy_conv_b": [1, 2304],
    "hy_fw1": [1, 33, 64], "hy_fb1": [1, 64], "hy_ff1": [1, 64], "hy_fw2": [1, 64, 64], "hy_fb2": [1, 64],
    "hy_ff2": [1, 64], "hy_fw3": [1, 64, 3072], "hy_bias": [1, 2, 768],
    "s5_lam_re": [1, 2, 16, 64], "s5_lam_im": [1, 2, 16, 64], "s5_log_step": [1, 2, 16],
    "s5_b_re": [1, 2, 16, 64, 16], "s5_b_im": [1, 2, 16, 64, 16], "s5_c_re": [1, 2, 16, 16, 64],
    "s5_c_im": [1, 2, 16, 16, 64], "s5_d": [1, 256], "s5_glu_w": [1, 256, 512], "s5_glu_b": [1, 512],
    "c_w_in": [1, 1024, 5120], "c_w_out": [1, 1024, 1024], "c_lower_bounds": [2, 2, 1024], "c_norm_g": [1, 1024],
    "moe_wg": [2, 1024, 4], "moe_bg": [2, 4], "moe_we": [2, 1024, 32], "moe_be": [2, 32],
    "moe_w_gate": [2, 4, 8, 1024, 256], "moe_w_up": [2, 4, 8, 1024, 256], "moe_w_down": [2, 4, 8, 256, 1024],
}


DBGF = set()


def build(stop=99, dbg=()):
    DBGF.clear()
    DBGF.update(dbg)
    nc = bass.Bass("TRN2", target_bir_lowering=False)
    k = KB(nc)
    I = {}
    I["x"] = k.dram("x", [NB, L, D], kind="ExternalInput")
    I["ctx"] = k.dram("ctx", [NB, LC, D], kind="ExternalInput")
    I["cvec"] = k.dram("cvec", [3, D], kind="ExternalInput")
    for n, s in WEIGHT_SHAPES.items():
        I[n] = k.dram(n, s, kind="ExternalInput")
    for n, s in CONST_SHAPES.items():
        I[n] = k.dram(n, s, kind="ExternalInput")
    OUT = k.dram("out", [NB, L, D], kind="ExternalOutput")

    def scratch(name, shape):
        return k.dram(name, shape, kind=("ExternalOutput" if name in dbg else "Internal"))

    S = {}
    S["MV"] = scratch("MV", [2, 3, 6, D])
    S["PT0"] = scratch("PT0", [NB, 2560, LC + L])
    S["UT0"] = scratch("UT0", [NB, LC + L, 256])
    S["YT0"] = scratch("YT0", [NB, 1024, LC + L])
    S["X1"] = scratch("X1", [NB, L, D])
    S["CTX1"] = scratch("CTX1", [NB, LC, D])
    S["HT1"] = scratch("HT1", [NB, D, TT])
    S["QT1"] = scratch("QT1", [NB, D, L])
    S["ZF1"] = scratch("ZF1", [NB, D, TT])
    S["ZB1"] = scratch("ZB1", [NB, D, TT])
    S["V1"] = scratch("V1", [NB, TT, D])
    S["G1"] = scratch("G1", [NB, L, D])
    S["O1"] = scratch("O1", [2, NB, L, D])
    S["OT1"] = scratch("OT1", [NB, D, L])
    S["DBG"] = scratch("DBG", [16, 128, 4096])
    S["YS5"] = scratch("YS5", [2, NB, LC + L, 256])

    with k.es:
        glob = k.es
        ident = k.tile(glob, "ident", [128, 128])
        antiI = k.tile(glob, "antiI", [128, 128])
        ones = k.tile(glob, "ones", [128, 128])
        k.dma("sp", ident[:], I["ident"][:, :])
        k.dma("sp", antiI[:], I["antiI"][:, :])
        k.dma("sp", ones[:], I["ones"][:, :])
        PS = [k.ptile(glob, f"ps{i}", [128, 512]) for i in range(8)]
        C = dict(ident=ident, antiI=antiI, ones=ones, PS=PS)

        phase_mod(k, I, S, C)
        k.barrier()
        if stop >= 1:
            phase_l0_inproj(k, I, S, C)
            k.barrier()
        if stop >= 2 and "nohy" not in dbg:
            phase_hyena(k, I, S, C)
            k.barrier()
        if stop >= 3:
            phase_s5(k, I, S, C)
            k.barrier()
            phase_s5_out(k, I, S, C)
            k.barrier()
        if stop >= 4:
            tl = l0_moe_tiles(I, S)
            if "moe1" in DBGF:
                tl = tl[:2]
            phase_moe(k, I, S, C, 0, S["YT0"], I["ab_w_out"][0], tl, final=False)
            k.barrier()
        if stop >= 5:
            phase_l1_norm(k, I, S, C)
            k.barrier()
            phase_l1_inproj(k, I, S, C)
            k.barrier()
        if stop >= 6:
            phase_hgrn2(k, I, S, C)
            k.barrier()
            phase_hgrn2_out(k, I, S, C)
            k.barrier()
        if stop >= 7:
            tl = l1_moe_tiles(I, S, OUT)
            if "moe1" in DBGF:
                tl = tl[:1]
            phase_moe(k, I, S, C, 1, S["OT1"], I["c_w_out"][0], tl, final=True)
            k.barrier()
        k.barrier()
    return nc


def phase_mod(k, I, S, C):
    PS = C["PS"]
    with contextlib.ExitStack() as st:
        cT = k.tile(st, "cT", [128, 3, 8])
        sT = k.tile(st, "sT", [128, 3, 8])
        for r in range(3):
            k.dma("sp", cT[:, r, :], I["cvec"][r].rearrange("(k p) -> p k", p=128), allow_slow_non_contiguous=True,
                  wb=[Buf("tmp")])
        k.barrier()
        k.act(sT[:], cT[:], AF.Silu)
        wt = [k.tile(st, f"modw{i}", [128, 8, 512]) for i in range(2)]
        mv = k.tile(st, "mv", [3, 6144])
        bias = k.tile(st, "modb", [3, 6144])
        g1 = k.tile(st, "g1b", [3, 1024])
        g2 = k.tile(st, "g2b", [3, 1024])
        for l in range(2):
            k.dma("sp", bias[:], I["mod_b"][l:l + 1, :].partition_broadcast(3))
            k.dma("sp", g1[:], I["norm1_g"][l:l + 1, :].partition_broadcast(3))
            k.dma("sp", g2[:], I["norm2_g"][l:l + 1, :].partition_broadcast(3))
            wv = I["mod_w"][l].rearrange("(k p) n -> p k n", p=128)
            for j in range(12):
                w = wt[j % 2]
                k.dma("sp" if j % 2 == 0 else "pool", w[:], wv[:, :, j * 512:(j + 1) * 512])
                ps = PS[j % 2]
                for kk in range(8):
                    k.mm(ps[0:3, :], sT[:, :, kk], w[:, kk, :], start=(kk == 0), stop=(kk == 7))
                k.tt("dve", mv[:, j * 512:(j + 1) * 512], ps[0:3, :], bias[:, j * 512:(j + 1) * 512], ALU.add)
            k.stt("dve", mv[:, 1024:2048], mv[:, 1024:2048], 1.0, g1[:], ALU.add, ALU.mult)
            k.stt("dve", mv[:, 4096:5120], mv[:, 4096:5120], 1.0, g2[:], ALU.add, ALU.mult)
            k.dma("sp", S["MV"][l].rearrange("r c d -> r (c d)"), mv[:])


def load_bcast(k, eng, tile_ap, dram_row_ap):
    return k.dma(eng, tile_ap, dram_row_ap.partition_broadcast(tile_ap.shape[0]))


def rms_mod(k, st_tmp, xt, A, sh, out, name):
    sq, ss, rstd = st_tmp
    k.act(sq[:], xt, AF.Square, accum_out=ss[:])
    k.ts("dve", ss[:], ss[:], 1.0 / D, EPS, ALU.mult, ALU.add)
    k.act(ss[:], ss[:], AF.Sqrt)
    k.recip(rstd[:], ss[:])
    k.stt("dve", out, xt, rstd[:, 0:1], A, ALU.mult, ALU.mult)
    k.tt("dve", out, out, sh, ALU.add)


def transpose_1024(k, C, src, dstT, pbase=4):
    PS = C["PS"]
    for half in range(2):
        ps = PS[pbase + half]
        for j in range(4):
            kk = half * 4 + j
            k.mm(ps[:, j * 128:(j + 1) * 128], src[:, kk * 128:(kk + 1) * 128], C["ident"][:], start=True, stop=True)
        k.copy("act", dstT[:, half * 4:(half + 1) * 4, :], ps[:].rearrange("p (j t) -> p j t", j=4))


TT = LC + L


def phase_l0_inproj(k, I, S, C):
    PS = C["PS"]
    with contextlib.ExitStack() as st:
        W = k.tile(st, "Win0", [128, 8, 2560])
        wv = I["ab_w_in"][0].rearrange("(k p) n -> p k n", p=128)
        for kk in range(8):
            k.dma("sp" if kk % 2 == 0 else "pool", W[:, kk, :], wv[:, kk, :], wb=[Buf("tmp")])
        k.barrier()
        A = k.tile(st, "A1", [128, 1024])
        sh = k.tile(st, "sh1", [128, 1024])
        xt = [k.tile(st, f"xt{i}", [128, 1024]) for i in range(2)]
        h = k.tile(st, "h", [128, 1024])
        hT = k.tile(st, "hT", [128, 8, 128])
        po = [k.tile(st, f"po{i}", [128, 20, 128]) for i in range(2)]
        pu = [k.tile(st, f"pu{i}", [128, 256]) for i in range(2)]
        sq = k.tile(st, "sq", [128, 1024])
        ss = k.tile(st, "ss", [128, 1])
        rstd = k.tile(st, "rstd", [128, 1])
        it = 0
        for b in range(NB):
            for (src, row, ntile, toff) in ((I["ctx"], 2, LC // 128, 0), (I["x"], b, L // 128, LC)):
                load_bcast(k, "sp", A[:], S["MV"][0, row, 1:2, :])
                load_bcast(k, "sp", sh[:], S["MV"][0, row, 0:1, :])
                for t in range(ntile):
                    x_ = xt[it % 2]
                    o_ = po[it % 2]
                    u_ = pu[it % 2]
                    k.dma("sp", x_[:], src[b, t * 128:(t + 1) * 128, :])
                    rms_mod(k, (sq, ss, rstd), x_[:], A[:], sh[:], h[:], "l0")
                    transpose_1024(k, C, h, hT)
                    for j4 in range(5):
                        ps = PS[j4 % 4]
                        for jj in range(4):
                            j = j4 * 4 + jj
                            for kk in range(8):
                                k.mm(ps[:, jj * 128:(jj + 1) * 128], W[:, kk, j * 128:(j + 1) * 128], hT[:, kk, :],
                                     start=(kk == 0), stop=(kk == 7))
                        k.copy("act" if j4 % 2 == 0 else "dve", o_[:, j4 * 4:(j4 + 1) * 4, :],
                               ps[:].rearrange("p (j t) -> p j t", j=4))
                    ps = PS[6]
                    for kk in range(8):
                        k.mm(ps[:, 0:256], hT[:, kk, :], W[:, kk, 2304:2560], start=(kk == 0), stop=(kk == 7))
                    k.copy("dve", u_[:], ps[:, 0:256])
                    t0 = toff + t * 128
                    k.dma("pool", S["PT0"][b].rearrange("(j p) t -> p j t", p=128)[:, :, t0:t0 + 128], o_[:],
                          wb=[S["PT0"].reg((b, t0))])
                    k.dma("pool", S["UT0"][b, t0:t0 + 128, :], u_[:], wb=[S["UT0"].reg((b, t0))])
                    it += 1


def sin_rr(k, out, src, tmp_r, tmp_n, pre_scale=None, pre_bias=None):
    if pre_scale is not None:
        k.ts("dve", tmp_r, src, pre_bias, pre_scale, ALU.add, ALU.mult)
        k.ts("dve", tmp_r, tmp_r, 1.0 / TWO_PI, None, ALU.mult)
    else:
        k.ts("dve", tmp_r, src, 1.0 / TWO_PI, None, ALU.mult)
    k.ts("dve", tmp_n, tmp_r, MAGIC, MAGIC, ALU.add, ALU.subtract)
    k.tt("dve", tmp_r, tmp_r, tmp_n, ALU.subtract)
    k.act(out, tmp_r, AF.Sin, scale=TWO_PI)


def phase_hyena(k, I, S, C):
    PS = C["PS"]
    for (T, toff, zname, tname) in ((LC, 0, "zT_ctx", "trow_ctx"), (L, LC, "zT_lat", "trow_lat")):
        if "hyctx" in DBGF and T == L:
            continue
        with contextlib.ExitStack() as st:
            h2 = k.tile(st, "hyh2", [64, T])
            with contextlib.ExitStack() as st2:
                zT = k.tile(st2, "zT", [33, T])
                k.dma("sp", zT[:], I[zname][:, :])
                fw1 = k.tile(st2, "fw1", [33, 64])
                fw2 = k.tile(st2, "fw2", [64, 64])
                k.dma("sp", fw1[:], I["hy_fw1"][0])
                k.dma("sp", fw2[:], I["hy_fw2"][0])
                prm = k.tile(st2, "hyprm", [64, 4])
                for i, n in enumerate(("hy_fb1", "hy_ff1", "hy_fb2", "hy_ff2")):
                    k.dma("sp", prm[:, i:i + 1], I[n][0].rearrange("(p o) -> p o", o=1), wb=[Buf("tmp")])
                k.barrier()
                h1 = k.tile(st2, "hyh1", [64, T])
                tr = k.tile(st2, "hytr", [64, 512])
                tn = k.tile(st2, "hytn", [64, 512])
                nb = min(T, 512)
                for blk in range(T // nb):
                    sl = slice(blk * nb, (blk + 1) * nb)
                    k.mm(PS[0][0:64, 0:nb], fw1[:], zT[:, sl])
                    sin_rr(k, h1[:, sl], PS[0][0:64, 0:nb], tr[:, 0:nb], tn[:, 0:nb], prm[:, 1:2], prm[:, 0:1])
                    k.mm(PS[1][0:64, 0:nb], fw2[:], h1[:, sl])
                    sin_rr(k, h2[:, sl], PS[1][0:64, 0:nb], tr[:, 0:nb], tn[:, 0:nb], prm[:, 3:4], prm[:, 2:3])
                if "DBG" in DBGF and T == LC:
                    k.dma("sp", S["DBG"][8, 0:64, 0:T], h1[:, :], wb=[Buf("x")])
                    k.dma("sp", S["DBG"][9, 0:64, 0:4], prm[:, :], wb=[Buf("x")])
                    k.dma("sp", S["DBG"][10, 0:33, 0:T], zT[:, :], wb=[Buf("x")])
                    k.dma("sp", S["DBG"][10, 64:97, 0:64], fw1[:, :], wb=[Buf("x")])
                k.barrier()
            fw3 = k.tile(st, "fw3", [64, 3072])
            k.dma("sp", fw3[:], I["hy_fw3"][0])
            trow = k.tile(st, "trow", [128, T])
            k.dma("sp", trow[:], I[tname][:, :])
            dec = k.tile(st, "dec", [128, T])
            kfb = k.tile(st, "kfb", [128, NB, T])
            raw = k.tile(st, "raw", [128, NB, T + 2])
            v = k.tile(st, "v", [128, NB, T])
            acc = k.tile(st, "acc", [128, NB, T])
            prm = k.tile(st, "cprm", [128, 3, 8])
            en = k.tile(st, "en", [128, 4])
            k.memset("dve", raw[:], 0.0)
            nb = min(T, 512)

            def load_conv(ct, blk, dst):
                col = blk * HY + ct * 128
                for b in range(NB):
                    k.dma("sp", raw[:, b, 1:T + 1], S["PT0"][b, col:col + 128, toff:toff + T])
                k.ts("dve", dst, raw[:, :, 0:T], prm[:, blk, 0:1], prm[:, blk, 3:4], ALU.mult, ALU.add)
                k.stt("dve", dst, raw[:, :, 1:T + 1], prm[:, blk, 1:2], dst, ALU.mult, ALU.add)
                k.stt("dve", dst, raw[:, :, 2:T + 2], prm[:, blk, 2:3], dst, ALU.mult, ALU.add)

            for ct in range(HY // 128):
                for blk in range(3):
                    col = blk * HY + ct * 128
                    k.dma("sp", prm[:, blk, 0:3], I["hy_conv_w"][0][:, col:col + 128].rearrange("j c -> c j"),
                          allow_slow_non_contiguous=True)
                    k.dma("sp", prm[:, blk, 3:4], I["hy_conv_b"][0][col:col + 128].rearrange("(p o) -> p o", o=1))
                for o in range(2):
                    k.dma("sp", prm[:, 0, 4 + o:5 + o], I["hy_bias"][0][o, ct * 128:(ct + 1) * 128].rearrange("(p o) -> p o", o=1))
                k.dma("sp", prm[:, 0, 6:7], I["hy_ndelta"][ct * 128:(ct + 1) * 128, :])
                k.act(dec[:], trow[:], AF.Exp, scale=prm[:, 0, 6:7])
                load_conv(ct, 2, v[:])
                for o in range(2):
                    for d_ in (0, 1):
                        col = o * 2 * HY + d_ * HY + ct * 128
                        for blk in range(T // nb):
                            sl = slice(blk * nb, (blk + 1) * nb)
                            ps = PS[blk % 4]
                            k.mm(ps[:, 0:nb], fw3[:, col:col + 128], h2[:, sl])
                            k.tt("dve", kfb[:, d_, sl], ps[:, 0:nb], dec[:, sl], ALU.mult)
                    k.memset("dve", kfb[:, 1, 0:1], 0.0)
                    k.act(acc[:, 0, :], kfb[:, 0, :], AF.Square, accum_out=en[:, 0:1])
                    k.act(acc[:, 0, :], kfb[:, 1, :], AF.Square, accum_out=en[:, 1:2])
                    k.tt("dve", en[:, 2:3], en[:, 0:1], en[:, 1:2], ALU.add)
                    k.act(en[:, 2:3], en[:, 2:3], AF.Sqrt)
                    k.recip(en[:, 3:4], en[:, 2:3])
                    k.ts("dve", kfb[:], kfb[:], en[:, 3:4], None, ALU.mult)
                    if "DBG" in DBGF and ct == 0 and T == LC:
                        k.dma("sp", S["DBG"][o * 4 + 0, :, 0:T], kfb[:, 0, :], wb=[Buf("x")])
                        k.dma("sp", S["DBG"][o * 4 + 1, :, 0:T], kfb[:, 1, :], wb=[Buf("x")])
                        k.dma("sp", S["DBG"][o * 4 + 2, :, 0:T], v[:, 0, :], wb=[Buf("x")])
                        k.dma("sp", S["DBG"][o * 4 + 3, 0:64, 0:T], h2[:, :], wb=[Buf("x")])
                    k.ts("dve", acc[:], v[:], kfb[:, 0, 0:1], None, ALU.mult)
                    for lag in range(1, T):
                        k.stt("dve", acc[:, :, lag:T], v[:, :, 0:T - lag], kfb[:, 0, lag:lag + 1], acc[:, :, lag:T],
                              ALU.mult, ALU.add)
                        k.stt("dve", acc[:, :, 0:T - lag], v[:, :, lag:T], kfb[:, 1, lag:lag + 1], acc[:, :, 0:T - lag],
                              ALU.mult, ALU.add)
                    k.stt("dve", acc[:], v[:], prm[:, 0, 4 + o:5 + o], acc[:], ALU.mult, ALU.add)
                    load_conv(ct, o, kfb[:])
                    if o == 0:
                        k.tt("dve", v[:], acc[:], kfb[:], ALU.mult)
                    else:
                        k.tt("dve", acc[:], acc[:], kfb[:], ALU.mult)
                        for b in range(NB):
                            k.dma("pool", S["YT0"][b, ct * 128:(ct + 1) * 128, toff:toff + T], acc[:, b, :],
                                  wb=[S["YT0"].reg((b, ct, toff))])
                    k.barrier()


TC = 256


def phase_s5(k, I, S, C):
    PS = C["PS"]
    ident, antiI = C["ident"], C["antiI"]
    nblk = TT // 128
    for d in range(2):
        with contextlib.ExitStack() as st:
            BTr = [k.tile(st, f"BTr{j}", [128, 128]) for j in range(8)]
            BTi = [k.tile(st, f"BTi{j}", [128, 128]) for j in range(8)]
            Cr = [k.tile(st, f"Cr{j}", [128, 256]) for j in range(8)]
            nCi = [k.tile(st, f"nCi{j}", [128, 256]) for j in range(8)]
            ctab = [k.tile(st, f"ctab{j}", [128, TC]) for j in range(8)]
            stab = [k.tile(st, f"stab{j}", [128, TC]) for j in range(8)]
            rt = [k.tile(st, f"rt{j}", [128, TC]) for j in range(8)]
            carry = [k.tile(st, f"carry{j}", [128, 2]) for j in range(8)]
            iot = k.tile(st, "iot", [128, TC])
            k.dma("sp", iot[:], I["iota1"][:, :])
            with contextlib.ExitStack() as st2:
                sc = k.tile(st2, "s5sc", [128, 24])
                tr = k.tile(st2, "s5tr", [128, TC])
                tn = k.tile(st2, "s5tn", [128, TC])
                bre = k.tile(st2, "s5bre", [128, 16])
                bim = k.tile(st2, "s5bim", [128, 16])
                bb = k.tile(st2, "s5bb", [128, 2, 16])
                t16 = k.tile(st2, "s5t16", [128, 16])
                pad = k.tile(st2, "s5pad", [128, 128])
                for j in range(8):
                    g0 = 2 * j
                    k.dma("sp", sc[:, 0:1], I["s5_lam_re"][0, d, g0:g0 + 2, :].rearrange("g (p o) -> (g p) o", o=1))
                    k.dma("sp", sc[:, 1:2], I["s5_lam_im"][0, d, g0:g0 + 2, :].rearrange("g (p o) -> (g p) o", o=1))
                    for gl in range(2):
                        k.dma("sp", sc[gl * 64:(gl + 1) * 64, 2:3],
                              I["s5_log_step"][0, d:d + 1, g0 + gl:g0 + gl + 1].partition_broadcast(64))
                    k.dma("sp", bre[:], I["s5_b_re"][0, d, g0:g0 + 2].rearrange("g p n -> (g p) n"))
                    k.dma("sp", bim[:], I["s5_b_im"][0, d, g0:g0 + 2].rearrange("g p n -> (g p) n"))
                    k.act(sc[:, 3:4], sc[:, 2:3], AF.Exp)
                    k.tt("dve", sc[:, 4:5], sc[:, 0:1], sc[:, 3:4], ALU.mult)
                    k.tt("dve", sc[:, 5:6], sc[:, 1:2], sc[:, 3:4], ALU.mult)
                    k.act(sc[:, 6:7], sc[:, 4:5], AF.Exp)
                    sin_rr(k, sc[:, 7:8], sc[:, 5:6], tr[:, 0:1], tn[:, 0:1])
                    k.ts("dve", sc[:, 9:10], sc[:, 5:6], math.pi / 2, None, ALU.add)
                    sin_rr(k, sc[:, 8:9], sc[:, 9:10], tr[:, 0:1], tn[:, 0:1])
                    k.tt("dve", sc[:, 10:11], sc[:, 6:7], sc[:, 8:9], ALU.mult)
                    k.tt("dve", sc[:, 11:12], sc[:, 6:7], sc[:, 7:8], ALU.mult)
                    k.ts("dve", sc[:, 12:13], sc[:, 10:11], -1.0, None, ALU.add)
                    k.tt("dve", sc[:, 13:14], sc[:, 0:1], sc[:, 0:1], ALU.mult)
                    k.stt("dve", sc[:, 13:14], sc[:, 1:2], sc[:, 1:2], sc[:, 13:14], ALU.mult, ALU.add)
                    k.recip(sc[:, 14:15], sc[:, 13:14])
                    k.tt("dve", sc[:, 15:16], sc[:, 12:13], sc[:, 0:1], ALU.mult)
                    k.stt("dve", sc[:, 15:16], sc[:, 11:12], sc[:, 1:2], sc[:, 15:16], ALU.mult, ALU.add)
                    k.tt("dve", sc[:, 15:16], sc[:, 15:16], sc[:, 14:15], ALU.mult)
                    k.tt("dve", sc[:, 16:17], sc[:, 12:13], sc[:, 1:2], ALU.mult)
                    k.stt("dve", sc[:, 16:17], sc[:, 11:12], sc[:, 0:1], sc[:, 16:17], ALU.mult, ALU.subtract)
                    k.tt("dve", sc[:, 16:17], sc[:, 16:17], sc[:, 14:15], ALU.mult)
                    k.ts("dve", bb[:, 0, :], bre[:], sc[:, 15:16], None, ALU.mult)
                    k.ts("dve", t16[:], bim[:], sc[:, 16:17], None, ALU.mult)
                    k.tt("dve", bb[:, 0, :], bb[:, 0, :], t16[:], ALU.subtract)
                    k.ts("dve", bb[:, 1, :], bim[:], sc[:, 15:16], None, ALU.mult)
                    k.ts("dve", t16[:], bre[:], sc[:, 16:17], None, ALU.mult)
                    k.tt("dve", bb[:, 1, :], bb[:, 1, :], t16[:], ALU.add)
                    for ri, BT in ((0, BTr), (1, BTi)):
                        k.memset("dve", pad[:], 0.0)
                        for gl in range(2):
                            cg = ((g0 + gl) % 8) * 16
                            k.copy("dve", pad[gl * 64:(gl + 1) * 64, cg:cg + 16], bb[gl * 64:(gl + 1) * 64, ri, :])
                        k.mm(PS[0][:, 0:128], pad[:], ident[:])
                        k.copy("act", BT[j][:], PS[0][:, 0:128])
                    k.memset("dve", Cr[j][:], 0.0)
                    k.memset("dve", nCi[j][:], 0.0)
                    for gl in range(2):
                        g = g0 + gl
                        k.dma("sp", Cr[j][gl * 64:(gl + 1) * 64, g * 16:(g + 1) * 16],
                              I["s5_c_re"][0, d, g].rearrange("n p -> p n"), allow_slow_non_contiguous=True)
                        k.dma("sp", nCi[j][gl * 64:(gl + 1) * 64, g * 16:(g + 1) * 16],
                              I["s5_c_im"][0, d, g].rearrange("n p -> p n"), allow_slow_non_contiguous=True)
                    k.ts("dve", nCi[j][:], nCi[j][:], -1.0, None, ALU.mult)
                    k.ts("dve", tn[:], iot[:], sc[:, 5:6], None, ALU.mult)
                    sin_rr(k, stab[j][:], tn[:], tr[:], tn[:])
                    k.ts("dve", tn[:], iot[:], sc[:, 5:6], math.pi / 2, ALU.mult, ALU.add)
                    sin_rr(k, ctab[j][:], tn[:], tr[:], tn[:])
                    k.memset("dve", rt[j][:], 0.0)
                    k.ts("dve", rt[j][:], rt[j][:], sc[:, 6:7], None, ALU.add)
                k.barrier()
            ut = [k.tile(st, f"s5ut{i}", [128, 256]) for i in range(2)]
            uT = k.tile(st, "s5uT", [128, 2, TC])
            Sre = [k.tile(st, f"Sre{j}", [128, TC]) for j in range(8)]
            Sim = [k.tile(st, f"Sim{j}", [128, TC]) for j in range(8)]
            m1 = k.tile(st, "s5m1", [128, TC])
            m2 = k.tile(st, "s5m2", [128, TC])
            mre = k.tile(st, "s5mre", [128, TC])
            mim = k.tile(st, "s5mim", [128, TC])
            vre = k.tile(st, "s5vre", [128, TC])
            vim = k.tile(st, "s5vim", [128, TC])
            yt = [k.tile(st, f"s5yt{i}", [128, 256]) for i in range(2)]
            y2 = [k.tile(st, f"s5y2{i}", [128, 256]) for i in range(2)]
            revm = antiI if d == 1 else ident

            def row0(m):
                if d == 0:
                    return 128 * m
                return 128 * (1 - m) if m < 2 else 4480 - 128 * m

            it = 0
            for b in range(NB):
                for j in range(8):
                    k.memset("dve", carry[j][:], 0.0)
                for ch in range(TT // TC):
                    for bl in range(2):
                        m = ch * 2 + bl
                        u_ = ut[(it + bl) % 2]
                        k.dma("sp", u_[:], S["UT0"][b, row0(m):row0(m) + 128, :])
                        for hf in range(2):
                            k.mm(PS[0][:, (hf * 2 + bl) * 128:(hf * 2 + bl + 1) * 128], u_[:, hf * 128:(hf + 1) * 128], revm[:])
                    k.copy("act", uT[:], PS[0][:].rearrange("p (h t) -> p h t", h=2))
                    for j in range(8):
                        hf = j // 4
                        pr, pi_ = PS[1 + (j % 2) * 2], PS[2 + (j % 2) * 2]
                        k.mm(pr[:, 0:TC], BTr[j][:], uT[:, hf, :])
                        k.mm(pi_[:, 0:TC], BTi[j][:], uT[:, hf, :])
                        k.tt("dve", m1[:], pr[:, 0:TC], ctab[j][:], ALU.mult)
                        k.tt("dve", m2[:], pi_[:, 0:TC], stab[j][:], ALU.mult)
                        k.tt("pool", mre[:], m1[:], m2[:], ALU.add)
                        k.tt("dve", m1[:], pi_[:, 0:TC], ctab[j][:], ALU.mult)
                        k.tt("dve", m2[:], pr[:, 0:TC], stab[j][:], ALU.mult)
                        k.tt("pool", mim[:], m1[:], m2[:], ALU.subtract)
                        k.scan("dve", vre[:], rt[j][:], mre[:], carry[j][:, 0:1], ALU.mult, ALU.add)
                        k.scan("dve", vim[:], rt[j][:], mim[:], carry[j][:, 1:2], ALU.mult, ALU.add)
                        k.tt("dve", m1[:], vre[:], ctab[j][:], ALU.mult)
                        k.tt("dve", m2[:], vim[:], stab[j][:], ALU.mult)
                        k.tt("pool", Sre[j][:], m1[:], m2[:], ALU.subtract)
                        k.tt("dve", m1[:], vre[:], stab[j][:], ALU.mult)
                        k.tt("dve", m2[:], vim[:], ctab[j][:], ALU.mult)
                        k.tt("pool", Sim[j][:], m1[:], m2[:], ALU.add)
                        k.copy("pool", carry[j][:, 0:1], Sre[j][:, TC - 1:TC])
                        k.copy("pool", carry[j][:, 1:2], Sim[j][:, TC - 1:TC])
                    for bl in range(2):
                        m = ch * 2 + bl
                        py = PS[5 + bl]
                        for j in range(8):
                            k.mm(py[:, 0:256], Sre[j][:, bl * 128:(bl + 1) * 128], Cr[j][:], start=(j == 0), stop=False)
                            k.mm(py[:, 0:256], Sim[j][:, bl * 128:(bl + 1) * 128], nCi[j][:], start=False, stop=(j == 7))
                        y_ = yt[(it + bl) % 2]
                        k.copy("act", y_[:], py[:, 0:256])
                        if d == 1:
                            k.mm(PS[7][:, 0:256], antiI[:], y_[:])
                            y2_ = y2[(it + bl) % 2]
                            k.copy("act", y2_[:], PS[7][:, 0:256])
                            y_ = y2_
                        k.dma("pool", S["YS5"][d, b, row0(m):row0(m) + 128, :], y_[:], wb=[S["YS5"].reg((d, b, m))])
                    it += 1
        k.barrier()


def phase_s5_out(k, I, S, C):
    PS = C["PS"]
    ident = C["ident"]
    with contextlib.ExitStack() as st:
        Db = k.tile(st, "s5D", [128, 256])
        gb = k.tile(st, "s5gb", [128, 512])
        gw = k.tile(st, "s5gw", [128, 2, 512])
        load_bcast(k, "sp", Db[:], I["s5_d"][0:1, :])
        load_bcast(k, "sp", gb[:], I["s5_glu_b"][0:1, :])
        k.dma("sp", gw[:], I["s5_glu_w"][0].rearrange("(h p) n -> p h n", p=128))
        yf = [k.tile(st, f"o5yf{i}", [128, 256]) for i in range(2)]
        yb = [k.tile(st, f"o5yb{i}", [128, 256]) for i in range(2)]
        uu = [k.tile(st, f"o5u{i}", [128, 256]) for i in range(2)]
        y = k.tile(st, "o5y", [128, 256])
        t1 = k.tile(st, "o5t1", [128, 256])
        geT = k.tile(st, "o5geT", [128, 2, 128])
        a = k.tile(st, "o5a", [128, 512])
        o = k.tile(st, "o5o", [128, 256])
        oT = [k.tile(st, f"o5oT{i}", [128, 2, 128]) for i in range(2)]
        it = 0
        for b in range(NB):
            for m in range(TT // 128):
                r0 = m * 128
                i2 = it % 2
                k.dma("sp", yf[i2][:], S["YS5"][0, b, r0:r0 + 128, :])
                k.dma("sp", yb[i2][:], S["YS5"][1, b, r0:r0 + 128, :])
                k.dma("sp", uu[i2][:], S["UT0"][b, r0:r0 + 128, :])
                k.tt("dve", y[:], yf[i2][:], yb[i2][:], ALU.add)
                k.tt("dve", t1[:], uu[i2][:], Db[:], ALU.mult)
                k.tt("dve", y[:], y[:], t1[:], ALU.add)
                k.act(t1[:], y[:], AF.Square)
                k.ts("dve", t1[:], t1[:], 0.044715, 1.0, ALU.mult, ALU.add)
                k.tt("dve", t1[:], t1[:], y[:], ALU.mult)
                k.act(t1[:], t1[:], AF.Sigmoid, scale=2.0 * math.sqrt(2.0 / math.pi))
                k.tt("dve", y[:], y[:], t1[:], ALU.mult)
                for hf in range(2):
                    k.mm(PS[0][:, hf * 128:(hf + 1) * 128], y[:, hf * 128:(hf + 1) * 128], ident[:])
                k.copy("act", geT[:], PS[0][:, 0:256].rearrange("p (h t) -> p h t", h=2))
                for hf in range(2):
                    k.mm(PS[1][:], geT[:, hf, :], gw[:, hf, :], start=(hf == 0), stop=(hf == 1))
                k.tt("dve", a[:], PS[1][:], gb[:], ALU.add)
                k.act(a[:, 256:512], a[:, 256:512], AF.Sigmoid)
                k.tt("dve", o[:], a[:, 0:256], a[:, 256:512], ALU.mult)
                for hf in range(2):
                    k.mm(PS[2][:, hf * 128:(hf + 1) * 128], o[:, hf * 128:(hf + 1) * 128], ident[:])
                k.copy("act", oT[i2][:], PS[2][:, 0:256].rearrange("p (h t) -> p h t", h=2))
                k.dma("pool", S["YT0"][b, 768:1024, r0:r0 + 128].rearrange("(h p) t -> p h t", p=128), oT[i2][:],
                      wb=[S["YT0"].reg(("s5", b, m))])
                it += 1


def phase_moe(k, I, S, C, layer, yT_src, wout, tiles, final):
    PS = C["PS"]
    ident = C["ident"]
    with contextlib.ExitStack() as st:
        Wo = k.tile(st, "Wo", [128, 8, 1024])
        k.dma("sp", Wo[:], wout.rearrange("(k p) n -> p k n", p=128))
        Wr = k.tile(st, "Wr", [128, 8, 36])
        k.dma("sp", Wr[:, :, 0:4], I["moe_wg"][layer].rearrange("(k p) g -> p k g", p=128), allow_slow_non_contiguous=True,
              wb=[Buf("x")])
        k.dma("sp", Wr[:, :, 4:36], I["moe_we"][layer].rearrange("(k p) g -> p k g", p=128), allow_slow_non_contiguous=True,
              wb=[Buf("x")])
        rb = k.tile(st, "rbias", [128, 36])
        k.dma("sp", rb[:, 0:4], I["moe_bg"][layer:layer + 1, :].partition_broadcast(128), wb=[Buf("x")])
        k.dma("sp", rb[:, 4:36], I["moe_be"][layer:layer + 1, :].partition_broadcast(128), wb=[Buf("x")])
        fg = k.tile(st, "fg", [128, 1024])
        if final:
            load_bcast(k, "sp", fg[:], I["final_g"].a.rearrange("(o d) -> o d", o=1))
        k.barrier()
        mvt = [k.tile(st, f"mv{i}", [128, 1024]) for i in range(4)]
        yT = k.tile(st, "yT", [128, 8, 512])
        xa = [k.tile(st, f"xa{i}", [128, 1024]) for i in range(4)]
        acc = [k.tile(st, f"acc{i}", [128, 1024]) for i in range(4)]
        gate = [k.tile(st, f"gate{i}", [128, 32]) for i in range(4)]
        xin = k.tile(st, "xin", [128, 1024])
        h = k.tile(st, "hm", [128, 1024])
        hT = k.tile(st, "hTm", [128, 8, 512])
        sq = k.tile(st, "sqm", [128, 1024])
        ss = k.tile(st, "ssm", [128, 1])
        rstd = k.tile(st, "rstdm", [128, 1])
        r_ = k.tile(st, "rt", [128, 64])
        lg = k.tile(st, "lg", [128, 36])
        Wg = [k.tile(st, f"Wg{i}", [128, 8, 256]) for i in range(2)]
        Wu = [k.tile(st, f"Wu{i}", [128, 8, 256]) for i in range(2)]
        Wd = [k.tile(st, f"Wd{i}", [128, 2, 1024]) for i in range(2)]
        sl_ = [k.tile(st, f"sil{i}", [128, 512]) for i in range(2)]
        hid = [k.tile(st, f"hid{i}", [128, 512]) for i in range(2)]
        cur_row = None
        ecount = 0
        for tl in tiles:
            if tl["row"] != cur_row:
                cur_row = tl["row"]
                for i, comp in enumerate((2, 3, 4, 5)):
                    load_bcast(k, "sp", mvt[i][:], S["MV"][layer, cur_row, comp:comp + 1, :])
            c0 = 0
            for (ap_, n) in tl["ysrc"]:
                k.dma("sp", yT[:, :, c0:c0 + n], ap_.rearrange("(k p) t -> p k t", p=128), wb=[Buf("x")])
                c0 += n
            k.barrier()
            for ts in range(4):
                k.dma("sp", xin[:], tl["xsrc"][ts])
                for half in range(2):
                    ps = PS[half]
                    for kk in range(8):
                        k.mm(ps[:], yT[:, kk, ts * 128:(ts + 1) * 128], Wo[:, kk, half * 512:(half + 1) * 512],
                             start=(kk == 0), stop=(kk == 7))
                    k.tt("dve", xa[ts][:, half * 512:(half + 1) * 512], ps[:], mvt[0][:, half * 512:(half + 1) * 512], ALU.mult)
                k.tt("dve", xa[ts][:], xa[ts][:], xin[:], ALU.add)
                rms_mod(k, (sq, ss, rstd), xa[ts][:], mvt[2][:], mvt[1][:], h[:], "m")
                for hf in range(2):
                    ps = PS[2 + hf]
                    for j in range(4):
                        kk = hf * 4 + j
                        k.mm(ps[:, j * 128:(j + 1) * 128], h[:, kk * 128:(kk + 1) * 128], ident[:])
                    k.copy("act", hT[:, hf * 4:(hf + 1) * 4, ts * 128:(ts + 1) * 128], ps[:].rearrange("p (j t) -> p j t", j=4))
                ps = PS[4]
                for kk in range(8):
                    k.mm(ps[:, 0:36], hT[:, kk, ts * 128:(ts + 1) * 128], Wr[:, kk, :], start=(kk == 0), stop=(kk == 7))
                k.tt("dve", lg[:], ps[:, 0:36], rb[:], ALU.add)
                g_ = gate[ts]
                k.op("dve", lambda: k.nc.vector.reduce_max(r_[:, 0:1], lg[:, 0:4], AX.X), [r_[:, 0:1]], [lg[:, 0:4]])
                k.ts("dve", r_[:, 1:2], r_[:, 0:1], -1.0, None, ALU.mult)
                k.act(r_[:, 4:8], lg[:, 0:4], AF.Exp, bias=r_[:, 1:2], accum_out=r_[:, 2:3])
                k.recip(r_[:, 3:4], r_[:, 2:3])
                k.ts("dve", r_[:, 8:12], lg[:, 0:4], r_[:, 0:1], None, ALU.is_ge)
                k.ts("dve", r_[:, 16:24], lg[:, 4:12], r_[:, 8:9], None, ALU.mult)
                for g in range(1, 4):
                    k.stt("dve", r_[:, 16:24], lg[:, 4 + 8 * g:12 + 8 * g], r_[:, 8 + g:9 + g], r_[:, 16:24], ALU.mult, ALU.add)
                k.op("dve", lambda: k.nc.vector.reduce_max(r_[:, 12:13], r_[:, 16:24], AX.X), [r_[:, 12:13]], [r_[:, 16:24]])
                k.ts("dve", r_[:, 24:32], r_[:, 16:24], r_[:, 12:13], None, ALU.is_ge)
                k.stt("dve", r_[:, 32:40], r_[:, 24:32], -1e30, r_[:, 16:24], ALU.mult, ALU.add)
                k.op("dve", lambda: k.nc.vector.reduce_max(r_[:, 13:14], r_[:, 32:40], AX.X), [r_[:, 13:14]], [r_[:, 32:40]])
                k.ts("dve", r_[:, 40:48], r_[:, 32:40], r_[:, 13:14], None, ALU.is_ge)
                k.tt("dve", r_[:, 14:15], r_[:, 13:14], r_[:, 12:13], ALU.subtract)
                k.act(r_[:, 14:15], r_[:, 14:15], AF.Exp)
                k.ts("dve", r_[:, 14:15], r_[:, 14:15], 1.0, None, ALU.add)
                k.recip(r_[:, 15:16], r_[:, 14:15])
                k.tt("dve", r_[:, 48:49], r_[:, 15:16], r_[:, 3:4], ALU.mult)
                k.tt("dve", r_[:, 49:50], r_[:, 3:4], r_[:, 48:49], ALU.subtract)
                k.ts("dve", r_[:, 50:58], r_[:, 24:32], r_[:, 48:49], None, ALU.mult)
                k.stt("dve", r_[:, 50:58], r_[:, 40:48], r_[:, 49:50], r_[:, 50:58], ALU.mult, ALU.add)
                for g in range(4):
                    k.ts("dve", g_[:, g * 8:(g + 1) * 8], r_[:, 50:58], r_[:, 8 + g:9 + g], None, ALU.mult)
            for e in range(32):
                gi, ei = e // 8, e % 8
                i2 = ecount % 2
                ecount += 1
                k.dma("sp", Wg[i2][:], I["moe_w_gate"][layer, gi, ei].rearrange("(k p) n -> p k n", p=128))
                k.dma("pool", Wu[i2][:], I["moe_w_up"][layer, gi, ei].rearrange("(k p) n -> p k n", p=128))
                k.dma("sp", Wd[i2][:], I["moe_w_down"][layer, gi, ei].rearrange("(k p) n -> p k n", p=128))
                for hc in range(2):
                    pa, pu = PS[hc * 2], PS[hc * 2 + 1]
                    for kk in range(8):
                        k.mm(pa[:], Wg[i2][:, kk, hc * 128:(hc + 1) * 128], hT[:, kk, :], start=(kk == 0), stop=(kk == 7))
                    for kk in range(8):
                        k.mm(pu[:], Wu[i2][:, kk, hc * 128:(hc + 1) * 128], hT[:, kk, :], start=(kk == 0), stop=(kk == 7))
                    k.act(sl_[hc][:], pa[:], AF.Silu)
                    k.tt("dve", hid[hc][:], sl_[hc][:], pu[:], ALU.mult)
                for ts in range(4):
                    for half in range(2):
                        po = PS[4 + (ts * 2 + half) % 4]
                        for hc in range(2):
                            k.mm(po[:], hid[hc][:, ts * 128:(ts + 1) * 128], Wd[i2][:, hc, half * 512:(half + 1) * 512],
                                 start=(hc == 0), stop=(hc == 1))
                        eng = "dve" if half == 0 else "pool"
                        asl = acc[ts][:, half * 512:(half + 1) * 512]
                        if e == 0:
                            k.ts("dve", asl, po[:], gate[ts][:, e:e + 1], None, ALU.mult)
                        else:
                            k.stt("dve", asl, po[:], gate[ts][:, e:e + 1], asl, ALU.mult, ALU.add)
            for ts in range(4):
                k.tt("dve", acc[ts][:], acc[ts][:], mvt[3][:], ALU.mult)
                k.tt("dve", acc[ts][:], acc[ts][:], xa[ts][:], ALU.add)
                if final:
                    k.act(sq[:], acc[ts][:], AF.Square, accum_out=ss[:])
                    k.ts("dve", ss[:], ss[:], 1.0 / D, EPS, ALU.mult, ALU.add)
                    k.act(ss[:], ss[:], AF.Sqrt)
                    k.recip(rstd[:], ss[:])
                    k.stt("dve", acc[ts][:], acc[ts][:], rstd[:, 0:1], fg[:], ALU.mult, ALU.mult)
                k.dma("pool", tl["dst"][ts], acc[ts][:], wb=[Buf("x")])
            k.barrier()


def l0_moe_tiles(I, S):
    tiles = []
    tiles.append(dict(row=2, ysrc=[(S["YT0"][b, :, 0:LC], LC) for b in range(NB)],
                      xsrc=[I["ctx"][b, j * 128:(j + 1) * 128, :] for b in range(NB) for j in range(2)],
                      dst=[S["CTX1"][b, j * 128:(j + 1) * 128, :] for b in range(NB) for j in range(2)]))
    for b in range(NB):
        for i in range(L // 512):
            t0 = i * 512
            tiles.append(dict(row=b, ysrc=[(S["YT0"][b, :, LC + t0:LC + t0 + 512], 512)],
                              xsrc=[I["x"][b, t0 + j * 128:t0 + (j + 1) * 128, :] for j in range(4)],
                              dst=[S["X1"][b, t0 + j * 128:t0 + (j + 1) * 128, :] for j in range(4)]))
    return tiles


def phase_l1_norm(k, I, S, C):
    with contextlib.ExitStack() as st:
        A = k.tile(st, "A1b", [128, 1024])
        sh = k.tile(st, "sh1b", [128, 1024])
        xt = [k.tile(st, f"xtb{i}", [128, 1024]) for i in range(2)]
        h = k.tile(st, "hb", [128, 1024])
        hT = [k.tile(st, f"hTb{i}", [128, 8, 128]) for i in range(2)]
        sq = k.tile(st, "sqb", [128, 1024])
        ss = k.tile(st, "ssb", [128, 1])
        rstd = k.tile(st, "rstdb", [128, 1])
        it = 0
        for b in range(NB):
            for (src, row, ntile, toff) in ((S["CTX1"], 2, LC // 128, 0), (S["X1"], b, L // 128, LC)):
                load_bcast(k, "sp", A[:], S["MV"][1, row, 1:2, :])
                load_bcast(k, "sp", sh[:], S["MV"][1, row, 0:1, :])
                for t in range(ntile):
                    x_ = xt[it % 2]
                    hT_ = hT[it % 2]
                    k.dma("sp", x_[:], src[b, t * 128:(t + 1) * 128, :])
                    rms_mod(k, (sq, ss, rstd), x_[:], A[:], sh[:], h[:], "l1")
                    transpose_1024(k, C, h, hT_)
                    t0 = toff + t * 128
                    k.dma("pool", S["HT1"][b].rearrange("(j p) t -> p j t", p=128)[:, :, t0:t0 + 128], hT_[:],
                          wb=[S["HT1"].reg((b, t0))])
                    it += 1


def phase_l1_inproj(k, I, S, C):
    PS = C["PS"]
    with contextlib.ExitStack() as st:
        W = k.tile(st, "W1g", [128, 8, 1024])
        hT = [k.tile(st, f"hT5{i}", [128, 8, 512]) for i in range(2)]
        of = [k.tile(st, f"of{i}", [128, 8, 512]) for i in range(2)]
        ot = [k.tile(st, f"ot{i}", [128, 4, 1024]) for i in range(2)]
        wv = I["c_w_in"][0].rearrange("(k p) n -> p k n", p=128)
        it = 0
        for cg in range(5):
            k.barrier()
            for kk in range(8):
                k.dma("sp" if kk % 2 == 0 else "pool", W[:, kk, :], wv[:, kk, cg * 1024:(cg + 1) * 1024], wb=[Buf("x")])
            k.barrier()
            for b in range(NB):
                segs = [(LC + i * 512, 512) for i in range(L // 512)]
                if cg >= 2:
                    segs = [(0, LC)] + segs
                for (t0, n) in segs:
                    h_ = hT[it % 2]
                    k.dma("sp", h_[:, :, 0:n], S["HT1"][b].rearrange("(j p) t -> p j t", p=128)[:, :, t0:t0 + n])
                    if cg in (0, 3, 4):
                        o_ = of[it % 2]
                        for j in range(8):
                            ps = PS[j % 4]
                            for kk in range(8):
                                k.mm(ps[:, 0:n], W[:, kk, j * 128:(j + 1) * 128], h_[:, kk, 0:n], start=(kk == 0), stop=(kk == 7))
                            k.copy("act" if j % 2 == 0 else "dve", o_[:, j, 0:n], ps[:, 0:n])
                        if cg == 0:
                            dst = S["QT1"][b].rearrange("(j p) t -> p j t", p=128)[:, :, t0 - LC:t0 - LC + n]
                        else:
                            dst = S["ZF1" if cg == 3 else "ZB1"][b].rearrange("(j p) t -> p j t", p=128)[:, :, t0:t0 + n]
                        k.dma("pool", dst, o_[:, :, 0:n], wb=[Buf("x")])
                    else:
                        o_ = ot[it % 2]
                        for ts in range(n // 128):
                            for half in range(2):
                                ps = PS[4 + (ts * 2 + half) % 4]
                                for kk in range(8):
                                    k.mm(ps[:], h_[:, kk, ts * 128:(ts + 1) * 128], W[:, kk, half * 512:(half + 1) * 512],
                                         start=(kk == 0), stop=(kk == 7))
                                k.copy("act" if half == 0 else "dve", o_[:, ts, half * 512:(half + 1) * 512], ps[:])
                        if cg == 1:
                            dst = S["G1"][b, t0 - LC:t0 - LC + n, :].rearrange("(s p) d -> p s d", p=128)
                        else:
                            dst = S["V1"][b, t0:t0 + n, :].rearrange("(s p) d -> p s d", p=128)
                        k.dma("pool", dst, o_[:, 0:n // 128, :], wb=[Buf("x")])
                    it += 1
        k.barrier()


def phase_hgrn2(k, I, S, C):
    PS = C["PS"]
    ident = C["ident"]
    CH = 64
    with contextlib.ExitStack() as st:
        cmask = k.tile(st, "cmask", [128, 512])
        k.dma("sp", cmask[:], I["cmask"][:, :])
        tri = [k.tile(st, f"tri{d}", [64, 64]) for d in range(2)]
        k.dma("sp", tri[0][:], I["triu"][:, :])
        k.dma("sp", tri[1][:], I["tril"][:, :])
        lbt = k.tile(st, "lbt", [128, 8])
        names = ["z", "f", "lf", "kk", "bc", "bb", "eb", "qt", "kt", "kh", "tmp"]
        T_ = [{n: k.tile(st, f"g{d}{n}", [128, 512]) for n in names} for d in range(2)]
        ebe = [k.tile(st, f"ebe{d}", [128, 8]) for d in range(2)]
        vt = [k.tile(st, f"vt{d}", [64, 8, 128]) for d in range(2)]
        kht = [k.tile(st, f"kht{d}", [64, 8, 128]) for d in range(2)]
        ot = [k.tile(st, f"oo{d}", [64, 8, 128]) for d in range(2)]
        am = [k.tile(st, f"am{d}", [64, 64]) for d in range(2)]
        Sst = [k.tile(st, f"Sst{d}", [128, 128]) for d in range(2)]
        for b in range(NB):
            for hh in range(8):
                hs = slice(hh * 128, (hh + 1) * 128)
                for d in range(2):
                    for l_ in range(2):
                        k.dma("sp", lbt[:, d * 4 + l_:d * 4 + l_ + 1],
                              I["c_lower_bounds"][d, l_, hs].rearrange("(p o) -> p o", o=1))
                    k.tt("dve", lbt[:, d * 4 + 2:d * 4 + 3], lbt[:, d * 4:d * 4 + 1], lbt[:, d * 4 + 1:d * 4 + 2], ALU.subtract)
                    k.act(lbt[:, d * 4 + 2:d * 4 + 3], lbt[:, d * 4 + 2:d * 4 + 3], AF.Sigmoid)
                    k.ts("dve", lbt[:, d * 4 + 3:d * 4 + 4], lbt[:, d * 4 + 2:d * 4 + 3], -1.0, 1.0, ALU.mult, ALU.add)
                    k.memset("dve", Sst[d][:], 0.0)
                segs = {0: [(0, LC, False)] + [(LC + i * 512, 512, True) for i in range(L // 512)],
                        1: [(0, LC, False)] + [(LC + i * 512, 512, True) for i in reversed(range(L // 512))]}
                for si in range(len(segs[0])):
                    for d in range(2):
                        t0, n, is_lat = segs[d][si]
                        nchunk = n // CH
                        Td = T_[d]
                        zsrc = S["ZF1" if d == 0 else "ZB1"]
                        k.dma("sp", Td["z"][:, 0:n], zsrc[b, hs, t0:t0 + n])
                        k.dma("sp", vt[d][:, 0:nchunk, :], S["V1"][b, t0:t0 + n, hs].rearrange("(c s) e -> s c e", s=CH))
                        k.act(Td["f"][:, 0:n], Td["z"][:, 0:n], AF.Sigmoid)
                        k.ts("dve", Td["f"][:, 0:n], Td["f"][:, 0:n], lbt[:, d * 4 + 3:d * 4 + 4], lbt[:, d * 4 + 2:d * 4 + 3],
                             ALU.mult, ALU.add)
                        k.act(Td["lf"][:, 0:n], Td["f"][:, 0:n], AF.Ln)
                        k.ts("dve", Td["kk"][:, 0:n], Td["f"][:, 0:n], -1.0, 1.0, ALU.mult, ALU.add)
                        k.scan("dve", Td["bc"][:, 0:n], cmask[:, 0:n], Td["lf"][:, 0:n], 0.0, ALU.mult, ALU.add)
                        bc3 = Td["bc"][:, 0:n].rearrange("p (c s) -> p c s", s=CH)
                        bend = bc3[:, :, CH - 1:CH]
                        if d == 0:
                            bbv = Td["bc"]
                        else:
                            k.tt("dve", Td["bb"][:, 0:n].rearrange("p (c s) -> p c s", s=CH), bend.to_broadcast([128, nchunk, CH]),
                                 bc3, ALU.subtract)
                            k.tt("dve", Td["bb"][:, 0:n], Td["bb"][:, 0:n], Td["lf"][:, 0:n], ALU.add)
                            bbv = Td["bb"]
                        k.act(ebe[d][:, 0:nchunk], bend.rearrange("p c o -> p (c o)"), AF.Exp)
                        k.act(Td["tmp"][:, 0:n], bbv[:, 0:n], AF.Exp, scale=-1.0)
                        k.tt("dve", Td["kt"][:, 0:n], Td["kk"][:, 0:n], Td["tmp"][:, 0:n], ALU.mult)
                        k.tt("dve", Td["kh"][:, 0:n].rearrange("p (c s) -> p c s", s=CH),
                             Td["kt"][:, 0:n].rearrange("p (c s) -> p c s", s=CH),
                             ebe[d][:, 0:nchunk].unsqueeze(2).to_broadcast([128, nchunk, CH]), ALU.mult)
                        if is_lat:
                            k.dma("sp", Td["z"][:, 0:n], S["QT1"][b, hs, t0 - LC:t0 - LC + n])
                            k.act(Td["eb"][:, 0:n], bbv[:, 0:n], AF.Exp)
                            k.act(Td["qt"][:, 0:n], Td["z"][:, 0:n], AF.Silu)
                            k.tt("dve", Td["qt"][:, 0:n], Td["qt"][:, 0:n], Td["eb"][:, 0:n], ALU.mult)
                        for half in range((nchunk + 3) // 4):
                            ps = PS[d * 4 + 3]
                            nn = min(4, nchunk - half * 4)
                            for c in range(nn):
                                cc = half * 4 + c
                                k.mm(ps[0:64, c * 128:(c + 1) * 128], Td["kh"][:, cc * CH:(cc + 1) * CH], ident[:])
                            k.copy("act", kht[d][:, half * 4:half * 4 + nn, :],
                                   ps[0:64, 0:nn * 128].rearrange("p (c e) -> p c e", e=128))
                        order = range(nchunk) if d == 0 else reversed(range(nchunk))
                        for c in order:
                            cs = slice(c * CH, (c + 1) * CH)
                            if is_lat:
                                pa = PS[d * 4 + 0]
                                k.mm(pa[0:64, 0:64], Td["kt"][:, cs], Td["qt"][:, cs])
                                k.tt("dve", am[d][:], pa[0:64, 0:64], tri[d][:], ALU.mult)
                                po = PS[d * 4 + 1]
                                k.mm(po[0:64, 0:128], am[d][:], vt[d][:, c, :], start=True, stop=False)
                                k.mm(po[0:64, 0:128], Td["qt"][:, cs], Sst[d][:], start=False, stop=True)
                                k.copy("act", ot[d][:, c, :], po[0:64, 0:128])
                            pd = PS[d * 4 + 2]
                            k.mm(pd[:, 0:128], kht[d][:, c, :], vt[d][:, c, :])
                            k.stt("dve", Sst[d][:], Sst[d][:], ebe[d][:, c:c + 1], pd[:, 0:128], ALU.mult, ALU.add)
                        if is_lat:
                            k.dma("pool", S["O1"][d, b, t0 - LC:t0 - LC + n, hs].rearrange("(c s) e -> s c e", s=CH),
                                  ot[d][:, 0:nchunk, :], wb=[Buf("x")])
                k.barrier()


def phase_hgrn2_out(k, I, S, C):
    PS = C["PS"]
    ident = C["ident"]
    with contextlib.ExitStack() as st:
        ng = k.tile(st, "ng", [128, 1024])
        load_bcast(k, "sp", ng[:], I["c_norm_g"][0:1, :])
        of_ = [k.tile(st, f"ro_f{i}", [128, 1024]) for i in range(2)]
        ob_ = [k.tile(st, f"ro_b{i}", [128, 1024]) for i in range(2)]
        gg = [k.tile(st, f"ro_g{i}", [128, 1024]) for i in range(2)]
        o = k.tile(st, "ro_o", [128, 1024])
        sq = k.tile(st, "ro_sq", [128, 1024])
        ss = k.tile(st, "ro_ss", [128, 8])
        oT = [k.tile(st, f"ro_oT{i}", [128, 8, 128]) for i in range(2)]
        it = 0
        for b in range(NB):
            for t in range(L // 128):
                i2 = it % 2
                r0 = t * 128
                k.dma("sp", of_[i2][:], S["O1"][0, b, r0:r0 + 128, :])
                k.dma("sp", ob_[i2][:], S["O1"][1, b, r0:r0 + 128, :])
                k.dma("sp", gg[i2][:], S["G1"][b, r0:r0 + 128, :])
                k.tt("dve", o[:], of_[i2][:], ob_[i2][:], ALU.add)
                k.act(sq[:], o[:], AF.Square)
                k.op("dve", lambda: k.nc.vector.reduce_sum(ss[:], sq[:].rearrange("p (h e) -> p h e", e=128), AX.X),
                     [ss[:]], [sq[:]])
                k.ts("dve", ss[:], ss[:], 1.0 / 128, EPS, ALU.mult, ALU.add)
                k.act(ss[:], ss[:], AF.Sqrt)
                k.recip(ss[:], ss[:])
                k.tt("dve", o[:].rearrange("p (h e) -> p h e", e=128), o[:].rearrange("p (h e) -> p h e", e=128),
                     ss[:].unsqueeze(2).to_broadcast([128, 8, 128]), ALU.mult)
                k.tt("dve", o[:], o[:], ng[:], ALU.mult)
                k.act(gg[i2][:], gg[i2][:], AF.Sigmoid)
                k.tt("dve", o[:], o[:], gg[i2][:], ALU.mult)
                transpose_1024(k, C, o, oT[i2])
                k.dma("pool", S["OT1"][b].rearrange("(j p) t -> p j t", p=128)[:, :, r0:r0 + 128], oT[i2][:], wb=[Buf("x")])
                it += 1


def l1_moe_tiles(I, S, OUT):
    tiles = []
    for b in range(NB):
        for i in range(L // 512):
            t0 = i * 512
            tiles.append(dict(row=b, ysrc=[(S["OT1"][b, :, t0:t0 + 512], 512)],
                              xsrc=[S["X1"][b, t0 + j * 128:t0 + (j + 1) * 128, :] for j in range(4)],
                              dst=[OUT[b, t0 + j * 128:t0 + (j + 1) * 128, :] for j in range(4)]))
    return tiles


_CACHE = {}


def kernel(**inputs):
    x = np.ascontiguousarray(inputs["x"], dtype=np.float32)
    c = np.asarray(inputs["c"], dtype=np.float32)
    ctx = np.ascontiguousarray(inputs["ctx"], dtype=np.float32)
    c_ctx = np.asarray(inputs["c_ctx"], dtype=np.float32)
    if "nc" not in _CACHE:
        _CACHE["nc"] = build()
    nc = _CACHE["nc"]
    consts = host_consts()
    shared = {n: np.ascontiguousarray(inputs[n], dtype=np.float32) for n in WEIGHT_SHAPES}
    shared.update(consts)
    in_maps = []
    for i in range(NCORES):
        m = dict(shared)
        m["x"] = x[i * NB:(i + 1) * NB]
        m["ctx"] = ctx[i * NB:(i + 1) * NB]
        m["cvec"] = np.concatenate([c[i * NB:(i + 1) * NB], c_ctx[None, :]], axis=0)
        in_maps.append(m)
    res = run_bass_kernel_spmd(nc, in_maps, core_ids=list(range(NCORES)))
    return np.concatenate([r["out"] for r in res.results], axis=0)
```

```python
import contextlib
import math
import numpy as np
import concourse.bass as bass
import concourse.mybir as mybir
from concourse.bass_utils import run_bass_kernel_spmd

F32 = mybir.dt.float32
BF16 = mybir.dt.bfloat16
ALU = mybir.AluOpType
AF = mybir.ActivationFunctionType
AX = mybir.AxisListType
AP = bass.AP

NCORES = 8
NB = 2
L = 4096
LC = 256
D = 1024
HY = 768
S5D = 256
EPS = 1e-6
MAGIC = 12582912.0
TWO_PI = 2.0 * math.pi


class Buf:
    __slots__ = ("w", "r", "name")

    def __init__(self, name):
        self.name = name
        self.w = None
        self.r = []


class Tile(Buf):
    __slots__ = ("t", "sub")

    def __init__(self, name, t):
        Buf.__init__(self, name)
        self.t = t
        self.sub = [Buf(f"{name}.{i}") for i in range(4)]

    def __getitem__(self, key):
        return self.t[key]


class DT(Buf):
    __slots__ = ("h", "a", "regions")

    def __init__(self, name, h):
        Buf.__init__(self, name)
        self.h = h
        self.a = h.ap()
        self.regions = {}

    def __getitem__(self, key):
        return self.a[key]

    def reg(self, key):
        b = self.regions.get(key)
        if b is None:
            b = Buf(f"{self.name}:{key}")
            self.regions[key] = b
        return b


class KB:
    ENG = ("pe", "act", "dve", "pool", "sp")

    def __init__(self, nc):
        self.nc = nc
        self.es = contextlib.ExitStack()
        self.eng = {"pe": nc.tensor, "act": nc.scalar, "dve": nc.vector, "pool": nc.gpsimd, "sp": nc.sync}
        self.sems = {}
        self.sem_list = []
        self.cnt = {}
        self.cur = {}
        self.spare = {e: [] for e in self.ENG}
        nsem = {"pe": 9, "act": 3, "dve": 12, "pool": 3, "sp": 1}
        for e in self.ENG:
            for i in range(nsem[e]):
                s = self.es.enter_context(nc.semaphore(f"s_{e}{i}"))
                self.spare[e].append(self._reg_sem(s))
            self.cur[e] = self.spare[e].pop(0)
        self.dring = {}
        for e in ("sp", "pool", "act"):
            ring = []
            for i in range(12):
                s = self.es.enter_context(nc.semaphore(f"d_{e}{i}"))
                ring.append(self._reg_sem(s))
            self.dring[e] = [ring, 0]
        self.waited = {e: {} for e in self.ENG}
        self.bufs = {}
        self.ninstr = 0

    def _reg_sem(self, s):
        self.sem_list.append(s)
        self.cnt[len(self.sem_list) - 1] = 0
        return len(self.sem_list) - 1

    def tile(self, stack, name, shape, dt=F32):
        self.ninstr += 1
        name = f"t{self.ninstr}_" + name
        t = stack.enter_context(self.nc.sbuf_tensor(name, list(shape), dt))
        tl = Tile(name, t)
        self.bufs[name] = tl
        return tl

    def ptile(self, stack, name, shape, dt=F32):
        name = "t_" + name
        t = stack.enter_context(self.nc.psum_tensor(name, list(shape), dt))
        tl = Tile(name, t)
        self.bufs[name] = tl
        return tl

    def dram(self, name, shape, dt=F32, kind="Internal"):
        h = self.nc.dram_tensor(name, list(shape), dt, kind=kind)
        d = DT(name, h)
        self.bufs[name] = d
        return d

    def buf_of(self, ap):
        return self.bufs[ap.tensor.name]

    def _wait(self, e, tok):
        si, val = tok
        if self.waited[e].get(si, 0) >= val:
            return
        self.eng[e].wait_ge(self.sem_list[si], val)
        self.waited[e][si] = val

    def _sync(self, e, rb, wb):
        own = self.cur[e]
        for b in rb:
            if b.w is not None and not (e == "pe" and b.w[0] == own):
                self._wait(e, b.w)
        for b in wb:
            if b.w is not None and not (e == "pe" and b.w[0] == own):
                self._wait(e, b.w)
            for t in b.r:
                if not (e == "pe" and t[0] == own):
                    self._wait(e, t)

    def _mark(self, tok, rb, wb):
        for b in rb:
            b.r.append(tok)
            if len(b.r) > 24:
                best = {}
                for t in b.r:
                    if best.get(t[0], 0) < t[1]:
                        best[t[0]] = t[1]
                b.r = list(best.items())
        for b in wb:
            b.w = tok
            b.r = []

    def op(self, e, fn, outs, ins, rb=None, wb=None):
        if rb is None:
            rb = [self.buf_of(a) for a in ins if isinstance(a, AP)]
        if wb is None:
            wb = [self.buf_of(a) for a in outs if isinstance(a, AP)]
        self._sync(e, rb, wb)
        ins_ = fn()
        si = self.cur[e]
        self.cnt[si] += 1
        ins_.then_inc(self.sem_list[si], 1)
        tok = (si, self.cnt[si])
        self._mark(tok, rb, wb)
        self.ninstr += 1
        return tok

    def dma(self, e, out, in_, rb=None, wb=None, **kw):
        if rb is None:
            rb = [self.buf_of(in_)]
        if wb is None:
            wb = [self.buf_of(out)]
        self._sync(e, rb, wb)
        ring, pos = self.dring[e]
        si = ring[pos % len(ring)]
        self.dring[e][1] = pos + 1
        self._wait(e, (si, self.cnt[si]))
        self.cnt[si] += 16
        self.eng[e].dma_start(out=out, in_=in_, **kw).then_inc(self.sem_list[si], 16)
        tok = (si, self.cnt[si])
        self._mark(tok, rb, wb)
        self.ninstr += 1
        return tok

    def barrier(self):
        toks = [(si, c) for si, c in self.cnt.items() if c > 0]
        for e in self.ENG:
            for t in toks:
                if t[0] == self.cur[e]:
                    continue
                self._wait(e, t)
        for e in self.ENG:
            if self.cnt[self.cur[e]] > 16000 and self.spare[e]:
                self.cur[e] = self.spare[e].pop(0)

    def mm(self, out, lhsT, rhs, start=True, stop=True):
        return self.op("pe", lambda: self.nc.tensor.matmul(out, lhsT, rhs, start=start, stop=stop), [out], [lhsT, rhs])

    def act(self, out, in_, func, bias=0.0, scale=1.0, accum_out=None, e="act"):
        outs = [out] + ([accum_out] if accum_out is not None else [])
        ins = [in_] + [a for a in (bias, scale) if isinstance(a, AP)]
        kw = {}
        if accum_out is not None:
            kw["accum_out"] = accum_out
        return self.op("act", lambda: self.nc.scalar.activation(out, in_, func, bias=bias, scale=scale, **kw), outs, ins)

    def copy(self, e, out, in_):
        if e == "act":
            return self.op("act", lambda: self.nc.scalar.copy(out, in_), [out], [in_])
        return self.op(e, lambda: self.eng[e].tensor_copy(out, in_), [out], [in_])

    def tt(self, e, out, a, b, op):
        return self.op(e, lambda: self.eng[e].tensor_tensor(out, a, b, op), [out], [a, b])

    def ts(self, e, out, a, s1, s2, op0, op1=None, accum_out=None):
        outs = [out] + ([accum_out] if accum_out is not None else [])
        ins = [a] + [s for s in (s1, s2) if isinstance(s, AP)]
        if op1 is None:
            return self.op(e, lambda: self.eng[e].tensor_single_scalar(out, a, s1, op0), outs, ins)
        kw = {}
        if accum_out is not None:
            kw["accum_out"] = accum_out
        return self.op(e, lambda: self.eng[e].tensor_scalar(out, a, s1, s2, op0, op1, **kw), outs, ins)

    def stt(self, e, out, a, s, b, op0, op1):
        ins = [a, b] + ([s] if isinstance(s, AP) else [])
        return self.op(e, lambda: self.eng[e].scalar_tensor_tensor(out, a, s, b, op0, op1), [out], ins)

    def memset(self, e, out, val):
        return self.op(e, lambda: self.eng[e].memset(out, val), [out], [])

    def recip(self, out, in_):
        return self.op("dve", lambda: self.nc.vector.reciprocal(out, in_), [out], [in_])

    def scan(self, e, out, d0, d1, init, op0, op1):
        ins = [d0, d1] + ([init] if isinstance(init, AP) else [])
        return self.op(e, lambda: self.eng[e].tensor_tensor_scan(out, d0, d1, init, op0, op1), [out], ins)


def _cplx_lhsT(W):
    return np.block([[W.real, W.imag], [-W.imag, W.real]])


def fft_tables():
    t = {}
    n1 = np.arange(128)[:, None].astype(np.float64)
    k1 = np.arange(128)[None, :].astype(np.float64)
    ang = 2.0 * np.pi * n1 * k1 / 128.0
    cs, sn = np.cos(ang), np.sin(ang)
    t["ff1"] = np.concatenate([cs, -sn], axis=1).astype(np.float32)
    t["f1d"] = np.concatenate([np.concatenate([cs[:64], -sn[:64]], axis=1),
                               np.concatenate([sn[:64], cs[:64]], axis=1)], axis=0).astype(np.float32)
    n2 = np.arange(64)[:, None].astype(np.float64)
    k2 = np.arange(64)[None, :].astype(np.float64)
    T2 = np.zeros((128, 128, 128), np.float32)
    for k1_ in range(128):
        W = np.exp(-2j * np.pi * n2 * (k1_ + 128.0 * k2) / 8192.0)
        T2[k1_] = _cplx_lhsT(W)
    t["fft_T2"] = T2
    Wi = np.exp(2j * np.pi * np.arange(64)[:, None] * np.arange(64)[None, :] / 64.0)
    G1 = _cplx_lhsT(Wi)
    Q = np.block([[np.zeros((64, 64)), -np.eye(64)], [np.eye(64), np.zeros((64, 64))]])
    t["fft_G1"] = G1.astype(np.float32)
    t["fft_G2"] = (Q.T @ G1).astype(np.float32)
    T4 = np.zeros((64, 2, 128, 128), np.float32)
    kk = np.arange(128)[:, None].astype(np.float64)
    nn = np.arange(64)[None, :].astype(np.float64)
    for n2_ in range(64):
        W = np.exp(2j * np.pi * (n2_ * kk / 8192.0 + nn * kk / 128.0)) / 8192.0
        T4[n2_, 0] = np.concatenate([W.real, W.imag], axis=1)
        T4[n2_, 1] = np.concatenate([-W.imag, W.real], axis=1)
    t["fft_T4"] = T4
    return t


def host_consts():
    c = {}
    c["ident"] = np.eye(128, dtype=np.float32)
    c["antiI"] = np.eye(128, dtype=np.float32)[::-1].copy()
    c["ones"] = np.ones((128, 128), np.float32)
    for nm, Lf in (("lat", L), ("ctx", LC)):
        pos = np.arange(Lf, dtype=np.float32)
        t = pos / np.float32(max(Lf - 1, 1))
        w = np.float32(2.0 * math.pi) * pos / np.float32(Lf)
        bands = np.linspace(1e-4, 15, 16, dtype=np.float32)
        ang = w[:, None] * bands[None, :]
        z = np.concatenate([t[:, None], np.cos(ang), -np.sin(ang)], axis=-1).astype(np.float32)
        c["zT_" + nm] = np.ascontiguousarray(z.T)
        c["trow_" + nm] = np.ascontiguousarray(np.broadcast_to(t[None, :], (128, Lf))).astype(np.float32)
    deltas = np.abs(np.linspace(math.log(1e-2) / 1.5, math.log(1e-2) / 0.3, HY, dtype=np.float32))
    c["hy_ndelta"] = (-deltas).reshape(HY, 1).astype(np.float32)
    c["ndelta_row"] = (-deltas).reshape(1, HY).astype(np.float32)
    tl = (np.arange(L, dtype=np.float32) / np.float32(L - 1)).reshape(32, 128).T
    c["tneg_lat"] = np.ascontiguousarray(tl).astype(np.float32)
    c.update(fft_tables())
    cm = np.ones((128, 512), np.float32)
    cm[:, ::64] = 0.0
    c["cmask"] = cm
    c["triu"] = np.triu(np.ones((64, 64), np.float32))
    c["tril"] = np.tril(np.ones((64, 64), np.float32))
    c["iota1"] = np.ascontiguousarray(np.broadcast_to(np.arange(1, 257, dtype=np.float32)[None, :], (128, 256)))
    return c


CONST_SHAPES = {"ident": [128, 128], "antiI": [128, 128], "ones": [128, 128], "zT_lat": [33, L], "zT_ctx": [33, LC],
                "trow_lat": [128, L], "trow_ctx": [128, LC], "hy_ndelta": [HY, 1], "iota1": [128, 256], "cmask": [128, 512], "triu": [64, 64], "tril": [64, 64],
                "ff1": [128, 256], "f1d": [128, 256], "fft_T2": [128, 128, 128], "fft_G1": [128, 128], "fft_G2": [128, 128],
                "fft_T4": [64, 2, 128, 128], "tneg_lat": [128, 32], "ndelta_row": [1, HY]}

WEIGHT_SHAPES = {
    "mod_w": [2, 1024, 6144], "mod_b": [2, 6144], "norm1_g": [2, 1024], "norm2_g": [2, 1024], "final_g": [1024],
    "ab_w_in": [1, 1024, 2560], "ab_w_out": [1, 1024, 1024], "hy_conv_w": [1, 3, 2304], "hy_conv_b": [1, 2304],
    "hy_fw1": [1, 33, 64], "hy_fb1": [1, 64], "hy_ff1": [1, 64], "hy_fw2": [1, 64, 64], "hy_fb2": [1, 64],
    "hy_ff2": [1, 64], "hy_fw3": [1, 64, 3072], "hy_bias": [1, 2, 768],
    "s5_lam_re": [1, 2, 16, 64], "s5_lam_im": [1, 2, 16, 64], "s5_log_step": [1, 2, 16],
    "s5_b_re": [1, 2, 16, 64, 16], "s5_b_im": [1, 2, 16, 64, 16], "s5_c_re": [1, 2, 16, 16, 64],
    "s5_c_im": [1, 2, 16, 16, 64], "s5_d": [1, 256], "s5_glu_w": [1, 256, 512], "s5_glu_b": [1, 512],
    "c_w_in": [1, 1024, 5120], "c_w_out": [1, 1024, 1024], "c_lower_bounds": [2, 2, 1024], "c_norm_g": [1, 1024],
    "moe_wg": [2, 1024, 4], "moe_bg": [2, 4], "moe_we": [2, 1024, 32], "moe_be": [2, 32],
    "moe_w_gate": [2, 4, 8, 1024, 256], "moe_w_up": [2, 4, 8, 1024, 256], "moe_w_down": [2, 4, 8, 256, 1024],
}


DBGF = set()
USE_FFT = True


def build(stop=99, dbg=()):
    DBGF.clear()
    DBGF.update(dbg)
    nc = bass.Bass("TRN2", target_bir_lowering=False)
    k = KB(nc)
    I = {}
    I["x"] = k.dram("x", [NB, L, D], kind="ExternalInput")
    I["ctx"] = k.dram("ctx", [NB, LC, D], kind="ExternalInput")
    I["cvec"] = k.dram("cvec", [3, D], kind="ExternalInput")
    for n, s in WEIGHT_SHAPES.items():
        I[n] = k.dram(n, s, kind="ExternalInput")
    for n, s in CONST_SHAPES.items():
        I[n] = k.dram(n, s, kind="ExternalInput")
    OUT = k.dram("out", [NB, L, D], kind="ExternalOutput")

    def scratch(name, shape):
        return k.dram(name, shape, kind=("ExternalOutput" if name in dbg else "Internal"))

    S = {}
    S["MV"] = scratch("MV", [2, 3, 6, D])
    S["PT0"] = scratch("PT0", [NB, 2560, LC + L])
    S["UT0"] = scratch("UT0", [NB, LC + L, 256])
    S["YT0"] = scratch("YT0", [NB, 1024, LC + L])
    S["P0"] = scratch("P0", [NB, L + 2, 2304])
    S["KT"] = scratch("KT", [2 * L, 2 * HY])
    S["HB"] = scratch("HB", [L + 128, 2 * HY])
    S["ESC"] = scratch("ESC", [1, 2 * HY])
    S["KF"] = scratch("KF", [2, 128, 128, HY])
    S["FS1"] = scratch("FS1", [2, 64, 128, CC])
    S["FS2"] = scratch("FS2", [2, 64, 128, CC])
    S["YH"] = scratch("YH", [NB, L, HY])
    S["X1"] = scratch("X1", [NB, L, D])
    S["CTX1"] = scratch("CTX1", [NB, LC, D])
    S["HT1"] = scratch("HT1", [NB, D, TT])
    S["QT1"] = scratch("QT1", [NB, D, L])
    S["ZF1"] = scratch("ZF1", [NB, D, TT])
    S["ZB1"] = scratch("ZB1", [NB, D, TT])
    S["V1"] = scratch("V1", [NB, TT, D])
    S["G1"] = scratch("G1", [NB, L, D])
    S["O1"] = scratch("O1", [2, NB, L, D])
    S["OT1"] = scratch("OT1", [NB, D, L])
    S["DBG"] = scratch("DBG", [16, 128, 4096])
    S["YS5"] = scratch("YS5", [2, NB, LC + L, 256])

    with k.es:
        glob = k.es
        ident = k.tile(glob, "ident", [128, 128])
        antiI = k.tile(glob, "antiI", [128, 128])
        ones = k.tile(glob, "ones", [128, 128])
        k.dma("sp", ident[:], I["ident"][:, :])
        k.dma("sp", antiI[:], I["antiI"][:, :])
        k.dma("sp", ones[:], I["ones"][:, :])
        PS = [k.ptile(glob, f"ps{i}", [128, 512]) for i in range(8)]
        C = dict(ident=ident, antiI=antiI, ones=ones, PS=PS)

        phase_mod(k, I, S, C)
        k.barrier()
        if stop >= 1:
            phase_l0_inproj(k, I, S, C)
            k.barrier()
        if stop >= 2 and "nohy" not in dbg:
            phase_hyena(k, I, S, C)
            k.barrier()
            if USE_FFT:
                phase_hyena_fft(k, I, S, C)
                k.barrier()
        if stop >= 3:
            phase_s5(k, I, S, C)
            k.barrier()
            phase_s5_out(k, I, S, C)
            k.barrier()
        if stop >= 4:
            tl = l0_moe_tiles(I, S)
            if "moe1" in DBGF:
                tl = tl[:2]
            phase_moe(k, I, S, C, 0, S["YT0"], I["ab_w_out"][0], tl, final=False)
            k.barrier()
        if stop >= 5:
            phase_l1_norm(k, I, S, C)
            k.barrier()
            phase_l1_inproj(k, I, S, C)
            k.barrier()
        if stop >= 6:
            phase_hgrn2(k, I, S, C)
            k.barrier()
            phase_hgrn2_out(k, I, S, C)
            k.barrier()
        if stop >= 7:
            tl = l1_moe_tiles(I, S, OUT)
            if "moe1" in DBGF:
                tl = tl[:1]
            phase_moe(k, I, S, C, 1, S["OT1"], I["c_w_out"][0], tl, final=True)
            k.barrier()
        k.barrier()
    return nc


def phase_mod(k, I, S, C):
    PS = C["PS"]
    with contextlib.ExitStack() as st:
        cT = k.tile(st, "cT", [128, 3, 8])
        sT = k.tile(st, "sT", [128, 3, 8])
        for r in range(3):
            k.dma("sp", cT[:, r, :], I["cvec"][r].rearrange("(k p) -> p k", p=128), allow_slow_non_contiguous=True,
                  wb=[Buf("tmp")])
        k.barrier()
        k.act(sT[:], cT[:], AF.Silu)
        wt = [k.tile(st, f"modw{i}", [128, 8, 512]) for i in range(2)]
        mv = k.tile(st, "mv", [3, 6144])
        bias = k.tile(st, "modb", [3, 6144])
        g1 = k.tile(st, "g1b", [3, 1024])
        g2 = k.tile(st, "g2b", [3, 1024])
        for l in range(2):
            k.dma("sp", bias[:], I["mod_b"][l:l + 1, :].partition_broadcast(3))
            k.dma("sp", g1[:], I["norm1_g"][l:l + 1, :].partition_broadcast(3))
            k.dma("sp", g2[:], I["norm2_g"][l:l + 1, :].partition_broadcast(3))
            wv = I["mod_w"][l].rearrange("(k p) n -> p k n", p=128)
            for j in range(12):
                w = wt[j % 2]
                k.dma("sp" if j % 2 == 0 else "pool", w[:], wv[:, :, j * 512:(j + 1) * 512])
                ps = PS[j % 2]
                for kk in range(8):
                    k.mm(ps[0:3, :], sT[:, :, kk], w[:, kk, :], start=(kk == 0), stop=(kk == 7))
                k.tt("dve", mv[:, j * 512:(j + 1) * 512], ps[0:3, :], bias[:, j * 512:(j + 1) * 512], ALU.add)
            k.stt("dve", mv[:, 1024:2048], mv[:, 1024:2048], 1.0, g1[:], ALU.add, ALU.mult)
            k.stt("dve", mv[:, 4096:5120], mv[:, 4096:5120], 1.0, g2[:], ALU.add, ALU.mult)
            k.dma("sp", S["MV"][l].rearrange("r c d -> r (c d)"), mv[:])


def load_bcast(k, eng, tile_ap, dram_row_ap):
    return k.dma(eng, tile_ap, dram_row_ap.partition_broadcast(tile_ap.shape[0]))


def rms_mod(k, st_tmp, xt, A, sh, out, name):
    sq, ss, rstd = st_tmp
    k.act(sq[:], xt, AF.Square, accum_out=ss[:])
    k.ts("dve", ss[:], ss[:], 1.0 / D, EPS, ALU.mult, ALU.add)
    k.act(ss[:], ss[:], AF.Sqrt)
    k.recip(rstd[:], ss[:])
    k.stt("dve", out, xt, rstd[:, 0:1], A, ALU.mult, ALU.mult)
    k.tt("dve", out, out, sh, ALU.add)


def transpose_1024(k, C, src, dstT, pbase=4):
    PS = C["PS"]
    for half in range(2):
        ps = PS[pbase + half]
        for j in range(4):
            kk = half * 4 + j
            k.mm(ps[:, j * 128:(j + 1) * 128], src[:, kk * 128:(kk + 1) * 128], C["ident"][:], start=True, stop=True)
        k.copy("act", dstT[:, half * 4:(half + 1) * 4, :], ps[:].rearrange("p (j t) -> p j t", j=4))


TT = LC + L


def phase_l0_inproj(k, I, S, C):
    PS = C["PS"]
    with contextlib.ExitStack() as st:
        W = k.tile(st, "Win0", [128, 8, 2560])
        wv = I["ab_w_in"][0].rearrange("(k p) n -> p k n", p=128)
        for kk in range(8):
            k.dma("sp" if kk % 2 == 0 else "pool", W[:, kk, :], wv[:, kk, :], wb=[Buf("tmp")])
        k.barrier()
        A = k.tile(st, "A1", [128, 1024])
        sh = k.tile(st, "sh1", [128, 1024])
        xt = [k.tile(st, f"xt{i}", [128, 1024]) for i in range(2)]
        h = k.tile(st, "h", [128, 1024])
        hT = k.tile(st, "hT", [128, 8, 128])
        po = [k.tile(st, f"po{i}", [128, 20, 128]) for i in range(2)]
        pu = [k.tile(st, f"pu{i}", [128, 256]) for i in range(2)]
        pt = [k.tile(st, f"pt{i}", [128, 2304]) for i in range(2)]
        zrow = k.tile(st, "zrow", [1, 2304])
        k.memset("dve", zrow[:], 0.0)
        for b in range(NB):
            k.dma("sp", S["P0"][b, 0:1, :], zrow[:], wb=[Buf("x")])
            k.dma("sp", S["P0"][b, L + 1:L + 2, :], zrow[:], wb=[Buf("x")])
        sq = k.tile(st, "sq", [128, 1024])
        ss = k.tile(st, "ss", [128, 1])
        rstd = k.tile(st, "rstd", [128, 1])
        it = 0
        for b in range(NB):
            for (src, row, ntile, toff) in ((I["ctx"], 2, LC // 128, 0), (I["x"], b, L // 128, LC)):
                load_bcast(k, "sp", A[:], S["MV"][0, row, 1:2, :])
                load_bcast(k, "sp", sh[:], S["MV"][0, row, 0:1, :])
                for t in range(ntile):
                    x_ = xt[it % 2]
                    o_ = po[it % 2]
                    u_ = pu[it % 2]
                    k.dma("sp", x_[:], src[b, t * 128:(t + 1) * 128, :])
                    rms_mod(k, (sq, ss, rstd), x_[:], A[:], sh[:], h[:], "l0")
                    transpose_1024(k, C, h, hT)
                    for j4 in range(5):
                        ps = PS[j4 % 4]
                        for jj in range(4):
                            j = j4 * 4 + jj
                            for kk in range(8):
                                k.mm(ps[:, jj * 128:(jj + 1) * 128], W[:, kk, j * 128:(j + 1) * 128], hT[:, kk, :],
                                     start=(kk == 0), stop=(kk == 7))
                        k.copy("act" if j4 % 2 == 0 else "dve", o_[:, j4 * 4:(j4 + 1) * 4, :],
                               ps[:].rearrange("p (j t) -> p j t", j=4))
                    ps = PS[6]
                    for kk in range(8):
                        k.mm(ps[:, 0:256], hT[:, kk, :], W[:, kk, 2304:2560], start=(kk == 0), stop=(kk == 7))
                    k.copy("dve", u_[:], ps[:, 0:256])
                    if toff == LC and USE_FFT:
                        p_ = pt[it % 2]
                        for j in range(5):
                            ps = PS[6 + j % 2]
                            w_ = 512 if j < 4 else 256
                            for kk in range(8):
                                k.mm(ps[:, 0:w_], hT[:, kk, :], W[:, kk, j * 512:j * 512 + w_], start=(kk == 0), stop=(kk == 7))
                            k.copy("act" if j % 2 == 0 else "dve", p_[:, j * 512:j * 512 + w_], ps[:, 0:w_])
                        k.dma("pool", S["P0"][b, 1 + t * 128:1 + (t + 1) * 128, :], p_[:], wb=[Buf("x")])
                    t0 = toff + t * 128
                    k.dma("pool", S["PT0"][b].rearrange("(j p) t -> p j t", p=128)[:, :, t0:t0 + 128], o_[:],
                          wb=[S["PT0"].reg((b, t0))])
                    k.dma("pool", S["UT0"][b, t0:t0 + 128, :], u_[:], wb=[S["UT0"].reg((b, t0))])
                    it += 1


def sin_rr(k, out, src, tmp_r, tmp_n, pre_scale=None, pre_bias=None):
    if pre_scale is not None:
        k.ts("dve", tmp_r, src, pre_bias, pre_scale, ALU.add, ALU.mult)
        k.ts("dve", tmp_r, tmp_r, 1.0 / TWO_PI, None, ALU.mult)
    else:
        k.ts("dve", tmp_r, src, 1.0 / TWO_PI, None, ALU.mult)
    k.ts("dve", tmp_n, tmp_r, MAGIC, MAGIC, ALU.add, ALU.subtract)
    k.tt("dve", tmp_r, tmp_r, tmp_n, ALU.subtract)
    k.act(out, tmp_r, AF.Sin, scale=TWO_PI)


def phase_hyena(k, I, S, C):
    PS = C["PS"]
    for (T, toff, zname, tname) in ((LC, 0, "zT_ctx", "trow_ctx"), (L, LC, "zT_lat", "trow_lat")):
        if ("hyctx" in DBGF or USE_FFT) and T == L:
            continue
        with contextlib.ExitStack() as st:
            h2 = k.tile(st, "hyh2", [64, T])
            with contextlib.ExitStack() as st2:
                zT = k.tile(st2, "zT", [33, T])
                k.dma("sp", zT[:], I[zname][:, :])
                fw1 = k.tile(st2, "fw1", [33, 64])
                fw2 = k.tile(st2, "fw2", [64, 64])
                k.dma("sp", fw1[:], I["hy_fw1"][0])
                k.dma("sp", fw2[:], I["hy_fw2"][0])
                prm = k.tile(st2, "hyprm", [64, 4])
                for i, n in enumerate(("hy_fb1", "hy_ff1", "hy_fb2", "hy_ff2")):
                    k.dma("sp", prm[:, i:i + 1], I[n][0].rearrange("(p o) -> p o", o=1), wb=[Buf("tmp")])
                k.barrier()
                h1 = k.tile(st2, "hyh1", [64, T])
                tr = k.tile(st2, "hytr", [64, 512])
                tn = k.tile(st2, "hytn", [64, 512])
                nb = min(T, 512)
                for blk in range(T // nb):
                    sl = slice(blk * nb, (blk + 1) * nb)
                    k.mm(PS[0][0:64, 0:nb], fw1[:], zT[:, sl])
                    sin_rr(k, h1[:, sl], PS[0][0:64, 0:nb], tr[:, 0:nb], tn[:, 0:nb], prm[:, 1:2], prm[:, 0:1])
                    k.mm(PS[1][0:64, 0:nb], fw2[:], h1[:, sl])
                    sin_rr(k, h2[:, sl], PS[1][0:64, 0:nb], tr[:, 0:nb], tn[:, 0:nb], prm[:, 3:4], prm[:, 2:3])
                if "DBG" in DBGF and T == LC:
                    k.dma("sp", S["DBG"][8, 0:64, 0:T], h1[:, :], wb=[Buf("x")])
                    k.dma("sp", S["DBG"][9, 0:64, 0:4], prm[:, :], wb=[Buf("x")])
                    k.dma("sp", S["DBG"][10, 0:33, 0:T], zT[:, :], wb=[Buf("x")])
                    k.dma("sp", S["DBG"][10, 64:97, 0:64], fw1[:, :], wb=[Buf("x")])
                k.barrier()
            fw3 = k.tile(st, "fw3", [64, 3072])
            k.dma("sp", fw3[:], I["hy_fw3"][0])
            trow = k.tile(st, "trow", [128, T])
            k.dma("sp", trow[:], I[tname][:, :])
            dec = k.tile(st, "dec", [128, T])
            kfb = k.tile(st, "kfb", [128, NB, T])
            raw = k.tile(st, "raw", [128, NB, T + 2])
            v = k.tile(st, "v", [128, NB, T])
            acc = k.tile(st, "acc", [128, NB, T])
            prm = k.tile(st, "cprm", [128, 3, 8])
            en = k.tile(st, "en", [128, 4])
            k.memset("dve", raw[:], 0.0)
            nb = min(T, 512)

            def load_conv(ct, blk, dst):
                col = blk * HY + ct * 128
                for b in range(NB):
                    k.dma("sp", raw[:, b, 1:T + 1], S["PT0"][b, col:col + 128, toff:toff + T])
                k.ts("dve", dst, raw[:, :, 0:T], prm[:, blk, 0:1], prm[:, blk, 3:4], ALU.mult, ALU.add)
                k.stt("dve", dst, raw[:, :, 1:T + 1], prm[:, blk, 1:2], dst, ALU.mult, ALU.add)
                k.stt("dve", dst, raw[:, :, 2:T + 2], prm[:, blk, 2:3], dst, ALU.mult, ALU.add)

            for ct in range(HY // 128):
                for blk in range(3):
                    col = blk * HY + ct * 128
                    k.dma("sp", prm[:, blk, 0:3], I["hy_conv_w"][0][:, col:col + 128].rearrange("j c -> c j"),
                          allow_slow_non_contiguous=True)
                    k.dma("sp", prm[:, blk, 3:4], I["hy_conv_b"][0][col:col + 128].rearrange("(p o) -> p o", o=1))
                for o in range(2):
                    k.dma("sp", prm[:, 0, 4 + o:5 + o], I["hy_bias"][0][o, ct * 128:(ct + 1) * 128].rearrange("(p o) -> p o", o=1))
                k.dma("sp", prm[:, 0, 6:7], I["hy_ndelta"][ct * 128:(ct + 1) * 128, :])
                k.act(dec[:], trow[:], AF.Exp, scale=prm[:, 0, 6:7])
                load_conv(ct, 2, v[:])
                for o in range(2):
                    for d_ in (0, 1):
                        col = o * 2 * HY + d_ * HY + ct * 128
                        for blk in range(T // nb):
                            sl = slice(blk * nb, (blk + 1) * nb)
                            ps = PS[blk % 4]
                            k.mm(ps[:, 0:nb], fw3[:, col:col + 128], h2[:, sl])
                            k.tt("dve", kfb[:, d_, sl], ps[:, 0:nb], dec[:, sl], ALU.mult)
                    k.memset("dve", kfb[:, 1, 0:1], 0.0)
                    k.act(acc[:, 0, :], kfb[:, 0, :], AF.Square, accum_out=en[:, 0:1])
                    k.act(acc[:, 0, :], kfb[:, 1, :], AF.Square, accum_out=en[:, 1:2])
                    k.tt("dve", en[:, 2:3], en[:, 0:1], en[:, 1:2], ALU.add)
                    k.act(en[:, 2:3], en[:, 2:3], AF.Sqrt)
                    k.recip(en[:, 3:4], en[:, 2:3])
                    k.ts("dve", kfb[:], kfb[:], en[:, 3:4], None, ALU.mult)
                    if "DBG" in DBGF and ct == 0 and T == LC:
                        k.dma("sp", S["DBG"][o * 4 + 0, :, 0:T], kfb[:, 0, :], wb=[Buf("x")])
                        k.dma("sp", S["DBG"][o * 4 + 1, :, 0:T], kfb[:, 1, :], wb=[Buf("x")])
                        k.dma("sp", S["DBG"][o * 4 + 2, :, 0:T], v[:, 0, :], wb=[Buf("x")])
                        k.dma("sp", S["DBG"][o * 4 + 3, 0:64, 0:T], h2[:, :], wb=[Buf("x")])
                    k.ts("dve", acc[:], v[:], kfb[:, 0, 0:1], None, ALU.mult)
                    for lag in range(1, T):
                        k.stt("dve", acc[:, :, lag:T], v[:, :, 0:T - lag], kfb[:, 0, lag:lag + 1], acc[:, :, lag:T],
                              ALU.mult, ALU.add)
                        k.stt("dve", acc[:, :, 0:T - lag], v[:, :, lag:T], kfb[:, 1, lag:lag + 1], acc[:, :, 0:T - lag],
                              ALU.mult, ALU.add)
                    k.stt("dve", acc[:], v[:], prm[:, 0, 4 + o:5 + o], acc[:], ALU.mult, ALU.add)
                    load_conv(ct, o, kfb[:])
                    if o == 0:
                        k.tt("dve", v[:], acc[:], kfb[:], ALU.mult)
                    else:
                        k.tt("dve", acc[:], acc[:], kfb[:], ALU.mult)
                        for b in range(NB):
                            k.dma("pool", S["YT0"][b, ct * 128:(ct + 1) * 128, toff:toff + T], acc[:, b, :],
                                  wb=[S["YT0"].reg((b, ct, toff))])
                    k.barrier()


CC = 64


def fwd_fft(k, C, I, S, st_tiles, Xin3, tab1, consumer):
    PS = C["PS"]
    A, A2, T2g = st_tiles["A"], st_tiles["A2"], st_tiles["T2g"]
    Xf = Xin3.rearrange("p n c -> p (n c)")
    ng = 64 * CC // 512
    for g in range(ng):
        pr, pi_ = PS[(g % 2) * 2], PS[(g % 2) * 2 + 1]
        k.mm(pr[:], tab1[:, 0:128], Xf[:, g * 512:(g + 1) * 512])
        k.mm(pi_[:], tab1[:, 128:256], Xf[:, g * 512:(g + 1) * 512])
        k.copy("act", A[:, 0].rearrange("p n c -> p (n c)")[:, g * 512:(g + 1) * 512], pr[:])
        k.copy("dve", A[:, 1].rearrange("p n c -> p (n c)")[:, g * 512:(g + 1) * 512], pi_[:])
    for r in range(2):
        k.dma("sp" if r == 0 else "pool", S["FS1"][r].rearrange("n k c -> k n c"), A[:, r])
    k.dma("sp", A2[:].rearrange("p k c -> p (k c)"), S["FS1"].a.rearrange("r n k c -> (r n) (k c)"))
    for k1g in range(16):
        tg = T2g[k1g % 2]
        k.dma("pool" if k1g % 2 else "sp", tg[:], I["fft_T2"][k1g * 8:(k1g + 1) * 8].rearrange("k r m -> r k m"))
        ps = PS[4 + k1g % 2]
        for j in range(8):
            k.mm(ps[:, j * CC:(j + 1) * CC], tg[:, j, :], A2[:, k1g * 8 + j, :])
        consumer(k1g, ps)


def phase_hyena_fft(k, I, S, C):
    PS = C["PS"]
    ident, antiI, ones = C["ident"], C["antiI"], C["ones"]
    T = L
    with contextlib.ExitStack() as st:
        h2 = k.tile(st, "fh2", [64, T])
        with contextlib.ExitStack() as st2:
            zT = k.tile(st2, "fzT", [33, T])
            k.dma("sp", zT[:], I["zT_lat"][:, :])
            fw1 = k.tile(st2, "ffw1", [33, 64])
            fw2 = k.tile(st2, "ffw2", [64, 64])
            k.dma("sp", fw1[:], I["hy_fw1"][0])
            k.dma("sp", fw2[:], I["hy_fw2"][0])
            prm = k.tile(st2, "fprm", [64, 4])
            for i, n in enumerate(("hy_fb1", "hy_ff1", "hy_fb2", "hy_ff2")):
                k.dma("sp", prm[:, i:i + 1], I[n][0].rearrange("(p o) -> p o", o=1), wb=[Buf("tmp")])
            k.barrier()
            h1 = k.tile(st2, "fh1", [64, T])
            tr = k.tile(st2, "ftr", [64, 512])
            tn = k.tile(st2, "ftn", [64, 512])
            for blk in range(T // 512):
                sl = slice(blk * 512, (blk + 1) * 512)
                k.mm(PS[0][0:64, :], fw1[:], zT[:, sl])
                sin_rr(k, h1[:, sl], PS[0][0:64, :], tr[:], tn[:], prm[:, 1:2], prm[:, 0:1])
                k.mm(PS[1][0:64, :], fw2[:], h1[:, sl])
                sin_rr(k, h2[:, sl], PS[1][0:64, :], tr[:], tn[:], prm[:, 3:4], prm[:, 2:3])
            k.barrier()
        fw3 = k.tile(st, "ffw3", [64, 3072])
        k.dma("sp", fw3[:], I["hy_fw3"][0])
        dlt = k.tile(st, "fdlt", [128, HY])
        load_bcast(k, "sp", dlt[:], I["ndelta_row"][0:1, :])
        tpos = k.tile(st, "ftpos", [128, 32])
        k.dma("sp", tpos[:], I["tneg_lat"][:, :])
        dec = k.tile(st, "fdec", [128, HY])
        hraw = [k.tile(st, f"fhraw{i}", [128, 3072]) for i in range(2)]
        accE = k.tile(st, "faccE", [128, 3072])
        sq = k.tile(st, "fsq", [128, 3072])
        for t in range(T // 128):
            hr = hraw[t % 2]
            k.act(dec[:], dlt[:], AF.Exp, scale=tpos[:, t:t + 1])
            for j in range(6):
                ps = PS[j % 4]
                k.mm(ps[:], h2[:, t * 128:(t + 1) * 128], fw3[:, j * 512:(j + 1) * 512])
                k.copy("act", hr[:, j * 512:(j + 1) * 512], ps[:])
            k.tt("dve", hr[:].rearrange("p (q c) -> p q c", c=HY), hr[:].rearrange("p (q c) -> p q c", c=HY),
                 dec[:].unsqueeze(1).to_broadcast([128, 4, HY]), ALU.mult)
            if t == 0:
                for o in range(2):
                    k.memset("dve", hr[0:1, o * 1536 + HY:o * 1536 + 2 * HY], 0.0)
            if t == 0:
                k.act(accE[:], hr[:], AF.Square)
            else:
                k.act(sq[:], hr[:], AF.Square)
                k.tt("pool", accE[:], accE[:], sq[:], ALU.add)
            for o in range(2):
                k.dma("sp", S["KT"][t * 128:(t + 1) * 128, o * HY:(o + 1) * HY], hr[:, o * 1536:o * 1536 + HY], wb=[Buf("x")])
                k.dma("pool", S["HB"][t * 128:(t + 1) * 128, o * HY:(o + 1) * HY], hr[:, o * 1536 + HY:(o + 1) * 1536],
                      wb=[Buf("x")])
        k.memset("dve", sq[:], 0.0)
        k.dma("sp", S["HB"][T:T + 128, :], sq[:, 0:1536], wb=[Buf("x")])
        en = k.tile(st, "fen", [1, 3072])
        for j in range(6):
            k.mm(PS[j % 4][0:1, :], ones[:, 0:1], accE[:, j * 512:(j + 1) * 512])
            k.copy("act", en[:, j * 512:(j + 1) * 512], PS[j % 4][0:1, :])
        es = k.tile(st, "fes", [1, 1536])
        for o in range(2):
            k.tt("dve", es[:, o * HY:(o + 1) * HY], en[:, o * 1536:o * 1536 + HY], en[:, o * 1536 + HY:(o + 1) * 1536], ALU.add)
        k.act(es[:], es[:], AF.Sqrt)
        k.recip(es[:], es[:])
        k.dma("sp", S["ESC"][0:1, :], es[:])
        k.barrier()
        for j in range(T // 128):
            g_ = hraw[j % 2]
            k.dma("sp", g_[:, 0:1536], S["HB"][128 * j + 1:128 * j + 129, :])
            fl = sq if j % 2 == 0 else accE
            for c3 in range(3):
                ps = PS[c3]
                k.mm(ps[:], antiI[:], g_[:, c3 * 512:(c3 + 1) * 512])
                k.copy("act" if c3 % 2 == 0 else "dve", fl[:, c3 * 512:(c3 + 1) * 512], ps[:])
            k.dma("pool", S["KT"][2 * T - 128 * (j + 1):2 * T - 128 * j, :], fl[:, 0:1536], wb=[Buf("x")])
        k.barrier()
    with contextlib.ExitStack() as st:
        tiles = dict(A=k.tile(st, "fA", [128, 2, 64, CC]), A2=k.tile(st, "fA2", [128, 128, CC]),
                     T2g=[k.tile(st, f"fT2g{i}", [128, 8, 128]) for i in range(2)])
        ff1 = k.tile(st, "fff1", [128, 256])
        f1d = k.tile(st, "ff1d", [128, 256])
        G1 = k.tile(st, "fG1", [128, 128])
        G2 = k.tile(st, "fG2", [128, 128])
        k.dma("sp", ff1[:], I["ff1"][:, :])
        k.dma("sp", f1d[:], I["f1d"][:, :])
        k.dma("sp", G1[:], I["fft_G1"][:, :])
        k.dma("sp", G2[:], I["fft_G2"][:, :])
        V = [k.tile(st, f"fV{i}", [128, 64, CC]) for i in range(2)]
        Xin = V[0]
        esc = k.tile(st, "fesc", [128, CC])
        okf = [k.tile(st, f"fokf{i}", [128, 8, CC]) for i in range(1)]
        KTv = S["KT"].a.rearrange("(a b) c -> a b c", b=64)
        for fc in range(24):
            o, c0 = fc // 12, (fc % 12) * CC
            k.dma("sp", Xin[:], KTv[:, :, fc * CC:(fc + 1) * CC])
            load_bcast(k, "sp", esc[:], S["ESC"][0:1, fc * CC:(fc + 1) * CC])

            def cons_f(k1g, ps, o=o, c0=c0):
                t_ = okf[0]
                k.tt("dve", t_[:], ps[:].rearrange("p (j c) -> p j c", c=CC), esc[:].unsqueeze(1).to_broadcast([128, 8, CC]), ALU.mult)
                k.dma("pool", S["KF"][o, :, k1g * 8:(k1g + 1) * 8, c0:c0 + CC], t_[:], wb=[Buf("x")])

            fwd_fft(k, C, I, S, tiles, Xin[:], ff1, cons_f)
        k.barrier()
        Bt = k.tile(st, "fB", [128, 128, CC])
        R = k.tile(st, "fR", [128, 66, CC])
        Gt = k.tile(st, "fGt", [128, 64, CC])
        wb_ = k.tile(st, "fwb", [128, 3, 4, CC])
        hb = k.tile(st, "fhb", [128, 2, CC])
        Kg = [k.tile(st, f"fKg{i}", [128, 2, 8, CC]) for i in range(2)]
        T12 = [k.tile(st, f"fT12{i}", [128, 2, 8 * CC]) for i in range(2)]
        T4g = [k.tile(st, f"fT4g{i}", [128, 4, 2, 128]) for i in range(2)]
        tmp = k.tile(st, "ftmp", [128, 4, CC])
        B2 = tiles["A"]

        def conv(blk, c0, dst):
            col = blk * HY + c0
            for b in range(NB):
                k.dma("sp", R[b * 64:(b + 1) * 64], AP(S["P0"].h, (b * (L + 2)) * 2304 + col, [[64 * 2304, 64], [2304, 66], [1, CC]]),
                      rb=[S["P0"]])
            w = lambda j: wb_[:, blk, j, :].unsqueeze(1).to_broadcast([128, 64, CC])
            k.tt("dve", dst, R[:, 0:64, :], w(0), ALU.mult)
            k.tt("pool", Gt_tmp[:, 0:64, :], R[:, 1:65, :], w(1), ALU.mult)
            k.tt("dve", dst, dst, Gt_tmp[:, 0:64, :], ALU.add)
            k.tt("pool", Gt_tmp[:, 0:64, :], R[:, 2:66, :], w(2), ALU.mult)
            k.tt("dve", dst, dst, Gt_tmp[:, 0:64, :], ALU.add)
            k.tt("dve", dst, dst, w(3), ALU.add)

        Gt_tmp = Bt
        for dc in range(HY // CC):
            c0 = dc * CC
            for blk in range(3):
                col = blk * HY + c0
                for j in range(3):
                    k.dma("sp", wb_[:, blk, j, :], I["hy_conv_w"][0][j:j + 1, col:col + CC].partition_broadcast(128), wb=[Buf("x")])
                k.dma("sp", wb_[:, blk, 3, :], I["hy_conv_b"][0:1, col:col + CC].partition_broadcast(128), wb=[Buf("x")])
            for o in range(2):
                k.dma("sp", hb[:, o, :], I["hy_bias"][0][o:o + 1, c0:c0 + CC].partition_broadcast(128), wb=[Buf("x")])
            k.barrier()
            conv(2, c0, V[0][:])
            for o in range(2):
                Vin, Vout = V[o], V[1 - o]

                def cons_d(k1g, ps, o=o, c0=c0):
                    kg = Kg[k1g % 2]
                    for r in range(2):
                        for hf in range(2):
                            k.dma("sp" if hf == 0 else "pool", kg[hf * 64:(hf + 1) * 64, r],
                                  S["KF"][o, r * 64:(r + 1) * 64, k1g * 8:(k1g + 1) * 8, c0:c0 + CC], rb=[S["KF"]], wb=[kg.sub[r * 2 + hf]])
                    t12 = T12[k1g % 2]
                    k.op("dve", lambda: k.nc.vector.tensor_tensor(t12[:, 0, :], ps[:], kg[:, 0].rearrange("p j c -> p (j c)"), ALU.mult),
                         [t12[:, 0, :]], [ps[:]], rb=[k.buf_of(ps[:])] + kg.sub, wb=[t12])
                    k.op("dve", lambda: k.nc.vector.tensor_tensor(t12[:, 1, :], ps[:], kg[:, 1].rearrange("p j c -> p (j c)"), ALU.mult),
                         [t12[:, 1, :]], [ps[:]], rb=[k.buf_of(ps[:])] + kg.sub, wb=[t12])
                    p2 = PS[6 + k1g % 2]
                    k.mm(p2[:], G1[:], t12[:, 0, :], start=True, stop=False)
                    k.mm(p2[:], G2[:], t12[:, 1, :], start=False, stop=True)
                    k.copy("act", Bt[:, k1g * 8:(k1g + 1) * 8, :], p2[:].rearrange("p (j c) -> p j c", c=CC))

                fwd_fft(k, C, I, S, tiles, Vin[:], f1d, cons_d)
                k.dma("sp", S["FS2"].a.rearrange("r n k c -> (r n) (k c)"), Bt[:].rearrange("p k c -> p (k c)"))
                for r in range(2):
                    k.dma("sp" if r == 0 else "pool", B2[:, r], S["FS2"][r].rearrange("n k c -> k n c"))
                conv(o, c0, Gt[:])
                for g in range(16):
                    t4 = T4g[g % 2]
                    for r in range(2):
                        k.dma("sp" if r == 0 else "pool", t4[:, :, r, :], I["fft_T4"][g * 4:(g + 1) * 4, r].rearrange("n k m -> k n m"),
                              wb=[t4.sub[r]])
                    ps = PS[g % 4]
                    for j in range(4):
                        n2 = g * 4 + j
                        k.op("pe", lambda: k.nc.tensor.matmul(ps[:, j * CC:(j + 1) * CC], t4[:, j, 0, :], B2[:, 0, n2, :], start=True, stop=False),
                             [ps[:]], [], rb=[B2] + t4.sub, wb=[k.buf_of(ps[:])])
                        k.op("pe", lambda: k.nc.tensor.matmul(ps[:, j * CC:(j + 1) * CC], t4[:, j, 1, :], B2[:, 1, n2, :], start=False, stop=True),
                             [ps[:]], [], rb=[B2] + t4.sub, wb=[k.buf_of(ps[:])])
                    gs = slice(g * 4, (g + 1) * 4)
                    k.tt("pool", tmp[:], Vin[:, gs, :], hb[:, o, :].unsqueeze(1).to_broadcast([128, 4, CC]), ALU.mult)
                    k.tt("dve", tmp[:], tmp[:], ps[:, 0:4 * CC].rearrange("p (j c) -> p j c", c=CC), ALU.add)
                    k.tt("dve", Vout[:, gs, :], tmp[:], Gt[:, gs, :], ALU.mult)
                if o == 1:
                    for b in range(NB):
                        k.dma("pool", AP(S["YH"].h, b * L * HY + c0, [[64 * HY, 64], [HY, 64], [1, CC]]), Vout[b * 64:(b + 1) * 64],
                              wb=[Buf("x")])
            k.barrier()


TC = 256


def phase_s5(k, I, S, C):
    PS = C["PS"]
    ident, antiI = C["ident"], C["antiI"]
    nblk = TT // 128
    for d in range(2):
        with contextlib.ExitStack() as st:
            BTr = [k.tile(st, f"BTr{j}", [128, 128]) for j in range(8)]
            BTi = [k.tile(st, f"BTi{j}", [128, 128]) for j in range(8)]
            Cr = [k.tile(st, f"Cr{j}", [128, 256]) for j in range(8)]
            nCi = [k.tile(st, f"nCi{j}", [128, 256]) for j in range(8)]
            ctab = [k.tile(st, f"ctab{j}", [128, TC]) for j in range(8)]
            stab = [k.tile(st, f"stab{j}", [128, TC]) for j in range(8)]
            rt = [k.tile(st, f"rt{j}", [128, TC]) for j in range(8)]
            carry = [k.tile(st, f"carry{j}", [128, 2]) for j in range(8)]
            iot = k.tile(st, "iot", [128, TC])
            k.dma("sp", iot[:], I["iota1"][:, :])
            with contextlib.ExitStack() as st2:
                sc = k.tile(st2, "s5sc", [128, 24])
                tr = k.tile(st2, "s5tr", [128, TC])
                tn = k.tile(st2, "s5tn", [128, TC])
                bre = k.tile(st2, "s5bre", [128, 16])
                bim = k.tile(st2, "s5bim", [128, 16])
                bb = k.tile(st2, "s5bb", [128, 2, 16])
                t16 = k.tile(st2, "s5t16", [128, 16])
                pad = k.tile(st2, "s5pad", [128, 128])
                for j in range(8):
                    g0 = 2 * j
                    k.dma("sp", sc[:, 0:1], I["s5_lam_re"][0, d, g0:g0 + 2, :].rearrange("g (p o) -> (g p) o", o=1))
                    k.dma("sp", sc[:, 1:2], I["s5_lam_im"][0, d, g0:g0 + 2, :].rearrange("g (p o) -> (g p) o", o=1))
                    for gl in range(2):
                        k.dma("sp", sc[gl * 64:(gl + 1) * 64, 2:3],
                              I["s5_log_step"][0, d:d + 1, g0 + gl:g0 + gl + 1].partition_broadcast(64))
                    k.dma("sp", bre[:], I["s5_b_re"][0, d, g0:g0 + 2].rearrange("g p n -> (g p) n"))
                    k.dma("sp", bim[:], I["s5_b_im"][0, d, g0:g0 + 2].rearrange("g p n -> (g p) n"))
                    k.act(sc[:, 3:4], sc[:, 2:3], AF.Exp)
                    k.tt("dve", sc[:, 4:5], sc[:, 0:1], sc[:, 3:4], ALU.mult)
                    k.tt("dve", sc[:, 5:6], sc[:, 1:2], sc[:, 3:4], ALU.mult)
                    k.act(sc[:, 6:7], sc[:, 4:5], AF.Exp)
                    sin_rr(k, sc[:, 7:8], sc[:, 5:6], tr[:, 0:1], tn[:, 0:1])
                    k.ts("dve", sc[:, 9:10], sc[:, 5:6], math.pi / 2, None, ALU.add)
                    sin_rr(k, sc[:, 8:9], sc[:, 9:10], tr[:, 0:1], tn[:, 0:1])
                    k.tt("dve", sc[:, 10:11], sc[:, 6:7], sc[:, 8:9], ALU.mult)
                    k.tt("dve", sc[:, 11:12], sc[:, 6:7], sc[:, 7:8], ALU.mult)
                    k.ts("dve", sc[:, 12:13], sc[:, 10:11], -1.0, None, ALU.add)
                    k.tt("dve", sc[:, 13:14], sc[:, 0:1], sc[:, 0:1], ALU.mult)
                    k.stt("dve", sc[:, 13:14], sc[:, 1:2], sc[:, 1:2], sc[:, 13:14], ALU.mult, ALU.add)
                    k.recip(sc[:, 14:15], sc[:, 13:14])
                    k.tt("dve", sc[:, 15:16], sc[:, 12:13], sc[:, 0:1], ALU.mult)
                    k.stt("dve", sc[:, 15:16], sc[:, 11:12], sc[:, 1:2], sc[:, 15:16], ALU.mult, ALU.add)
                    k.tt("dve", sc[:, 15:16], sc[:, 15:16], sc[:, 14:15], ALU.mult)
                    k.tt("dve", sc[:, 16:17], sc[:, 12:13], sc[:, 1:2], ALU.mult)
                    k.stt("dve", sc[:, 16:17], sc[:, 11:12], sc[:, 0:1], sc[:, 16:17], ALU.mult, ALU.subtract)
                    k.tt("dve", sc[:, 16:17], sc[:, 16:17], sc[:, 14:15], ALU.mult)
                    k.ts("dve", bb[:, 0, :], bre[:], sc[:, 15:16], None, ALU.mult)
                    k.ts("dve", t16[:], bim[:], sc[:, 16:17], None, ALU.mult)
                    k.tt("dve", bb[:, 0, :], bb[:, 0, :], t16[:], ALU.subtract)
                    k.ts("dve", bb[:, 1, :], bim[:], sc[:, 15:16], None, ALU.mult)
                    k.ts("dve", t16[:], bre[:], sc[:, 16:17], None, ALU.mult)
                    k.tt("dve", bb[:, 1, :], bb[:, 1, :], t16[:], ALU.add)
                    for ri, BT in ((0, BTr), (1, BTi)):
                        k.memset("dve", pad[:], 0.0)
                        for gl in range(2):
                            cg = ((g0 + gl) % 8) * 16
                            k.copy("dve", pad[gl * 64:(gl + 1) * 64, cg:cg + 16], bb[gl * 64:(gl + 1) * 64, ri, :])
                        k.mm(PS[0][:, 0:128], pad[:], ident[:])
                        k.copy("act", BT[j][:], PS[0][:, 0:128])
                    k.memset("dve", Cr[j][:], 0.0)
                    k.memset("dve", nCi[j][:], 0.0)
                    for gl in range(2):
                        g = g0 + gl
                        k.dma("sp", Cr[j][gl * 64:(gl + 1) * 64, g * 16:(g + 1) * 16],
                              I["s5_c_re"][0, d, g].rearrange("n p -> p n"), allow_slow_non_contiguous=True)
                        k.dma("sp", nCi[j][gl * 64:(gl + 1) * 64, g * 16:(g + 1) * 16],
                              I["s5_c_im"][0, d, g].rearrange("n p -> p n"), allow_slow_non_contiguous=True)
                    k.ts("dve", nCi[j][:], nCi[j][:], -1.0, None, ALU.mult)
                    k.ts("dve", tn[:], iot[:], sc[:, 5:6], None, ALU.mult)
                    sin_rr(k, stab[j][:], tn[:], tr[:], tn[:])
                    k.ts("dve", tn[:], iot[:], sc[:, 5:6], math.pi / 2, ALU.mult, ALU.add)
                    sin_rr(k, ctab[j][:], tn[:], tr[:], tn[:])
                    k.memset("dve", rt[j][:], 0.0)
                    k.ts("dve", rt[j][:], rt[j][:], sc[:, 6:7], None, ALU.add)
                k.barrier()
            ut = [k.tile(st, f"s5ut{i}", [128, 256]) for i in range(2)]
            uT = k.tile(st, "s5uT", [128, 2, TC])
            Sre = [k.tile(st, f"Sre{j}", [128, TC]) for j in range(8)]
            Sim = [k.tile(st, f"Sim{j}", [128, TC]) for j in range(8)]
            m1 = k.tile(st, "s5m1", [128, TC])
            m2 = k.tile(st, "s5m2", [128, TC])
            mre = k.tile(st, "s5mre", [128, TC])
            mim = k.tile(st, "s5mim", [128, TC])
            vre = k.tile(st, "s5vre", [128, TC])
            vim = k.tile(st, "s5vim", [128, TC])
            yt = [k.tile(st, f"s5yt{i}", [128, 256]) for i in range(2)]
            y2 = [k.tile(st, f"s5y2{i}", [128, 256]) for i in range(2)]
            revm = antiI if d == 1 else ident

            def row0(m):
                if d == 0:
                    return 128 * m
                return 128 * (1 - m) if m < 2 else 4480 - 128 * m

            it = 0
            for b in range(NB):
                for j in range(8):
                    k.memset("dve", carry[j][:], 0.0)
                for ch in range(TT // TC):
                    for bl in range(2):
                        m = ch * 2 + bl
                        u_ = ut[(it + bl) % 2]
                        k.dma("sp", u_[:], S["UT0"][b, row0(m):row0(m) + 128, :])
                        for hf in range(2):
                            k.mm(PS[0][:, (hf * 2 + bl) * 128:(hf * 2 + bl + 1) * 128], u_[:, hf * 128:(hf + 1) * 128], revm[:])
                    k.copy("act", uT[:], PS[0][:].rearrange("p (h t) -> p h t", h=2))
                    for j in range(8):
                        hf = j // 4
                        pr, pi_ = PS[1 + (j % 2) * 2], PS[2 + (j % 2) * 2]
                        k.mm(pr[:, 0:TC], BTr[j][:], uT[:, hf, :])
                        k.mm(pi_[:, 0:TC], BTi[j][:], uT[:, hf, :])
                        k.tt("dve", m1[:], pr[:, 0:TC], ctab[j][:], ALU.mult)
                        k.tt("dve", m2[:], pi_[:, 0:TC], stab[j][:], ALU.mult)
                        k.tt("pool", mre[:], m1[:], m2[:], ALU.add)
                        k.tt("dve", m1[:], pi_[:, 0:TC], ctab[j][:], ALU.mult)
                        k.tt("dve", m2[:], pr[:, 0:TC], stab[j][:], ALU.mult)
                        k.tt("pool", mim[:], m1[:], m2[:], ALU.subtract)
                        k.scan("dve", vre[:], rt[j][:], mre[:], carry[j][:, 0:1], ALU.mult, ALU.add)
                        k.scan("dve", vim[:], rt[j][:], mim[:], carry[j][:, 1:2], ALU.mult, ALU.add)
                        k.tt("dve", m1[:], vre[:], ctab[j][:], ALU.mult)
                        k.tt("dve", m2[:], vim[:], stab[j][:], ALU.mult)
                        k.tt("pool", Sre[j][:], m1[:], m2[:], ALU.subtract)
                        k.tt("dve", m1[:], vre[:], stab[j][:], ALU.mult)
                        k.tt("dve", m2[:], vim[:], ctab[j][:], ALU.mult)
                        k.tt("pool", Sim[j][:], m1[:], m2[:], ALU.add)
                        k.copy("pool", carry[j][:, 0:1], Sre[j][:, TC - 1:TC])
                        k.copy("pool", carry[j][:, 1:2], Sim[j][:, TC - 1:TC])
                    for bl in range(2):
                        m = ch * 2 + bl
                        py = PS[5 + bl]
                        for j in range(8):
                            k.mm(py[:, 0:256], Sre[j][:, bl * 128:(bl + 1) * 128], Cr[j][:], start=(j == 0), stop=False)
                            k.mm(py[:, 0:256], Sim[j][:, bl * 128:(bl + 1) * 128], nCi[j][:], start=False, stop=(j == 7))
                        y_ = yt[(it + bl) % 2]
                        k.copy("act", y_[:], py[:, 0:256])
                        if d == 1:
                            k.mm(PS[7][:, 0:256], antiI[:], y_[:])
                            y2_ = y2[(it + bl) % 2]
                            k.copy("act", y2_[:], PS[7][:, 0:256])
                            y_ = y2_
                        k.dma("pool", S["YS5"][d, b, row0(m):row0(m) + 128, :], y_[:], wb=[S["YS5"].reg((d, b, m))])
                    it += 1
        k.barrier()


def phase_s5_out(k, I, S, C):
    PS = C["PS"]
    ident = C["ident"]
    with contextlib.ExitStack() as st:
        Db = k.tile(st, "s5D", [128, 256])
        gb = k.tile(st, "s5gb", [128, 512])
        gw = k.tile(st, "s5gw", [128, 2, 512])
        load_bcast(k, "sp", Db[:], I["s5_d"][0:1, :])
        load_bcast(k, "sp", gb[:], I["s5_glu_b"][0:1, :])
        k.dma("sp", gw[:], I["s5_glu_w"][0].rearrange("(h p) n -> p h n", p=128))
        yf = [k.tile(st, f"o5yf{i}", [128, 256]) for i in range(2)]
        yb = [k.tile(st, f"o5yb{i}", [128, 256]) for i in range(2)]
        uu = [k.tile(st, f"o5u{i}", [128, 256]) for i in range(2)]
        y = k.tile(st, "o5y", [128, 256])
        t1 = k.tile(st, "o5t1", [128, 256])
        geT = k.tile(st, "o5geT", [128, 2, 128])
        a = k.tile(st, "o5a", [128, 512])
        o = k.tile(st, "o5o", [128, 256])
        oT = [k.tile(st, f"o5oT{i}", [128, 2, 128]) for i in range(2)]
        it = 0
        for b in range(NB):
            for m in range(TT // 128):
                r0 = m * 128
                i2 = it % 2
                k.dma("sp", yf[i2][:], S["YS5"][0, b, r0:r0 + 128, :])
                k.dma("sp", yb[i2][:], S["YS5"][1, b, r0:r0 + 128, :])
                k.dma("sp", uu[i2][:], S["UT0"][b, r0:r0 + 128, :])
                k.tt("dve", y[:], yf[i2][:], yb[i2][:], ALU.add)
                k.tt("dve", t1[:], uu[i2][:], Db[:], ALU.mult)
                k.tt("dve", y[:], y[:], t1[:], ALU.add)
                k.act(t1[:], y[:], AF.Square)
                k.ts("dve", t1[:], t1[:], 0.044715, 1.0, ALU.mult, ALU.add)
                k.tt("dve", t1[:], t1[:], y[:], ALU.mult)
                k.act(t1[:], t1[:], AF.Sigmoid, scale=2.0 * math.sqrt(2.0 / math.pi))
                k.tt("dve", y[:], y[:], t1[:], ALU.mult)
                for hf in range(2):
                    k.mm(PS[0][:, hf * 128:(hf + 1) * 128], y[:, hf * 128:(hf + 1) * 128], ident[:])
                k.copy("act", geT[:], PS[0][:, 0:256].rearrange("p (h t) -> p h t", h=2))
                for hf in range(2):
                    k.mm(PS[1][:], geT[:, hf, :], gw[:, hf, :], start=(hf == 0), stop=(hf == 1))
                k.tt("dve", a[:], PS[1][:], gb[:], ALU.add)
                k.act(a[:, 256:512], a[:, 256:512], AF.Sigmoid)
                k.tt("dve", o[:], a[:, 0:256], a[:, 256:512], ALU.mult)
                for hf in range(2):
                    k.mm(PS[2][:, hf * 128:(hf + 1) * 128], o[:, hf * 128:(hf + 1) * 128], ident[:])
                k.copy("act", oT[i2][:], PS[2][:, 0:256].rearrange("p (h t) -> p h t", h=2))
                k.dma("pool", S["YT0"][b, 768:1024, r0:r0 + 128].rearrange("(h p) t -> p h t", p=128), oT[i2][:],
                      wb=[S["YT0"].reg(("s5", b, m))])
                it += 1


def phase_moe(k, I, S, C, layer, yT_src, wout, tiles, final):
    PS = C["PS"]
    ident = C["ident"]
    with contextlib.ExitStack() as st:
        Wo = k.tile(st, "Wo", [128, 8, 1024])
        k.dma("sp", Wo[:], wout.rearrange("(k p) n -> p k n", p=128))
        Wr = k.tile(st, "Wr", [128, 8, 36])
        k.dma("sp", Wr[:, :, 0:4], I["moe_wg"][layer].rearrange("(k p) g -> p k g", p=128), allow_slow_non_contiguous=True,
              wb=[Buf("x")])
        k.dma("sp", Wr[:, :, 4:36], I["moe_we"][layer].rearrange("(k p) g -> p k g", p=128), allow_slow_non_contiguous=True,
              wb=[Buf("x")])
        rb = k.tile(st, "rbias", [128, 36])
        k.dma("sp", rb[:, 0:4], I["moe_bg"][layer:layer + 1, :].partition_broadcast(128), wb=[Buf("x")])
        k.dma("sp", rb[:, 4:36], I["moe_be"][layer:layer + 1, :].partition_broadcast(128), wb=[Buf("x")])
        fg = k.tile(st, "fg", [128, 1024])
        if final:
            load_bcast(k, "sp", fg[:], I["final_g"].a.rearrange("(o d) -> o d", o=1))
        k.barrier()
        mvt = [k.tile(st, f"mv{i}", [128, 1024]) for i in range(4)]
        yT = k.tile(st, "yT", [128, 8, 512])
        xa = [k.tile(st, f"xa{i}", [128, 1024]) for i in range(4)]
        acc = [k.tile(st, f"acc{i}", [128, 1024]) for i in range(4)]
        gate = [k.tile(st, f"gate{i}", [128, 32]) for i in range(4)]
        xin = k.tile(st, "xin", [128, 1024])
        h = k.tile(st, "hm", [128, 1024])
        hT = k.tile(st, "hTm", [128, 8, 512])
        sq = k.tile(st, "sqm", [128, 1024])
        ss = k.tile(st, "ssm", [128, 1])
        rstd = k.tile(st, "rstdm", [128, 1])
        r_ = k.tile(st, "rt", [128, 64])
        lg = k.tile(st, "lg", [128, 36])
        Wg = [k.tile(st, f"Wg{i}", [128, 8, 256]) for i in range(2)]
        Wu = [k.tile(st, f"Wu{i}", [128, 8, 256]) for i in range(2)]
        Wd = [k.tile(st, f"Wd{i}", [128, 2, 1024]) for i in range(2)]
        sl_ = [k.tile(st, f"sil{i}", [128, 512]) for i in range(2)]
        hid = [k.tile(st, f"hid{i}", [128, 512]) for i in range(2)]
        cur_row = None
        ecount = 0
        for tl in tiles:
            if tl["row"] != cur_row:
                cur_row = tl["row"]
                for i, comp in enumerate((2, 3, 4, 5)):
                    load_bcast(k, "sp", mvt[i][:], S["MV"][layer, cur_row, comp:comp + 1, :])
            c0 = 0
            for (ap_, n) in tl["ysrc"]:
                if "yh" in tl:
                    k.dma("sp", yT[:, 6:8, c0:c0 + n], ap_.rearrange("(k p) t -> p k t", p=128), wb=[Buf("x")])
                else:
                    k.dma("sp", yT[:, :, c0:c0 + n], ap_.rearrange("(k p) t -> p k t", p=128), wb=[Buf("x")])
                c0 += n
            k.barrier()
            if "yh" in tl:
                for ts in range(4):
                    k.dma("sp", h[:, 0:HY], tl["yh"][ts])
                    for hf in range(2):
                        ps = PS[2 + hf]
                        for j in range(3):
                            kk = hf * 3 + j
                            k.mm(ps[:, j * 128:(j + 1) * 128], h[:, kk * 128:(kk + 1) * 128], ident[:])
                        k.copy("act", yT[:, hf * 3:(hf + 1) * 3, ts * 128:(ts + 1) * 128],
                               ps[:, 0:384].rearrange("p (j t) -> p j t", j=3))
            for ts in range(4):
                k.dma("sp", xin[:], tl["xsrc"][ts])
                for half in range(2):
                    ps = PS[half]
                    for kk in range(8):
                        k.mm(ps[:], yT[:, kk, ts * 128:(ts + 1) * 128], Wo[:, kk, half * 512:(half + 1) * 512],
                             start=(kk == 0), stop=(kk == 7))
                    k.tt("dve", xa[ts][:, half * 512:(half + 1) * 512], ps[:], mvt[0][:, half * 512:(half + 1) * 512], ALU.mult)
                k.tt("dve", xa[ts][:], xa[ts][:], xin[:], ALU.add)
                rms_mod(k, (sq, ss, rstd), xa[ts][:], mvt[2][:], mvt[1][:], h[:], "m")
                for hf in range(2):
                    ps = PS[2 + hf]
                    for j in range(4):
                        kk = hf * 4 + j
                        k.mm(ps[:, j * 128:(j + 1) * 128], h[:, kk * 128:(kk + 1) * 128], ident[:])
                    k.copy("act", hT[:, hf * 4:(hf + 1) * 4, ts * 128:(ts + 1) * 128], ps[:].rearrange("p (j t) -> p j t", j=4))
                ps = PS[4]
                for kk in range(8):
                    k.mm(ps[:, 0:36], hT[:, kk, ts * 128:(ts + 1) * 128], Wr[:, kk, :], start=(kk == 0), stop=(kk == 7))
                k.tt("dve", lg[:], ps[:, 0:36], rb[:], ALU.add)
                g_ = gate[ts]
                k.op("dve", lambda: k.nc.vector.reduce_max(r_[:, 0:1], lg[:, 0:4], AX.X), [r_[:, 0:1]], [lg[:, 0:4]])
                k.ts("dve", r_[:, 1:2], r_[:, 0:1], -1.0, None, ALU.mult)
                k.act(r_[:, 4:8], lg[:, 0:4], AF.Exp, bias=r_[:, 1:2], accum_out=r_[:, 2:3])
                k.recip(r_[:, 3:4], r_[:, 2:3])
                k.ts("dve", r_[:, 8:12], lg[:, 0:4], r_[:, 0:1], None, ALU.is_ge)
                k.ts("dve", r_[:, 16:24], lg[:, 4:12], r_[:, 8:9], None, ALU.mult)
                for g in range(1, 4):
                    k.stt("dve", r_[:, 16:24], lg[:, 4 + 8 * g:12 + 8 * g], r_[:, 8 + g:9 + g], r_[:, 16:24], ALU.mult, ALU.add)
                k.op("dve", lambda: k.nc.vector.reduce_max(r_[:, 12:13], r_[:, 16:24], AX.X), [r_[:, 12:13]], [r_[:, 16:24]])
                k.ts("dve", r_[:, 24:32], r_[:, 16:24], r_[:, 12:13], None, ALU.is_ge)
                k.stt("dve", r_[:, 32:40], r_[:, 24:32], -1e30, r_[:, 16:24], ALU.mult, ALU.add)
                k.op("dve", lambda: k.nc.vector.reduce_max(r_[:, 13:14], r_[:, 32:40], AX.X), [r_[:, 13:14]], [r_[:, 32:40]])
                k.ts("dve", r_[:, 40:48], r_[:, 32:40], r_[:, 13:14], None, ALU.is_ge)
                k.tt("dve", r_[:, 14:15], r_[:, 13:14], r_[:, 12:13], ALU.subtract)
                k.act(r_[:, 14:15], r_[:, 14:15], AF.Exp)
                k.ts("dve", r_[:, 14:15], r_[:, 14:15], 1.0, None, ALU.add)
                k.recip(r_[:, 15:16], r_[:, 14:15])
                k.tt("dve", r_[:, 48:49], r_[:, 15:16], r_[:, 3:4], ALU.mult)
                k.tt("dve", r_[:, 49:50], r_[:, 3:4], r_[:, 48:49], ALU.subtract)
                k.ts("dve", r_[:, 50:58], r_[:, 24:32], r_[:, 48:49], None, ALU.mult)
                k.stt("dve", r_[:, 50:58], r_[:, 40:48], r_[:, 49:50], r_[:, 50:58], ALU.mult, ALU.add)
                for g in range(4):
                    k.ts("dve", g_[:, g * 8:(g + 1) * 8], r_[:, 50:58], r_[:, 8 + g:9 + g], None, ALU.mult)
            for e in range(32):
                gi, ei = e // 8, e % 8
                i2 = ecount % 2
                ecount += 1
                k.dma("sp", Wg[i2][:], I["moe_w_gate"][layer, gi, ei].rearrange("(k p) n -> p k n", p=128))
                k.dma("pool", Wu[i2][:], I["moe_w_up"][layer, gi, ei].rearrange("(k p) n -> p k n", p=128))
                k.dma("sp", Wd[i2][:], I["moe_w_down"][layer, gi, ei].rearrange("(k p) n -> p k n", p=128))
                for hc in range(2):
                    pa, pu = PS[hc * 2], PS[hc * 2 + 1]
                    for kk in range(8):
                        k.mm(pa[:], Wg[i2][:, kk, hc * 128:(hc + 1) * 128], hT[:, kk, :], start=(kk == 0), stop=(kk == 7))
                    for kk in range(8):
                        k.mm(pu[:], Wu[i2][:, kk, hc * 128:(hc + 1) * 128], hT[:, kk, :], start=(kk == 0), stop=(kk == 7))
                    k.act(sl_[hc][:], pa[:], AF.Silu)
                    k.tt("dve", hid[hc][:], sl_[hc][:], pu[:], ALU.mult)
                for ts in range(4):
                    for half in range(2):
                        po = PS[4 + (ts * 2 + half) % 4]
                        for hc in range(2):
                            k.mm(po[:], hid[hc][:, ts * 128:(ts + 1) * 128], Wd[i2][:, hc, half * 512:(half + 1) * 512],
                                 start=(hc == 0), stop=(hc == 1))
                        eng = "dve" if half == 0 else "pool"
                        asl = acc[ts][:, half * 512:(half + 1) * 512]
                        if e == 0:
                            k.ts("dve", asl, po[:], gate[ts][:, e:e + 1], None, ALU.mult)
                        else:
                            k.stt("dve", asl, po[:], gate[ts][:, e:e + 1], asl, ALU.mult, ALU.add)
            for ts in range(4):
                k.tt("dve", acc[ts][:], acc[ts][:], mvt[3][:], ALU.mult)
                k.tt("dve", acc[ts][:], acc[ts][:], xa[ts][:], ALU.add)
                if final:
                    k.act(sq[:], acc[ts][:], AF.Square, accum_out=ss[:])
                    k.ts("dve", ss[:], ss[:], 1.0 / D, EPS, ALU.mult, ALU.add)
                    k.act(ss[:], ss[:], AF.Sqrt)
                    k.recip(rstd[:], ss[:])
                    k.stt("dve", acc[ts][:], acc[ts][:], rstd[:, 0:1], fg[:], ALU.mult, ALU.mult)
                k.dma("pool", tl["dst"][ts], acc[ts][:], wb=[Buf("x")])
            k.barrier()


def l0_moe_tiles(I, S):
    tiles = []
    tiles.append(dict(row=2, ysrc=[(S["YT0"][b, :, 0:LC], LC) for b in range(NB)],
                      xsrc=[I["ctx"][b, j * 128:(j + 1) * 128, :] for b in range(NB) for j in range(2)],
                      dst=[S["CTX1"][b, j * 128:(j + 1) * 128, :] for b in range(NB) for j in range(2)]))
    for b in range(NB):
        for i in range(L // 512):
            t0 = i * 512
            td = dict(row=b, ysrc=[(S["YT0"][b, :, LC + t0:LC + t0 + 512], 512)])
            if USE_FFT:
                td = dict(row=b, ysrc=[(S["YT0"][b, 768:1024, LC + t0:LC + t0 + 512], 512)],
                          yh=[S["YH"][b, t0 + j * 128:t0 + (j + 1) * 128, :] for j in range(4)])
            tiles.append(dict(td,
                              xsrc=[I["x"][b, t0 + j * 128:t0 + (j + 1) * 128, :] for j in range(4)],
                              dst=[S["X1"][b, t0 + j * 128:t0 + (j + 1) * 128, :] for j in range(4)]))
    return tiles


def phase_l1_norm(k, I, S, C):
    with contextlib.ExitStack() as st:
        A = k.tile(st, "A1b", [128, 1024])
        sh = k.tile(st, "sh1b", [128, 1024])
        xt = [k.tile(st, f"xtb{i}", [128, 1024]) for i in range(2)]
        h = k.tile(st, "hb", [128, 1024])
        hT = [k.tile(st, f"hTb{i}", [128, 8, 128]) for i in range(2)]
        sq = k.tile(st, "sqb", [128, 1024])
        ss = k.tile(st, "ssb", [128, 1])
        rstd = k.tile(st, "rstdb", [128, 1])
        it = 0
        for b in range(NB):
            for (src, row, ntile, toff) in ((S["CTX1"], 2, LC // 128, 0), (S["X1"], b, L // 128, LC)):
                load_bcast(k, "sp", A[:], S["MV"][1, row, 1:2, :])
                load_bcast(k, "sp", sh[:], S["MV"][1, row, 0:1, :])
                for t in range(ntile):
                    x_ = xt[it % 2]
                    hT_ = hT[it % 2]
                    k.dma("sp", x_[:], src[b, t * 128:(t + 1) * 128, :])
                    rms_mod(k, (sq, ss, rstd), x_[:], A[:], sh[:], h[:], "l1")
                    transpose_1024(k, C, h, hT_)
                    t0 = toff + t * 128
                    k.dma("pool", S["HT1"][b].rearrange("(j p) t -> p j t", p=128)[:, :, t0:t0 + 128], hT_[:],
                          wb=[S["HT1"].reg((b, t0))])
                    it += 1


def phase_l1_inproj(k, I, S, C):
    PS = C["PS"]
    with contextlib.ExitStack() as st:
        W = k.tile(st, "W1g", [128, 8, 1024])
        hT = [k.tile(st, f"hT5{i}", [128, 8, 512]) for i in range(2)]
        of = [k.tile(st, f"of{i}", [128, 8, 512]) for i in range(2)]
        ot = [k.tile(st, f"ot{i}", [128, 4, 1024]) for i in range(2)]
        wv = I["c_w_in"][0].rearrange("(k p) n -> p k n", p=128)
        it = 0
        for cg in range(5):
            k.barrier()
            for kk in range(8):
                k.dma("sp" if kk % 2 == 0 else "pool", W[:, kk, :], wv[:, kk, cg * 1024:(cg + 1) * 1024], wb=[Buf("x")])
            k.barrier()
            for b in range(NB):
                segs = [(LC + i * 512, 512) for i in range(L // 512)]
                if cg >= 2:
                    segs = [(0, LC)] + segs
                for (t0, n) in segs:
                    h_ = hT[it % 2]
                    k.dma("sp", h_[:, :, 0:n], S["HT1"][b].rearrange("(j p) t -> p j t", p=128)[:, :, t0:t0 + n])
                    if cg in (0, 3, 4):
                        o_ = of[it % 2]
                        for j in range(8):
                            ps = PS[j % 4]
                            for kk in range(8):
                                k.mm(ps[:, 0:n], W[:, kk, j * 128:(j + 1) * 128], h_[:, kk, 0:n], start=(kk == 0), stop=(kk == 7))
                            k.copy("act" if j % 2 == 0 else "dve", o_[:, j, 0:n], ps[:, 0:n])
                        if cg == 0:
                            dst = S["QT1"][b].rearrange("(j p) t -> p j t", p=128)[:, :, t0 - LC:t0 - LC + n]
                        else:
                            dst = S["ZF1" if cg == 3 else "ZB1"][b].rearrange("(j p) t -> p j t", p=128)[:, :, t0:t0 + n]
                        k.dma("pool", dst, o_[:, :, 0:n], wb=[Buf("x")])
                    else:
                        o_ = ot[it % 2]
                        for ts in range(n // 128):
                            for half in range(2):
                                ps = PS[4 + (ts * 2 + half) % 4]
                                for kk in range(8):
                                    k.mm(ps[:], h_[:, kk, ts * 128:(ts + 1) * 128], W[:, kk, half * 512:(half + 1) * 512],
                                         start=(kk == 0), stop=(kk == 7))
                                k.copy("act" if half == 0 else "dve", o_[:, ts, half * 512:(half + 1) * 512], ps[:])
                        if cg == 1:
                            dst = S["G1"][b, t0 - LC:t0 - LC + n, :].rearrange("(s p) d -> p s d", p=128)
                        else:
                            dst = S["V1"][b, t0:t0 + n, :].rearrange("(s p) d -> p s d", p=128)
                        k.dma("pool", dst, o_[:, 0:n // 128, :], wb=[Buf("x")])
                    it += 1
        k.barrier()


def phase_hgrn2(k, I, S, C):
    PS = C["PS"]
    ident = C["ident"]
    CH = 64
    with contextlib.ExitStack() as st:
        cmask = k.tile(st, "cmask", [128, 512])
        k.dma("sp", cmask[:], I["cmask"][:, :])
        tri = [k.tile(st, f"tri{d}", [64, 64]) for d in range(2)]
        k.dma("sp", tri[0][:], I["triu"][:, :])
        k.dma("sp", tri[1][:], I["tril"][:, :])
        lbt = k.tile(st, "lbt", [128, 8])
        names = ["z", "f", "lf", "kk", "bc", "bb", "eb", "qt", "kt", "kh", "tmp"]
        T_ = [{n: k.tile(st, f"g{d}{n}", [128, 512]) for n in names} for d in range(2)]
        ebe = [k.tile(st, f"ebe{d}", [128, 8]) for d in range(2)]
        vt = [k.tile(st, f"vt{d}", [64, 8, 128]) for d in range(2)]
        kht = [k.tile(st, f"kht{d}", [64, 8, 128]) for d in range(2)]
        ot = [k.tile(st, f"oo{d}", [64, 8, 128]) for d in range(2)]
        am = [k.tile(st, f"am{d}", [64, 64]) for d in range(2)]
        Sst = [k.tile(st, f"Sst{d}", [128, 128]) for d in range(2)]
        for b in range(NB):
            for hh in range(8):
                hs = slice(hh * 128, (hh + 1) * 128)
                for d in range(2):
                    for l_ in range(2):
                        k.dma("sp", lbt[:, d * 4 + l_:d * 4 + l_ + 1],
                              I["c_lower_bounds"][d, l_, hs].rearrange("(p o) -> p o", o=1))
                    k.tt("dve", lbt[:, d * 4 + 2:d * 4 + 3], lbt[:, d * 4:d * 4 + 1], lbt[:, d * 4 + 1:d * 4 + 2], ALU.subtract)
                    k.act(lbt[:, d * 4 + 2:d * 4 + 3], lbt[:, d * 4 + 2:d * 4 + 3], AF.Sigmoid)
                    k.ts("dve", lbt[:, d * 4 + 3:d * 4 + 4], lbt[:, d * 4 + 2:d * 4 + 3], -1.0, 1.0, ALU.mult, ALU.add)
                    k.memset("dve", Sst[d][:], 0.0)
                segs = {0: [(0, LC, False)] + [(LC + i * 512, 512, True) for i in range(L // 512)],
                        1: [(0, LC, False)] + [(LC + i * 512, 512, True) for i in reversed(range(L // 512))]}
                for si in range(len(segs[0])):
                    for d in range(2):
                        t0, n, is_lat = segs[d][si]
                        nchunk = n // CH
                        Td = T_[d]
                        zsrc = S["ZF1" if d == 0 else "ZB1"]
                        k.dma("sp", Td["z"][:, 0:n], zsrc[b, hs, t0:t0 + n])
                        k.dma("sp", vt[d][:, 0:nchunk, :], S["V1"][b, t0:t0 + n, hs].rearrange("(c s) e -> s c e", s=CH))
                        k.act(Td["f"][:, 0:n], Td["z"][:, 0:n], AF.Sigmoid)
                        k.ts("dve", Td["f"][:, 0:n], Td["f"][:, 0:n], lbt[:, d * 4 + 3:d * 4 + 4], lbt[:, d * 4 + 2:d * 4 + 3],
                             ALU.mult, ALU.add)
                        k.act(Td["lf"][:, 0:n], Td["f"][:, 0:n], AF.Ln)
                        k.ts("dve", Td["kk"][:, 0:n], Td["f"][:, 0:n], -1.0, 1.0, ALU.mult, ALU.add)
                        k.scan("dve", Td["bc"][:, 0:n], cmask[:, 0:n], Td["lf"][:, 0:n], 0.0, ALU.mult, ALU.add)
                        bc3 = Td["bc"][:, 0:n].rearrange("p (c s) -> p c s", s=CH)
                        bend = bc3[:, :, CH - 1:CH]
                        if d == 0:
                            bbv = Td["bc"]
                        else:
                            k.tt("dve", Td["bb"][:, 0:n].rearrange("p (c s) -> p c s", s=CH), bend.to_broadcast([128, nchunk, CH]),
                                 bc3, ALU.subtract)
                            k.tt("dve", Td["bb"][:, 0:n], Td["bb"][:, 0:n], Td["lf"][:, 0:n], ALU.add)
                            bbv = Td["bb"]
                        k.act(ebe[d][:, 0:nchunk], bend.rearrange("p c o -> p (c o)"), AF.Exp)
                        k.act(Td["tmp"][:, 0:n], bbv[:, 0:n], AF.Exp, scale=-1.0)
                        k.tt("dve", Td["kt"][:, 0:n], Td["kk"][:, 0:n], Td["tmp"][:, 0:n], ALU.mult)
                        k.tt("dve", Td["kh"][:, 0:n].rearrange("p (c s) -> p c s", s=CH),
                             Td["kt"][:, 0:n].rearrange("p (c s) -> p c s", s=CH),
                             ebe[d][:, 0:nchunk].unsqueeze(2).to_broadcast([128, nchunk, CH]), ALU.mult)
                        if is_lat:
                            k.dma("sp", Td["z"][:, 0:n], S["QT1"][b, hs, t0 - LC:t0 - LC + n])
                            k.act(Td["eb"][:, 0:n], bbv[:, 0:n], AF.Exp)
                            k.act(Td["qt"][:, 0:n], Td["z"][:, 0:n], AF.Silu)
                            k.tt("dve", Td["qt"][:, 0:n], Td["qt"][:, 0:n], Td["eb"][:, 0:n], ALU.mult)
                        for half in range((nchunk + 3) // 4):
                            ps = PS[d * 4 + 3]
                            nn = min(4, nchunk - half * 4)
                            for c in range(nn):
                                cc = half * 4 + c
                                k.mm(ps[0:64, c * 128:(c + 1) * 128], Td["kh"][:, cc * CH:(cc + 1) * CH], ident[:])
                            k.copy("act", kht[d][:, half * 4:half * 4 + nn, :],
                                   ps[0:64, 0:nn * 128].rearrange("p (c e) -> p c e", e=128))
                        order = range(nchunk) if d == 0 else reversed(range(nchunk))
                        for c in order:
                            cs = slice(c * CH, (c + 1) * CH)
                            if is_lat:
                                pa = PS[d * 4 + 0]
                                k.mm(pa[0:64, 0:64], Td["kt"][:, cs], Td["qt"][:, cs])
                                k.tt("dve", am[d][:], pa[0:64, 0:64], tri[d][:], ALU.mult)
                                po = PS[d * 4 + 1]
                                k.mm(po[0:64, 0:128], am[d][:], vt[d][:, c, :], start=True, stop=False)
                                k.mm(po[0:64, 0:128], Td["qt"][:, cs], Sst[d][:], start=False, stop=True)
                                k.copy("act", ot[d][:, c, :], po[0:64, 0:128])
                            pd = PS[d * 4 + 2]
                            k.mm(pd[:, 0:128], kht[d][:, c, :], vt[d][:, c, :])
                            k.stt("dve", Sst[d][:], Sst[d][:], ebe[d][:, c:c + 1], pd[:, 0:128], ALU.mult, ALU.add)
                        if is_lat:
                            k.dma("pool", S["O1"][d, b, t0 - LC:t0 - LC + n, hs].rearrange("(c s) e -> s c e", s=CH),
                                  ot[d][:, 0:nchunk, :], wb=[Buf("x")])
                k.barrier()


def phase_hgrn2_out(k, I, S, C):
    PS = C["PS"]
    ident = C["ident"]
    with contextlib.ExitStack() as st:
        ng = k.tile(st, "ng", [128, 1024])
        load_bcast(k, "sp", ng[:], I["c_norm_g"][0:1, :])
        of_ = [k.tile(st, f"ro_f{i}", [128, 1024]) for i in range(2)]
        ob_ = [k.tile(st, f"ro_b{i}", [128, 1024]) for i in range(2)]
        gg = [k.tile(st, f"ro_g{i}", [128, 1024]) for i in range(2)]
        o = k.tile(st, "ro_o", [128, 1024])
        sq = k.tile(st, "ro_sq", [128, 1024])
        ss = k.tile(st, "ro_ss", [128, 8])
        oT = [k.tile(st, f"ro_oT{i}", [128, 8, 128]) for i in range(2)]
        it = 0
        for b in range(NB):
            for t in range(L // 128):
                i2 = it % 2
                r0 = t * 128
                k.dma("sp", of_[i2][:], S["O1"][0, b, r0:r0 + 128, :])
                k.dma("sp", ob_[i2][:], S["O1"][1, b, r0:r0 + 128, :])
                k.dma("sp", gg[i2][:], S["G1"][b, r0:r0 + 128, :])
                k.tt("dve", o[:], of_[i2][:], ob_[i2][:], ALU.add)
                k.act(sq[:], o[:], AF.Square)
                k.op("dve", lambda: k.nc.vector.reduce_sum(ss[:], sq[:].rearrange("p (h e) -> p h e", e=128), AX.X),
                     [ss[:]], [sq[:]])
                k.ts("dve", ss[:], ss[:], 1.0 / 128, EPS, ALU.mult, ALU.add)
                k.act(ss[:], ss[:], AF.Sqrt)
                k.recip(ss[:], ss[:])
                k.tt("dve", o[:].rearrange("p (h e) -> p h e", e=128), o[:].rearrange("p (h e) -> p h e", e=128),
                     ss[:].unsqueeze(2).to_broadcast([128, 8, 128]), ALU.mult)
                k.tt("dve", o[:], o[:], ng[:], ALU.mult)
                k.act(gg[i2][:], gg[i2][:], AF.Sigmoid)
                k.tt("dve", o[:], o[:], gg[i2][:], ALU.mult)
                transpose_1024(k, C, o, oT[i2])
                k.dma("pool", S["OT1"][b].rearrange("(j p) t -> p j t", p=128)[:, :, r0:r0 + 128], oT[i2][:], wb=[Buf("x")])
                it += 1


def l1_moe_tiles(I, S, OUT):
    tiles = []
    for b in range(NB):
        for i in range(L // 512):
            t0 = i * 512
            tiles.append(dict(row=b, ysrc=[(S["OT1"][b, :, t0:t0 + 512], 512)],
                              xsrc=[S["X1"][b, t0 + j * 128:t0 + (j + 1) * 128, :] for j in range(4)],
                              dst=[OUT[b, t0 + j * 128:t0 + (j + 1) * 128, :] for j in range(4)]))
    return tiles


_CACHE = {}


def kernel(**inputs):
    x = np.ascontiguousarray(inputs["x"], dtype=np.float32)
    c = np.asarray(inputs["c"], dtype=np.float32)
    ctx = np.ascontiguousarray(inputs["ctx"], dtype=np.float32)
    c_ctx = np.asarray(inputs["c_ctx"], dtype=np.float32)
    if "nc" not in _CACHE:
        _CACHE["nc"] = build()
    nc = _CACHE["nc"]
    consts = host_consts()
    shared = {n: np.ascontiguousarray(inputs[n], dtype=np.float32) for n in WEIGHT_SHAPES}
    shared.update(consts)
    in_maps = []
    for i in range(NCORES):
        m = dict(shared)
        m["x"] = x[i * NB:(i + 1) * NB]
        m["ctx"] = ctx[i * NB:(i + 1) * NB]
        m["cvec"] = np.concatenate([c[i * NB:(i + 1) * NB], c_ctx[None, :]], axis=0)
        in_maps.append(m)
    res = run_bass_kernel_spmd(nc, in_maps, core_ids=list(range(NCORES)))
    return np.concatenate([r["out"] for r in res.results], axis=0)
```

```python
import contextlib
import math
import numpy as np
import concourse.bass as bass
import concourse.mybir as mybir
from concourse.bass_utils import run_bass_kernel_spmd

F32 = mybir.dt.float32
BF16 = mybir.dt.bfloat16
ALU = mybir.AluOpType
AF = mybir.ActivationFunctionType
AX = mybir.AxisListType
AP = bass.AP

NCORES = 8
NB = 2
L = 4096
LC = 256
D = 1024
HY = 768
S5D = 256
EPS = 1e-6
MAGIC = 12582912.0
TWO_PI = 2.0 * math.pi


class Buf:
    __slots__ = ("w", "r", "name")

    def __init__(self, name):
        self.name = name
        self.w = None
        self.r = []


class Tile(Buf):
    __slots__ = ("t", "sub")

    def __init__(self, name, t):
        Buf.__init__(self, name)
        self.t = t
        self.sub = [Buf(f"{name}.{i}") for i in range(4)]

    def __getitem__(self, key):
        return self.t[key]


class DT(Buf):
    __slots__ = ("h", "a", "regions")

    def __init__(self, name, h):
        Buf.__init__(self, name)
        self.h = h
        self.a = h.ap()
        self.regions = {}

    def __getitem__(self, key):
        return self.a[key]

    def reg(self, key):
        b = self.regions.get(key)
        if b is None:
            b = Buf(f"{self.name}:{key}")
            self.regions[key] = b
        return b


class KB:
    ENG = ("pe", "act", "dve", "pool", "sp")

    def __init__(self, nc):
        self.nc = nc
        self.es = contextlib.ExitStack()
        self.eng = {"pe": nc.tensor, "act": nc.scalar, "dve": nc.vector, "pool": nc.gpsimd, "sp": nc.sync}
        self.sems = {}
        self.sem_list = []
        self.cnt = {}
        self.cur = {}
        self.spare = {e: [] for e in self.ENG}
        nsem = {"pe": 9, "act": 3, "dve": 12, "pool": 3, "sp": 1}
        for e in self.ENG:
            for i in range(nsem[e]):
                s = self.es.enter_context(nc.semaphore(f"s_{e}{i}"))
                self.spare[e].append(self._reg_sem(s))
            self.cur[e] = self.spare[e].pop(0)
        self.dring = {}
        for e in ("sp", "pool", "act"):
            ring = []
            for i in range(12):
                s = self.es.enter_context(nc.semaphore(f"d_{e}{i}"))
                ring.append(self._reg_sem(s))
            self.dring[e] = [ring, 0]
        self.waited = {e: {} for e in self.ENG}
        self.bufs = {}
        self.ninstr = 0

    def _reg_sem(self, s):
        self.sem_list.append(s)
        self.cnt[len(self.sem_list) - 1] = 0
        return len(self.sem_list) - 1

    def tile(self, stack, name, shape, dt=F32):
        self.ninstr += 1
        name = f"t{self.ninstr}_" + name
        t = stack.enter_context(self.nc.sbuf_tensor(name, list(shape), dt))
        tl = Tile(name, t)
        self.bufs[name] = tl
        return tl

    def ptile(self, stack, name, shape, dt=F32):
        name = "t_" + name
        t = stack.enter_context(self.nc.psum_tensor(name, list(shape), dt))
        tl = Tile(name, t)
        self.bufs[name] = tl
        return tl

    def dram(self, name, shape, dt=F32, kind="Internal"):
        h = self.nc.dram_tensor(name, list(shape), dt, kind=kind)
        d = DT(name, h)
        self.bufs[name] = d
        return d

    def buf_of(self, ap):
        return self.bufs[ap.tensor.name]

    def _wait(self, e, tok):
        si, val = tok
        if self.waited[e].get(si, 0) >= val:
            return
        self.eng[e].wait_ge(self.sem_list[si], val)
        self.waited[e][si] = val

    def _sync(self, e, rb, wb):
        own = self.cur[e]
        for b in rb:
            if b.w is not None and not (e == "pe" and b.w[0] == own):
                self._wait(e, b.w)
        for b in wb:
            if b.w is not None and not (e == "pe" and b.w[0] == own):
                self._wait(e, b.w)
            for t in b.r:
                if not (e == "pe" and t[0] == own):
                    self._wait(e, t)

    def _mark(self, tok, rb, wb):
        for b in rb:
            b.r.append(tok)
            if len(b.r) > 24:
                best = {}
                for t in b.r:
                    if best.get(t[0], 0) < t[1]:
                        best[t[0]] = t[1]
                b.r = list(best.items())
        for b in wb:
            b.w = tok
            b.r = []

    def op(self, e, fn, outs, ins, rb=None, wb=None):
        if rb is None:
            rb = [self.buf_of(a) for a in ins if isinstance(a, AP)]
        if wb is None:
            wb = [self.buf_of(a) for a in outs if isinstance(a, AP)]
        self._sync(e, rb, wb)
        ins_ = fn()
        si = self.cur[e]
        self.cnt[si] += 1
        ins_.then_inc(self.sem_list[si], 1)
        tok = (si, self.cnt[si])
        self._mark(tok, rb, wb)
        self.ninstr += 1
        return tok

    def dma(self, e, out, in_, rb=None, wb=None, **kw):
        if rb is None:
            rb = [self.buf_of(in_)]
        if wb is None:
            wb = [self.buf_of(out)]
        self._sync(e, rb, wb)
        ring, pos = self.dring[e]
        si = ring[pos % len(ring)]
        self.dring[e][1] = pos + 1
        self._wait(e, (si, self.cnt[si]))
        self.cnt[si] += 16
        self.eng[e].dma_start(out=out, in_=in_, **kw).then_inc(self.sem_list[si], 16)
        tok = (si, self.cnt[si])
        self._mark(tok, rb, wb)
        self.ninstr += 1
        return tok

    def barrier(self):
        toks = [(si, c) for si, c in self.cnt.items() if c > 0]
        for e in self.ENG:
            for t in toks:
                if t[0] == self.cur[e]:
                    continue
                self._wait(e, t)
        for e in self.ENG:
            if self.cnt[self.cur[e]] > 16000 and self.spare[e]:
                self.cur[e] = self.spare[e].pop(0)

    def mm(self, out, lhsT, rhs, start=True, stop=True):
        return self.op("pe", lambda: self.nc.tensor.matmul(out, lhsT, rhs, start=start, stop=stop), [out], [lhsT, rhs])

    def act(self, out, in_, func, bias=0.0, scale=1.0, accum_out=None, e="act"):
        outs = [out] + ([accum_out] if accum_out is not None else [])
        ins = [in_] + [a for a in (bias, scale) if isinstance(a, AP)]
        kw = {}
        if accum_out is not None:
            kw["accum_out"] = accum_out
        return self.op("act", lambda: self.nc.scalar.activation(out, in_, func, bias=bias, scale=scale, **kw), outs, ins)

    def copy(self, e, out, in_):
        if e == "act":
            return self.op("act", lambda: self.nc.scalar.copy(out, in_), [out], [in_])
        return self.op(e, lambda: self.eng[e].tensor_copy(out, in_), [out], [in_])

    def tt(self, e, out, a, b, op):
        return self.op(e, lambda: self.eng[e].tensor_tensor(out, a, b, op), [out], [a, b])

    def ts(self, e, out, a, s1, s2, op0, op1=None, accum_out=None):
        outs = [out] + ([accum_out] if accum_out is not None else [])
        ins = [a] + [s for s in (s1, s2) if isinstance(s, AP)]
        if op1 is None:
            return self.op(e, lambda: self.eng[e].tensor_single_scalar(out, a, s1, op0), outs, ins)
        kw = {}
        if accum_out is not None:
            kw["accum_out"] = accum_out
        return self.op(e, lambda: self.eng[e].tensor_scalar(out, a, s1, s2, op0, op1, **kw), outs, ins)

    def stt(self, e, out, a, s, b, op0, op1):
        ins = [a, b] + ([s] if isinstance(s, AP) else [])
        return self.op(e, lambda: self.eng[e].scalar_tensor_tensor(out, a, s, b, op0, op1), [out], ins)

    def memset(self, e, out, val):
        return self.op(e, lambda: self.eng[e].memset(out, val), [out], [])

    def recip(self, out, in_):
        return self.op("dve", lambda: self.nc.vector.reciprocal(out, in_), [out], [in_])

    def scan(self, e, out, d0, d1, init, op0, op1):
        ins = [d0, d1] + ([init] if isinstance(init, AP) else [])
        return self.op(e, lambda: self.eng[e].tensor_tensor_scan(out, d0, d1, init, op0, op1), [out], ins)


def _cplx_lhsT(W):
    return np.block([[W.real, W.imag], [-W.imag, W.real]])


def fft_tables():
    t = {}
    n1 = np.arange(128)[:, None].astype(np.float64)
    k1 = np.arange(128)[None, :].astype(np.float64)
    ang = 2.0 * np.pi * n1 * k1 / 128.0
    cs, sn = np.cos(ang), np.sin(ang)
    t["ff1"] = np.concatenate([cs, -sn], axis=1).astype(np.float32)
    t["f1d"] = np.concatenate([np.concatenate([cs[:64], -sn[:64]], axis=1),
                               np.concatenate([sn[:64], cs[:64]], axis=1)], axis=0).astype(np.float32)
    n2 = np.arange(64)[:, None].astype(np.float64)
    k2 = np.arange(64)[None, :].astype(np.float64)
    T2 = np.zeros((128, 128, 128), np.float32)
    for k1_ in range(128):
        W = np.exp(-2j * np.pi * n2 * (k1_ + 128.0 * k2) / 8192.0)
        T2[k1_] = _cplx_lhsT(W)
    t["fft_T2"] = T2
    Wi = np.exp(2j * np.pi * np.arange(64)[:, None] * np.arange(64)[None, :] / 64.0)
    G1 = _cplx_lhsT(Wi)
    Q = np.block([[np.zeros((64, 64)), -np.eye(64)], [np.eye(64), np.zeros((64, 64))]])
    t["fft_G1"] = G1.astype(np.float32)
    t["fft_G2"] = (Q.T @ G1).astype(np.float32)
    T4 = np.zeros((64, 2, 128, 128), np.float32)
    kk = np.arange(128)[:, None].astype(np.float64)
    nn = np.arange(64)[None, :].astype(np.float64)
    for n2_ in range(64):
        W = np.exp(2j * np.pi * (n2_ * kk / 8192.0 + nn * kk / 128.0)) / 8192.0
        T4[n2_, 0] = np.concatenate([W.real, W.imag], axis=1)
        T4[n2_, 1] = np.concatenate([-W.imag, W.real], axis=1)
    t["fft_T4"] = T4
    return t


def host_consts():
    c = {}
    c["ident"] = np.eye(128, dtype=np.float32)
    c["antiI"] = np.eye(128, dtype=np.float32)[::-1].copy()
    c["ones"] = np.ones((128, 128), np.float32)
    for nm, Lf in (("lat", L), ("ctx", LC)):
        pos = np.arange(Lf, dtype=np.float32)
        t = pos / np.float32(max(Lf - 1, 1))
        w = np.float32(2.0 * math.pi) * pos / np.float32(Lf)
        bands = np.linspace(1e-4, 15, 16, dtype=np.float32)
        ang = w[:, None] * bands[None, :]
        z = np.concatenate([t[:, None], np.cos(ang), -np.sin(ang)], axis=-1).astype(np.float32)
        c["zT_" + nm] = np.ascontiguousarray(z.T)
        c["trow_" + nm] = np.ascontiguousarray(np.broadcast_to(t[None, :], (128, Lf))).astype(np.float32)
    deltas = np.abs(np.linspace(math.log(1e-2) / 1.5, math.log(1e-2) / 0.3, HY, dtype=np.float32))
    c["hy_ndelta"] = (-deltas).reshape(HY, 1).astype(np.float32)
    c["ndelta_row"] = (-deltas).reshape(1, HY).astype(np.float32)
    tl = (np.arange(L, dtype=np.float32) / np.float32(L - 1)).reshape(32, 128).T
    c["tneg_lat"] = np.ascontiguousarray(tl).astype(np.float32)
    c.update(fft_tables())
    cm = np.ones((128, 512), np.float32)
    cm[:, ::64] = 0.0
    c["cmask"] = cm
    c["triu"] = np.triu(np.ones((64, 64), np.float32))
    c["tril"] = np.tril(np.ones((64, 64), np.float32))
    c["iota1"] = np.ascontiguousarray(np.broadcast_to(np.arange(1, 257, dtype=np.float32)[None, :], (128, 256)))
    return c


CONST_SHAPES = {"ident": [128, 128], "antiI": [128, 128], "ones": [128, 128], "zT_lat": [33, L], "zT_ctx": [33, LC],
                "trow_lat": [128, L], "trow_ctx": [128, LC], "hy_ndelta": [HY, 1], "iota1": [128, 256], "cmask": [128, 512], "triu": [64, 64], "tril": [64, 64],
                "ff1": [128, 256], "f1d": [128, 256], "fft_T2": [128, 128, 128], "fft_G1": [128, 128], "fft_G2": [128, 128],
                "fft_T4": [64, 2, 128, 128], "tneg_lat": [128, 32], "ndelta_row": [1, HY]}

WEIGHT_SHAPES = {
    "mod_w": [2, 1024, 6144], "mod_b": [2, 6144], "norm1_g": [2, 1024], "norm2_g": [2, 1024], "final_g": [1024],
    "ab_w_in": [1, 1024, 2560], "ab_w_out": [1, 1024, 1024], "hy_conv_w": [1, 3, 2304], "hy_conv_b": [1, 2304],
    "hy_fw1": [1, 33, 64], "hy_fb1": [1, 64], "hy_ff1": [1, 64], "hy_fw2": [1, 64, 64], "hy_fb2": [1, 64],
    "hy_ff2": [1, 64], "hy_fw3": [1, 64, 3072], "hy_bias": [1, 2, 768],
    "s5_lam_re": [1, 2, 16, 64], "s5_lam_im": [1, 2, 16, 64], "s5_log_step": [1, 2, 16],
    "s5_b_re": [1, 2, 16, 64, 16], "s5_b_im": [1, 2, 16, 64, 16], "s5_c_re": [1, 2, 16, 16, 64],
    "s5_c_im": [1, 2, 16, 16, 64], "s5_d": [1, 256], "s5_glu_w": [1, 256, 512], "s5_glu_b": [1, 512],
    "c_w_in": [1, 1024, 5120], "c_w_out": [1, 1024, 1024], "c_lower_bounds": [2, 2, 1024], "c_norm_g": [1, 1024],
    "moe_wg": [2, 1024, 4], "moe_bg": [2, 4], "moe_we": [2, 1024, 32], "moe_be": [2, 32],
    "moe_w_gate": [2, 4, 8, 1024, 256], "moe_w_up": [2, 4, 8, 1024, 256], "moe_w_down": [2, 4, 8, 256, 1024],
}


DBGF = set()
USE_FFT = True


def build(stop=99, dbg=()):
    DBGF.clear()
    DBGF.update(dbg)
    nc = bass.Bass("TRN2", target_bir_lowering=False)
    k = KB(nc)
    I = {}
    I["x"] = k.dram("x", [NB, L, D], kind="ExternalInput")
    I["ctx"] = k.dram("ctx", [NB, LC, D], kind="ExternalInput")
    I["cvec"] = k.dram("cvec", [3, D], kind="ExternalInput")
    for n, s in WEIGHT_SHAPES.items():
        I[n] = k.dram(n, s, kind="ExternalInput")
    for n, s in CONST_SHAPES.items():
        I[n] = k.dram(n, s, kind="ExternalInput")
    OUT = k.dram("out", [NB, L, D], kind="ExternalOutput")

    def scratch(name, shape):
        return k.dram(name, shape, kind=("ExternalOutput" if name in dbg else "Internal"))

    S = {}
    S["MV"] = scratch("MV", [2, 3, 6, D])
    S["PT0"] = scratch("PT0", [NB, 2560, LC + L])
    S["UT0"] = scratch("UT0", [NB, LC + L, 256])
    S["YT0"] = scratch("YT0", [NB, 1024, LC + L])
    S["P0"] = scratch("P0", [NB, L + 2, 2304])
    S["KT"] = scratch("KT", [2 * L, 2 * HY])
    S["HB"] = scratch("HB", [L + 128, 2 * HY])
    S["ESC"] = scratch("ESC", [1, 2 * HY])
    S["KF"] = scratch("KF", [2, 128, 128, HY])
    S["FS1"] = scratch("FS1", [2, 64, 128, CC])
    S["FS2"] = scratch("FS2", [2, 64, 128, CC])
    S["YH"] = scratch("YH", [NB, L, HY])
    S["X1"] = scratch("X1", [NB, L, D])
    S["CTX1"] = scratch("CTX1", [NB, LC, D])
    S["HT1"] = scratch("HT1", [NB, D, TT])
    S["QT1"] = scratch("QT1", [NB, D, L])
    S["ZF1"] = scratch("ZF1", [NB, D, TT])
    S["ZB1"] = scratch("ZB1", [NB, D, TT])
    S["V1"] = scratch("V1", [NB, TT, D])
    S["G1"] = scratch("G1", [NB, L, D])
    S["O1"] = scratch("O1", [2, NB, L, D])
    S["OT1"] = scratch("OT1", [NB, D, L])
    S["DBG"] = scratch("DBG", [16, 128, 4096])
    S["YS5"] = scratch("YS5", [2, NB, LC + L, 256])

    with k.es:
        glob = k.es
        ident = k.tile(glob, "ident", [128, 128])
        antiI = k.tile(glob, "antiI", [128, 128])
        ones = k.tile(glob, "ones", [128, 128])
        k.dma("sp", ident[:], I["ident"][:, :])
        k.dma("sp", antiI[:], I["antiI"][:, :])
        k.dma("sp", ones[:], I["ones"][:, :])
        PS = [k.ptile(glob, f"ps{i}", [128, 512]) for i in range(8)]
        C = dict(ident=ident, antiI=antiI, ones=ones, PS=PS)

        phase_mod(k, I, S, C)
        k.barrier()
        if stop >= 1:
            phase_l0_inproj(k, I, S, C)
            k.barrier()
        if stop >= 2 and "nohy" not in dbg:
            phase_hyena(k, I, S, C)
            k.barrier()
            if USE_FFT:
                phase_hyena_fft(k, I, S, C)
                k.barrier()
        if stop >= 3:
            phase_s5(k, I, S, C)
            k.barrier()
            phase_s5_out(k, I, S, C)
            k.barrier()
        if stop >= 4:
            tl = l0_moe_tiles(I, S)
            if "moe1" in DBGF:
                tl = tl[:2]
            phase_moe(k, I, S, C, 0, S["YT0"], I["ab_w_out"][0], tl, final=False)
            k.barrier()
        if stop >= 5:
            phase_l1_norm(k, I, S, C)
            k.barrier()
            phase_l1_inproj(k, I, S, C)
            k.barrier()
        if stop >= 6:
            phase_hgrn2(k, I, S, C)
            k.barrier()
            phase_hgrn2_out(k, I, S, C)
            k.barrier()
        if stop >= 7:
            tl = l1_moe_tiles(I, S, OUT)
            if "moe1" in DBGF:
                tl = tl[:1]
            phase_moe(k, I, S, C, 1, S["OT1"], I["c_w_out"][0], tl, final=True)
            k.barrier()
        k.barrier()
    return nc


def phase_mod(k, I, S, C):
    PS = C["PS"]
    with contextlib.ExitStack() as st:
        cT = k.tile(st, "cT", [128, 3, 8])
        sT = k.tile(st, "sT", [128, 3, 8])
        for r in range(3):
            k.dma("sp", cT[:, r, :], I["cvec"][r].rearrange("(k p) -> p k", p=128), allow_slow_non_contiguous=True,
                  wb=[Buf("tmp")])
        k.barrier()
        k.act(sT[:], cT[:], AF.Silu)
        wt = [k.tile(st, f"modw{i}", [128, 8, 512]) for i in range(2)]
        mv = k.tile(st, "mv", [3, 6144])
        bias = k.tile(st, "modb", [3, 6144])
        g1 = k.tile(st, "g1b", [3, 1024])
        g2 = k.tile(st, "g2b", [3, 1024])
        for l in range(2):
            k.dma("sp", bias[:], I["mod_b"][l:l + 1, :].partition_broadcast(3))
            k.dma("sp", g1[:], I["norm1_g"][l:l + 1, :].partition_broadcast(3))
            k.dma("sp", g2[:], I["norm2_g"][l:l + 1, :].partition_broadcast(3))
            wv = I["mod_w"][l].rearrange("(k p) n -> p k n", p=128)
            for j in range(12):
                w = wt[j % 2]
                k.dma("sp" if j % 2 == 0 else "pool", w[:], wv[:, :, j * 512:(j + 1) * 512])
                ps = PS[j % 2]
                for kk in range(8):
                    k.mm(ps[0:3, :], sT[:, :, kk], w[:, kk, :], start=(kk == 0), stop=(kk == 7))
                k.tt("dve", mv[:, j * 512:(j + 1) * 512], ps[0:3, :], bias[:, j * 512:(j + 1) * 512], ALU.add)
            k.stt("dve", mv[:, 1024:2048], mv[:, 1024:2048], 1.0, g1[:], ALU.add, ALU.mult)
            k.stt("dve", mv[:, 4096:5120], mv[:, 4096:5120], 1.0, g2[:], ALU.add, ALU.mult)
            k.dma("sp", S["MV"][l].rearrange("r c d -> r (c d)"), mv[:])


def load_bcast(k, eng, tile_ap, dram_row_ap):
    return k.dma(eng, tile_ap, dram_row_ap.partition_broadcast(tile_ap.shape[0]))


def rms_mod(k, st_tmp, xt, A, sh, out, name):
    sq, ss, rstd = st_tmp
    k.act(sq[:], xt, AF.Square, accum_out=ss[:])
    k.ts("dve", ss[:], ss[:], 1.0 / D, EPS, ALU.mult, ALU.add)
    k.act(ss[:], ss[:], AF.Sqrt)
    k.recip(rstd[:], ss[:])
    k.stt("dve", out, xt, rstd[:, 0:1], A, ALU.mult, ALU.mult)
    k.tt("dve", out, out, sh, ALU.add)


def transpose_1024(k, C, src, dstT, pbase=4):
    PS = C["PS"]
    for half in range(2):
        ps = PS[pbase + half]
        for j in range(4):
            kk = half * 4 + j
            k.mm(ps[:, j * 128:(j + 1) * 128], src[:, kk * 128:(kk + 1) * 128], C["ident"][:], start=True, stop=True)
        k.copy("act", dstT[:, half * 4:(half + 1) * 4, :], ps[:].rearrange("p (j t) -> p j t", j=4))


TT = LC + L


def phase_l0_inproj(k, I, S, C):
    PS = C["PS"]
    with contextlib.ExitStack() as st:
        W = k.tile(st, "Win0", [128, 8, 2560])
        wv = I["ab_w_in"][0].rearrange("(k p) n -> p k n", p=128)
        for kk in range(8):
            k.dma("sp" if kk % 2 == 0 else "pool", W[:, kk, :], wv[:, kk, :], wb=[Buf("tmp")])
        k.barrier()
        A = k.tile(st, "A1", [128, 1024])
        sh = k.tile(st, "sh1", [128, 1024])
        xt = [k.tile(st, f"xt{i}", [128, 1024]) for i in range(2)]
        h = k.tile(st, "h", [128, 1024])
        hT = k.tile(st, "hT", [128, 8, 128])
        po = [k.tile(st, f"po{i}", [128, 20, 128]) for i in range(2)]
        pu = [k.tile(st, f"pu{i}", [128, 256]) for i in range(2)]
        pt = [k.tile(st, f"pt{i}", [128, 2304]) for i in range(2)]
        zrow = k.tile(st, "zrow", [1, 2304])
        k.memset("dve", zrow[:], 0.0)
        for b in range(NB):
            k.dma("sp", S["P0"][b, 0:1, :], zrow[:], wb=[Buf("x")])
            k.dma("sp", S["P0"][b, L + 1:L + 2, :], zrow[:], wb=[Buf("x")])
        sq = k.tile(st, "sq", [128, 1024])
        ss = k.tile(st, "ss", [128, 1])
        rstd = k.tile(st, "rstd", [128, 1])
        it = 0
        for b in range(NB):
            for (src, row, ntile, toff) in ((I["ctx"], 2, LC // 128, 0), (I["x"], b, L // 128, LC)):
                load_bcast(k, "sp", A[:], S["MV"][0, row, 1:2, :])
                load_bcast(k, "sp", sh[:], S["MV"][0, row, 0:1, :])
                for t in range(ntile):
                    x_ = xt[it % 2]
                    o_ = po[it % 2]
                    u_ = pu[it % 2]
                    k.dma("sp", x_[:], src[b, t * 128:(t + 1) * 128, :])
                    rms_mod(k, (sq, ss, rstd), x_[:], A[:], sh[:], h[:], "l0")
                    transpose_1024(k, C, h, hT)
                    for j4 in range(5):
                        ps = PS[j4 % 4]
                        for jj in range(4):
                            j = j4 * 4 + jj
                            for kk in range(8):
                                k.mm(ps[:, jj * 128:(jj + 1) * 128], W[:, kk, j * 128:(j + 1) * 128], hT[:, kk, :],
                                     start=(kk == 0), stop=(kk == 7))
                        k.copy("act" if j4 % 2 == 0 else "dve", o_[:, j4 * 4:(j4 + 1) * 4, :],
                               ps[:].rearrange("p (j t) -> p j t", j=4))
                    ps = PS[6]
                    for kk in range(8):
                        k.mm(ps[:, 0:256], hT[:, kk, :], W[:, kk, 2304:2560], start=(kk == 0), stop=(kk == 7))
                    k.copy("dve", u_[:], ps[:, 0:256])
                    if toff == LC and USE_FFT:
                        p_ = pt[it % 2]
                        for j in range(5):
                            ps = PS[6 + j % 2]
                            w_ = 512 if j < 4 else 256
                            for kk in range(8):
                                k.mm(ps[:, 0:w_], hT[:, kk, :], W[:, kk, j * 512:j * 512 + w_], start=(kk == 0), stop=(kk == 7))
                            k.copy("act" if j % 2 == 0 else "dve", p_[:, j * 512:j * 512 + w_], ps[:, 0:w_])
                        k.dma("pool", S["P0"][b, 1 + t * 128:1 + (t + 1) * 128, :], p_[:], wb=[Buf("x")])
                    t0 = toff + t * 128
                    k.dma("pool", S["PT0"][b].rearrange("(j p) t -> p j t", p=128)[:, :, t0:t0 + 128], o_[:],
                          wb=[S["PT0"].reg((b, t0))])
                    k.dma("pool", S["UT0"][b, t0:t0 + 128, :], u_[:], wb=[S["UT0"].reg((b, t0))])
                    it += 1


def sin_rr(k, out, src, tmp_r, tmp_n, pre_scale=None, pre_bias=None):
    if pre_scale is not None:
        k.ts("dve", tmp_r, src, pre_bias, pre_scale, ALU.add, ALU.mult)
        k.ts("dve", tmp_r, tmp_r, 1.0 / TWO_PI, None, ALU.mult)
    else:
        k.ts("dve", tmp_r, src, 1.0 / TWO_PI, None, ALU.mult)
    k.ts("dve", tmp_n, tmp_r, MAGIC, MAGIC, ALU.add, ALU.subtract)
    k.tt("dve", tmp_r, tmp_r, tmp_n, ALU.subtract)
    k.act(out, tmp_r, AF.Sin, scale=TWO_PI)


def phase_hyena(k, I, S, C):
    PS = C["PS"]
    for (T, toff, zname, tname) in ((LC, 0, "zT_ctx", "trow_ctx"), (L, LC, "zT_lat", "trow_lat")):
        if ("hyctx" in DBGF or USE_FFT) and T == L:
            continue
        with contextlib.ExitStack() as st:
            h2 = k.tile(st, "hyh2", [64, T])
            with contextlib.ExitStack() as st2:
                zT = k.tile(st2, "zT", [33, T])
                k.dma("sp", zT[:], I[zname][:, :])
                fw1 = k.tile(st2, "fw1", [33, 64])
                fw2 = k.tile(st2, "fw2", [64, 64])
                k.dma("sp", fw1[:], I["hy_fw1"][0])
                k.dma("sp", fw2[:], I["hy_fw2"][0])
                prm = k.tile(st2, "hyprm", [64, 4])
                for i, n in enumerate(("hy_fb1", "hy_ff1", "hy_fb2", "hy_ff2")):
                    k.dma("sp", prm[:, i:i + 1], I[n][0].rearrange("(p o) -> p o", o=1), wb=[Buf("tmp")])
                k.barrier()
                h1 = k.tile(st2, "hyh1", [64, T])
                tr = k.tile(st2, "hytr", [64, 512])
                tn = k.tile(st2, "hytn", [64, 512])
                nb = min(T, 512)
                for blk in range(T // nb):
                    sl = slice(blk * nb, (blk + 1) * nb)
                    k.mm(PS[0][0:64, 0:nb], fw1[:], zT[:, sl])
                    sin_rr(k, h1[:, sl], PS[0][0:64, 0:nb], tr[:, 0:nb], tn[:, 0:nb], prm[:, 1:2], prm[:, 0:1])
                    k.mm(PS[1][0:64, 0:nb], fw2[:], h1[:, sl])
                    sin_rr(k, h2[:, sl], PS[1][0:64, 0:nb], tr[:, 0:nb], tn[:, 0:nb], prm[:, 3:4], prm[:, 2:3])
                if "DBG" in DBGF and T == LC:
                    k.dma("sp", S["DBG"][8, 0:64, 0:T], h1[:, :], wb=[Buf("x")])
                    k.dma("sp", S["DBG"][9, 0:64, 0:4], prm[:, :], wb=[Buf("x")])
                    k.dma("sp", S["DBG"][10, 0:33, 0:T], zT[:, :], wb=[Buf("x")])
                    k.dma("sp", S["DBG"][10, 64:97, 0:64], fw1[:, :], wb=[Buf("x")])
                k.barrier()
            fw3 = k.tile(st, "fw3", [64, 3072])
            k.dma("sp", fw3[:], I["hy_fw3"][0])
            trow = k.tile(st, "trow", [128, T])
            k.dma("sp", trow[:], I[tname][:, :])
            dec = k.tile(st, "dec", [128, T])
            kfb = k.tile(st, "kfb", [128, NB, T])
            raw = k.tile(st, "raw", [128, NB, T + 2])
            v = k.tile(st, "v", [128, NB, T])
            acc = k.tile(st, "acc", [128, NB, T])
            prm = k.tile(st, "cprm", [128, 3, 8])
            en = k.tile(st, "en", [128, 4])
            k.memset("dve", raw[:], 0.0)
            nb = min(T, 512)

            def load_conv(ct, blk, dst):
                col = blk * HY + ct * 128
                for b in range(NB):
                    k.dma("sp", raw[:, b, 1:T + 1], S["PT0"][b, col:col + 128, toff:toff + T])
                k.ts("dve", dst, raw[:, :, 0:T], prm[:, blk, 0:1], prm[:, blk, 3:4], ALU.mult, ALU.add)
                k.stt("dve", dst, raw[:, :, 1:T + 1], prm[:, blk, 1:2], dst, ALU.mult, ALU.add)
                k.stt("dve", dst, raw[:, :, 2:T + 2], prm[:, blk, 2:3], dst, ALU.mult, ALU.add)

            for ct in range(HY // 128):
                for blk in range(3):
                    col = blk * HY + ct * 128
                    k.dma("sp", prm[:, blk, 0:3], I["hy_conv_w"][0][:, col:col + 128].rearrange("j c -> c j"),
                          allow_slow_non_contiguous=True)
                    k.dma("sp", prm[:, blk, 3:4], I["hy_conv_b"][0][col:col + 128].rearrange("(p o) -> p o", o=1))
                for o in range(2):
                    k.dma("sp", prm[:, 0, 4 + o:5 + o], I["hy_bias"][0][o, ct * 128:(ct + 1) * 128].rearrange("(p o) -> p o", o=1))
                k.dma("sp", prm[:, 0, 6:7], I["hy_ndelta"][ct * 128:(ct + 1) * 128, :])
                k.act(dec[:], trow[:], AF.Exp, scale=prm[:, 0, 6:7])
                load_conv(ct, 2, v[:])
                for o in range(2):
                    for d_ in (0, 1):
                        col = o * 2 * HY + d_ * HY + ct * 128
                        for blk in range(T // nb):
                            sl = slice(blk * nb, (blk + 1) * nb)
                            ps = PS[blk % 4]
                            k.mm(ps[:, 0:nb], fw3[:, col:col + 128], h2[:, sl])
                            k.tt("dve", kfb[:, d_, sl], ps[:, 0:nb], dec[:, sl], ALU.mult)
                    k.memset("dve", kfb[:, 1, 0:1], 0.0)
                    k.act(acc[:, 0, :], kfb[:, 0, :], AF.Square, accum_out=en[:, 0:1])
                    k.act(acc[:, 0, :], kfb[:, 1, :], AF.Square, accum_out=en[:, 1:2])
                    k.tt("dve", en[:, 2:3], en[:, 0:1], en[:, 1:2], ALU.add)
                    k.act(en[:, 2:3], en[:, 2:3], AF.Sqrt)
                    k.recip(en[:, 3:4], en[:, 2:3])
                    k.ts("dve", kfb[:], kfb[:], en[:, 3:4], None, ALU.mult)
                    if "DBG" in DBGF and ct == 0 and T == LC:
                        k.dma("sp", S["DBG"][o * 4 + 0, :, 0:T], kfb[:, 0, :], wb=[Buf("x")])
                        k.dma("sp", S["DBG"][o * 4 + 1, :, 0:T], kfb[:, 1, :], wb=[Buf("x")])
                        k.dma("sp", S["DBG"][o * 4 + 2, :, 0:T], v[:, 0, :], wb=[Buf("x")])
                        k.dma("sp", S["DBG"][o * 4 + 3, 0:64, 0:T], h2[:, :], wb=[Buf("x")])
                    k.ts("dve", acc[:], v[:], kfb[:, 0, 0:1], None, ALU.mult)
                    for lag in range(1, T):
                        k.stt("dve", acc[:, :, lag:T], v[:, :, 0:T - lag], kfb[:, 0, lag:lag + 1], acc[:, :, lag:T],
                              ALU.mult, ALU.add)
                        k.stt("dve", acc[:, :, 0:T - lag], v[:, :, lag:T], kfb[:, 1, lag:lag + 1], acc[:, :, 0:T - lag],
                              ALU.mult, ALU.add)
                    k.stt("dve", acc[:], v[:], prm[:, 0, 4 + o:5 + o], acc[:], ALU.mult, ALU.add)
                    load_conv(ct, o, kfb[:])
                    if o == 0:
                        k.tt("dve", v[:], acc[:], kfb[:], ALU.mult)
                    else:
                        k.tt("dve", acc[:], acc[:], kfb[:], ALU.mult)
                        for b in range(NB):
                            k.dma("pool", S["YT0"][b, ct * 128:(ct + 1) * 128, toff:toff + T], acc[:, b, :],
                                  wb=[S["YT0"].reg((b, ct, toff))])
                    k.barrier()


CC = 64


def fwd_fft(k, C, I, S, st_tiles, Xin3, tab1, consumer):
    PS = C["PS"]
    A, A2, T2g = st_tiles["A"], st_tiles["A2"], st_tiles["T2g"]
    Xf = Xin3.rearrange("p n c -> p (n c)")
    ng = 64 * CC // 512
    for g in range(ng):
        pr, pi_ = PS[(g % 2) * 2], PS[(g % 2) * 2 + 1]
        k.mm(pr[:], tab1[:, 0:128], Xf[:, g * 512:(g + 1) * 512])
        k.mm(pi_[:], tab1[:, 128:256], Xf[:, g * 512:(g + 1) * 512])
        k.copy("act", A[:, 0].rearrange("p n c -> p (n c)")[:, g * 512:(g + 1) * 512], pr[:])
        k.copy("dve", A[:, 1].rearrange("p n c -> p (n c)")[:, g * 512:(g + 1) * 512], pi_[:])
    for r in range(2):
        k.dma("sp" if r == 0 else "pool", S["FS1"][r].rearrange("n k c -> k n c"), A[:, r])
    k.dma("sp", A2[:].rearrange("p k c -> p (k c)"), S["FS1"].a.rearrange("r n k c -> (r n) (k c)"))
    for k1g in range(16):
        tg = T2g[k1g % 2]
        k.dma("pool" if k1g % 2 else "sp", tg[:], I["fft_T2"][k1g * 8:(k1g + 1) * 8].rearrange("k r m -> r k m"))
        ps = PS[4 + k1g % 2]
        for j in range(8):
            k.mm(ps[:, j * CC:(j + 1) * CC], tg[:, j, :], A2[:, k1g * 8 + j, :])
        consumer(k1g, ps)


def phase_hyena_fft(k, I, S, C):
    PS = C["PS"]
    ident, antiI, ones = C["ident"], C["antiI"], C["ones"]
    T = L
    with contextlib.ExitStack() as st:
        h2 = k.tile(st, "fh2", [64, T])
        with contextlib.ExitStack() as st2:
            zT = k.tile(st2, "fzT", [33, T])
            k.dma("sp", zT[:], I["zT_lat"][:, :])
            fw1 = k.tile(st2, "ffw1", [33, 64])
            fw2 = k.tile(st2, "ffw2", [64, 64])
            k.dma("sp", fw1[:], I["hy_fw1"][0])
            k.dma("sp", fw2[:], I["hy_fw2"][0])
            prm = k.tile(st2, "fprm", [64, 4])
            for i, n in enumerate(("hy_fb1", "hy_ff1", "hy_fb2", "hy_ff2")):
                k.dma("sp", prm[:, i:i + 1], I[n][0].rearrange("(p o) -> p o", o=1), wb=[Buf("tmp")])
            k.barrier()
            h1 = k.tile(st2, "fh1", [64, T])
            tr = k.tile(st2, "ftr", [64, 512])
            tn = k.tile(st2, "ftn", [64, 512])
            for blk in range(T // 512):
                sl = slice(blk * 512, (blk + 1) * 512)
                k.mm(PS[0][0:64, :], fw1[:], zT[:, sl])
                sin_rr(k, h1[:, sl], PS[0][0:64, :], tr[:], tn[:], prm[:, 1:2], prm[:, 0:1])
                k.mm(PS[1][0:64, :], fw2[:], h1[:, sl])
                sin_rr(k, h2[:, sl], PS[1][0:64, :], tr[:], tn[:], prm[:, 3:4], prm[:, 2:3])
            k.barrier()
        fw3 = k.tile(st, "ffw3", [64, 3072])
        k.dma("sp", fw3[:], I["hy_fw3"][0])
        dlt = k.tile(st, "fdlt", [128, HY])
        load_bcast(k, "sp", dlt[:], I["ndelta_row"][0:1, :])
        tpos = k.tile(st, "ftpos", [128, 32])
        k.dma("sp", tpos[:], I["tneg_lat"][:, :])
        dec = k.tile(st, "fdec", [128, HY])
        hraw = [k.tile(st, f"fhraw{i}", [128, 3072]) for i in range(2)]
        accE = k.tile(st, "faccE", [128, 3072])
        sq = k.tile(st, "fsq", [128, 3072])
        for t in range(T // 128):
            hr = hraw[t % 2]
            k.act(dec[:], dlt[:], AF.Exp, scale=tpos[:, t:t + 1])
            for j in range(6):
                ps = PS[j % 4]
                k.mm(ps[:], h2[:, t * 128:(t + 1) * 128], fw3[:, j * 512:(j + 1) * 512])
                k.copy("act", hr[:, j * 512:(j + 1) * 512], ps[:])
            k.tt("dve", hr[:].rearrange("p (q c) -> p q c", c=HY), hr[:].rearrange("p (q c) -> p q c", c=HY),
                 dec[:].unsqueeze(1).to_broadcast([128, 4, HY]), ALU.mult)
            if t == 0:
                for o in range(2):
                    k.memset("dve", hr[0:1, o * 1536 + HY:o * 1536 + 2 * HY], 0.0)
            if t == 0:
                k.act(accE[:], hr[:], AF.Square)
            else:
                k.act(sq[:], hr[:], AF.Square)
                k.tt("pool", accE[:], accE[:], sq[:], ALU.add)
            for o in range(2):
                k.dma("sp", S["KT"][t * 128:(t + 1) * 128, o * HY:(o + 1) * HY], hr[:, o * 1536:o * 1536 + HY], wb=[Buf("x")])
                k.dma("pool", S["HB"][t * 128:(t + 1) * 128, o * HY:(o + 1) * HY], hr[:, o * 1536 + HY:(o + 1) * 1536],
                      wb=[Buf("x")])
        k.memset("dve", sq[:], 0.0)
        k.dma("sp", S["HB"][T:T + 128, :], sq[:, 0:1536], wb=[Buf("x")])
        en = k.tile(st, "fen", [1, 3072])
        for j in range(6):
            k.mm(PS[j % 4][0:1, :], ones[:, 0:1], accE[:, j * 512:(j + 1) * 512])
            k.copy("act", en[:, j * 512:(j + 1) * 512], PS[j % 4][0:1, :])
        es = k.tile(st, "fes", [1, 1536])
        for o in range(2):
            k.tt("dve", es[:, o * HY:(o + 1) * HY], en[:, o * 1536:o * 1536 + HY], en[:, o * 1536 + HY:(o + 1) * 1536], ALU.add)
        k.act(es[:], es[:], AF.Sqrt)
        k.recip(es[:], es[:])
        k.dma("sp", S["ESC"][0:1, :], es[:])
        k.barrier()
        for j in range(T // 128):
            g_ = hraw[j % 2]
            k.dma("sp", g_[:, 0:1536], S["HB"][128 * j + 1:128 * j + 129, :])
            fl = sq if j % 2 == 0 else accE
            for c3 in range(3):
                ps = PS[c3]
                k.mm(ps[:], antiI[:], g_[:, c3 * 512:(c3 + 1) * 512])
                k.copy("act" if c3 % 2 == 0 else "dve", fl[:, c3 * 512:(c3 + 1) * 512], ps[:])
            k.dma("pool", S["KT"][2 * T - 128 * (j + 1):2 * T - 128 * j, :], fl[:, 0:1536], wb=[Buf("x")])
        k.barrier()
    with contextlib.ExitStack() as st:
        tiles = dict(A=k.tile(st, "fA", [128, 2, 64, CC]), A2=k.tile(st, "fA2", [128, 128, CC]),
                     T2g=[k.tile(st, f"fT2g{i}", [128, 8, 128]) for i in range(2)])
        ff1 = k.tile(st, "fff1", [128, 256])
        f1d = k.tile(st, "ff1d", [128, 256])
        G1 = k.tile(st, "fG1", [128, 128])
        G2 = k.tile(st, "fG2", [128, 128])
        k.dma("sp", ff1[:], I["ff1"][:, :])
        k.dma("sp", f1d[:], I["f1d"][:, :])
        k.dma("sp", G1[:], I["fft_G1"][:, :])
        k.dma("sp", G2[:], I["fft_G2"][:, :])
        V = [k.tile(st, f"fV{i}", [128, 64, CC]) for i in range(2)]
        Xin = V[0]
        esc = k.tile(st, "fesc", [128, CC])
        okf = [k.tile(st, f"fokf{i}", [128, 8, CC]) for i in range(1)]
        KTv = S["KT"].a.rearrange("(a b) c -> a b c", b=64)
        for fc in range(24):
            o, c0 = fc // 12, (fc % 12) * CC
            k.dma("sp", Xin[:], KTv[:, :, fc * CC:(fc + 1) * CC])
            load_bcast(k, "sp", esc[:], S["ESC"][0:1, fc * CC:(fc + 1) * CC])

            def cons_f(k1g, ps, o=o, c0=c0):
                t_ = okf[0]
                k.tt("dve", t_[:], ps[:].rearrange("p (j c) -> p j c", c=CC), esc[:].unsqueeze(1).to_broadcast([128, 8, CC]), ALU.mult)
                k.dma("pool", S["KF"][o, :, k1g * 8:(k1g + 1) * 8, c0:c0 + CC], t_[:], wb=[Buf("x")])

            fwd_fft(k, C, I, S, tiles, Xin[:], ff1, cons_f)
        k.barrier()
        Bt = k.tile(st, "fB", [128, 128, CC])
        R = k.tile(st, "fR", [128, 66, CC])
        Gt = k.tile(st, "fGt", [128, 64, CC])
        wb_ = k.tile(st, "fwb", [128, 3, 4, CC])
        hb = k.tile(st, "fhb", [128, 2, CC])
        Kg = [k.tile(st, f"fKg{i}", [128, 2, 8, CC]) for i in range(2)]
        T12 = [k.tile(st, f"fT12{i}", [128, 2, 8 * CC]) for i in range(2)]
        T4g = [k.tile(st, f"fT4g{i}", [128, 4, 2, 128]) for i in range(2)]
        tmp = k.tile(st, "ftmp", [128, 4, CC])
        B2 = tiles["A"]

        def conv(blk, c0, dst):
            col = blk * HY + c0
            for b in range(NB):
                k.dma("sp", R[b * 64:(b + 1) * 64], AP(S["P0"].h, (b * (L + 2)) * 2304 + col, [[64 * 2304, 64], [2304, 66], [1, CC]]),
                      rb=[S["P0"]])
            w = lambda j: wb_[:, blk, j, :].unsqueeze(1).to_broadcast([128, 64, CC])
            k.tt("dve", dst, R[:, 0:64, :], w(0), ALU.mult)
            k.tt("pool", Gt_tmp[:, 0:64, :], R[:, 1:65, :], w(1), ALU.mult)
            k.tt("dve", dst, dst, Gt_tmp[:, 0:64, :], ALU.add)
            k.tt("pool", Gt_tmp[:, 0:64, :], R[:, 2:66, :], w(2), ALU.mult)
            k.tt("dve", dst, dst, Gt_tmp[:, 0:64, :], ALU.add)
            k.tt("dve", dst, dst, w(3), ALU.add)

        Gt_tmp = Bt
        for dc in range(HY // CC):
            c0 = dc * CC
            for blk in range(3):
                col = blk * HY + c0
                for j in range(3):
                    k.dma("sp", wb_[:, blk, j, :], I["hy_conv_w"][0][j:j + 1, col:col + CC].partition_broadcast(128), wb=[Buf("x")])
                k.dma("sp", wb_[:, blk, 3, :], I["hy_conv_b"][0:1, col:col + CC].partition_broadcast(128), wb=[Buf("x")])
            for o in range(2):
                k.dma("sp", hb[:, o, :], I["hy_bias"][0][o:o + 1, c0:c0 + CC].partition_broadcast(128), wb=[Buf("x")])
            k.barrier()
            conv(2, c0, V[0][:])
            for o in range(2):
                Vin, Vout = V[o], V[1 - o]

                def cons_d(k1g, ps, o=o, c0=c0):
                    kg = Kg[k1g % 2]
                    for r in range(2):
                        for hf in range(2):
                            k.dma("sp" if hf == 0 else "pool", kg[hf * 64:(hf + 1) * 64, r],
                                  S["KF"][o, r * 64:(r + 1) * 64, k1g * 8:(k1g + 1) * 8, c0:c0 + CC], rb=[S["KF"]], wb=[kg.sub[r * 2 + hf]])
                    t12 = T12[k1g % 2]
                    k.op("dve", lambda: k.nc.vector.tensor_tensor(t12[:, 0, :], ps[:], kg[:, 0].rearrange("p j c -> p (j c)"), ALU.mult),
                         [t12[:, 0, :]], [ps[:]], rb=[k.buf_of(ps[:])] + kg.sub, wb=[t12])
                    k.op("dve", lambda: k.nc.vector.tensor_tensor(t12[:, 1, :], ps[:], kg[:, 1].rearrange("p j c -> p (j c)"), ALU.mult),
                         [t12[:, 1, :]], [ps[:]], rb=[k.buf_of(ps[:])] + kg.sub, wb=[t12])
                    p2 = PS[6 + k1g % 2]
                    k.mm(p2[:], G1[:], t12[:, 0, :], start=True, stop=False)
                    k.mm(p2[:], G2[:], t12[:, 1, :], start=False, stop=True)
                    k.copy("act", Bt[:, k1g * 8:(k1g + 1) * 8, :], p2[:].rearrange("p (j c) -> p j c", c=CC))

                fwd_fft(k, C, I, S, tiles, Vin[:], f1d, cons_d)
                k.dma("sp", S["FS2"].a.rearrange("r n k c -> (r n) (k c)"), Bt[:].rearrange("p k c -> p (k c)"))
                for r in range(2):
                    k.dma("sp" if r == 0 else "pool", B2[:, r], S["FS2"][r].rearrange("n k c -> k n c"))
                conv(o, c0, Gt[:])
                for g in range(16):
                    t4 = T4g[g % 2]
                    for r in range(2):
                        k.dma("sp" if r == 0 else "pool", t4[:, :, r, :], I["fft_T4"][g * 4:(g + 1) * 4, r].rearrange("n k m -> k n m"),
                              wb=[t4.sub[r]])
                    ps = PS[g % 4]
                    for j in range(4):
                        n2 = g * 4 + j
                        k.op("pe", lambda: k.nc.tensor.matmul(ps[:, j * CC:(j + 1) * CC], t4[:, j, 0, :], B2[:, 0, n2, :], start=True, stop=False),
                             [ps[:]], [], rb=[B2] + t4.sub, wb=[k.buf_of(ps[:])])
                        k.op("pe", lambda: k.nc.tensor.matmul(ps[:, j * CC:(j + 1) * CC], t4[:, j, 1, :], B2[:, 1, n2, :], start=False, stop=True),
                             [ps[:]], [], rb=[B2] + t4.sub, wb=[k.buf_of(ps[:])])
                    gs = slice(g * 4, (g + 1) * 4)
                    k.tt("pool", tmp[:], Vin[:, gs, :], hb[:, o, :].unsqueeze(1).to_broadcast([128, 4, CC]), ALU.mult)
                    k.tt("dve", tmp[:], tmp[:], ps[:, 0:4 * CC].rearrange("p (j c) -> p j c", c=CC), ALU.add)
                    k.tt("dve", Vout[:, gs, :], tmp[:], Gt[:, gs, :], ALU.mult)
                if o == 1:
                    for b in range(NB):
                        k.dma("pool", AP(S["YH"].h, b * L * HY + c0, [[64 * HY, 64], [HY, 64], [1, CC]]), Vout[b * 64:(b + 1) * 64],
                              wb=[Buf("x")])
            k.barrier()


TC = 256


def phase_s5(k, I, S, C):
    PS = C["PS"]
    ident, antiI = C["ident"], C["antiI"]
    nblk = TT // 128
    for d in range(2):
        with contextlib.ExitStack() as st:
            BTr = [k.tile(st, f"BTr{j}", [128, 128]) for j in range(8)]
            BTi = [k.tile(st, f"BTi{j}", [128, 128]) for j in range(8)]
            Cr = [k.tile(st, f"Cr{j}", [128, 256]) for j in range(8)]
            nCi = [k.tile(st, f"nCi{j}", [128, 256]) for j in range(8)]
            ctab = [k.tile(st, f"ctab{j}", [128, TC]) for j in range(8)]
            stab = [k.tile(st, f"stab{j}", [128, TC]) for j in range(8)]
            rt = [k.tile(st, f"rt{j}", [128, TC]) for j in range(8)]
            carry = [k.tile(st, f"carry{j}", [128, 2]) for j in range(8)]
            iot = k.tile(st, "iot", [128, TC])
            k.dma("sp", iot[:], I["iota1"][:, :])
            with contextlib.ExitStack() as st2:
                sc = k.tile(st2, "s5sc", [128, 24])
                tr = k.tile(st2, "s5tr", [128, TC])
                tn = k.tile(st2, "s5tn", [128, TC])
                bre = k.tile(st2, "s5bre", [128, 16])
                bim = k.tile(st2, "s5bim", [128, 16])
                bb = k.tile(st2, "s5bb", [128, 2, 16])
                t16 = k.tile(st2, "s5t16", [128, 16])
                pad = k.tile(st2, "s5pad", [128, 128])
                for j in range(8):
                    g0 = 2 * j
                    k.dma("sp", sc[:, 0:1], I["s5_lam_re"][0, d, g0:g0 + 2, :].rearrange("g (p o) -> (g p) o", o=1))
                    k.dma("sp", sc[:, 1:2], I["s5_lam_im"][0, d, g0:g0 + 2, :].rearrange("g (p o) -> (g p) o", o=1))
                    for gl in range(2):
                        k.dma("sp", sc[gl * 64:(gl + 1) * 64, 2:3],
                              I["s5_log_step"][0, d:d + 1, g0 + gl:g0 + gl + 1].partition_broadcast(64))
                    k.dma("sp", bre[:], I["s5_b_re"][0, d, g0:g0 + 2].rearrange("g p n -> (g p) n"))
                    k.dma("sp", bim[:], I["s5_b_im"][0, d, g0:g0 + 2].rearrange("g p n -> (g p) n"))
                    k.act(sc[:, 3:4], sc[:, 2:3], AF.Exp)
                    k.tt("dve", sc[:, 4:5], sc[:, 0:1], sc[:, 3:4], ALU.mult)
                    k.tt("dve", sc[:, 5:6], sc[:, 1:2], sc[:, 3:4], ALU.mult)
                    k.act(sc[:, 6:7], sc[:, 4:5], AF.Exp)
                    sin_rr(k, sc[:, 7:8], sc[:, 5:6], tr[:, 0:1], tn[:, 0:1])
                    k.ts("dve", sc[:, 9:10], sc[:, 5:6], math.pi / 2, None, ALU.add)
                    sin_rr(k, sc[:, 8:9], sc[:, 9:10], tr[:, 0:1], tn[:, 0:1])
                    k.tt("dve", sc[:, 10:11], sc[:, 6:7], sc[:, 8:9], ALU.mult)
                    k.tt("dve", sc[:, 11:12], sc[:, 6:7], sc[:, 7:8], ALU.mult)
                    k.ts("dve", sc[:, 12:13], sc[:, 10:11], -1.0, None, ALU.add)
                    k.tt("dve", sc[:, 13:14], sc[:, 0:1], sc[:, 0:1], ALU.mult)
                    k.stt("dve", sc[:, 13:14], sc[:, 1:2], sc[:, 1:2], sc[:, 13:14], ALU.mult, ALU.add)
                    k.recip(sc[:, 14:15], sc[:, 13:14])
                    k.tt("dve", sc[:, 15:16], sc[:, 12:13], sc[:, 0:1], ALU.mult)
                    k.stt("dve", sc[:, 15:16], sc[:, 11:12], sc[:, 1:2], sc[:, 15:16], ALU.mult, ALU.add)
                    k.tt("dve", sc[:, 15:16], sc[:, 15:16], sc[:, 14:15], ALU.mult)
                    k.tt("dve", sc[:, 16:17], sc[:, 12:13], sc[:, 1:2], ALU.mult)
                    k.stt("dve", sc[:, 16:17], sc[:, 11:12], sc[:, 0:1], sc[:, 16:17], ALU.mult, ALU.subtract)
                    k.tt("dve", sc[:, 16:17], sc[:, 16:17], sc[:, 14:15], ALU.mult)
                    k.ts("dve", bb[:, 0, :], bre[:], sc[:, 15:16], None, ALU.mult)
                    k.ts("dve", t16[:], bim[:], sc[:, 16:17], None, ALU.mult)
                    k.tt("dve", bb[:, 0, :], bb[:, 0, :], t16[:], ALU.subtract)
                    k.ts("dve", bb[:, 1, :], bim[:], sc[:, 15:16], None, ALU.mult)
                    k.ts("dve", t16[:], bre[:], sc[:, 16:17], None, ALU.mult)
                    k.tt("dve", bb[:, 1, :], bb[:, 1, :], t16[:], ALU.add)
                    for ri, BT in ((0, BTr), (1, BTi)):
                        k.memset("dve", pad[:], 0.0)
                        for gl in range(2):
                            cg = ((g0 + gl) % 8) * 16
                            k.copy("dve", pad[gl * 64:(gl + 1) * 64, cg:cg + 16], bb[gl * 64:(gl + 1) * 64, ri, :])
                        k.mm(PS[0][:, 0:128], pad[:], ident[:])
                        k.copy("act", BT[j][:], PS[0][:, 0:128])
                    k.memset("dve", Cr[j][:], 0.0)
                    k.memset("dve", nCi[j][:], 0.0)
                    for gl in range(2):
                        g = g0 + gl
                        k.dma("sp", Cr[j][gl * 64:(gl + 1) * 64, g * 16:(g + 1) * 16],
                              I["s5_c_re"][0, d, g].rearrange("n p -> p n"), allow_slow_non_contiguous=True)
                        k.dma("sp", nCi[j][gl * 64:(gl + 1) * 64, g * 16:(g + 1) * 16],
                              I["s5_c_im"][0, d, g].rearrange("n p -> p n"), allow_slow_non_contiguous=True)
                    k.ts("dve", nCi[j][:], nCi[j][:], -1.0, None, ALU.mult)
                    k.ts("dve", tn[:], iot[:], sc[:, 5:6], None, ALU.mult)
                    sin_rr(k, stab[j][:], tn[:], tr[:], tn[:])
                    k.ts("dve", tn[:], iot[:], sc[:, 5:6], math.pi / 2, ALU.mult, ALU.add)
                    sin_rr(k, ctab[j][:], tn[:], tr[:], tn[:])
                    k.memset("dve", rt[j][:], 0.0)
                    k.ts("dve", rt[j][:], rt[j][:], sc[:, 6:7], None, ALU.add)
                k.barrier()
            ut = [k.tile(st, f"s5ut{i}", [128, 256]) for i in range(2)]
            uT = k.tile(st, "s5uT", [128, 2, TC])
            Sre = [k.tile(st, f"Sre{j}", [128, TC]) for j in range(8)]
            Sim = [k.tile(st, f"Sim{j}", [128, TC]) for j in range(8)]
            m1 = k.tile(st, "s5m1", [128, TC])
            m2 = k.tile(st, "s5m2", [128, TC])
            mre = k.tile(st, "s5mre", [128, TC])
            mim = k.tile(st, "s5mim", [128, TC])
            vre = k.tile(st, "s5vre", [128, TC])
            vim = k.tile(st, "s5vim", [128, TC])
            yt = [k.tile(st, f"s5yt{i}", [128, 256]) for i in range(2)]
            y2 = [k.tile(st, f"s5y2{i}", [128, 256]) for i in range(2)]
            revm = antiI if d == 1 else ident

            def row0(m):
                if d == 0:
                    return 128 * m
                return 128 * (1 - m) if m < 2 else 4480 - 128 * m

            it = 0
            for b in range(NB):
                for j in range(8):
                    k.memset("dve", carry[j][:], 0.0)
                for ch in range(TT // TC):
                    for bl in range(2):
                        m = ch * 2 + bl
                        u_ = ut[(it + bl) % 2]
                        k.dma("sp", u_[:], S["UT0"][b, row0(m):row0(m) + 128, :])
                        for hf in range(2):
                            k.mm(PS[0][:, (hf * 2 + bl) * 128:(hf * 2 + bl + 1) * 128], u_[:, hf * 128:(hf + 1) * 128], revm[:])
                    k.copy("act", uT[:], PS[0][:].rearrange("p (h t) -> p h t", h=2))
                    for j in range(8):
                        hf = j // 4
                        pr, pi_ = PS[1 + (j % 2) * 2], PS[2 + (j % 2) * 2]
                        k.mm(pr[:, 0:TC], BTr[j][:], uT[:, hf, :])
                        k.mm(pi_[:, 0:TC], BTi[j][:], uT[:, hf, :])
                        k.tt("dve", m1[:], pr[:, 0:TC], ctab[j][:], ALU.mult)
                        k.tt("dve", m2[:], pi_[:, 0:TC], stab[j][:], ALU.mult)
                        k.tt("pool", mre[:], m1[:], m2[:], ALU.add)
                        k.tt("dve", m1[:], pi_[:, 0:TC], ctab[j][:], ALU.mult)
                        k.tt("dve", m2[:], pr[:, 0:TC], stab[j][:], ALU.mult)
                        k.tt("pool", mim[:], m1[:], m2[:], ALU.subtract)
                        k.scan("dve", vre[:], rt[j][:], mre[:], carry[j][:, 0:1], ALU.mult, ALU.add)
                        k.scan("dve", vim[:], rt[j][:], mim[:], carry[j][:, 1:2], ALU.mult, ALU.add)
                        k.tt("dve", m1[:], vre[:], ctab[j][:], ALU.mult)
                        k.tt("dve", m2[:], vim[:], stab[j][:], ALU.mult)
                        k.tt("pool", Sre[j][:], m1[:], m2[:], ALU.subtract)
                        k.tt("dve", m1[:], vre[:], stab[j][:], ALU.mult)
                        k.tt("dve", m2[:], vim[:], ctab[j][:], ALU.mult)
                        k.tt("pool", Sim[j][:], m1[:], m2[:], ALU.add)
                        k.copy("pool", carry[j][:, 0:1], Sre[j][:, TC - 1:TC])
                        k.copy("pool", carry[j][:, 1:2], Sim[j][:, TC - 1:TC])
                    for bl in range(2):
                        m = ch * 2 + bl
                        py = PS[5 + bl]
                        for j in range(8):
                            k.mm(py[:, 0:256], Sre[j][:, bl * 128:(bl + 1) * 128], Cr[j][:], start=(j == 0), stop=False)
                            k.mm(py[:, 0:256], Sim[j][:, bl * 128:(bl + 1) * 128], nCi[j][:], start=False, stop=(j == 7))
                        y_ = yt[(it + bl) % 2]
                        k.copy("act", y_[:], py[:, 0:256])
                        if d == 1:
                            k.mm(PS[7][:, 0:256], antiI[:], y_[:])
                            y2_ = y2[(it + bl) % 2]
                            k.copy("act", y2_[:], PS[7][:, 0:256])
                            y_ = y2_
                        k.dma("pool", S["YS5"][d, b, row0(m):row0(m) + 128, :], y_[:], wb=[S["YS5"].reg((d, b, m))])
                    it += 1
        k.barrier()


def phase_s5_out(k, I, S, C):
    PS = C["PS"]
    ident = C["ident"]
    with contextlib.ExitStack() as st:
        Db = k.tile(st, "s5D", [128, 256])
        gb = k.tile(st, "s5gb", [128, 512])
        gw = k.tile(st, "s5gw", [128, 2, 512])
        load_bcast(k, "sp", Db[:], I["s5_d"][0:1, :])
        load_bcast(k, "sp", gb[:], I["s5_glu_b"][0:1, :])
        k.dma("sp", gw[:], I["s5_glu_w"][0].rearrange("(h p) n -> p h n", p=128))
        yf = [k.tile(st, f"o5yf{i}", [128, 256]) for i in range(2)]
        yb = [k.tile(st, f"o5yb{i}", [128, 256]) for i in range(2)]
        uu = [k.tile(st, f"o5u{i}", [128, 256]) for i in range(2)]
        y = k.tile(st, "o5y", [128, 256])
        t1 = k.tile(st, "o5t1", [128, 256])
        geT = k.tile(st, "o5geT", [128, 2, 128])
        a = k.tile(st, "o5a", [128, 512])
        o = k.tile(st, "o5o", [128, 256])
        oT = [k.tile(st, f"o5oT{i}", [128, 2, 128]) for i in range(2)]
        it = 0
        for b in range(NB):
            for m in range(TT // 128):
                r0 = m * 128
                i2 = it % 2
                k.dma("sp", yf[i2][:], S["YS5"][0, b, r0:r0 + 128, :])
                k.dma("sp", yb[i2][:], S["YS5"][1, b, r0:r0 + 128, :])
                k.dma("sp", uu[i2][:], S["UT0"][b, r0:r0 + 128, :])
                k.tt("dve", y[:], yf[i2][:], yb[i2][:], ALU.add)
                k.tt("dve", t1[:], uu[i2][:], Db[:], ALU.mult)
                k.tt("dve", y[:], y[:], t1[:], ALU.add)
                k.act(t1[:], y[:], AF.Square)
                k.ts("dve", t1[:], t1[:], 0.044715, 1.0, ALU.mult, ALU.add)
                k.tt("dve", t1[:], t1[:], y[:], ALU.mult)
                k.act(t1[:], t1[:], AF.Sigmoid, scale=2.0 * math.sqrt(2.0 / math.pi))
                k.tt("dve", y[:], y[:], t1[:], ALU.mult)
                for hf in range(2):
                    k.mm(PS[0][:, hf * 128:(hf + 1) * 128], y[:, hf * 128:(hf + 1) * 128], ident[:])
                k.copy("act", geT[:], PS[0][:, 0:256].rearrange("p (h t) -> p h t", h=2))
                for hf in range(2):
                    k.mm(PS[1][:], geT[:, hf, :], gw[:, hf, :], start=(hf == 0), stop=(hf == 1))
                k.tt("dve", a[:], PS[1][:], gb[:], ALU.add)
                k.act(a[:, 256:512], a[:, 256:512], AF.Sigmoid)
                k.tt("dve", o[:], a[:, 0:256], a[:, 256:512], ALU.mult)
                for hf in range(2):
                    k.mm(PS[2][:, hf * 128:(hf + 1) * 128], o[:, hf * 128:(hf + 1) * 128], ident[:])
                k.copy("act", oT[i2][:], PS[2][:, 0:256].rearrange("p (h t) -> p h t", h=2))
                k.dma("pool", S["YT0"][b, 768:1024, r0:r0 + 128].rearrange("(h p) t -> p h t", p=128), oT[i2][:],
                      wb=[S["YT0"].reg(("s5", b, m))])
                it += 1


def phase_moe(k, I, S, C, layer, yT_src, wout, tiles, final):
    PS = C["PS"]
    ident = C["ident"]
    with contextlib.ExitStack() as st:
        Wo = k.tile(st, "Wo", [128, 8, 1024])
        k.dma("sp", Wo[:], wout.rearrange("(k p) n -> p k n", p=128))
        Wr = k.tile(st, "Wr", [128, 8, 36])
        k.dma("sp", Wr[:, :, 0:4], I["moe_wg"][layer].rearrange("(k p) g -> p k g", p=128), allow_slow_non_contiguous=True,
              wb=[Buf("x")])
        k.dma("sp", Wr[:, :, 4:36], I["moe_we"][layer].rearrange("(k p) g -> p k g", p=128), allow_slow_non_contiguous=True,
              wb=[Buf("x")])
        rb = k.tile(st, "rbias", [128, 36])
        k.dma("sp", rb[:, 0:4], I["moe_bg"][layer:layer + 1, :].partition_broadcast(128), wb=[Buf("x")])
        k.dma("sp", rb[:, 4:36], I["moe_be"][layer:layer + 1, :].partition_broadcast(128), wb=[Buf("x")])
        fg = k.tile(st, "fg", [128, 1024])
        if final:
            load_bcast(k, "sp", fg[:], I["final_g"].a.rearrange("(o d) -> o d", o=1))
        k.barrier()
        mvt = [k.tile(st, f"mv{i}", [128, 1024]) for i in range(4)]
        yT = k.tile(st, "yT", [128, 8, 512])
        xa = [k.tile(st, f"xa{i}", [128, 1024]) for i in range(4)]
        acc = [k.tile(st, f"acc{i}", [128, 1024]) for i in range(4)]
        gate = [k.tile(st, f"gate{i}", [128, 32]) for i in range(4)]
        xin = k.tile(st, "xin", [128, 1024])
        h = k.tile(st, "hm", [128, 1024])
        hT = yT
        hTb = k.tile(st, "hTb", [128, 8, 512], BF16)
        sq = xin
        ss = k.tile(st, "ssm", [128, 1])
        rstd = k.tile(st, "rstdm", [128, 1])
        r_ = k.tile(st, "rt", [128, 64])
        lg = k.tile(st, "lg", [128, 36])
        Wg = [k.tile(st, f"Wg{i}", [128, 8, 256]) for i in range(2)]
        Wu = [k.tile(st, f"Wu{i}", [128, 8, 256]) for i in range(2)]
        Wd = [k.tile(st, f"Wd{i}", [128, 2, 1024]) for i in range(2)]
        Wgb = [k.tile(st, f"Wgb{i}", [128, 8, 256], BF16) for i in range(2)]
        Wub = [k.tile(st, f"Wub{i}", [128, 8, 256], BF16) for i in range(2)]
        Wdb = [k.tile(st, f"Wdb{i}", [128, 2, 1024], BF16) for i in range(2)]
        sl_ = [k.tile(st, f"sil{i}", [128, 512]) for i in range(2)]
        hid = [k.tile(st, f"hid{i}", [128, 512], BF16) for i in range(2)]
        cur_row = None
        ecount = 0
        for tl in tiles:
            if tl["row"] != cur_row:
                cur_row = tl["row"]
                for i, comp in enumerate((2, 3, 4, 5)):
                    load_bcast(k, "sp", mvt[i][:], S["MV"][layer, cur_row, comp:comp + 1, :])
            c0 = 0
            for (ap_, n) in tl["ysrc"]:
                if "yh" in tl:
                    k.dma("sp", yT[:, 6:8, c0:c0 + n], ap_.rearrange("(k p) t -> p k t", p=128), wb=[Buf("x")])
                else:
                    k.dma("sp", yT[:, :, c0:c0 + n], ap_.rearrange("(k p) t -> p k t", p=128), wb=[Buf("x")])
                c0 += n
            k.barrier()
            if "yh" in tl:
                for ts in range(4):
                    k.dma("sp", h[:, 0:HY], tl["yh"][ts])
                    for hf in range(2):
                        ps = PS[2 + hf]
                        for j in range(3):
                            kk = hf * 3 + j
                            k.mm(ps[:, j * 128:(j + 1) * 128], h[:, kk * 128:(kk + 1) * 128], ident[:])
                        k.copy("act", yT[:, hf * 3:(hf + 1) * 3, ts * 128:(ts + 1) * 128],
                               ps[:, 0:384].rearrange("p (j t) -> p j t", j=3))
            for ts in range(4):
                k.dma("sp", xin[:], tl["xsrc"][ts])
                for half in range(2):
                    ps = PS[half]
                    for kk in range(8):
                        k.mm(ps[:], yT[:, kk, ts * 128:(ts + 1) * 128], Wo[:, kk, half * 512:(half + 1) * 512],
                             start=(kk == 0), stop=(kk == 7))
                    k.tt("dve", xa[ts][:, half * 512:(half + 1) * 512], ps[:], mvt[0][:, half * 512:(half + 1) * 512], ALU.mult)
                k.tt("dve", xa[ts][:], xa[ts][:], xin[:], ALU.add)
                rms_mod(k, (sq, ss, rstd), xa[ts][:], mvt[2][:], mvt[1][:], h[:], "m")
                for hf in range(2):
                    ps = PS[2 + hf]
                    for j in range(4):
                        kk = hf * 4 + j
                        k.mm(ps[:, j * 128:(j + 1) * 128], h[:, kk * 128:(kk + 1) * 128], ident[:])
                    k.copy("act", hT[:, hf * 4:(hf + 1) * 4, ts * 128:(ts + 1) * 128], ps[:].rearrange("p (j t) -> p j t", j=4))
                    k.copy("dve", hTb[:, hf * 4:(hf + 1) * 4, ts * 128:(ts + 1) * 128], hT[:, hf * 4:(hf + 1) * 4, ts * 128:(ts + 1) * 128])
                ps = PS[4]
                for kk in range(8):
                    k.mm(ps[:, 0:36], hT[:, kk, ts * 128:(ts + 1) * 128], Wr[:, kk, :], start=(kk == 0), stop=(kk == 7))
                k.tt("dve", lg[:], ps[:, 0:36], rb[:], ALU.add)
                g_ = gate[ts]
                k.op("dve", lambda: k.nc.vector.reduce_max(r_[:, 0:1], lg[:, 0:4], AX.X), [r_[:, 0:1]], [lg[:, 0:4]])
                k.ts("dve", r_[:, 1:2], r_[:, 0:1], -1.0, None, ALU.mult)
                k.act(r_[:, 4:8], lg[:, 0:4], AF.Exp, bias=r_[:, 1:2], accum_out=r_[:, 2:3])
                k.recip(r_[:, 3:4], r_[:, 2:3])
                k.ts("dve", r_[:, 8:12], lg[:, 0:4], r_[:, 0:1], None, ALU.is_ge)
                k.ts("dve", r_[:, 16:24], lg[:, 4:12], r_[:, 8:9], None, ALU.mult)
                for g in range(1, 4):
                    k.stt("dve", r_[:, 16:24], lg[:, 4 + 8 * g:12 + 8 * g], r_[:, 8 + g:9 + g], r_[:, 16:24], ALU.mult, ALU.add)
                k.op("dve", lambda: k.nc.vector.reduce_max(r_[:, 12:13], r_[:, 16:24], AX.X), [r_[:, 12:13]], [r_[:, 16:24]])
                k.ts("dve", r_[:, 24:32], r_[:, 16:24], r_[:, 12:13], None, ALU.is_ge)
                k.stt("dve", r_[:, 32:40], r_[:, 24:32], -1e30, r_[:, 16:24], ALU.mult, ALU.add)
                k.op("dve", lambda: k.nc.vector.reduce_max(r_[:, 13:14], r_[:, 32:40], AX.X), [r_[:, 13:14]], [r_[:, 32:40]])
                k.ts("dve", r_[:, 40:48], r_[:, 32:40], r_[:, 13:14], None, ALU.is_ge)
                k.tt("dve", r_[:, 14:15], r_[:, 13:14], r_[:, 12:13], ALU.subtract)
                k.act(r_[:, 14:15], r_[:, 14:15], AF.Exp)
                k.ts("dve", r_[:, 14:15], r_[:, 14:15], 1.0, None, ALU.add)
                k.recip(r_[:, 15:16], r_[:, 14:15])
                k.tt("dve", r_[:, 48:49], r_[:, 15:16], r_[:, 3:4], ALU.mult)
                k.tt("dve", r_[:, 49:50], r_[:, 3:4], r_[:, 48:49], ALU.subtract)
                k.ts("dve", r_[:, 50:58], r_[:, 24:32], r_[:, 48:49], None, ALU.mult)
                k.stt("dve", r_[:, 50:58], r_[:, 40:48], r_[:, 49:50], r_[:, 50:58], ALU.mult, ALU.add)
                for g in range(4):
                    k.ts("dve", g_[:, g * 8:(g + 1) * 8], r_[:, 50:58], r_[:, 8 + g:9 + g], None, ALU.mult)
            for e in range(32):
                gi, ei = e // 8, e % 8
                i2 = ecount % 2
                ecount += 1
                k.dma("sp", Wg[i2][:], I["moe_w_gate"][layer, gi, ei].rearrange("(k p) n -> p k n", p=128))
                k.dma("pool", Wu[i2][:], I["moe_w_up"][layer, gi, ei].rearrange("(k p) n -> p k n", p=128))
                k.dma("sp", Wd[i2][:], I["moe_w_down"][layer, gi, ei].rearrange("(k p) n -> p k n", p=128))
                k.copy("act", Wgb[i2][:], Wg[i2][:])
                k.copy("act", Wub[i2][:], Wu[i2][:])
                k.copy("dve", Wdb[i2][:], Wd[i2][:])
                for hc in range(2):
                    pa, pu = PS[hc * 2], PS[hc * 2 + 1]
                    for kk in range(8):
                        k.mm(pa[:], Wgb[i2][:, kk, hc * 128:(hc + 1) * 128], hTb[:, kk, :], start=(kk == 0), stop=(kk == 7))
                    for kk in range(8):
                        k.mm(pu[:], Wub[i2][:, kk, hc * 128:(hc + 1) * 128], hTb[:, kk, :], start=(kk == 0), stop=(kk == 7))
                    k.act(sl_[hc][:], pa[:], AF.Silu)
                    k.tt("dve", hid[hc][:], sl_[hc][:], pu[:], ALU.mult)
                for ts in range(4):
                    for half in range(2):
                        po = PS[4 + (ts * 2 + half) % 4]
                        for hc in range(2):
                            k.mm(po[:], hid[hc][:, ts * 128:(ts + 1) * 128], Wdb[i2][:, hc, half * 512:(half + 1) * 512],
                                 start=(hc == 0), stop=(hc == 1))
                        eng = "dve" if half == 0 else "pool"
                        asl = acc[ts][:, half * 512:(half + 1) * 512]
                        if e == 0:
                            k.ts("dve", asl, po[:], gate[ts][:, e:e + 1], None, ALU.mult)
                        else:
                            k.stt("dve", asl, po[:], gate[ts][:, e:e + 1], asl, ALU.mult, ALU.add)
            for ts in range(4):
                k.tt("dve", acc[ts][:], acc[ts][:], mvt[3][:], ALU.mult)
                k.tt("dve", acc[ts][:], acc[ts][:], xa[ts][:], ALU.add)
                if final:
                    k.act(sq[:], acc[ts][:], AF.Square, accum_out=ss[:])
                    k.ts("dve", ss[:], ss[:], 1.0 / D, EPS, ALU.mult, ALU.add)
                    k.act(ss[:], ss[:], AF.Sqrt)
                    k.recip(rstd[:], ss[:])
                    k.stt("dve", acc[ts][:], acc[ts][:], rstd[:, 0:1], fg[:], ALU.mult, ALU.mult)
                k.dma("pool", tl["dst"][ts], acc[ts][:], wb=[Buf("x")])
            k.barrier()


def l0_moe_tiles(I, S):
    tiles = []
    tiles.append(dict(row=2, ysrc=[(S["YT0"][b, :, 0:LC], LC) for b in range(NB)],
                      xsrc=[I["ctx"][b, j * 128:(j + 1) * 128, :] for b in range(NB) for j in range(2)],
                      dst=[S["CTX1"][b, j * 128:(j + 1) * 128, :] for b in range(NB) for j in range(2)]))
    for b in range(NB):
        for i in range(L // 512):
            t0 = i * 512
            td = dict(row=b, ysrc=[(S["YT0"][b, :, LC + t0:LC + t0 + 512], 512)])
            if USE_FFT:
                td = dict(row=b, ysrc=[(S["YT0"][b, 768:1024, LC + t0:LC + t0 + 512], 512)],
                          yh=[S["YH"][b, t0 + j * 128:t0 + (j + 1) * 128, :] for j in range(4)])
            tiles.append(dict(td,
                              xsrc=[I["x"][b, t0 + j * 128:t0 + (j + 1) * 128, :] for j in range(4)],
                              dst=[S["X1"][b, t0 + j * 128:t0 + (j + 1) * 128, :] for j in range(4)]))
    return tiles


def phase_l1_norm(k, I, S, C):
    with contextlib.ExitStack() as st:
        A = k.tile(st, "A1b", [128, 1024])
        sh = k.tile(st, "sh1b", [128, 1024])
        xt = [k.tile(st, f"xtb{i}", [128, 1024]) for i in range(2)]
        h = k.tile(st, "hb", [128, 1024])
        hT = [k.tile(st, f"hTb{i}", [128, 8, 128]) for i in range(2)]
        sq = k.tile(st, "sqb", [128, 1024])
        ss = k.tile(st, "ssb", [128, 1])
        rstd = k.tile(st, "rstdb", [128, 1])
        it = 0
        for b in range(NB):
            for (src, row, ntile, toff) in ((S["CTX1"], 2, LC // 128, 0), (S["X1"], b, L // 128, LC)):
                load_bcast(k, "sp", A[:], S["MV"][1, row, 1:2, :])
                load_bcast(k, "sp", sh[:], S["MV"][1, row, 0:1, :])
                for t in range(ntile):
                    x_ = xt[it % 2]
                    hT_ = hT[it % 2]
                    k.dma("sp", x_[:], src[b, t * 128:(t + 1) * 128, :])
                    rms_mod(k, (sq, ss, rstd), x_[:], A[:], sh[:], h[:], "l1")
                    transpose_1024(k, C, h, hT_)
                    t0 = toff + t * 128
                    k.dma("pool", S["HT1"][b].rearrange("(j p) t -> p j t", p=128)[:, :, t0:t0 + 128], hT_[:],
                          wb=[S["HT1"].reg((b, t0))])
                    it += 1


def phase_l1_inproj(k, I, S, C):
    PS = C["PS"]
    with contextlib.ExitStack() as st:
        W = k.tile(st, "W1g", [128, 8, 1024])
        hT = [k.tile(st, f"hT5{i}", [128, 8, 512]) for i in range(2)]
        of = [k.tile(st, f"of{i}", [128, 8, 512]) for i in range(2)]
        ot = [k.tile(st, f"ot{i}", [128, 4, 1024]) for i in range(2)]
        wv = I["c_w_in"][0].rearrange("(k p) n -> p k n", p=128)
        it = 0
        for cg in range(5):
            k.barrier()
            for kk in range(8):
                k.dma("sp" if kk % 2 == 0 else "pool", W[:, kk, :], wv[:, kk, cg * 1024:(cg + 1) * 1024], wb=[Buf("x")])
            k.barrier()
            for b in range(NB):
                segs = [(LC + i * 512, 512) for i in range(L // 512)]
                if cg >= 2:
                    segs = [(0, LC)] + segs
                for (t0, n) in segs:
                    h_ = hT[it % 2]
                    k.dma("sp", h_[:, :, 0:n], S["HT1"][b].rearrange("(j p) t -> p j t", p=128)[:, :, t0:t0 + n])
                    if cg in (0, 3, 4):
                        o_ = of[it % 2]
                        for j in range(8):
                            ps = PS[j % 4]
                            for kk in range(8):
                                k.mm(ps[:, 0:n], W[:, kk, j * 128:(j + 1) * 128], h_[:, kk, 0:n], start=(kk == 0), stop=(kk == 7))
                            k.copy("act" if j % 2 == 0 else "dve", o_[:, j, 0:n], ps[:, 0:n])
                        if cg == 0:
                            dst = S["QT1"][b].rearrange("(j p) t -> p j t", p=128)[:, :, t0 - LC:t0 - LC + n]
                        else:
                            dst = S["ZF1" if cg == 3 else "ZB1"][b].rearrange("(j p) t -> p j t", p=128)[:, :, t0:t0 + n]
                        k.dma("pool", dst, o_[:, :, 0:n], wb=[Buf("x")])
                    else:
                        o_ = ot[it % 2]
                        for ts in range(n // 128):
                            for half in range(2):
                                ps = PS[4 + (ts * 2 + half) % 4]
                                for kk in range(8):
                                    k.mm(ps[:], h_[:, kk, ts * 128:(ts + 1) * 128], W[:, kk, half * 512:(half + 1) * 512],
                                         start=(kk == 0), stop=(kk == 7))
                                k.copy("act" if half == 0 else "dve", o_[:, ts, half * 512:(half + 1) * 512], ps[:])
                        if cg == 1:
                            dst = S["G1"][b, t0 - LC:t0 - LC + n, :].rearrange("(s p) d -> p s d", p=128)
                        else:
                            dst = S["V1"][b, t0:t0 + n, :].rearrange("(s p) d -> p s d", p=128)
                        k.dma("pool", dst, o_[:, 0:n // 128, :], wb=[Buf("x")])
                    it += 1
        k.barrier()


def phase_hgrn2(k, I, S, C):
    PS = C["PS"]
    ident = C["ident"]
    CH = 64
    with contextlib.ExitStack() as st:
        cmask = k.tile(st, "cmask", [128, 512])
        k.dma("sp", cmask[:], I["cmask"][:, :])
        tri = [k.tile(st, f"tri{d}", [64, 64]) for d in range(2)]
        k.dma("sp", tri[0][:], I["triu"][:, :])
        k.dma("sp", tri[1][:], I["tril"][:, :])
        lbt = k.tile(st, "lbt", [128, 8])
        names = ["z", "f", "lf", "kk", "bc", "bb", "eb", "qt", "kt", "kh", "tmp"]
        T_ = [{n: k.tile(st, f"g{d}{n}", [128, 512]) for n in names} for d in range(2)]
        ebe = [k.tile(st, f"ebe{d}", [128, 8]) for d in range(2)]
        vt = [k.tile(st, f"vt{d}", [64, 8, 128]) for d in range(2)]
        kht = [k.tile(st, f"kht{d}", [64, 8, 128]) for d in range(2)]
        ot = [k.tile(st, f"oo{d}", [64, 8, 128]) for d in range(2)]
        am = [k.tile(st, f"am{d}", [64, 64]) for d in range(2)]
        Sst = [k.tile(st, f"Sst{d}", [128, 128]) for d in range(2)]
        for b in range(NB):
            for hh in range(8):
                hs = slice(hh * 128, (hh + 1) * 128)
                for d in range(2):
                    for l_ in range(2):
                        k.dma("sp", lbt[:, d * 4 + l_:d * 4 + l_ + 1],
                              I["c_lower_bounds"][d, l_, hs].rearrange("(p o) -> p o", o=1))
                    k.tt("dve", lbt[:, d * 4 + 2:d * 4 + 3], lbt[:, d * 4:d * 4 + 1], lbt[:, d * 4 + 1:d * 4 + 2], ALU.subtract)
                    k.act(lbt[:, d * 4 + 2:d * 4 + 3], lbt[:, d * 4 + 2:d * 4 + 3], AF.Sigmoid)
                    k.ts("dve", lbt[:, d * 4 + 3:d * 4 + 4], lbt[:, d * 4 + 2:d * 4 + 3], -1.0, 1.0, ALU.mult, ALU.add)
                    k.memset("dve", Sst[d][:], 0.0)
                segs = {0: [(0, LC, False)] + [(LC + i * 512, 512, True) for i in range(L // 512)],
                        1: [(0, LC, False)] + [(LC + i * 512, 512, True) for i in reversed(range(L // 512))]}
                for si in range(len(segs[0])):
                    for d in range(2):
                        t0, n, is_lat = segs[d][si]
                        nchunk = n // CH
                        Td = T_[d]
                        zsrc = S["ZF1" if d == 0 else "ZB1"]
                        k.dma("sp", Td["z"][:, 0:n], zsrc[b, hs, t0:t0 + n])
                        k.dma("sp", vt[d][:, 0:nchunk, :], S["V1"][b, t0:t0 + n, hs].rearrange("(c s) e -> s c e", s=CH))
                        k.act(Td["f"][:, 0:n], Td["z"][:, 0:n], AF.Sigmoid)
                        k.ts("dve", Td["f"][:, 0:n], Td["f"][:, 0:n], lbt[:, d * 4 + 3:d * 4 + 4], lbt[:, d * 4 + 2:d * 4 + 3],
                             ALU.mult, ALU.add)
                        k.act(Td["lf"][:, 0:n], Td["f"][:, 0:n], AF.Ln)
                        k.ts("dve", Td["kk"][:, 0:n], Td["f"][:, 0:n], -1.0, 1.0, ALU.mult, ALU.add)
                        k.scan("dve", Td["bc"][:, 0:n], cmask[:, 0:n], Td["lf"][:, 0:n], 0.0, ALU.mult, ALU.add)
                        bc3 = Td["bc"][:, 0:n].rearrange("p (c s) -> p c s", s=CH)
                        bend = bc3[:, :, CH - 1:CH]
                        if d == 0:
                            bbv = Td["bc"]
                        else:
                            k.tt("dve", Td["bb"][:, 0:n].rearrange("p (c s) -> p c s", s=CH), bend.to_broadcast([128, nchunk, CH]),
                                 bc3, ALU.subtract)
                            k.tt("dve", Td["bb"][:, 0:n], Td["bb"][:, 0:n], Td["lf"][:, 0:n], ALU.add)
                            bbv = Td["bb"]
                        k.act(ebe[d][:, 0:nchunk], bend.rearrange("p c o -> p (c o)"), AF.Exp)
                        k.act(Td["tmp"][:, 0:n], bbv[:, 0:n], AF.Exp, scale=-1.0)
                        k.tt("dve", Td["kt"][:, 0:n], Td["kk"][:, 0:n], Td["tmp"][:, 0:n], ALU.mult)
                        k.tt("dve", Td["kh"][:, 0:n].rearrange("p (c s) -> p c s", s=CH),
                             Td["kt"][:, 0:n].rearrange("p (c s) -> p c s", s=CH),
                             ebe[d][:, 0:nchunk].unsqueeze(2).to_broadcast([128, nchunk, CH]), ALU.mult)
                        if is_lat:
                            k.dma("sp", Td["z"][:, 0:n], S["QT1"][b, hs, t0 - LC:t0 - LC + n])
                            k.act(Td["eb"][:, 0:n], bbv[:, 0:n], AF.Exp)
                            k.act(Td["qt"][:, 0:n], Td["z"][:, 0:n], AF.Silu)
                            k.tt("dve", Td["qt"][:, 0:n], Td["qt"][:, 0:n], Td["eb"][:, 0:n], ALU.mult)
                        for half in range((nchunk + 3) // 4):
                            ps = PS[d * 4 + 3]
                            nn = min(4, nchunk - half * 4)
                            for c in range(nn):
                                cc = half * 4 + c
                                k.mm(ps[0:64, c * 128:(c + 1) * 128], Td["kh"][:, cc * CH:(cc + 1) * CH], ident[:])
                            k.copy("act", kht[d][:, half * 4:half * 4 + nn, :],
                                   ps[0:64, 0:nn * 128].rearrange("p (c e) -> p c e", e=128))
                        order = range(nchunk) if d == 0 else reversed(range(nchunk))
                        for c in order:
                            cs = slice(c * CH, (c + 1) * CH)
                            if is_lat:
                                pa = PS[d * 4 + 0]
                                k.mm(pa[0:64, 0:64], Td["kt"][:, cs], Td["qt"][:, cs])
                                k.tt("dve", am[d][:], pa[0:64, 0:64], tri[d][:], ALU.mult)
                                po = PS[d * 4 + 1]
                                k.mm(po[0:64, 0:128], am[d][:], vt[d][:, c, :], start=True, stop=False)
                                k.mm(po[0:64, 0:128], Td["qt"][:, cs], Sst[d][:], start=False, stop=True)
                                k.copy("act", ot[d][:, c, :], po[0:64, 0:128])
                            pd = PS[d * 4 + 2]
                            k.mm(pd[:, 0:128], kht[d][:, c, :], vt[d][:, c, :])
                            k.stt("dve", Sst[d][:], Sst[d][:], ebe[d][:, c:c + 1], pd[:, 0:128], ALU.mult, ALU.add)
                        if is_lat:
                            k.dma("pool", S["O1"][d, b, t0 - LC:t0 - LC + n, hs].rearrange("(c s) e -> s c e", s=CH),
                                  ot[d][:, 0:nchunk, :], wb=[Buf("x")])
                k.barrier()


def phase_hgrn2_out(k, I, S, C):
    PS = C["PS"]
    ident = C["ident"]
    with contextlib.ExitStack() as st:
        ng = k.tile(st, "ng", [128, 1024])
        load_bcast(k, "sp", ng[:], I["c_norm_g"][0:1, :])
        of_ = [k.tile(st, f"ro_f{i}", [128, 1024]) for i in range(2)]
        ob_ = [k.tile(st, f"ro_b{i}", [128, 1024]) for i in range(2)]
        gg = [k.tile(st, f"ro_g{i}", [128, 1024]) for i in range(2)]
        o = k.tile(st, "ro_o", [128, 1024])
        sq = k.tile(st, "ro_sq", [128, 1024])
        ss = k.tile(st, "ro_ss", [128, 8])
        oT = [k.tile(st, f"ro_oT{i}", [128, 8, 128]) for i in range(2)]
        it = 0
        for b in range(NB):
            for t in range(L // 128):
                i2 = it % 2
                r0 = t * 128
                k.dma("sp", of_[i2][:], S["O1"][0, b, r0:r0 + 128, :])
                k.dma("sp", ob_[i2][:], S["O1"][1, b, r0:r0 + 128, :])
                k.dma("sp", gg[i2][:], S["G1"][b, r0:r0 + 128, :])
                k.tt("dve", o[:], of_[i2][:], ob_[i2][:], ALU.add)
                k.act(sq[:], o[:], AF.Square)
                k.op("dve", lambda: k.nc.vector.reduce_sum(ss[:], sq[:].rearrange("p (h e) -> p h e", e=128), AX.X),
                     [ss[:]], [sq[:]])
                k.ts("dve", ss[:], ss[:], 1.0 / 128, EPS, ALU.mult, ALU.add)
                k.act(ss[:], ss[:], AF.Sqrt)
                k.recip(ss[:], ss[:])
                k.tt("dve", o[:].rearrange("p (h e) -> p h e", e=128), o[:].rearrange("p (h e) -> p h e", e=128),
                     ss[:].unsqueeze(2).to_broadcast([128, 8, 128]), ALU.mult)
                k.tt("dve", o[:], o[:], ng[:], ALU.mult)
                k.act(gg[i2][:], gg[i2][:], AF.Sigmoid)
                k.tt("dve", o[:], o[:], gg[i2][:], ALU.mult)
                transpose_1024(k, C, o, oT[i2])
                k.dma("pool", S["OT1"][b].rearrange("(j p) t -> p j t", p=128)[:, :, r0:r0 + 128], oT[i2][:], wb=[Buf("x")])
                it += 1


def l1_moe_tiles(I, S, OUT):
    tiles = []
    for b in range(NB):
        for i in range(L // 512):
            t0 = i * 512
            tiles.append(dict(row=b, ysrc=[(S["OT1"][b, :, t0:t0 + 512], 512)],
                              xsrc=[S["X1"][b, t0 + j * 128:t0 + (j + 1) * 128, :] for j in range(4)],
                              dst=[OUT[b, t0 + j * 128:t0 + (j + 1) * 128, :] for j in range(4)]))
    return tiles


_CACHE = {}


def kernel(**inputs):
    x = np.ascontiguousarray(inputs["x"], dtype=np.float32)
    c = np.asarray(inputs["c"], dtype=np.float32)
    ctx = np.ascontiguousarray(inputs["ctx"], dtype=np.float32)
    c_ctx = np.asarray(inputs["c_ctx"], dtype=np.float32)
    if "nc" not in _CACHE:
        _CACHE["nc"] = build()
    nc = _CACHE["nc"]
    consts = host_consts()
    shared = {n: np.ascontiguousarray(inputs[n], dtype=np.float32) for n in WEIGHT_SHAPES}
    shared.update(consts)
    in_maps = []
    for i in range(NCORES):
        m = dict(shared)
        m["x"] = x[i * NB:(i + 1) * NB]
        m["ctx"] = ctx[i * NB:(i + 1) * NB]
        m["cvec"] = np.concatenate([c[i * NB:(i + 1) * NB], c_ctx[None, :]], axis=0)
        in_maps.append(m)
    res = run_bass_kernel_spmd(nc, in_maps, core_ids=list(range(NCORES)))
    return np.concatenate([r["out"] for r in res.results], axis=0)
```

```python
import contextlib
import math
import numpy as np
import concourse.bass as bass
import concourse.mybir as mybir
from concourse.bass_utils import run_bass_kernel_spmd

F32 = mybir.dt.float32
BF16 = mybir.dt.bfloat16
ALU = mybir.AluOpType
AF = mybir.ActivationFunctionType
AX = mybir.AxisListType
AP = bass.AP

NCORES = 8
NB = 2
L = 4096
LC = 256
D = 1024
HY = 768
S5D = 256
EPS = 1e-6
MAGIC = 12582912.0
TWO_PI = 2.0 * math.pi


class Buf:
    __slots__ = ("w", "r", "name")

    def __init__(self, name):
        self.name = name
        self.w = None
        self.r = []


class Tile(Buf):
    __slots__ = ("t", "sub")

    def __init__(self, name, t):
        Buf.__init__(self, name)
        self.t = t
        self.sub = [Buf(f"{name}.{i}") for i in range(4)]

    def __getitem__(self, key):
        return self.t[key]


class DT(Buf):
    __slots__ = ("h", "a", "regions")

    def __init__(self, name, h):
        Buf.__init__(self, name)
        self.h = h
        self.a = h.ap()
        self.regions = {}

    def __getitem__(self, key):
        return self.a[key]

    def reg(self, key):
        b = self.regions.get(key)
        if b is None:
            b = Buf(f"{self.name}:{key}")
            self.regions[key] = b
        return b


class KB:
    ENG = ("pe", "act", "dve", "pool", "sp")

    def __init__(self, nc):
        self.nc = nc
        self.es = contextlib.ExitStack()
        self.eng = {"pe": nc.tensor, "act": nc.scalar, "dve": nc.vector, "pool": nc.gpsimd, "sp": nc.sync}
        self.sems = {}
        self.sem_list = []
        self.cnt = {}
        self.cur = {}
        self.spare = {e: [] for e in self.ENG}
        nsem = {"pe": 9, "act": 3, "dve": 12, "pool": 3, "sp": 1}
        for e in self.ENG:
            for i in range(nsem[e]):
                s = self.es.enter_context(nc.semaphore(f"s_{e}{i}"))
                self.spare[e].append(self._reg_sem(s))
            self.cur[e] = self.spare[e].pop(0)
        self.dring = {}
        for e in ("sp", "pool", "act"):
            ring = []
            for i in range(12):
                s = self.es.enter_context(nc.semaphore(f"d_{e}{i}"))
                ring.append(self._reg_sem(s))
            self.dring[e] = [ring, 0]
        self.waited = {e: {} for e in self.ENG}
        self.bufs = {}
        self.ninstr = 0

    def _reg_sem(self, s):
        self.sem_list.append(s)
        self.cnt[len(self.sem_list) - 1] = 0
        return len(self.sem_list) - 1

    def tile(self, stack, name, shape, dt=F32):
        self.ninstr += 1
        name = f"t{self.ninstr}_" + name
        t = stack.enter_context(self.nc.sbuf_tensor(name, list(shape), dt))
        tl = Tile(name, t)
        self.bufs[name] = tl
        return tl

    def ptile(self, stack, name, shape, dt=F32):
        name = "t_" + name
        t = stack.enter_context(self.nc.psum_tensor(name, list(shape), dt))
        tl = Tile(name, t)
        self.bufs[name] = tl
        return tl

    def dram(self, name, shape, dt=F32, kind="Internal"):
        h = self.nc.dram_tensor(name, list(shape), dt, kind=kind)
        d = DT(name, h)
        self.bufs[name] = d
        return d

    def buf_of(self, ap):
        return self.bufs[ap.tensor.name]

    def _wait(self, e, tok):
        si, val = tok
        if self.waited[e].get(si, 0) >= val:
            return
        self.eng[e].wait_ge(self.sem_list[si], val)
        self.waited[e][si] = val

    def _sync(self, e, rb, wb):
        own = self.cur[e]
        for b in rb:
            if b.w is not None and not (e == "pe" and b.w[0] == own):
                self._wait(e, b.w)
        for b in wb:
            if b.w is not None and not (e == "pe" and b.w[0] == own):
                self._wait(e, b.w)
            for t in b.r:
                if not (e == "pe" and t[0] == own):
                    self._wait(e, t)

    def _mark(self, tok, rb, wb):
        for b in rb:
            b.r.append(tok)
            if len(b.r) > 24:
                best = {}
                for t in b.r:
                    if best.get(t[0], 0) < t[1]:
                        best[t[0]] = t[1]
                b.r = list(best.items())
        for b in wb:
            b.w = tok
            b.r = []

    def op(self, e, fn, outs, ins, rb=None, wb=None):
        if rb is None:
            rb = [self.buf_of(a) for a in ins if isinstance(a, AP)]
        if wb is None:
            wb = [self.buf_of(a) for a in outs if isinstance(a, AP)]
        self._sync(e, rb, wb)
        ins_ = fn()
        si = self.cur[e]
        self.cnt[si] += 1
        ins_.then_inc(self.sem_list[si], 1)
        tok = (si, self.cnt[si])
        self._mark(tok, rb, wb)
        self.ninstr += 1
        return tok

    def dma(self, e, out, in_, rb=None, wb=None, **kw):
        if rb is None:
            rb = [self.buf_of(in_)]
        if wb is None:
            wb = [self.buf_of(out)]
        self._sync(e, rb, wb)
        ring, pos = self.dring[e]
        si = ring[pos % len(ring)]
        self.dring[e][1] = pos + 1
        self._wait(e, (si, self.cnt[si]))
        self.cnt[si] += 16
        self.eng[e].dma_start(out=out, in_=in_, **kw).then_inc(self.sem_list[si], 16)
        tok = (si, self.cnt[si])
        self._mark(tok, rb, wb)
        self.ninstr += 1
        return tok

    def barrier(self):
        toks = [(si, c) for si, c in self.cnt.items() if c > 0]
        for e in self.ENG:
            for t in toks:
                if t[0] == self.cur[e]:
                    continue
                self._wait(e, t)
        for e in self.ENG:
            if self.cnt[self.cur[e]] > 16000 and self.spare[e]:
                self.cur[e] = self.spare[e].pop(0)

    def mm(self, out, lhsT, rhs, start=True, stop=True):
        return self.op("pe", lambda: self.nc.tensor.matmul(out, lhsT, rhs, start=start, stop=stop), [out], [lhsT, rhs])

    def act(self, out, in_, func, bias=0.0, scale=1.0, accum_out=None, e="act"):
        outs = [out] + ([accum_out] if accum_out is not None else [])
        ins = [in_] + [a for a in (bias, scale) if isinstance(a, AP)]
        kw = {}
        if accum_out is not None:
            kw["accum_out"] = accum_out
        return self.op("act", lambda: self.nc.scalar.activation(out, in_, func, bias=bias, scale=scale, **kw), outs, ins)

    def copy(self, e, out, in_):
        if e == "act":
            return self.op("act", lambda: self.nc.scalar.copy(out, in_), [out], [in_])
        return self.op(e, lambda: self.eng[e].tensor_copy(out, in_), [out], [in_])

    def tt(self, e, out, a, b, op):
        return self.op(e, lambda: self.eng[e].tensor_tensor(out, a, b, op), [out], [a, b])

    def ts(self, e, out, a, s1, s2, op0, op1=None, accum_out=None):
        outs = [out] + ([accum_out] if accum_out is not None else [])
        ins = [a] + [s for s in (s1, s2) if isinstance(s, AP)]
        if op1 is None:
            return self.op(e, lambda: self.eng[e].tensor_single_scalar(out, a, s1, op0), outs, ins)
        kw = {}
        if accum_out is not None:
            kw["accum_out"] = accum_out
        return self.op(e, lambda: self.eng[e].tensor_scalar(out, a, s1, s2, op0, op1, **kw), outs, ins)

    def stt(self, e, out, a, s, b, op0, op1):
        ins = [a, b] + ([s] if isinstance(s, AP) else [])
        return self.op(e, lambda: self.eng[e].scalar_tensor_tensor(out, a, s, b, op0, op1), [out], ins)

    def memset(self, e, out, val):
        return self.op(e, lambda: self.eng[e].memset(out, val), [out], [])

    def recip(self, out, in_):
        return self.op("dve", lambda: self.nc.vector.reciprocal(out, in_), [out], [in_])

    def scan(self, e, out, d0, d1, init, op0, op1):
        ins = [d0, d1] + ([init] if isinstance(init, AP) else [])
        return self.op(e, lambda: self.eng[e].tensor_tensor_scan(out, d0, d1, init, op0, op1), [out], ins)


def _cplx_lhsT(W):
    return np.block([[W.real, W.imag], [-W.imag, W.real]])


def fft_tables():
    t = {}
    n1 = np.arange(128)[:, None].astype(np.float64)
    k1 = np.arange(128)[None, :].astype(np.float64)
    ang = 2.0 * np.pi * n1 * k1 / 128.0
    cs, sn = np.cos(ang), np.sin(ang)
    t["ff1"] = np.concatenate([cs, -sn], axis=1).astype(np.float32)
    t["f1d"] = np.concatenate([np.concatenate([cs[:64], -sn[:64]], axis=1),
                               np.concatenate([sn[:64], cs[:64]], axis=1)], axis=0).astype(np.float32)
    n2 = np.arange(64)[:, None].astype(np.float64)
    k2 = np.arange(64)[None, :].astype(np.float64)
    T2 = np.zeros((128, 128, 128), np.float32)
    for k1_ in range(128):
        W = np.exp(-2j * np.pi * n2 * (k1_ + 128.0 * k2) / 8192.0)
        T2[k1_] = _cplx_lhsT(W)
    t["fft_T2"] = T2
    Wi = np.exp(2j * np.pi * np.arange(64)[:, None] * np.arange(64)[None, :] / 64.0)
    G1 = _cplx_lhsT(Wi)
    Q = np.block([[np.zeros((64, 64)), -np.eye(64)], [np.eye(64), np.zeros((64, 64))]])
    t["fft_G1"] = G1.astype(np.float32)
    t["fft_G2"] = (Q.T @ G1).astype(np.float32)
    T4 = np.zeros((64, 2, 128, 128), np.float32)
    kk = np.arange(128)[:, None].astype(np.float64)
    nn = np.arange(64)[None, :].astype(np.float64)
    for n2_ in range(64):
        W = np.exp(2j * np.pi * (n2_ * kk / 8192.0 + nn * kk / 128.0)) / 8192.0
        T4[n2_, 0] = np.concatenate([W.real, W.imag], axis=1)
        T4[n2_, 1] = np.concatenate([-W.imag, W.real], axis=1)
    t["fft_T4"] = T4
    return t


def host_consts():
    c = {}
    c["ident"] = np.eye(128, dtype=np.float32)
    c["antiI"] = np.eye(128, dtype=np.float32)[::-1].copy()
    c["ones"] = np.ones((128, 128), np.float32)
    for nm, Lf in (("lat", L), ("ctx", LC)):
        pos = np.arange(Lf, dtype=np.float32)
        t = pos / np.float32(max(Lf - 1, 1))
        w = np.float32(2.0 * math.pi) * pos / np.float32(Lf)
        bands = np.linspace(1e-4, 15, 16, dtype=np.float32)
        ang = w[:, None] * bands[None, :]
        z = np.concatenate([t[:, None], np.cos(ang), -np.sin(ang)], axis=-1).astype(np.float32)
        c["zT_" + nm] = np.ascontiguousarray(z.T)
        c["trow_" + nm] = np.ascontiguousarray(np.broadcast_to(t[None, :], (128, Lf))).astype(np.float32)
    deltas = np.abs(np.linspace(math.log(1e-2) / 1.5, math.log(1e-2) / 0.3, HY, dtype=np.float32))
    c["hy_ndelta"] = (-deltas).reshape(HY, 1).astype(np.float32)
    c["ndelta_row"] = (-deltas).reshape(1, HY).astype(np.float32)
    tl = (np.arange(L, dtype=np.float32) / np.float32(L - 1)).reshape(32, 128).T
    c["tneg_lat"] = np.ascontiguousarray(tl).astype(np.float32)
    c.update(fft_tables())
    cm = np.ones((128, 512), np.float32)
    cm[:, ::64] = 0.0
    c["cmask"] = cm
    c["triu"] = np.triu(np.ones((64, 64), np.float32))
    c["tril"] = np.tril(np.ones((64, 64), np.float32))
    c["iota1"] = np.ascontiguousarray(np.broadcast_to(np.arange(1, 257, dtype=np.float32)[None, :], (128, 256)))
    return c


CONST_SHAPES = {"ident": [128, 128], "antiI": [128, 128], "ones": [128, 128], "zT_lat": [33, L], "zT_ctx": [33, LC],
                "trow_lat": [128, L], "trow_ctx": [128, LC], "hy_ndelta": [HY, 1], "iota1": [128, 256], "cmask": [128, 512], "triu": [64, 64], "tril": [64, 64],
                "ff1": [128, 256], "f1d": [128, 256], "fft_T2": [128, 128, 128], "fft_G1": [128, 128], "fft_G2": [128, 128],
                "fft_T4": [64, 2, 128, 128], "tneg_lat": [128, 32], "ndelta_row": [1, HY]}

WEIGHT_SHAPES = {
    "mod_w": [2, 1024, 6144], "mod_b": [2, 6144], "norm1_g": [2, 1024], "norm2_g": [2, 1024], "final_g": [1024],
    "ab_w_in": [1, 1024, 2560], "ab_w_out": [1, 1024, 1024], "hy_conv_w": [1, 3, 2304], "hy_conv_b": [1, 2304],
    "hy_fw1": [1, 33, 64], "hy_fb1": [1, 64], "hy_ff1": [1, 64], "hy_fw2": [1, 64, 64], "hy_fb2": [1, 64],
    "hy_ff2": [1, 64], "hy_fw3": [1, 64, 3072], "hy_bias": [1, 2, 768],
    "s5_lam_re": [1, 2, 16, 64], "s5_lam_im": [1, 2, 16, 64], "s5_log_step": [1, 2, 16],
    "s5_b_re": [1, 2, 16, 64, 16], "s5_b_im": [1, 2, 16, 64, 16], "s5_c_re": [1, 2, 16, 16, 64],
    "s5_c_im": [1, 2, 16, 16, 64], "s5_d": [1, 256], "s5_glu_w": [1, 256, 512], "s5_glu_b": [1, 512],
    "c_w_in": [1, 1024, 5120], "c_w_out": [1, 1024, 1024], "c_lower_bounds": [2, 2, 1024], "c_norm_g": [1, 1024],
    "moe_wg": [2, 1024, 4], "moe_bg": [2, 4], "moe_we": [2, 1024, 32], "moe_be": [2, 32],
    "moe_w_gate": [2, 4, 8, 1024, 256], "moe_w_up": [2, 4, 8, 1024, 256], "moe_w_down": [2, 4, 8, 256, 1024],
}


DBGF = set()
USE_FFT = True


def build(stop=99, dbg=()):
    DBGF.clear()
    DBGF.update(dbg)
    nc = bass.Bass("TRN2", target_bir_lowering=False)
    k = KB(nc)
    I = {}
    I["x"] = k.dram("x", [NB, L, D], kind="ExternalInput")
    I["ctx"] = k.dram("ctx", [NB, LC, D], kind="ExternalInput")
    I["cvec"] = k.dram("cvec", [3, D], kind="ExternalInput")
    for n, s in WEIGHT_SHAPES.items():
        I[n] = k.dram(n, s, kind="ExternalInput")
    for n, s in CONST_SHAPES.items():
        I[n] = k.dram(n, s, kind="ExternalInput")
    OUT = k.dram("out", [NB, L, D], kind="ExternalOutput")

    def scratch(name, shape):
        return k.dram(name, shape, kind=("ExternalOutput" if name in dbg else "Internal"))

    S = {}
    S["MV"] = scratch("MV", [2, 3, 6, D])
    S["PT0"] = scratch("PT0", [NB, 2560, LC + L])
    S["UT0"] = scratch("UT0", [NB, LC + L, 256])
    S["YT0"] = scratch("YT0", [NB, 1024, LC + L])
    S["P0"] = scratch("P0", [NB, L + 2, 2304])
    S["KT"] = scratch("KT", [2 * L, 2 * HY])
    S["HB"] = scratch("HB", [L + 128, 2 * HY])
    S["ESC"] = scratch("ESC", [1, 2 * HY])
    S["KF"] = scratch("KF", [2, 128, 128, HY])
    S["FS1"] = scratch("FS1", [2, 64, 128, CC])
    S["FS2"] = scratch("FS2", [2, 64, 128, CC])
    S["YH"] = scratch("YH", [NB, L, HY])
    S["X1"] = scratch("X1", [NB, L, D])
    S["CTX1"] = scratch("CTX1", [NB, LC, D])
    S["HT1"] = k.dram("HT1", [NB, D, TT], dt=BF16)
    S["QT1"] = scratch("QT1", [NB, D, L])
    S["ZF1"] = scratch("ZF1", [NB, D, TT])
    S["ZB1"] = scratch("ZB1", [NB, D, TT])
    S["V1"] = scratch("V1", [NB, TT, D])
    S["G1"] = scratch("G1", [NB, L, D])
    S["O1"] = scratch("O1", [2, NB, L, D])
    S["OT1"] = scratch("OT1", [NB, D, L])
    S["DBG"] = scratch("DBG", [16, 128, 4096])
    S["YS5"] = scratch("YS5", [2, NB, LC + L, 256])

    with k.es:
        glob = k.es
        ident = k.tile(glob, "ident", [128, 128])
        antiI = k.tile(glob, "antiI", [128, 128])
        ones = k.tile(glob, "ones", [128, 128])
        k.dma("sp", ident[:], I["ident"][:, :])
        k.dma("sp", antiI[:], I["antiI"][:, :])
        k.dma("sp", ones[:], I["ones"][:, :])
        PS = [k.ptile(glob, f"ps{i}", [128, 512]) for i in range(8)]
        C = dict(ident=ident, antiI=antiI, ones=ones, PS=PS)

        phase_mod(k, I, S, C)
        k.barrier()
        if stop >= 1:
            phase_l0_inproj(k, I, S, C)
            k.barrier()
        if stop >= 2 and "nohy" not in dbg:
            phase_hyena(k, I, S, C)
            k.barrier()
            if USE_FFT:
                phase_hyena_fft(k, I, S, C)
                k.barrier()
        if stop >= 3:
            phase_s5(k, I, S, C)
            k.barrier()
            phase_s5_out(k, I, S, C)
            k.barrier()
        if stop >= 4:
            tl = l0_moe_tiles(I, S)
            if "moe1" in DBGF:
                tl = tl[:2]
            phase_moe(k, I, S, C, 0, S["YT0"], I["ab_w_out"][0], tl, final=False)
            k.barrier()
        if stop >= 5:
            phase_l1_norm(k, I, S, C)
            k.barrier()
            phase_l1_inproj(k, I, S, C)
            k.barrier()
        if stop >= 6:
            phase_hgrn2(k, I, S, C)
            k.barrier()
            phase_hgrn2_out(k, I, S, C)
            k.barrier()
        if stop >= 7:
            tl = l1_moe_tiles(I, S, OUT)
            if "moe1" in DBGF:
                tl = tl[:1]
            phase_moe(k, I, S, C, 1, S["OT1"], I["c_w_out"][0], tl, final=True)
            k.barrier()
        k.barrier()
    return nc


def phase_mod(k, I, S, C):
    PS = C["PS"]
    with contextlib.ExitStack() as st:
        cT = k.tile(st, "cT", [128, 3, 8])
        sT = k.tile(st, "sT", [128, 3, 8])
        for r in range(3):
            k.dma("sp", cT[:, r, :], I["cvec"][r].rearrange("(k p) -> p k", p=128), allow_slow_non_contiguous=True,
                  wb=[Buf("tmp")])
        k.barrier()
        k.act(sT[:], cT[:], AF.Silu)
        wt = [k.tile(st, f"modw{i}", [128, 8, 512]) for i in range(2)]
        mv = k.tile(st, "mv", [3, 6144])
        bias = k.tile(st, "modb", [3, 6144])
        g1 = k.tile(st, "g1b", [3, 1024])
        g2 = k.tile(st, "g2b", [3, 1024])
        for l in range(2):
            k.dma("sp", bias[:], I["mod_b"][l:l + 1, :].partition_broadcast(3))
            k.dma("sp", g1[:], I["norm1_g"][l:l + 1, :].partition_broadcast(3))
            k.dma("sp", g2[:], I["norm2_g"][l:l + 1, :].partition_broadcast(3))
            wv = I["mod_w"][l].rearrange("(k p) n -> p k n", p=128)
            for j in range(12):
                w = wt[j % 2]
                k.dma("sp" if j % 2 == 0 else "pool", w[:], wv[:, :, j * 512:(j + 1) * 512])
                ps = PS[j % 2]
                for kk in range(8):
                    k.mm(ps[0:3, :], sT[:, :, kk], w[:, kk, :], start=(kk == 0), stop=(kk == 7))
                k.tt("dve", mv[:, j * 512:(j + 1) * 512], ps[0:3, :], bias[:, j * 512:(j + 1) * 512], ALU.add)
            k.stt("dve", mv[:, 1024:2048], mv[:, 1024:2048], 1.0, g1[:], ALU.add, ALU.mult)
            k.stt("dve", mv[:, 4096:5120], mv[:, 4096:5120], 1.0, g2[:], ALU.add, ALU.mult)
            k.dma("sp", S["MV"][l].rearrange("r c d -> r (c d)"), mv[:])


def load_bcast(k, eng, tile_ap, dram_row_ap):
    return k.dma(eng, tile_ap, dram_row_ap.partition_broadcast(tile_ap.shape[0]))


def rms_mod(k, st_tmp, xt, A, sh, out, name):
    sq, ss, rstd = st_tmp
    k.act(sq[:], xt, AF.Square, accum_out=ss[:])
    k.ts("dve", ss[:], ss[:], 1.0 / D, EPS, ALU.mult, ALU.add)
    k.act(ss[:], ss[:], AF.Sqrt)
    k.recip(rstd[:], ss[:])
    k.stt("dve", out, xt, rstd[:, 0:1], A, ALU.mult, ALU.mult)
    k.tt("dve", out, out, sh, ALU.add)


def transpose_1024(k, C, src, dstT, pbase=4):
    PS = C["PS"]
    for half in range(2):
        ps = PS[pbase + half]
        for j in range(4):
            kk = half * 4 + j
            k.mm(ps[:, j * 128:(j + 1) * 128], src[:, kk * 128:(kk + 1) * 128], C["ident"][:], start=True, stop=True)
        k.copy("act", dstT[:, half * 4:(half + 1) * 4, :], ps[:].rearrange("p (j t) -> p j t", j=4))


TT = LC + L


def phase_l0_inproj(k, I, S, C):
    PS = C["PS"]
    with contextlib.ExitStack() as st:
        W = k.tile(st, "Win0", [128, 8, 2560], BF16)
        wst = [k.tile(st, f"Wst{i}", [128, 2560]) for i in range(2)]
        wv = I["ab_w_in"][0].rearrange("(k p) n -> p k n", p=128)
        for kk in range(8):
            k.dma("sp" if kk % 2 == 0 else "pool", wst[kk % 2][:], wv[:, kk, :])
            k.copy("act" if kk % 2 == 0 else "dve", W[:, kk, :], wst[kk % 2][:])
        k.barrier()
        A = k.tile(st, "A1", [128, 1024])
        sh = k.tile(st, "sh1", [128, 1024])
        xt = [k.tile(st, f"xt{i}", [128, 1024]) for i in range(2)]
        h = k.tile(st, "h", [128, 1024])
        hT = k.tile(st, "hT", [128, 8, 128], BF16)
        po = [k.tile(st, f"po{i}", [128, 20, 128]) for i in range(2)]
        pu = [k.tile(st, f"pu{i}", [128, 256]) for i in range(2)]
        pt = [k.tile(st, f"pt{i}", [128, 2304]) for i in range(2)]
        zrow = k.tile(st, "zrow", [1, 2304])
        k.memset("dve", zrow[:], 0.0)
        for b in range(NB):
            k.dma("sp", S["P0"][b, 0:1, :], zrow[:], wb=[Buf("x")])
            k.dma("sp", S["P0"][b, L + 1:L + 2, :], zrow[:], wb=[Buf("x")])
        sq = k.tile(st, "sq", [128, 1024])
        ss = k.tile(st, "ss", [128, 1])
        rstd = k.tile(st, "rstd", [128, 1])
        it = 0
        for b in range(NB):
            for (src, row, ntile, toff) in ((I["ctx"], 2, LC // 128, 0), (I["x"], b, L // 128, LC)):
                load_bcast(k, "sp", A[:], S["MV"][0, row, 1:2, :])
                load_bcast(k, "sp", sh[:], S["MV"][0, row, 0:1, :])
                for t in range(ntile):
                    x_ = xt[it % 2]
                    o_ = po[it % 2]
                    u_ = pu[it % 2]
                    k.dma("sp", x_[:], src[b, t * 128:(t + 1) * 128, :])
                    rms_mod(k, (sq, ss, rstd), x_[:], A[:], sh[:], h[:], "l0")
                    transpose_1024(k, C, h, hT)
                    for j4 in range(5):
                        ps = PS[j4 % 4]
                        for jj in range(4):
                            j = j4 * 4 + jj
                            for kk in range(8):
                                k.mm(ps[:, jj * 128:(jj + 1) * 128], W[:, kk, j * 128:(j + 1) * 128], hT[:, kk, :],
                                     start=(kk == 0), stop=(kk == 7))
                        k.copy("act" if j4 % 2 == 0 else "dve", o_[:, j4 * 4:(j4 + 1) * 4, :],
                               ps[:].rearrange("p (j t) -> p j t", j=4))
                    ps = PS[6]
                    for kk in range(8):
                        k.mm(ps[:, 0:256], hT[:, kk, :], W[:, kk, 2304:2560], start=(kk == 0), stop=(kk == 7))
                    k.copy("dve", u_[:], ps[:, 0:256])
                    if toff == LC and USE_FFT:
                        p_ = pt[it % 2]
                        for j in range(5):
                            ps = PS[6 + j % 2]
                            w_ = 512 if j < 4 else 256
                            for kk in range(8):
                                k.mm(ps[:, 0:w_], hT[:, kk, :], W[:, kk, j * 512:j * 512 + w_], start=(kk == 0), stop=(kk == 7))
                            k.copy("act" if j % 2 == 0 else "dve", p_[:, j * 512:j * 512 + w_], ps[:, 0:w_])
                        k.dma("pool", S["P0"][b, 1 + t * 128:1 + (t + 1) * 128, :], p_[:], wb=[Buf("x")])
                    t0 = toff + t * 128
                    k.dma("pool", S["PT0"][b].rearrange("(j p) t -> p j t", p=128)[:, :, t0:t0 + 128], o_[:],
                          wb=[S["PT0"].reg((b, t0))])
                    k.dma("pool", S["UT0"][b, t0:t0 + 128, :], u_[:], wb=[S["UT0"].reg((b, t0))])
                    it += 1


def sin_rr(k, out, src, tmp_r, tmp_n, pre_scale=None, pre_bias=None):
    if pre_scale is not None:
        k.ts("dve", tmp_r, src, pre_bias, pre_scale, ALU.add, ALU.mult)
        k.ts("dve", tmp_r, tmp_r, 1.0 / TWO_PI, None, ALU.mult)
    else:
        k.ts("dve", tmp_r, src, 1.0 / TWO_PI, None, ALU.mult)
    k.ts("dve", tmp_n, tmp_r, MAGIC, MAGIC, ALU.add, ALU.subtract)
    k.tt("dve", tmp_r, tmp_r, tmp_n, ALU.subtract)
    k.act(out, tmp_r, AF.Sin, scale=TWO_PI)


def phase_hyena(k, I, S, C):
    PS = C["PS"]
    for (T, toff, zname, tname) in ((LC, 0, "zT_ctx", "trow_ctx"), (L, LC, "zT_lat", "trow_lat")):
        if ("hyctx" in DBGF or USE_FFT) and T == L:
            continue
        with contextlib.ExitStack() as st:
            h2 = k.tile(st, "hyh2", [64, T])
            with contextlib.ExitStack() as st2:
                zT = k.tile(st2, "zT", [33, T])
                k.dma("sp", zT[:], I[zname][:, :])
                fw1 = k.tile(st2, "fw1", [33, 64])
                fw2 = k.tile(st2, "fw2", [64, 64])
                k.dma("sp", fw1[:], I["hy_fw1"][0])
                k.dma("sp", fw2[:], I["hy_fw2"][0])
                prm = k.tile(st2, "hyprm", [64, 4])
                for i, n in enumerate(("hy_fb1", "hy_ff1", "hy_fb2", "hy_ff2")):
                    k.dma("sp", prm[:, i:i + 1], I[n][0].rearrange("(p o) -> p o", o=1), wb=[Buf("tmp")])
                k.barrier()
                h1 = k.tile(st2, "hyh1", [64, T])
                tr = k.tile(st2, "hytr", [64, 512])
                tn = k.tile(st2, "hytn", [64, 512])
                nb = min(T, 512)
                for blk in range(T // nb):
                    sl = slice(blk * nb, (blk + 1) * nb)
                    k.mm(PS[0][0:64, 0:nb], fw1[:], zT[:, sl])
                    sin_rr(k, h1[:, sl], PS[0][0:64, 0:nb], tr[:, 0:nb], tn[:, 0:nb], prm[:, 1:2], prm[:, 0:1])
                    k.mm(PS[1][0:64, 0:nb], fw2[:], h1[:, sl])
                    sin_rr(k, h2[:, sl], PS[1][0:64, 0:nb], tr[:, 0:nb], tn[:, 0:nb], prm[:, 3:4], prm[:, 2:3])
                if "DBG" in DBGF and T == LC:
                    k.dma("sp", S["DBG"][8, 0:64, 0:T], h1[:, :], wb=[Buf("x")])
                    k.dma("sp", S["DBG"][9, 0:64, 0:4], prm[:, :], wb=[Buf("x")])
                    k.dma("sp", S["DBG"][10, 0:33, 0:T], zT[:, :], wb=[Buf("x")])
                    k.dma("sp", S["DBG"][10, 64:97, 0:64], fw1[:, :], wb=[Buf("x")])
                k.barrier()
            fw3 = k.tile(st, "fw3", [64, 3072])
            k.dma("sp", fw3[:], I["hy_fw3"][0])
            trow = k.tile(st, "trow", [128, T])
            k.dma("sp", trow[:], I[tname][:, :])
            dec = k.tile(st, "dec", [128, T])
            kfb = k.tile(st, "kfb", [128, NB, T])
            raw = k.tile(st, "raw", [128, NB, T + 2])
            v = k.tile(st, "v", [128, NB, T])
            acc = k.tile(st, "acc", [128, NB, T])
            prm = k.tile(st, "cprm", [128, 3, 8])
            en = k.tile(st, "en", [128, 4])
            k.memset("dve", raw[:], 0.0)
            nb = min(T, 512)

            def load_conv(ct, blk, dst):
                col = blk * HY + ct * 128
                for b in range(NB):
                    k.dma("sp", raw[:, b, 1:T + 1], S["PT0"][b, col:col + 128, toff:toff + T])
                k.ts("dve", dst, raw[:, :, 0:T], prm[:, blk, 0:1], prm[:, blk, 3:4], ALU.mult, ALU.add)
                k.stt("dve", dst, raw[:, :, 1:T + 1], prm[:, blk, 1:2], dst, ALU.mult, ALU.add)
                k.stt("dve", dst, raw[:, :, 2:T + 2], prm[:, blk, 2:3], dst, ALU.mult, ALU.add)

            for ct in range(HY // 128):
                for blk in range(3):
                    col = blk * HY + ct * 128
                    k.dma("sp", prm[:, blk, 0:3], I["hy_conv_w"][0][:, col:col + 128].rearrange("j c -> c j"),
                          allow_slow_non_contiguous=True)
                    k.dma("sp", prm[:, blk, 3:4], I["hy_conv_b"][0][col:col + 128].rearrange("(p o) -> p o", o=1))
                for o in range(2):
                    k.dma("sp", prm[:, 0, 4 + o:5 + o], I["hy_bias"][0][o, ct * 128:(ct + 1) * 128].rearrange("(p o) -> p o", o=1))
                k.dma("sp", prm[:, 0, 6:7], I["hy_ndelta"][ct * 128:(ct + 1) * 128, :])
                k.act(dec[:], trow[:], AF.Exp, scale=prm[:, 0, 6:7])
                load_conv(ct, 2, v[:])
                for o in range(2):
                    for d_ in (0, 1):
                        col = o * 2 * HY + d_ * HY + ct * 128
                        for blk in range(T // nb):
                            sl = slice(blk * nb, (blk + 1) * nb)
                            ps = PS[blk % 4]
                            k.mm(ps[:, 0:nb], fw3[:, col:col + 128], h2[:, sl])
                            k.tt("dve", kfb[:, d_, sl], ps[:, 0:nb], dec[:, sl], ALU.mult)
                    k.memset("dve", kfb[:, 1, 0:1], 0.0)
                    k.act(acc[:, 0, :], kfb[:, 0, :], AF.Square, accum_out=en[:, 0:1])
                    k.act(acc[:, 0, :], kfb[:, 1, :], AF.Square, accum_out=en[:, 1:2])
                    k.tt("dve", en[:, 2:3], en[:, 0:1], en[:, 1:2], ALU.add)
                    k.act(en[:, 2:3], en[:, 2:3], AF.Sqrt)
                    k.recip(en[:, 3:4], en[:, 2:3])
                    k.ts("dve", kfb[:], kfb[:], en[:, 3:4], None, ALU.mult)
                    if "DBG" in DBGF and ct == 0 and T == LC:
                        k.dma("sp", S["DBG"][o * 4 + 0, :, 0:T], kfb[:, 0, :], wb=[Buf("x")])
                        k.dma("sp", S["DBG"][o * 4 + 1, :, 0:T], kfb[:, 1, :], wb=[Buf("x")])
                        k.dma("sp", S["DBG"][o * 4 + 2, :, 0:T], v[:, 0, :], wb=[Buf("x")])
                        k.dma("sp", S["DBG"][o * 4 + 3, 0:64, 0:T], h2[:, :], wb=[Buf("x")])
                    k.ts("dve", acc[:], v[:], kfb[:, 0, 0:1], None, ALU.mult)
                    for lag in range(1, T):
                        k.stt("dve", acc[:, :, lag:T], v[:, :, 0:T - lag], kfb[:, 0, lag:lag + 1], acc[:, :, lag:T],
                              ALU.mult, ALU.add)
                        k.stt("dve", acc[:, :, 0:T - lag], v[:, :, lag:T], kfb[:, 1, lag:lag + 1], acc[:, :, 0:T - lag],
                              ALU.mult, ALU.add)
                    k.stt("dve", acc[:], v[:], prm[:, 0, 4 + o:5 + o], acc[:], ALU.mult, ALU.add)
                    load_conv(ct, o, kfb[:])
                    if o == 0:
                        k.tt("dve", v[:], acc[:], kfb[:], ALU.mult)
                    else:
                        k.tt("dve", acc[:], acc[:], kfb[:], ALU.mult)
                        for b in range(NB):
                            k.dma("pool", S["YT0"][b, ct * 128:(ct + 1) * 128, toff:toff + T], acc[:, b, :],
                                  wb=[S["YT0"].reg((b, ct, toff))])
                    k.barrier()


CC = 64


def fwd_fft(k, C, I, S, st_tiles, Xin3, tab1, consumer):
    PS = C["PS"]
    A, A2, T2g = st_tiles["A"], st_tiles["A2"], st_tiles["T2g"]
    Xf = Xin3.rearrange("p n c -> p (n c)")
    ng = 64 * CC // 512
    for g in range(ng):
        pr, pi_ = PS[(g % 2) * 2], PS[(g % 2) * 2 + 1]
        k.mm(pr[:], tab1[:, 0:128], Xf[:, g * 512:(g + 1) * 512])
        k.mm(pi_[:], tab1[:, 128:256], Xf[:, g * 512:(g + 1) * 512])
        k.copy("act", A[:, 0].rearrange("p n c -> p (n c)")[:, g * 512:(g + 1) * 512], pr[:])
        k.copy("dve", A[:, 1].rearrange("p n c -> p (n c)")[:, g * 512:(g + 1) * 512], pi_[:])
    for r in range(2):
        k.dma("sp" if r == 0 else "pool", S["FS1"][r].rearrange("n k c -> k n c"), A[:, r])
    k.dma("sp", A2[:].rearrange("p k c -> p (k c)"), S["FS1"].a.rearrange("r n k c -> (r n) (k c)"))
    for k1g in range(16):
        tg = T2g[k1g % 2]
        k.dma("pool" if k1g % 2 else "sp", tg[:], I["fft_T2"][k1g * 8:(k1g + 1) * 8].rearrange("k r m -> r k m"))
        ps = PS[4 + k1g % 2]
        for j in range(8):
            k.mm(ps[:, j * CC:(j + 1) * CC], tg[:, j, :], A2[:, k1g * 8 + j, :])
        consumer(k1g, ps)


def phase_hyena_fft(k, I, S, C):
    PS = C["PS"]
    ident, antiI, ones = C["ident"], C["antiI"], C["ones"]
    T = L
    with contextlib.ExitStack() as st:
        h2 = k.tile(st, "fh2", [64, T])
        with contextlib.ExitStack() as st2:
            zT = k.tile(st2, "fzT", [33, T])
            k.dma("sp", zT[:], I["zT_lat"][:, :])
            fw1 = k.tile(st2, "ffw1", [33, 64])
            fw2 = k.tile(st2, "ffw2", [64, 64])
            k.dma("sp", fw1[:], I["hy_fw1"][0])
            k.dma("sp", fw2[:], I["hy_fw2"][0])
            prm = k.tile(st2, "fprm", [64, 4])
            for i, n in enumerate(("hy_fb1", "hy_ff1", "hy_fb2", "hy_ff2")):
                k.dma("sp", prm[:, i:i + 1], I[n][0].rearrange("(p o) -> p o", o=1), wb=[Buf("tmp")])
            k.barrier()
            h1 = k.tile(st2, "fh1", [64, T])
            tr = k.tile(st2, "ftr", [64, 512])
            tn = k.tile(st2, "ftn", [64, 512])
            for blk in range(T // 512):
                sl = slice(blk * 512, (blk + 1) * 512)
                k.mm(PS[0][0:64, :], fw1[:], zT[:, sl])
                sin_rr(k, h1[:, sl], PS[0][0:64, :], tr[:], tn[:], prm[:, 1:2], prm[:, 0:1])
                k.mm(PS[1][0:64, :], fw2[:], h1[:, sl])
                sin_rr(k, h2[:, sl], PS[1][0:64, :], tr[:], tn[:], prm[:, 3:4], prm[:, 2:3])
            k.barrier()
        fw3 = k.tile(st, "ffw3", [64, 3072])
        k.dma("sp", fw3[:], I["hy_fw3"][0])
        dlt = k.tile(st, "fdlt", [128, HY])
        load_bcast(k, "sp", dlt[:], I["ndelta_row"][0:1, :])
        tpos = k.tile(st, "ftpos", [128, 32])
        k.dma("sp", tpos[:], I["tneg_lat"][:, :])
        dec = k.tile(st, "fdec", [128, HY])
        hraw = [k.tile(st, f"fhraw{i}", [128, 3072]) for i in range(2)]
        accE = k.tile(st, "faccE", [128, 3072])
        sq = k.tile(st, "fsq", [128, 3072])
        for t in range(T // 128):
            hr = hraw[t % 2]
            k.act(dec[:], dlt[:], AF.Exp, scale=tpos[:, t:t + 1])
            for j in range(6):
                ps = PS[j % 4]
                k.mm(ps[:], h2[:, t * 128:(t + 1) * 128], fw3[:, j * 512:(j + 1) * 512])
                k.copy("act", hr[:, j * 512:(j + 1) * 512], ps[:])
            k.tt("dve", hr[:].rearrange("p (q c) -> p q c", c=HY), hr[:].rearrange("p (q c) -> p q c", c=HY),
                 dec[:].unsqueeze(1).to_broadcast([128, 4, HY]), ALU.mult)
            if t == 0:
                for o in range(2):
                    k.memset("dve", hr[0:1, o * 1536 + HY:o * 1536 + 2 * HY], 0.0)
            if t == 0:
                k.act(accE[:], hr[:], AF.Square)
            else:
                k.act(sq[:], hr[:], AF.Square)
                k.tt("pool", accE[:], accE[:], sq[:], ALU.add)
            for o in range(2):
                k.dma("sp", S["KT"][t * 128:(t + 1) * 128, o * HY:(o + 1) * HY], hr[:, o * 1536:o * 1536 + HY], wb=[Buf("x")])
                k.dma("pool", S["HB"][t * 128:(t + 1) * 128, o * HY:(o + 1) * HY], hr[:, o * 1536 + HY:(o + 1) * 1536],
                      wb=[Buf("x")])
        k.memset("dve", sq[:], 0.0)
        k.dma("sp", S["HB"][T:T + 128, :], sq[:, 0:1536], wb=[Buf("x")])
        en = k.tile(st, "fen", [1, 3072])
        for j in range(6):
            k.mm(PS[j % 4][0:1, :], ones[:, 0:1], accE[:, j * 512:(j + 1) * 512])
            k.copy("act", en[:, j * 512:(j + 1) * 512], PS[j % 4][0:1, :])
        es = k.tile(st, "fes", [1, 1536])
        for o in range(2):
            k.tt("dve", es[:, o * HY:(o + 1) * HY], en[:, o * 1536:o * 1536 + HY], en[:, o * 1536 + HY:(o + 1) * 1536], ALU.add)
        k.act(es[:], es[:], AF.Sqrt)
        k.recip(es[:], es[:])
        k.dma("sp", S["ESC"][0:1, :], es[:])
        k.barrier()
        for j in range(T // 128):
            g_ = hraw[j % 2]
            k.dma("sp", g_[:, 0:1536], S["HB"][128 * j + 1:128 * j + 129, :])
            fl = sq if j % 2 == 0 else accE
            for c3 in range(3):
                ps = PS[c3]
                k.mm(ps[:], antiI[:], g_[:, c3 * 512:(c3 + 1) * 512])
                k.copy("act" if c3 % 2 == 0 else "dve", fl[:, c3 * 512:(c3 + 1) * 512], ps[:])
            k.dma("pool", S["KT"][2 * T - 128 * (j + 1):2 * T - 128 * j, :], fl[:, 0:1536], wb=[Buf("x")])
        k.barrier()
    with contextlib.ExitStack() as st:
        tiles = dict(A=k.tile(st, "fA", [128, 2, 64, CC]), A2=k.tile(st, "fA2", [128, 128, CC]),
                     T2g=[k.tile(st, f"fT2g{i}", [128, 8, 128]) for i in range(2)])
        ff1 = k.tile(st, "fff1", [128, 256])
        f1d = k.tile(st, "ff1d", [128, 256])
        G1 = k.tile(st, "fG1", [128, 128])
        G2 = k.tile(st, "fG2", [128, 128])
        k.dma("sp", ff1[:], I["ff1"][:, :])
        k.dma("sp", f1d[:], I["f1d"][:, :])
        k.dma("sp", G1[:], I["fft_G1"][:, :])
        k.dma("sp", G2[:], I["fft_G2"][:, :])
        V = [k.tile(st, f"fV{i}", [128, 64, CC]) for i in range(2)]
        Xin = V[0]
        esc = k.tile(st, "fesc", [128, CC])
        okf = [k.tile(st, f"fokf{i}", [128, 8, CC]) for i in range(1)]
        KTv = S["KT"].a.rearrange("(a b) c -> a b c", b=64)
        for fc in range(24):
            o, c0 = fc // 12, (fc % 12) * CC
            k.dma("sp", Xin[:], KTv[:, :, fc * CC:(fc + 1) * CC])
            load_bcast(k, "sp", esc[:], S["ESC"][0:1, fc * CC:(fc + 1) * CC])

            def cons_f(k1g, ps, o=o, c0=c0):
                t_ = okf[0]
                k.tt("dve", t_[:], ps[:].rearrange("p (j c) -> p j c", c=CC), esc[:].unsqueeze(1).to_broadcast([128, 8, CC]), ALU.mult)
                k.dma("pool", S["KF"][o, :, k1g * 8:(k1g + 1) * 8, c0:c0 + CC], t_[:], wb=[Buf("x")])

            fwd_fft(k, C, I, S, tiles, Xin[:], ff1, cons_f)
        k.barrier()
        Bt = k.tile(st, "fB", [128, 128, CC])
        R = k.tile(st, "fR", [128, 66, CC])
        Gt = k.tile(st, "fGt", [128, 64, CC])
        wb_ = k.tile(st, "fwb", [128, 3, 4, CC])
        hb = k.tile(st, "fhb", [128, 2, CC])
        Kg = [k.tile(st, f"fKg{i}", [128, 2, 8, CC]) for i in range(2)]
        T12 = [k.tile(st, f"fT12{i}", [128, 2, 8 * CC]) for i in range(2)]
        T4g = [k.tile(st, f"fT4g{i}", [128, 4, 2, 128]) for i in range(2)]
        tmp = k.tile(st, "ftmp", [128, 4, CC])
        B2 = tiles["A"]

        def conv(blk, c0, dst):
            col = blk * HY + c0
            for b in range(NB):
                k.dma("sp", R[b * 64:(b + 1) * 64], AP(S["P0"].h, (b * (L + 2)) * 2304 + col, [[64 * 2304, 64], [2304, 66], [1, CC]]),
                      rb=[S["P0"]])
            w = lambda j: wb_[:, blk, j, :].unsqueeze(1).to_broadcast([128, 64, CC])
            k.tt("dve", dst, R[:, 0:64, :], w(0), ALU.mult)
            k.tt("pool", Gt_tmp[:, 0:64, :], R[:, 1:65, :], w(1), ALU.mult)
            k.tt("dve", dst, dst, Gt_tmp[:, 0:64, :], ALU.add)
            k.tt("pool", Gt_tmp[:, 0:64, :], R[:, 2:66, :], w(2), ALU.mult)
            k.tt("dve", dst, dst, Gt_tmp[:, 0:64, :], ALU.add)
            k.tt("dve", dst, dst, w(3), ALU.add)

        Gt_tmp = Bt
        for dc in range(HY // CC):
            c0 = dc * CC
            for blk in range(3):
                col = blk * HY + c0
                for j in range(3):
                    k.dma("sp", wb_[:, blk, j, :], I["hy_conv_w"][0][j:j + 1, col:col + CC].partition_broadcast(128), wb=[Buf("x")])
                k.dma("sp", wb_[:, blk, 3, :], I["hy_conv_b"][0:1, col:col + CC].partition_broadcast(128), wb=[Buf("x")])
            for o in range(2):
                k.dma("sp", hb[:, o, :], I["hy_bias"][0][o:o + 1, c0:c0 + CC].partition_broadcast(128), wb=[Buf("x")])
            k.barrier()
            conv(2, c0, V[0][:])
            for o in range(2):
                Vin, Vout = V[o], V[1 - o]

                def cons_d(k1g, ps, o=o, c0=c0):
                    kg = Kg[k1g % 2]
                    for r in range(2):
                        for hf in range(2):
                            k.dma("sp" if hf == 0 else "pool", kg[hf * 64:(hf + 1) * 64, r],
                                  S["KF"][o, r * 64:(r + 1) * 64, k1g * 8:(k1g + 1) * 8, c0:c0 + CC], rb=[S["KF"]], wb=[kg.sub[r * 2 + hf]])
                    t12 = T12[k1g % 2]
                    k.op("dve", lambda: k.nc.vector.tensor_tensor(t12[:, 0, :], ps[:], kg[:, 0].rearrange("p j c -> p (j c)"), ALU.mult),
                         [t12[:, 0, :]], [ps[:]], rb=[k.buf_of(ps[:])] + kg.sub, wb=[t12])
                    k.op("dve", lambda: k.nc.vector.tensor_tensor(t12[:, 1, :], ps[:], kg[:, 1].rearrange("p j c -> p (j c)"), ALU.mult),
                         [t12[:, 1, :]], [ps[:]], rb=[k.buf_of(ps[:])] + kg.sub, wb=[t12])
                    p2 = PS[6 + k1g % 2]
                    k.mm(p2[:], G1[:], t12[:, 0, :], start=True, stop=False)
                    k.mm(p2[:], G2[:], t12[:, 1, :], start=False, stop=True)
                    k.copy("act", Bt[:, k1g * 8:(k1g + 1) * 8, :], p2[:].rearrange("p (j c) -> p j c", c=CC))

                fwd_fft(k, C, I, S, tiles, Vin[:], f1d, cons_d)
                k.dma("sp", S["FS2"].a.rearrange("r n k c -> (r n) (k c)"), Bt[:].rearrange("p k c -> p (k c)"))
                for r in range(2):
                    k.dma("sp" if r == 0 else "pool", B2[:, r], S["FS2"][r].rearrange("n k c -> k n c"))
                conv(o, c0, Gt[:])
                for g in range(16):
                    t4 = T4g[g % 2]
                    for r in range(2):
                        k.dma("sp" if r == 0 else "pool", t4[:, :, r, :], I["fft_T4"][g * 4:(g + 1) * 4, r].rearrange("n k m -> k n m"),
                              wb=[t4.sub[r]])
                    ps = PS[g % 4]
                    for j in range(4):
                        n2 = g * 4 + j
                        k.op("pe", lambda: k.nc.tensor.matmul(ps[:, j * CC:(j + 1) * CC], t4[:, j, 0, :], B2[:, 0, n2, :], start=True, stop=False),
                             [ps[:]], [], rb=[B2] + t4.sub, wb=[k.buf_of(ps[:])])
                        k.op("pe", lambda: k.nc.tensor.matmul(ps[:, j * CC:(j + 1) * CC], t4[:, j, 1, :], B2[:, 1, n2, :], start=False, stop=True),
                             [ps[:]], [], rb=[B2] + t4.sub, wb=[k.buf_of(ps[:])])
                    gs = slice(g * 4, (g + 1) * 4)
                    k.tt("pool", tmp[:], Vin[:, gs, :], hb[:, o, :].unsqueeze(1).to_broadcast([128, 4, CC]), ALU.mult)
                    k.tt("dve", tmp[:], tmp[:], ps[:, 0:4 * CC].rearrange("p (j c) -> p j c", c=CC), ALU.add)
                    k.tt("dve", Vout[:, gs, :], tmp[:], Gt[:, gs, :], ALU.mult)
                if o == 1:
                    for b in range(NB):
                        k.dma("pool", AP(S["YH"].h, b * L * HY + c0, [[64 * HY, 64], [HY, 64], [1, CC]]), Vout[b * 64:(b + 1) * 64],
                              wb=[Buf("x")])
            k.barrier()


TC = 256


def phase_s5(k, I, S, C):
    PS = C["PS"]
    ident, antiI = C["ident"], C["antiI"]
    nblk = TT // 128
    for d in range(2):
        with contextlib.ExitStack() as st:
            BTr = [k.tile(st, f"BTr{j}", [128, 128]) for j in range(8)]
            BTi = [k.tile(st, f"BTi{j}", [128, 128]) for j in range(8)]
            Cr = [k.tile(st, f"Cr{j}", [128, 256]) for j in range(8)]
            nCi = [k.tile(st, f"nCi{j}", [128, 256]) for j in range(8)]
            ctab = [k.tile(st, f"ctab{j}", [128, TC]) for j in range(8)]
            stab = [k.tile(st, f"stab{j}", [128, TC]) for j in range(8)]
            rt = [k.tile(st, f"rt{j}", [128, TC]) for j in range(8)]
            carry = [k.tile(st, f"carry{j}", [128, 2]) for j in range(8)]
            iot = k.tile(st, "iot", [128, TC])
            k.dma("sp", iot[:], I["iota1"][:, :])
            with contextlib.ExitStack() as st2:
                sc = k.tile(st2, "s5sc", [128, 24])
                tr = k.tile(st2, "s5tr", [128, TC])
                tn = k.tile(st2, "s5tn", [128, TC])
                bre = k.tile(st2, "s5bre", [128, 16])
                bim = k.tile(st2, "s5bim", [128, 16])
                bb = k.tile(st2, "s5bb", [128, 2, 16])
                t16 = k.tile(st2, "s5t16", [128, 16])
                pad = k.tile(st2, "s5pad", [128, 128])
                for j in range(8):
                    g0 = 2 * j
                    k.dma("sp", sc[:, 0:1], I["s5_lam_re"][0, d, g0:g0 + 2, :].rearrange("g (p o) -> (g p) o", o=1))
                    k.dma("sp", sc[:, 1:2], I["s5_lam_im"][0, d, g0:g0 + 2, :].rearrange("g (p o) -> (g p) o", o=1))
                    for gl in range(2):
                        k.dma("sp", sc[gl * 64:(gl + 1) * 64, 2:3],
                              I["s5_log_step"][0, d:d + 1, g0 + gl:g0 + gl + 1].partition_broadcast(64))
                    k.dma("sp", bre[:], I["s5_b_re"][0, d, g0:g0 + 2].rearrange("g p n -> (g p) n"))
                    k.dma("sp", bim[:], I["s5_b_im"][0, d, g0:g0 + 2].rearrange("g p n -> (g p) n"))
                    k.act(sc[:, 3:4], sc[:, 2:3], AF.Exp)
                    k.tt("dve", sc[:, 4:5], sc[:, 0:1], sc[:, 3:4], ALU.mult)
                    k.tt("dve", sc[:, 5:6], sc[:, 1:2], sc[:, 3:4], ALU.mult)
                    k.act(sc[:, 6:7], sc[:, 4:5], AF.Exp)
                    sin_rr(k, sc[:, 7:8], sc[:, 5:6], tr[:, 0:1], tn[:, 0:1])
                    k.ts("dve", sc[:, 9:10], sc[:, 5:6], math.pi / 2, None, ALU.add)
                    sin_rr(k, sc[:, 8:9], sc[:, 9:10], tr[:, 0:1], tn[:, 0:1])
                    k.tt("dve", sc[:, 10:11], sc[:, 6:7], sc[:, 8:9], ALU.mult)
                    k.tt("dve", sc[:, 11:12], sc[:, 6:7], sc[:, 7:8], ALU.mult)
                    k.ts("dve", sc[:, 12:13], sc[:, 10:11], -1.0, None, ALU.add)
                    k.tt("dve", sc[:, 13:14], sc[:, 0:1], sc[:, 0:1], ALU.mult)
                    k.stt("dve", sc[:, 13:14], sc[:, 1:2], sc[:, 1:2], sc[:, 13:14], ALU.mult, ALU.add)
                    k.recip(sc[:, 14:15], sc[:, 13:14])
                    k.tt("dve", sc[:, 15:16], sc[:, 12:13], sc[:, 0:1], ALU.mult)
                    k.stt("dve", sc[:, 15:16], sc[:, 11:12], sc[:, 1:2], sc[:, 15:16], ALU.mult, ALU.add)
                    k.tt("dve", sc[:, 15:16], sc[:, 15:16], sc[:, 14:15], ALU.mult)
                    k.tt("dve", sc[:, 16:17], sc[:, 12:13], sc[:, 1:2], ALU.mult)
                    k.stt("dve", sc[:, 16:17], sc[:, 11:12], sc[:, 0:1], sc[:, 16:17], ALU.mult, ALU.subtract)
                    k.tt("dve", sc[:, 16:17], sc[:, 16:17], sc[:, 14:15], ALU.mult)
                    k.ts("dve", bb[:, 0, :], bre[:], sc[:, 15:16], None, ALU.mult)
                    k.ts("dve", t16[:], bim[:], sc[:, 16:17], None, ALU.mult)
                    k.tt("dve", bb[:, 0, :], bb[:, 0, :], t16[:], ALU.subtract)
                    k.ts("dve", bb[:, 1, :], bim[:], sc[:, 15:16], None, ALU.mult)
                    k.ts("dve", t16[:], bre[:], sc[:, 16:17], None, ALU.mult)
                    k.tt("dve", bb[:, 1, :], bb[:, 1, :], t16[:], ALU.add)
                    for ri, BT in ((0, BTr), (1, BTi)):
                        k.memset("dve", pad[:], 0.0)
                        for gl in range(2):
                            cg = ((g0 + gl) % 8) * 16
                            k.copy("dve", pad[gl * 64:(gl + 1) * 64, cg:cg + 16], bb[gl * 64:(gl + 1) * 64, ri, :])
                        k.mm(PS[0][:, 0:128], pad[:], ident[:])
                        k.copy("act", BT[j][:], PS[0][:, 0:128])
                    k.memset("dve", Cr[j][:], 0.0)
                    k.memset("dve", nCi[j][:], 0.0)
                    for gl in range(2):
                        g = g0 + gl
                        k.dma("sp", Cr[j][gl * 64:(gl + 1) * 64, g * 16:(g + 1) * 16],
                              I["s5_c_re"][0, d, g].rearrange("n p -> p n"), allow_slow_non_contiguous=True)
                        k.dma("sp", nCi[j][gl * 64:(gl + 1) * 64, g * 16:(g + 1) * 16],
                              I["s5_c_im"][0, d, g].rearrange("n p -> p n"), allow_slow_non_contiguous=True)
                    k.ts("dve", nCi[j][:], nCi[j][:], -1.0, None, ALU.mult)
                    k.ts("dve", tn[:], iot[:], sc[:, 5:6], None, ALU.mult)
                    sin_rr(k, stab[j][:], tn[:], tr[:], tn[:])
                    k.ts("dve", tn[:], iot[:], sc[:, 5:6], math.pi / 2, ALU.mult, ALU.add)
                    sin_rr(k, ctab[j][:], tn[:], tr[:], tn[:])
                    k.memset("dve", rt[j][:], 0.0)
                    k.ts("dve", rt[j][:], rt[j][:], sc[:, 6:7], None, ALU.add)
                k.barrier()
            ut = [k.tile(st, f"s5ut{i}", [128, 256]) for i in range(2)]
            uT = k.tile(st, "s5uT", [128, 2, TC])
            Sre = [k.tile(st, f"Sre{j}", [128, TC]) for j in range(8)]
            Sim = [k.tile(st, f"Sim{j}", [128, TC]) for j in range(8)]
            m1 = k.tile(st, "s5m1", [128, TC])
            m2 = k.tile(st, "s5m2", [128, TC])
            mre = k.tile(st, "s5mre", [128, TC])
            mim = k.tile(st, "s5mim", [128, TC])
            vre = k.tile(st, "s5vre", [128, TC])
            vim = k.tile(st, "s5vim", [128, TC])
            yt = [k.tile(st, f"s5yt{i}", [128, 256]) for i in range(2)]
            y2 = [k.tile(st, f"s5y2{i}", [128, 256]) for i in range(2)]
            revm = antiI if d == 1 else ident

            def row0(m):
                if d == 0:
                    return 128 * m
                return 128 * (1 - m) if m < 2 else 4480 - 128 * m

            it = 0
            for b in range(NB):
                for j in range(8):
                    k.memset("dve", carry[j][:], 0.0)
                for ch in range(TT // TC):
                    for bl in range(2):
                        m = ch * 2 + bl
                        u_ = ut[(it + bl) % 2]
                        k.dma("sp", u_[:], S["UT0"][b, row0(m):row0(m) + 128, :])
                        for hf in range(2):
                            k.mm(PS[0][:, (hf * 2 + bl) * 128:(hf * 2 + bl + 1) * 128], u_[:, hf * 128:(hf + 1) * 128], revm[:])
                    k.copy("act", uT[:], PS[0][:].rearrange("p (h t) -> p h t", h=2))
                    for j in range(8):
                        hf = j // 4
                        pr, pi_ = PS[1 + (j % 2) * 2], PS[2 + (j % 2) * 2]
                        k.mm(pr[:, 0:TC], BTr[j][:], uT[:, hf, :])
                        k.mm(pi_[:, 0:TC], BTi[j][:], uT[:, hf, :])
                        k.tt("dve", m1[:], pr[:, 0:TC], ctab[j][:], ALU.mult)
                        k.tt("dve", m2[:], pi_[:, 0:TC], stab[j][:], ALU.mult)
                        k.tt("pool", mre[:], m1[:], m2[:], ALU.add)
                        k.tt("dve", m1[:], pi_[:, 0:TC], ctab[j][:], ALU.mult)
                        k.tt("dve", m2[:], pr[:, 0:TC], stab[j][:], ALU.mult)
                        k.tt("pool", mim[:], m1[:], m2[:], ALU.subtract)
                        k.scan("dve", vre[:], rt[j][:], mre[:], carry[j][:, 0:1], ALU.mult, ALU.add)
                        k.scan("dve", vim[:], rt[j][:], mim[:], carry[j][:, 1:2], ALU.mult, ALU.add)
                        k.tt("dve", m1[:], vre[:], ctab[j][:], ALU.mult)
                        k.tt("dve", m2[:], vim[:], stab[j][:], ALU.mult)
                        k.tt("pool", Sre[j][:], m1[:], m2[:], ALU.subtract)
                        k.tt("dve", m1[:], vre[:], stab[j][:], ALU.mult)
                        k.tt("dve", m2[:], vim[:], ctab[j][:], ALU.mult)
                        k.tt("pool", Sim[j][:], m1[:], m2[:], ALU.add)
                        k.copy("pool", carry[j][:, 0:1], Sre[j][:, TC - 1:TC])
                        k.copy("pool", carry[j][:, 1:2], Sim[j][:, TC - 1:TC])
                    for bl in range(2):
                        m = ch * 2 + bl
                        py = PS[5 + bl]
                        for j in range(8):
                            k.mm(py[:, 0:256], Sre[j][:, bl * 128:(bl + 1) * 128], Cr[j][:], start=(j == 0), stop=False)
                            k.mm(py[:, 0:256], Sim[j][:, bl * 128:(bl + 1) * 128], nCi[j][:], start=False, stop=(j == 7))
                        y_ = yt[(it + bl) % 2]
                        k.copy("act", y_[:], py[:, 0:256])
                        if d == 1:
                            k.mm(PS[7][:, 0:256], antiI[:], y_[:])
                            y2_ = y2[(it + bl) % 2]
                            k.copy("act", y2_[:], PS[7][:, 0:256])
                            y_ = y2_
                        k.dma("pool", S["YS5"][d, b, row0(m):row0(m) + 128, :], y_[:], wb=[S["YS5"].reg((d, b, m))])
                    it += 1
        k.barrier()


def phase_s5_out(k, I, S, C):
    PS = C["PS"]
    ident = C["ident"]
    with contextlib.ExitStack() as st:
        Db = k.tile(st, "s5D", [128, 256])
        gb = k.tile(st, "s5gb", [128, 512])
        gw = k.tile(st, "s5gw", [128, 2, 512])
        load_bcast(k, "sp", Db[:], I["s5_d"][0:1, :])
        load_bcast(k, "sp", gb[:], I["s5_glu_b"][0:1, :])
        k.dma("sp", gw[:], I["s5_glu_w"][0].rearrange("(h p) n -> p h n", p=128))
        yf = [k.tile(st, f"o5yf{i}", [128, 256]) for i in range(2)]
        yb = [k.tile(st, f"o5yb{i}", [128, 256]) for i in range(2)]
        uu = [k.tile(st, f"o5u{i}", [128, 256]) for i in range(2)]
        y = k.tile(st, "o5y", [128, 256])
        t1 = k.tile(st, "o5t1", [128, 256])
        geT = k.tile(st, "o5geT", [128, 2, 128])
        a = k.tile(st, "o5a", [128, 512])
        o = k.tile(st, "o5o", [128, 256])
        oT = [k.tile(st, f"o5oT{i}", [128, 2, 128]) for i in range(2)]
        it = 0
        for b in range(NB):
            for m in range(TT // 128):
                r0 = m * 128
                i2 = it % 2
                k.dma("sp", yf[i2][:], S["YS5"][0, b, r0:r0 + 128, :])
                k.dma("sp", yb[i2][:], S["YS5"][1, b, r0:r0 + 128, :])
                k.dma("sp", uu[i2][:], S["UT0"][b, r0:r0 + 128, :])
                k.tt("dve", y[:], yf[i2][:], yb[i2][:], ALU.add)
                k.tt("dve", t1[:], uu[i2][:], Db[:], ALU.mult)
                k.tt("dve", y[:], y[:], t1[:], ALU.add)
                k.act(t1[:], y[:], AF.Square)
                k.ts("dve", t1[:], t1[:], 0.044715, 1.0, ALU.mult, ALU.add)
                k.tt("dve", t1[:], t1[:], y[:], ALU.mult)
                k.act(t1[:], t1[:], AF.Sigmoid, scale=2.0 * math.sqrt(2.0 / math.pi))
                k.tt("dve", y[:], y[:], t1[:], ALU.mult)
                for hf in range(2):
                    k.mm(PS[0][:, hf * 128:(hf + 1) * 128], y[:, hf * 128:(hf + 1) * 128], ident[:])
                k.copy("act", geT[:], PS[0][:, 0:256].rearrange("p (h t) -> p h t", h=2))
                for hf in range(2):
                    k.mm(PS[1][:], geT[:, hf, :], gw[:, hf, :], start=(hf == 0), stop=(hf == 1))
                k.tt("dve", a[:], PS[1][:], gb[:], ALU.add)
                k.act(a[:, 256:512], a[:, 256:512], AF.Sigmoid)
                k.tt("dve", o[:], a[:, 0:256], a[:, 256:512], ALU.mult)
                for hf in range(2):
                    k.mm(PS[2][:, hf * 128:(hf + 1) * 128], o[:, hf * 128:(hf + 1) * 128], ident[:])
                k.copy("act", oT[i2][:], PS[2][:, 0:256].rearrange("p (h t) -> p h t", h=2))
                k.dma("pool", S["YT0"][b, 768:1024, r0:r0 + 128].rearrange("(h p) t -> p h t", p=128), oT[i2][:],
                      wb=[S["YT0"].reg(("s5", b, m))])
                it += 1


def phase_moe(k, I, S, C, layer, yT_src, wout, tiles, final):
    PS = C["PS"]
    ident = C["ident"]
    with contextlib.ExitStack() as st:
        Wo = k.tile(st, "Wo", [128, 8, 1024])
        k.dma("sp", Wo[:], wout.rearrange("(k p) n -> p k n", p=128))
        Wr = k.tile(st, "Wr", [128, 8, 36])
        k.dma("sp", Wr[:, :, 0:4], I["moe_wg"][layer].rearrange("(k p) g -> p k g", p=128), allow_slow_non_contiguous=True,
              wb=[Buf("x")])
        k.dma("sp", Wr[:, :, 4:36], I["moe_we"][layer].rearrange("(k p) g -> p k g", p=128), allow_slow_non_contiguous=True,
              wb=[Buf("x")])
        rb = k.tile(st, "rbias", [128, 36])
        k.dma("sp", rb[:, 0:4], I["moe_bg"][layer:layer + 1, :].partition_broadcast(128), wb=[Buf("x")])
        k.dma("sp", rb[:, 4:36], I["moe_be"][layer:layer + 1, :].partition_broadcast(128), wb=[Buf("x")])
        fg = k.tile(st, "fg", [128, 1024])
        if final:
            load_bcast(k, "sp", fg[:], I["final_g"].a.rearrange("(o d) -> o d", o=1))
        k.barrier()
        mvt = [k.tile(st, f"mv{i}", [128, 1024]) for i in range(4)]
        yT = k.tile(st, "yT", [128, 8, 512])
        xa = [k.tile(st, f"xa{i}", [128, 1024]) for i in range(4)]
        acc = [k.tile(st, f"acc{i}", [128, 1024]) for i in range(4)]
        gate = [k.tile(st, f"gate{i}", [128, 32]) for i in range(4)]
        xin = k.tile(st, "xin", [128, 1024])
        h = k.tile(st, "hm", [128, 1024])
        hT = yT
        hTb = k.tile(st, "hTb", [128, 8, 512], BF16)
        sq = xin
        ss = k.tile(st, "ssm", [128, 1])
        rstd = k.tile(st, "rstdm", [128, 1])
        r_ = k.tile(st, "rt", [128, 64])
        lg = k.tile(st, "lg", [128, 36])
        Wg = [k.tile(st, f"Wg{i}", [128, 8, 256]) for i in range(2)]
        Wu = [k.tile(st, f"Wu{i}", [128, 8, 256]) for i in range(2)]
        Wd = [k.tile(st, f"Wd{i}", [128, 2, 1024]) for i in range(2)]
        Wgb = [k.tile(st, f"Wgb{i}", [128, 8, 256], BF16) for i in range(2)]
        Wub = [k.tile(st, f"Wub{i}", [128, 8, 256], BF16) for i in range(2)]
        Wdb = [k.tile(st, f"Wdb{i}", [128, 2, 1024], BF16) for i in range(2)]
        sl_ = [k.tile(st, f"sil{i}", [128, 512]) for i in range(2)]
        hid = [k.tile(st, f"hid{i}", [128, 512], BF16) for i in range(2)]
        cur_row = None
        ecount = 0
        for tl in tiles:
            if tl["row"] != cur_row:
                cur_row = tl["row"]
                for i, comp in enumerate((2, 3, 4, 5)):
                    load_bcast(k, "sp", mvt[i][:], S["MV"][layer, cur_row, comp:comp + 1, :])
            c0 = 0
            for (ap_, n) in tl["ysrc"]:
                if "yh" in tl:
                    k.dma("sp", yT[:, 6:8, c0:c0 + n], ap_.rearrange("(k p) t -> p k t", p=128), wb=[Buf("x")])
                else:
                    k.dma("sp", yT[:, :, c0:c0 + n], ap_.rearrange("(k p) t -> p k t", p=128), wb=[Buf("x")])
                c0 += n
            k.barrier()
            if "yh" in tl:
                for ts in range(4):
                    k.dma("sp", h[:, 0:HY], tl["yh"][ts])
                    for hf in range(2):
                        ps = PS[2 + hf]
                        for j in range(3):
                            kk = hf * 3 + j
                            k.mm(ps[:, j * 128:(j + 1) * 128], h[:, kk * 128:(kk + 1) * 128], ident[:])
                        k.copy("act", yT[:, hf * 3:(hf + 1) * 3, ts * 128:(ts + 1) * 128],
                               ps[:, 0:384].rearrange("p (j t) -> p j t", j=3))
            for ts in range(4):
                k.dma("sp", xin[:], tl["xsrc"][ts])
                for half in range(2):
                    ps = PS[half]
                    for kk in range(8):
                        k.mm(ps[:], yT[:, kk, ts * 128:(ts + 1) * 128], Wo[:, kk, half * 512:(half + 1) * 512],
                             start=(kk == 0), stop=(kk == 7))
                    k.tt("dve", xa[ts][:, half * 512:(half + 1) * 512], ps[:], mvt[0][:, half * 512:(half + 1) * 512], ALU.mult)
                k.tt("dve", xa[ts][:], xa[ts][:], xin[:], ALU.add)
                rms_mod(k, (sq, ss, rstd), xa[ts][:], mvt[2][:], mvt[1][:], h[:], "m")
                for hf in range(2):
                    ps = PS[2 + hf]
                    for j in range(4):
                        kk = hf * 4 + j
                        k.mm(ps[:, j * 128:(j + 1) * 128], h[:, kk * 128:(kk + 1) * 128], ident[:])
                    k.copy("act", hT[:, hf * 4:(hf + 1) * 4, ts * 128:(ts + 1) * 128], ps[:].rearrange("p (j t) -> p j t", j=4))
                    k.copy("dve", hTb[:, hf * 4:(hf + 1) * 4, ts * 128:(ts + 1) * 128], hT[:, hf * 4:(hf + 1) * 4, ts * 128:(ts + 1) * 128])
                ps = PS[4]
                for kk in range(8):
                    k.mm(ps[:, 0:36], hT[:, kk, ts * 128:(ts + 1) * 128], Wr[:, kk, :], start=(kk == 0), stop=(kk == 7))
                k.tt("dve", lg[:], ps[:, 0:36], rb[:], ALU.add)
                g_ = gate[ts]
                k.op("dve", lambda: k.nc.vector.reduce_max(r_[:, 0:1], lg[:, 0:4], AX.X), [r_[:, 0:1]], [lg[:, 0:4]])
                k.ts("dve", r_[:, 1:2], r_[:, 0:1], -1.0, None, ALU.mult)
                k.act(r_[:, 4:8], lg[:, 0:4], AF.Exp, bias=r_[:, 1:2], accum_out=r_[:, 2:3])
                k.recip(r_[:, 3:4], r_[:, 2:3])
                k.ts("dve", r_[:, 8:12], lg[:, 0:4], r_[:, 0:1], None, ALU.is_ge)
                k.ts("dve", r_[:, 16:24], lg[:, 4:12], r_[:, 8:9], None, ALU.mult)
                for g in range(1, 4):
                    k.stt("dve", r_[:, 16:24], lg[:, 4 + 8 * g:12 + 8 * g], r_[:, 8 + g:9 + g], r_[:, 16:24], ALU.mult, ALU.add)
                k.op("dve", lambda: k.nc.vector.reduce_max(r_[:, 12:13], r_[:, 16:24], AX.X), [r_[:, 12:13]], [r_[:, 16:24]])
                k.ts("dve", r_[:, 24:32], r_[:, 16:24], r_[:, 12:13], None, ALU.is_ge)
                k.stt("dve", r_[:, 32:40], r_[:, 24:32], -1e30, r_[:, 16:24], ALU.mult, ALU.add)
                k.op("dve", lambda: k.nc.vector.reduce_max(r_[:, 13:14], r_[:, 32:40], AX.X), [r_[:, 13:14]], [r_[:, 32:40]])
                k.ts("dve", r_[:, 40:48], r_[:, 32:40], r_[:, 13:14], None, ALU.is_ge)
                k.tt("dve", r_[:, 14:15], r_[:, 13:14], r_[:, 12:13], ALU.subtract)
                k.act(r_[:, 14:15], r_[:, 14:15], AF.Exp)
                k.ts("dve", r_[:, 14:15], r_[:, 14:15], 1.0, None, ALU.add)
                k.recip(r_[:, 15:16], r_[:, 14:15])
                k.tt("dve", r_[:, 48:49], r_[:, 15:16], r_[:, 3:4], ALU.mult)
                k.tt("dve", r_[:, 49:50], r_[:, 3:4], r_[:, 48:49], ALU.subtract)
                k.ts("dve", r_[:, 50:58], r_[:, 24:32], r_[:, 48:49], None, ALU.mult)
                k.stt("dve", r_[:, 50:58], r_[:, 40:48], r_[:, 49:50], r_[:, 50:58], ALU.mult, ALU.add)
                for g in range(4):
                    k.ts("dve", g_[:, g * 8:(g + 1) * 8], r_[:, 50:58], r_[:, 8 + g:9 + g], None, ALU.mult)
            for e in range(32):
                gi, ei = e // 8, e % 8
                i2 = ecount % 2
                ecount += 1
                k.dma("sp", Wg[i2][:], I["moe_w_gate"][layer, gi, ei].rearrange("(k p) n -> p k n", p=128))
                k.dma("pool", Wu[i2][:], I["moe_w_up"][layer, gi, ei].rearrange("(k p) n -> p k n", p=128))
                k.dma("sp", Wd[i2][:], I["moe_w_down"][layer, gi, ei].rearrange("(k p) n -> p k n", p=128))
                k.copy("act", Wgb[i2][:], Wg[i2][:])
                k.copy("act", Wub[i2][:], Wu[i2][:])
                k.copy("dve", Wdb[i2][:], Wd[i2][:])
                for hc in range(2):
                    pa, pu = PS[hc * 2], PS[hc * 2 + 1]
                    for kk in range(8):
                        k.mm(pa[:], Wgb[i2][:, kk, hc * 128:(hc + 1) * 128], hTb[:, kk, :], start=(kk == 0), stop=(kk == 7))
                    for kk in range(8):
                        k.mm(pu[:], Wub[i2][:, kk, hc * 128:(hc + 1) * 128], hTb[:, kk, :], start=(kk == 0), stop=(kk == 7))
                    k.act(sl_[hc][:], pa[:], AF.Silu)
                    k.tt("dve", hid[hc][:], sl_[hc][:], pu[:], ALU.mult)
                for ts in range(4):
                    for half in range(2):
                        po = PS[4 + (ts * 2 + half) % 4]
                        for hc in range(2):
                            k.mm(po[:], hid[hc][:, ts * 128:(ts + 1) * 128], Wdb[i2][:, hc, half * 512:(half + 1) * 512],
                                 start=(hc == 0), stop=(hc == 1))
                        eng = "dve" if half == 0 else "pool"
                        asl = acc[ts][:, half * 512:(half + 1) * 512]
                        if e == 0:
                            k.ts("dve", asl, po[:], gate[ts][:, e:e + 1], None, ALU.mult)
                        else:
                            k.stt("dve", asl, po[:], gate[ts][:, e:e + 1], asl, ALU.mult, ALU.add)
            for ts in range(4):
                k.tt("dve", acc[ts][:], acc[ts][:], mvt[3][:], ALU.mult)
                k.tt("dve", acc[ts][:], acc[ts][:], xa[ts][:], ALU.add)
                if final:
                    k.act(sq[:], acc[ts][:], AF.Square, accum_out=ss[:])
                    k.ts("dve", ss[:], ss[:], 1.0 / D, EPS, ALU.mult, ALU.add)
                    k.act(ss[:], ss[:], AF.Sqrt)
                    k.recip(rstd[:], ss[:])
                    k.stt("dve", acc[ts][:], acc[ts][:], rstd[:, 0:1], fg[:], ALU.mult, ALU.mult)
                k.dma("pool", tl["dst"][ts], acc[ts][:], wb=[Buf("x")])
            k.barrier()


def l0_moe_tiles(I, S):
    tiles = []
    tiles.append(dict(row=2, ysrc=[(S["YT0"][b, :, 0:LC], LC) for b in range(NB)],
                      xsrc=[I["ctx"][b, j * 128:(j + 1) * 128, :] for b in range(NB) for j in range(2)],
                      dst=[S["CTX1"][b, j * 128:(j + 1) * 128, :] for b in range(NB) for j in range(2)]))
    for b in range(NB):
        for i in range(L // 512):
            t0 = i * 512
            td = dict(row=b, ysrc=[(S["YT0"][b, :, LC + t0:LC + t0 + 512], 512)])
            if USE_FFT:
                td = dict(row=b, ysrc=[(S["YT0"][b, 768:1024, LC + t0:LC + t0 + 512], 512)],
                          yh=[S["YH"][b, t0 + j * 128:t0 + (j + 1) * 128, :] for j in range(4)])
            tiles.append(dict(td,
                              xsrc=[I["x"][b, t0 + j * 128:t0 + (j + 1) * 128, :] for j in range(4)],
                              dst=[S["X1"][b, t0 + j * 128:t0 + (j + 1) * 128, :] for j in range(4)]))
    return tiles


def phase_l1_norm(k, I, S, C):
    with contextlib.ExitStack() as st:
        A = k.tile(st, "A1b", [128, 1024])
        sh = k.tile(st, "sh1b", [128, 1024])
        xt = [k.tile(st, f"xtb{i}", [128, 1024]) for i in range(2)]
        h = k.tile(st, "hb", [128, 1024])
        hT = [k.tile(st, f"hTb{i}", [128, 8, 128], BF16) for i in range(2)]
        sq = k.tile(st, "sqb", [128, 1024])
        ss = k.tile(st, "ssb", [128, 1])
        rstd = k.tile(st, "rstdb", [128, 1])
        it = 0
        for b in range(NB):
            for (src, row, ntile, toff) in ((S["CTX1"], 2, LC // 128, 0), (S["X1"], b, L // 128, LC)):
                load_bcast(k, "sp", A[:], S["MV"][1, row, 1:2, :])
                load_bcast(k, "sp", sh[:], S["MV"][1, row, 0:1, :])
                for t in range(ntile):
                    x_ = xt[it % 2]
                    hT_ = hT[it % 2]
                    k.dma("sp", x_[:], src[b, t * 128:(t + 1) * 128, :])
                    rms_mod(k, (sq, ss, rstd), x_[:], A[:], sh[:], h[:], "l1")
                    transpose_1024(k, C, h, hT_)
                    t0 = toff + t * 128
                    k.dma("pool", S["HT1"][b].rearrange("(j p) t -> p j t", p=128)[:, :, t0:t0 + 128], hT_[:],
                          wb=[S["HT1"].reg((b, t0))])
                    it += 1


def phase_l1_inproj(k, I, S, C):
    PS = C["PS"]
    with contextlib.ExitStack() as st:
        W = k.tile(st, "W1g", [128, 8, 1024], BF16)
        wst = [k.tile(st, f"W1st{i}", [128, 1024]) for i in range(2)]
        hT = [k.tile(st, f"hT5{i}", [128, 8, 512], BF16) for i in range(2)]
        of = [k.tile(st, f"of{i}", [128, 8, 512]) for i in range(2)]
        ot = [k.tile(st, f"ot{i}", [128, 4, 1024]) for i in range(2)]
        wv = I["c_w_in"][0].rearrange("(k p) n -> p k n", p=128)
        it = 0
        for cg in range(5):
            k.barrier()
            for kk in range(8):
                k.dma("sp" if kk % 2 == 0 else "pool", wst[kk % 2][:], wv[:, kk, cg * 1024:(cg + 1) * 1024])
                k.copy("act" if kk % 2 == 0 else "dve", W[:, kk, :], wst[kk % 2][:])
            k.barrier()
            for b in range(NB):
                segs = [(LC + i * 512, 512) for i in range(L // 512)]
                if cg >= 2:
                    segs = [(0, LC)] + segs
                for (t0, n) in segs:
                    h_ = hT[it % 2]
                    k.dma("sp", h_[:, :, 0:n], S["HT1"][b].rearrange("(j p) t -> p j t", p=128)[:, :, t0:t0 + n])
                    if cg in (0, 3, 4):
                        o_ = of[it % 2]
                        for j in range(8):
                            ps = PS[j % 4]
                            for kk in range(8):
                                k.mm(ps[:, 0:n], W[:, kk, j * 128:(j + 1) * 128], h_[:, kk, 0:n], start=(kk == 0), stop=(kk == 7))
                            k.copy("act" if j % 2 == 0 else "dve", o_[:, j, 0:n], ps[:, 0:n])
                        if cg == 0:
                            dst = S["QT1"][b].rearrange("(j p) t -> p j t", p=128)[:, :, t0 - LC:t0 - LC + n]
                        else:
                            dst = S["ZF1" if cg == 3 else "ZB1"][b].rearrange("(j p) t -> p j t", p=128)[:, :, t0:t0 + n]
                        k.dma("pool", dst, o_[:, :, 0:n], wb=[Buf("x")])
                    else:
                        o_ = ot[it % 2]
                        for ts in range(n // 128):
                            for half in range(2):
                                ps = PS[4 + (ts * 2 + half) % 4]
                                for kk in range(8):
                                    k.mm(ps[:], h_[:, kk, ts * 128:(ts + 1) * 128], W[:, kk, half * 512:(half + 1) * 512],
                                         start=(kk == 0), stop=(kk == 7))
                                k.copy("act" if half == 0 else "dve", o_[:, ts, half * 512:(half + 1) * 512], ps[:])
                        if cg == 1:
                            dst = S["G1"][b, t0 - LC:t0 - LC + n, :].rearrange("(s p) d -> p s d", p=128)
                        else:
                            dst = S["V1"][b, t0:t0 + n, :].rearrange("(s p) d -> p s d", p=128)
                        k.dma("pool", dst, o_[:, 0:n // 128, :], wb=[Buf("x")])
                    it += 1
        k.barrier()


def phase_hgrn2(k, I, S, C):
    PS = C["PS"]
    ident = C["ident"]
    CH = 64
    with contextlib.ExitStack() as st:
        cmask = k.tile(st, "cmask", [128, 512])
        k.dma("sp", cmask[:], I["cmask"][:, :])
        tri = [k.tile(st, f"tri{d}", [64, 64]) for d in range(2)]
        k.dma("sp", tri[0][:], I["triu"][:, :])
        k.dma("sp", tri[1][:], I["tril"][:, :])
        lbt = k.tile(st, "lbt", [128, 8])
        names = ["z", "f", "lf", "kk", "bc", "bb", "eb", "qt", "kt", "kh", "tmp"]
        T_ = [{n: k.tile(st, f"g{d}{n}", [128, 512]) for n in names} for d in range(2)]
        ebe = [k.tile(st, f"ebe{d}", [128, 8]) for d in range(2)]
        vt = [k.tile(st, f"vt{d}", [64, 8, 128]) for d in range(2)]
        kht = [k.tile(st, f"kht{d}", [64, 8, 128]) for d in range(2)]
        ot = [k.tile(st, f"oo{d}", [64, 8, 128]) for d in range(2)]
        am = [k.tile(st, f"am{d}", [64, 64]) for d in range(2)]
        Sst = [k.tile(st, f"Sst{d}", [128, 128]) for d in range(2)]
        for b in range(NB):
            for hh in range(8):
                hs = slice(hh * 128, (hh + 1) * 128)
                for d in range(2):
                    for l_ in range(2):
                        k.dma("sp", lbt[:, d * 4 + l_:d * 4 + l_ + 1],
                              I["c_lower_bounds"][d, l_, hs].rearrange("(p o) -> p o", o=1))
                    k.tt("dve", lbt[:, d * 4 + 2:d * 4 + 3], lbt[:, d * 4:d * 4 + 1], lbt[:, d * 4 + 1:d * 4 + 2], ALU.subtract)
                    k.act(lbt[:, d * 4 + 2:d * 4 + 3], lbt[:, d * 4 + 2:d * 4 + 3], AF.Sigmoid)
                    k.ts("dve", lbt[:, d * 4 + 3:d * 4 + 4], lbt[:, d * 4 + 2:d * 4 + 3], -1.0, 1.0, ALU.mult, ALU.add)
                    k.memset("dve", Sst[d][:], 0.0)
                segs = {0: [(0, LC, False)] + [(LC + i * 512, 512, True) for i in range(L // 512)],
                        1: [(0, LC, False)] + [(LC + i * 512, 512, True) for i in reversed(range(L // 512))]}
                for si in range(len(segs[0])):
                    for d in range(2):
                        t0, n, is_lat = segs[d][si]
                        nchunk = n // CH
                        Td = T_[d]
                        zsrc = S["ZF1" if d == 0 else "ZB1"]
                        k.dma("sp", Td["z"][:, 0:n], zsrc[b, hs, t0:t0 + n])
                        k.dma("sp", vt[d][:, 0:nchunk, :], S["V1"][b, t0:t0 + n, hs].rearrange("(c s) e -> s c e", s=CH))
                        k.act(Td["f"][:, 0:n], Td["z"][:, 0:n], AF.Sigmoid)
                        k.ts("dve", Td["f"][:, 0:n], Td["f"][:, 0:n], lbt[:, d * 4 + 3:d * 4 + 4], lbt[:, d * 4 + 2:d * 4 + 3],
                             ALU.mult, ALU.add)
                        k.act(Td["lf"][:, 0:n], Td["f"][:, 0:n], AF.Ln)
                        k.ts("dve", Td["kk"][:, 0:n], Td["f"][:, 0:n], -1.0, 1.0, ALU.mult, ALU.add)
                        k.scan("dve", Td["bc"][:, 0:n], cmask[:, 0:n], Td["lf"][:, 0:n], 0.0, ALU.mult, ALU.add)
                        bc3 = Td["bc"][:, 0:n].rearrange("p (c s) -> p c s", s=CH)
                        bend = bc3[:, :, CH - 1:CH]
                        if d == 0:
                            bbv = Td["bc"]
                        else:
                            k.tt("dve", Td["bb"][:, 0:n].rearrange("p (c s) -> p c s", s=CH), bend.to_broadcast([128, nchunk, CH]),
                                 bc3, ALU.subtract)
                            k.tt("dve", Td["bb"][:, 0:n], Td["bb"][:, 0:n], Td["lf"][:, 0:n], ALU.add)
                            bbv = Td["bb"]
                        k.act(ebe[d][:, 0:nchunk], bend.rearrange("p c o -> p (c o)"), AF.Exp)
                        k.act(Td["tmp"][:, 0:n], bbv[:, 0:n], AF.Exp, scale=-1.0)
                        k.tt("dve", Td["kt"][:, 0:n], Td["kk"][:, 0:n], Td["tmp"][:, 0:n], ALU.mult)
                        k.tt("dve", Td["kh"][:, 0:n].rearrange("p (c s) -> p c s", s=CH),
                             Td["kt"][:, 0:n].rearrange("p (c s) -> p c s", s=CH),
                             ebe[d][:, 0:nchunk].unsqueeze(2).to_broadcast([128, nchunk, CH]), ALU.mult)
                        if is_lat:
                            k.dma("sp", Td["z"][:, 0:n], S["QT1"][b, hs, t0 - LC:t0 - LC + n])
                            k.act(Td["eb"][:, 0:n], bbv[:, 0:n], AF.Exp)
                            k.act(Td["qt"][:, 0:n], Td["z"][:, 0:n], AF.Silu)
                            k.tt("dve", Td["qt"][:, 0:n], Td["qt"][:, 0:n], Td["eb"][:, 0:n], ALU.mult)
                        for half in range((nchunk + 3) // 4):
                            ps = PS[d * 4 + 3]
                            nn = min(4, nchunk - half * 4)
                            for c in range(nn):
                                cc = half * 4 + c
                                k.mm(ps[0:64, c * 128:(c + 1) * 128], Td["kh"][:, cc * CH:(cc + 1) * CH], ident[:])
                            k.copy("act", kht[d][:, half * 4:half * 4 + nn, :],
                                   ps[0:64, 0:nn * 128].rearrange("p (c e) -> p c e", e=128))
                        order = range(nchunk) if d == 0 else reversed(range(nchunk))
                        for c in order:
                            cs = slice(c * CH, (c + 1) * CH)
                            if is_lat:
                                pa = PS[d * 4 + 0]
                                k.mm(pa[0:64, 0:64], Td["kt"][:, cs], Td["qt"][:, cs])
                                k.tt("dve", am[d][:], pa[0:64, 0:64], tri[d][:], ALU.mult)
                                po = PS[d * 4 + 1]
                                k.mm(po[0:64, 0:128], am[d][:], vt[d][:, c, :], start=True, stop=False)
                                k.mm(po[0:64, 0:128], Td["qt"][:, cs], Sst[d][:], start=False, stop=True)
                                k.copy("act", ot[d][:, c, :], po[0:64, 0:128])
                            pd = PS[d * 4 + 2]
                            k.mm(pd[:, 0:128], kht[d][:, c, :], vt[d][:, c, :])
                            k.stt("dve", Sst[d][:], Sst[d][:], ebe[d][:, c:c + 1], pd[:, 0:128], ALU.mult, ALU.add)
                        if is_lat:
                            k.dma("pool", S["O1"][d, b, t0 - LC:t0 - LC + n, hs].rearrange("(c s) e -> s c e", s=CH),
                                  ot[d][:, 0:nchunk, :], wb=[Buf("x")])
                k.barrier()


def phase_hgrn2_out(k, I, S, C):
    PS = C["PS"]
    ident = C["ident"]
    with contextlib.ExitStack() as st:
        ng = k.tile(st, "ng", [128, 1024])
        load_bcast(k, "sp", ng[:], I["c_norm_g"][0:1, :])
        of_ = [k.tile(st, f"ro_f{i}", [128, 1024]) for i in range(2)]
        ob_ = [k.tile(st, f"ro_b{i}", [128, 1024]) for i in range(2)]
        gg = [k.tile(st, f"ro_g{i}", [128, 1024]) for i in range(2)]
        o = k.tile(st, "ro_o", [128, 1024])
        sq = k.tile(st, "ro_sq", [128, 1024])
        ss = k.tile(st, "ro_ss", [128, 8])
        oT = [k.tile(st, f"ro_oT{i}", [128, 8, 128]) for i in range(2)]
        it = 0
        for b in range(NB):
            for t in range(L // 128):
                i2 = it % 2
                r0 = t * 128
                k.dma("sp", of_[i2][:], S["O1"][0, b, r0:r0 + 128, :])
                k.dma("sp", ob_[i2][:], S["O1"][1, b, r0:r0 + 128, :])
                k.dma("sp", gg[i2][:], S["G1"][b, r0:r0 + 128, :])
                k.tt("dve", o[:], of_[i2][:], ob_[i2][:], ALU.add)
                k.act(sq[:], o[:], AF.Square)
                k.op("dve", lambda: k.nc.vector.reduce_sum(ss[:], sq[:].rearrange("p (h e) -> p h e", e=128), AX.X),
                     [ss[:]], [sq[:]])
                k.ts("dve", ss[:], ss[:], 1.0 / 128, EPS, ALU.mult, ALU.add)
                k.act(ss[:], ss[:], AF.Sqrt)
                k.recip(ss[:], ss[:])
                k.tt("dve", o[:].rearrange("p (h e) -> p h e", e=128), o[:].rearrange("p (h e) -> p h e", e=128),
                     ss[:].unsqueeze(2).to_broadcast([128, 8, 128]), ALU.mult)
                k.tt("dve", o[:], o[:], ng[:], ALU.mult)
                k.act(gg[i2][:], gg[i2][:], AF.Sigmoid)
                k.tt("dve", o[:], o[:], gg[i2][:], ALU.mult)
                transpose_1024(k, C, o, oT[i2])
                k.dma("pool", S["OT1"][b].rearrange("(j p) t -> p j t", p=128)[:, :, r0:r0 + 128], oT[i2][:], wb=[Buf("x")])
                it += 1


def l1_moe_tiles(I, S, OUT):
    tiles = []
    for b in range(NB):
        for i in range(L // 512):
            t0 = i * 512
            tiles.append(dict(row=b, ysrc=[(S["OT1"][b, :, t0:t0 + 512], 512)],
                              xsrc=[S["X1"][b, t0 + j * 128:t0 + (j + 1) * 128, :] for j in range(4)],
                              dst=[OUT[b, t0 + j * 128:t0 + (j + 1) * 128, :] for j in range(4)]))
    return tiles


_CACHE = {}


def kernel(**inputs):
    x = np.ascontiguousarray(inputs["x"], dtype=np.float32)
    c = np.asarray(inputs["c"], dtype=np.float32)
    ctx = np.ascontiguousarray(inputs["ctx"], dtype=np.float32)
    c_ctx = np.asarray(inputs["c_ctx"], dtype=np.float32)
    if "nc" not in _CACHE:
        _CACHE["nc"] = build()
    nc = _CACHE["nc"]
    consts = host_consts()
    shared = {n: np.ascontiguousarray(inputs[n], dtype=np.float32) for n in WEIGHT_SHAPES}
    shared.update(consts)
    in_maps = []
    for i in range(NCORES):
        m = dict(shared)
        m["x"] = x[i * NB:(i + 1) * NB]
        m["ctx"] = ctx[i * NB:(i + 1) * NB]
        m["cvec"] = np.concatenate([c[i * NB:(i + 1) * NB], c_ctx[None, :]], axis=0)
        in_maps.append(m)
    res = run_bass_kernel_spmd(nc, in_maps, core_ids=list(range(NCORES)))
    return np.concatenate([r["out"] for r in res.results], axis=0)
```

```python
import contextlib
import math
import numpy as np
import concourse.bass as bass
import concourse.mybir as mybir
from concourse.bass_utils import run_bass_kernel_spmd

F32 = mybir.dt.float32
BF16 = mybir.dt.bfloat16
ALU = mybir.AluOpType
AF = mybir.ActivationFunctionType
AX = mybir.AxisListType
AP = bass.AP

NCORES = 8
NB = 2
L = 4096
LC = 256
D = 1024
HY = 768
S5D = 256
EPS = 1e-6
MAGIC = 12582912.0
TWO_PI = 2.0 * math.pi


class Buf:
    __slots__ = ("w", "r", "name")

    def __init__(self, name):
        self.name = name
        self.w = None
        self.r = []


class Tile(Buf):
    __slots__ = ("t", "sub")

    def __init__(self, name, t):
        Buf.__init__(self, name)
        self.t = t
        self.sub = [Buf(f"{name}.{i}") for i in range(4)]

    def __getitem__(self, key):
        return self.t[key]


class DT(Buf):
    __slots__ = ("h", "a", "regions")

    def __init__(self, name, h):
        Buf.__init__(self, name)
        self.h = h
        self.a = h.ap()
        self.regions = {}

    def __getitem__(self, key):
        return self.a[key]

    def reg(self, key):
        b = self.regions.get(key)
        if b is None:
            b = Buf(f"{self.name}:{key}")
            self.regions[key] = b
        return b


class KB:
    ENG = ("pe", "act", "dve", "pool", "sp")

    def __init__(self, nc):
        self.nc = nc
        self.es = contextlib.ExitStack()
        self.eng = {"pe": nc.tensor, "act": nc.scalar, "dve": nc.vector, "pool": nc.gpsimd, "sp": nc.sync}
        self.sems = {}
        self.sem_list = []
        self.cnt = {}
        self.cur = {}
        self.spare = {e: [] for e in self.ENG}
        nsem = {"pe": 9, "act": 3, "dve": 12, "pool": 3, "sp": 1}
        for e in self.ENG:
            for i in range(nsem[e]):
                s = self.es.enter_context(nc.semaphore(f"s_{e}{i}"))
                self.spare[e].append(self._reg_sem(s))
            self.cur[e] = self.spare[e].pop(0)
        self.dring = {}
        for e in ("sp", "pool", "act"):
            ring = []
            for i in range(12):
                s = self.es.enter_context(nc.semaphore(f"d_{e}{i}"))
                ring.append(self._reg_sem(s))
            self.dring[e] = [ring, 0]
        self.waited = {e: {} for e in self.ENG}
        self.bufs = {}
        self.ninstr = 0

    def _reg_sem(self, s):
        self.sem_list.append(s)
        self.cnt[len(self.sem_list) - 1] = 0
        return len(self.sem_list) - 1

    def tile(self, stack, name, shape, dt=F32):
        self.ninstr += 1
        name = f"t{self.ninstr}_" + name
        t = stack.enter_context(self.nc.sbuf_tensor(name, list(shape), dt))
        tl = Tile(name, t)
        self.bufs[name] = tl
        return tl

    def ptile(self, stack, name, shape, dt=F32):
        name = "t_" + name
        t = stack.enter_context(self.nc.psum_tensor(name, list(shape), dt))
        tl = Tile(name, t)
        self.bufs[name] = tl
        return tl

    def dram(self, name, shape, dt=F32, kind="Internal"):
        h = self.nc.dram_tensor(name, list(shape), dt, kind=kind)
        d = DT(name, h)
        self.bufs[name] = d
        return d

    def buf_of(self, ap):
        return self.bufs[ap.tensor.name]

    def _wait(self, e, tok):
        si, val = tok
        if self.waited[e].get(si, 0) >= val:
            return
        self.eng[e].wait_ge(self.sem_list[si], val)
        self.waited[e][si] = val

    def _sync(self, e, rb, wb):
        own = self.cur[e]
        for b in rb:
            if b.w is not None and not (e == "pe" and b.w[0] == own):
                self._wait(e, b.w)
        for b in wb:
            if b.w is not None and not (e == "pe" and b.w[0] == own):
                self._wait(e, b.w)
            for t in b.r:
                if not (e == "pe" and t[0] == own):
                    self._wait(e, t)

    def _mark(self, tok, rb, wb):
        for b in rb:
            b.r.append(tok)
            if len(b.r) > 24:
                best = {}
                for t in b.r:
                    if best.get(t[0], 0) < t[1]:
                        best[t[0]] = t[1]
                b.r = list(best.items())
        for b in wb:
            b.w = tok
            b.r = []

    def op(self, e, fn, outs, ins, rb=None, wb=None):
        if rb is None:
            rb = [self.buf_of(a) for a in ins if isinstance(a, AP)]
        if wb is None:
            wb = [self.buf_of(a) for a in outs if isinstance(a, AP)]
        self._sync(e, rb, wb)
        ins_ = fn()
        si = self.cur[e]
        self.cnt[si] += 1
        ins_.then_inc(self.sem_list[si], 1)
        tok = (si, self.cnt[si])
        self._mark(tok, rb, wb)
        self.ninstr += 1
        return tok

    def dma(self, e, out, in_, rb=None, wb=None, **kw):
        if rb is None:
            rb = [self.buf_of(in_)]
        if wb is None:
            wb = [self.buf_of(out)]
        self._sync(e, rb, wb)
        ring, pos = self.dring[e]
        si = ring[pos % len(ring)]
        self.dring[e][1] = pos + 1
        self._wait(e, (si, self.cnt[si]))
        self.cnt[si] += 16
        self.eng[e].dma_start(out=out, in_=in_, **kw).then_inc(self.sem_list[si], 16)
        tok = (si, self.cnt[si])
        self._mark(tok, rb, wb)
        self.ninstr += 1
        return tok

    def barrier(self):
        toks = [(si, c) for si, c in self.cnt.items() if c > 0]
        for e in self.ENG:
            for t in toks:
                if t[0] == self.cur[e]:
                    continue
                self._wait(e, t)
        for e in self.ENG:
            if self.cnt[self.cur[e]] > 16000 and self.spare[e]:
                self.cur[e] = self.spare[e].pop(0)

    def mm(self, out, lhsT, rhs, start=True, stop=True):
        return self.op("pe", lambda: self.nc.tensor.matmul(out, lhsT, rhs, start=start, stop=stop), [out], [lhsT, rhs])

    def act(self, out, in_, func, bias=0.0, scale=1.0, accum_out=None, e="act"):
        outs = [out] + ([accum_out] if accum_out is not None else [])
        ins = [in_] + [a for a in (bias, scale) if isinstance(a, AP)]
        kw = {}
        if accum_out is not None:
            kw["accum_out"] = accum_out
        return self.op("act", lambda: self.nc.scalar.activation(out, in_, func, bias=bias, scale=scale, **kw), outs, ins)

    def copy(self, e, out, in_):
        if e == "act":
            return self.op("act", lambda: self.nc.scalar.copy(out, in_), [out], [in_])
        return self.op(e, lambda: self.eng[e].tensor_copy(out, in_), [out], [in_])

    def tt(self, e, out, a, b, op):
        return self.op(e, lambda: self.eng[e].tensor_tensor(out, a, b, op), [out], [a, b])

    def ts(self, e, out, a, s1, s2, op0, op1=None, accum_out=None):
        outs = [out] + ([accum_out] if accum_out is not None else [])
        ins = [a] + [s for s in (s1, s2) if isinstance(s, AP)]
        if op1 is None:
            return self.op(e, lambda: self.eng[e].tensor_single_scalar(out, a, s1, op0), outs, ins)
        kw = {}
        if accum_out is not None:
            kw["accum_out"] = accum_out
        return self.op(e, lambda: self.eng[e].tensor_scalar(out, a, s1, s2, op0, op1, **kw), outs, ins)

    def stt(self, e, out, a, s, b, op0, op1):
        ins = [a, b] + ([s] if isinstance(s, AP) else [])
        return self.op(e, lambda: self.eng[e].scalar_tensor_tensor(out, a, s, b, op0, op1), [out], ins)

    def memset(self, e, out, val):
        return self.op(e, lambda: self.eng[e].memset(out, val), [out], [])

    def recip(self, out, in_):
        return self.op("dve", lambda: self.nc.vector.reciprocal(out, in_), [out], [in_])

    def scan(self, e, out, d0, d1, init, op0, op1):
        ins = [d0, d1] + ([init] if isinstance(init, AP) else [])
        return self.op(e, lambda: self.eng[e].tensor_tensor_scan(out, d0, d1, init, op0, op1), [out], ins)


def _cplx_lhsT(W):
    return np.block([[W.real, W.imag], [-W.imag, W.real]])


def fft_tables():
    t = {}
    n1 = np.arange(128)[:, None].astype(np.float64)
    k1 = np.arange(128)[None, :].astype(np.float64)
    ang = 2.0 * np.pi * n1 * k1 / 128.0
    cs, sn = np.cos(ang), np.sin(ang)
    t["ff1"] = np.concatenate([cs, -sn], axis=1).astype(np.float32)
    t["f1d"] = np.concatenate([np.concatenate([cs[:64], -sn[:64]], axis=1),
                               np.concatenate([sn[:64], cs[:64]], axis=1)], axis=0).astype(np.float32)
    n2 = np.arange(64)[:, None].astype(np.float64)
    k2 = np.arange(64)[None, :].astype(np.float64)
    T2 = np.zeros((128, 128, 128), np.float32)
    for k1_ in range(128):
        W = np.exp(-2j * np.pi * n2 * (k1_ + 128.0 * k2) / 8192.0)
        T2[k1_] = _cplx_lhsT(W)
    t["fft_T2"] = T2
    Wi = np.exp(2j * np.pi * np.arange(64)[:, None] * np.arange(64)[None, :] / 64.0)
    G1 = _cplx_lhsT(Wi)
    Q = np.block([[np.zeros((64, 64)), -np.eye(64)], [np.eye(64), np.zeros((64, 64))]])
    t["fft_G1"] = G1.astype(np.float32)
    t["fft_G2"] = (Q.T @ G1).astype(np.float32)
    T4 = np.zeros((64, 2, 128, 128), np.float32)
    kk = np.arange(128)[:, None].astype(np.float64)
    nn = np.arange(64)[None, :].astype(np.float64)
    for n2_ in range(64):
        W = np.exp(2j * np.pi * (n2_ * kk / 8192.0 + nn * kk / 128.0)) / 8192.0
        T4[n2_, 0] = np.concatenate([W.real, W.imag], axis=1)
        T4[n2_, 1] = np.concatenate([-W.imag, W.real], axis=1)
    t["fft_T4"] = T4
    return t


def host_consts():
    c = {}
    c["ident"] = np.eye(128, dtype=np.float32)
    c["antiI"] = np.eye(128, dtype=np.float32)[::-1].copy()
    c["ones"] = np.ones((128, 128), np.float32)
    for nm, Lf in (("lat", L), ("ctx", LC)):
        pos = np.arange(Lf, dtype=np.float32)
        t = pos / np.float32(max(Lf - 1, 1))
        w = np.float32(2.0 * math.pi) * pos / np.float32(Lf)
        bands = np.linspace(1e-4, 15, 16, dtype=np.float32)
        ang = w[:, None] * bands[None, :]
        z = np.concatenate([t[:, None], np.cos(ang), -np.sin(ang)], axis=-1).astype(np.float32)
        c["zT_" + nm] = np.ascontiguousarray(z.T)
        c["trow_" + nm] = np.ascontiguousarray(np.broadcast_to(t[None, :], (128, Lf))).astype(np.float32)
    deltas = np.abs(np.linspace(math.log(1e-2) / 1.5, math.log(1e-2) / 0.3, HY, dtype=np.float32))
    c["hy_ndelta"] = (-deltas).reshape(HY, 1).astype(np.float32)
    c["ndelta_row"] = (-deltas).reshape(1, HY).astype(np.float32)
    tl = (np.arange(L, dtype=np.float32) / np.float32(L - 1)).reshape(32, 128).T
    c["tneg_lat"] = np.ascontiguousarray(tl).astype(np.float32)
    c.update(fft_tables())
    cm = np.ones((128, 512), np.float32)
    cm[:, ::64] = 0.0
    c["cmask"] = cm
    c["triu"] = np.triu(np.ones((64, 64), np.float32))
    c["tril"] = np.tril(np.ones((64, 64), np.float32))
    c["iota1"] = np.ascontiguousarray(np.broadcast_to(np.arange(1, 257, dtype=np.float32)[None, :], (128, 256)))
    return c


CONST_SHAPES = {"ident": [128, 128], "antiI": [128, 128], "ones": [128, 128], "zT_lat": [33, L], "zT_ctx": [33, LC],
                "trow_lat": [128, L], "trow_ctx": [128, LC], "hy_ndelta": [HY, 1], "iota1": [128, 256], "cmask": [128, 512], "triu": [64, 64], "tril": [64, 64],
                "ff1": [128, 256], "f1d": [128, 256], "fft_T2": [128, 128, 128], "fft_G1": [128, 128], "fft_G2": [128, 128],
                "fft_T4": [64, 2, 128, 128], "tneg_lat": [128, 32], "ndelta_row": [1, HY]}

WEIGHT_SHAPES = {
    "mod_w": [2, 1024, 6144], "mod_b": [2, 6144], "norm1_g": [2, 1024], "norm2_g": [2, 1024], "final_g": [1024],
    "ab_w_in": [1, 1024, 2560], "ab_w_out": [1, 1024, 1024], "hy_conv_w": [1, 3, 2304], "hy_conv_b": [1, 2304],
    "hy_fw1": [1, 33, 64], "hy_fb1": [1, 64], "hy_ff1": [1, 64], "hy_fw2": [1, 64, 64], "hy_fb2": [1, 64],
    "hy_ff2": [1, 64], "hy_fw3": [1, 64, 3072], "hy_bias": [1, 2, 768],
    "s5_lam_re": [1, 2, 16, 64], "s5_lam_im": [1, 2, 16, 64], "s5_log_step": [1, 2, 16],
    "s5_b_re": [1, 2, 16, 64, 16], "s5_b_im": [1, 2, 16, 64, 16], "s5_c_re": [1, 2, 16, 16, 64],
    "s5_c_im": [1, 2, 16, 16, 64], "s5_d": [1, 256], "s5_glu_w": [1, 256, 512], "s5_glu_b": [1, 512],
    "c_w_in": [1, 1024, 5120], "c_w_out": [1, 1024, 1024], "c_lower_bounds": [2, 2, 1024], "c_norm_g": [1, 1024],
    "moe_wg": [2, 1024, 4], "moe_bg": [2, 4], "moe_we": [2, 1024, 32], "moe_be": [2, 32],
    "moe_w_gate": [2, 4, 8, 1024, 256], "moe_w_up": [2, 4, 8, 1024, 256], "moe_w_down": [2, 4, 8, 256, 1024],
}


DBGF = set()
USE_FFT = True


def build(stop=99, dbg=()):
    DBGF.clear()
    DBGF.update(dbg)
    nc = bass.Bass("TRN2", target_bir_lowering=False)
    k = KB(nc)
    I = {}
    I["x"] = k.dram("x", [NB, L, D], kind="ExternalInput")
    I["ctx"] = k.dram("ctx", [NB, LC, D], kind="ExternalInput")
    I["cvec"] = k.dram("cvec", [3, D], kind="ExternalInput")
    for n, s in WEIGHT_SHAPES.items():
        I[n] = k.dram(n, s, kind="ExternalInput")
    for n, s in CONST_SHAPES.items():
        I[n] = k.dram(n, s, kind="ExternalInput")
    OUT = k.dram("out", [NB, L, D], kind="ExternalOutput")

    def scratch(name, shape):
        return k.dram(name, shape, kind=("ExternalOutput" if name in dbg else "Internal"))

    S = {}
    S["MV"] = scratch("MV", [2, 3, 6, D])
    S["PT0"] = scratch("PT0", [NB, 2560, LC + L])
    S["UT0"] = scratch("UT0", [NB, LC + L, 256])
    S["YT0"] = scratch("YT0", [NB, 1024, LC + L])
    S["P0"] = scratch("P0", [NB, L + 2, 2304])
    S["KT"] = scratch("KT", [2 * L, 2 * HY])
    S["HB"] = scratch("HB", [L + 128, 2 * HY])
    S["ESC"] = scratch("ESC", [1, 2 * HY])
    S["KF"] = scratch("KF", [2, 128, 128, HY])
    S["FS1"] = scratch("FS1", [2, 64, 128, CC])
    S["FS2"] = scratch("FS2", [2, 64, 128, CC])
    S["YH"] = scratch("YH", [NB, L, HY])
    S["X1"] = scratch("X1", [NB, L, D])
    S["CTX1"] = scratch("CTX1", [NB, LC, D])
    S["HT1"] = k.dram("HT1", [NB, D, TT], dt=BF16)
    S["QT1"] = scratch("QT1", [NB, D, L])
    S["ZF1"] = scratch("ZF1", [NB, D, TT])
    S["ZB1"] = scratch("ZB1", [NB, D, TT])
    S["V1"] = scratch("V1", [NB, TT, D])
    S["G1"] = scratch("G1", [NB, L, D])
    S["O1"] = scratch("O1", [2, NB, L, D])
    S["OT1"] = scratch("OT1", [NB, D, L])
    S["DBG"] = scratch("DBG", [16, 128, 4096])
    S["YS5"] = scratch("YS5", [2, NB, LC + L, 256])

    with k.es:
        glob = k.es
        ident = k.tile(glob, "ident", [128, 128])
        antiI = k.tile(glob, "antiI", [128, 128])
        ones = k.tile(glob, "ones", [128, 128])
        k.dma("sp", ident[:], I["ident"][:, :])
        k.dma("sp", antiI[:], I["antiI"][:, :])
        k.dma("sp", ones[:], I["ones"][:, :])
        PS = [k.ptile(glob, f"ps{i}", [128, 512]) for i in range(8)]
        C = dict(ident=ident, antiI=antiI, ones=ones, PS=PS)

        phase_mod(k, I, S, C)
        k.barrier()
        if stop >= 1:
            phase_l0_inproj(k, I, S, C)
            k.barrier()
        if stop >= 2 and "nohy" not in dbg:
            phase_hyena(k, I, S, C)
            k.barrier()
            if USE_FFT:
                phase_hyena_fft(k, I, S, C)
                k.barrier()
        if stop >= 3:
            phase_s5(k, I, S, C)
            k.barrier()
            phase_s5_out(k, I, S, C)
            k.barrier()
        if stop >= 4:
            tl = l0_moe_tiles(I, S)
            if "moe1" in DBGF:
                tl = tl[:2]
            phase_moe(k, I, S, C, 0, S["YT0"], I["ab_w_out"][0], tl, final=False)
            k.barrier()
        if stop >= 5:
            phase_l1_norm(k, I, S, C)
            k.barrier()
            phase_l1_inproj(k, I, S, C)
            k.barrier()
        if stop >= 6:
            phase_hgrn2(k, I, S, C)
            k.barrier()
            phase_hgrn2_out(k, I, S, C)
            k.barrier()
        if stop >= 7:
            tl = l1_moe_tiles(I, S, OUT)
            if "moe1" in DBGF:
                tl = tl[:1]
            phase_moe(k, I, S, C, 1, S["OT1"], I["c_w_out"][0], tl, final=True)
            k.barrier()
        k.barrier()
    return nc


def phase_mod(k, I, S, C):
    PS = C["PS"]
    with contextlib.ExitStack() as st:
        cT = k.tile(st, "cT", [128, 3, 8])
        sT = k.tile(st, "sT", [128, 3, 8])
        for r in range(3):
            k.dma("sp", cT[:, r, :], I["cvec"][r].rearrange("(k p) -> p k", p=128), allow_slow_non_contiguous=True,
                  wb=[Buf("tmp")])
        k.barrier()
        k.act(sT[:], cT[:], AF.Silu)
        wt = [k.tile(st, f"modw{i}", [128, 8, 512]) for i in range(2)]
        mv = k.tile(st, "mv", [3, 6144])
        bias = k.tile(st, "modb", [3, 6144])
        g1 = k.tile(st, "g1b", [3, 1024])
        g2 = k.tile(st, "g2b", [3, 1024])
        for l in range(2):
            k.dma("sp", bias[:], I["mod_b"][l:l + 1, :].partition_broadcast(3))
            k.dma("sp", g1[:], I["norm1_g"][l:l + 1, :].partition_broadcast(3))
            k.dma("sp", g2[:], I["norm2_g"][l:l + 1, :].partition_broadcast(3))
            wv = I["mod_w"][l].rearrange("(k p) n -> p k n", p=128)
            for j in range(12):
                w = wt[j % 2]
                k.dma("sp" if j % 2 == 0 else "pool", w[:], wv[:, :, j * 512:(j + 1) * 512])
                ps = PS[j % 2]
                for kk in range(8):
                    k.mm(ps[0:3, :], sT[:, :, kk], w[:, kk, :], start=(kk == 0), stop=(kk == 7))
                k.tt("dve", mv[:, j * 512:(j + 1) * 512], ps[0:3, :], bias[:, j * 512:(j + 1) * 512], ALU.add)
            k.stt("dve", mv[:, 1024:2048], mv[:, 1024:2048], 1.0, g1[:], ALU.add, ALU.mult)
            k.stt("dve", mv[:, 4096:5120], mv[:, 4096:5120], 1.0, g2[:], ALU.add, ALU.mult)
            k.dma("sp", S["MV"][l].rearrange("r c d -> r (c d)"), mv[:])


def load_bcast(k, eng, tile_ap, dram_row_ap):
    return k.dma(eng, tile_ap, dram_row_ap.partition_broadcast(tile_ap.shape[0]))


def rms_mod(k, st_tmp, xt, A, sh, out, name):
    sq, ss, rstd = st_tmp
    k.act(sq[:], xt, AF.Square, accum_out=ss[:])
    k.ts("dve", ss[:], ss[:], 1.0 / D, EPS, ALU.mult, ALU.add)
    k.act(ss[:], ss[:], AF.Sqrt)
    k.recip(rstd[:], ss[:])
    k.stt("dve", out, xt, rstd[:, 0:1], A, ALU.mult, ALU.mult)
    k.tt("dve", out, out, sh, ALU.add)


def transpose_1024(k, C, src, dstT, pbase=4):
    PS = C["PS"]
    for half in range(2):
        ps = PS[pbase + half]
        for j in range(4):
            kk = half * 4 + j
            k.mm(ps[:, j * 128:(j + 1) * 128], src[:, kk * 128:(kk + 1) * 128], C["ident"][:], start=True, stop=True)
        k.copy("act", dstT[:, half * 4:(half + 1) * 4, :], ps[:].rearrange("p (j t) -> p j t", j=4))


TT = LC + L


def phase_l0_inproj(k, I, S, C):
    PS = C["PS"]
    with contextlib.ExitStack() as st:
        W = k.tile(st, "Win0", [128, 8, 2560], BF16)
        wst = [k.tile(st, f"Wst{i}", [128, 2560]) for i in range(2)]
        wv = I["ab_w_in"][0].rearrange("(k p) n -> p k n", p=128)
        for kk in range(8):
            k.dma("sp" if kk % 2 == 0 else "pool", wst[kk % 2][:], wv[:, kk, :])
            k.copy("act" if kk % 2 == 0 else "dve", W[:, kk, :], wst[kk % 2][:])
        k.barrier()
        A = k.tile(st, "A1", [128, 1024])
        sh = k.tile(st, "sh1", [128, 1024])
        xt = [k.tile(st, f"xt{i}", [128, 1024]) for i in range(2)]
        h = k.tile(st, "h", [128, 1024])
        hT = k.tile(st, "hT", [128, 8, 128], BF16)
        po = [k.tile(st, f"po{i}", [128, 20, 128]) for i in range(2)]
        pu = [k.tile(st, f"pu{i}", [128, 256]) for i in range(2)]
        pt = [k.tile(st, f"pt{i}", [128, 2304]) for i in range(2)]
        zrow = k.tile(st, "zrow", [1, 2304])
        k.memset("dve", zrow[:], 0.0)
        for b in range(NB):
            k.dma("sp", S["P0"][b, 0:1, :], zrow[:], wb=[Buf("x")])
            k.dma("sp", S["P0"][b, L + 1:L + 2, :], zrow[:], wb=[Buf("x")])
        sq = k.tile(st, "sq", [128, 1024])
        ss = k.tile(st, "ss", [128, 1])
        rstd = k.tile(st, "rstd", [128, 1])
        it = 0
        for b in range(NB):
            for (src, row, ntile, toff) in ((I["ctx"], 2, LC // 128, 0), (I["x"], b, L // 128, LC)):
                load_bcast(k, "sp", A[:], S["MV"][0, row, 1:2, :])
                load_bcast(k, "sp", sh[:], S["MV"][0, row, 0:1, :])
                for t in range(ntile):
                    x_ = xt[it % 2]
                    o_ = po[it % 2]
                    u_ = pu[it % 2]
                    k.dma("sp", x_[:], src[b, t * 128:(t + 1) * 128, :])
                    rms_mod(k, (sq, ss, rstd), x_[:], A[:], sh[:], h[:], "l0")
                    transpose_1024(k, C, h, hT)
                    for j4 in range(5):
                        ps = PS[j4 % 4]
                        for jj in range(4):
                            j = j4 * 4 + jj
                            for kk in range(8):
                                k.mm(ps[:, jj * 128:(jj + 1) * 128], W[:, kk, j * 128:(j + 1) * 128], hT[:, kk, :],
                                     start=(kk == 0), stop=(kk == 7))
                        k.copy("act" if j4 % 2 == 0 else "dve", o_[:, j4 * 4:(j4 + 1) * 4, :],
                               ps[:].rearrange("p (j t) -> p j t", j=4))
                    ps = PS[6]
                    for kk in range(8):
                        k.mm(ps[:, 0:256], hT[:, kk, :], W[:, kk, 2304:2560], start=(kk == 0), stop=(kk == 7))
                    k.copy("dve", u_[:], ps[:, 0:256])
                    if toff == LC and USE_FFT:
                        p_ = pt[it % 2]
                        for j in range(5):
                            ps = PS[6 + j % 2]
                            w_ = 512 if j < 4 else 256
                            for kk in range(8):
                                k.mm(ps[:, 0:w_], hT[:, kk, :], W[:, kk, j * 512:j * 512 + w_], start=(kk == 0), stop=(kk == 7))
                            k.copy("act" if j % 2 == 0 else "dve", p_[:, j * 512:j * 512 + w_], ps[:, 0:w_])
                        k.dma("pool", S["P0"][b, 1 + t * 128:1 + (t + 1) * 128, :], p_[:], wb=[Buf("x")])
                    t0 = toff + t * 128
                    k.dma("pool", S["PT0"][b].rearrange("(j p) t -> p j t", p=128)[:, :, t0:t0 + 128], o_[:],
                          wb=[S["PT0"].reg((b, t0))])
                    k.dma("pool", S["UT0"][b, t0:t0 + 128, :], u_[:], wb=[S["UT0"].reg((b, t0))])
                    it += 1


def sin_rr(k, out, src, tmp_r, tmp_n, pre_scale=None, pre_bias=None):
    if pre_scale is not None:
        k.ts("dve", tmp_r, src, pre_bias, pre_scale, ALU.add, ALU.mult)
        k.ts("dve", tmp_r, tmp_r, 1.0 / TWO_PI, None, ALU.mult)
    else:
        k.ts("dve", tmp_r, src, 1.0 / TWO_PI, None, ALU.mult)
    k.ts("dve", tmp_n, tmp_r, MAGIC, MAGIC, ALU.add, ALU.subtract)
    k.tt("dve", tmp_r, tmp_r, tmp_n, ALU.subtract)
    k.act(out, tmp_r, AF.Sin, scale=TWO_PI)


def phase_hyena(k, I, S, C):
    PS = C["PS"]
    for (T, toff, zname, tname) in ((LC, 0, "zT_ctx", "trow_ctx"), (L, LC, "zT_lat", "trow_lat")):
        if ("hyctx" in DBGF or USE_FFT) and T == L:
            continue
        with contextlib.ExitStack() as st:
            h2 = k.tile(st, "hyh2", [64, T])
            with contextlib.ExitStack() as st2:
                zT = k.tile(st2, "zT", [33, T])
                k.dma("sp", zT[:], I[zname][:, :])
                fw1 = k.tile(st2, "fw1", [33, 64])
                fw2 = k.tile(st2, "fw2", [64, 64])
                k.dma("sp", fw1[:], I["hy_fw1"][0])
                k.dma("sp", fw2[:], I["hy_fw2"][0])
                prm = k.tile(st2, "hyprm", [64, 4])
                for i, n in enumerate(("hy_fb1", "hy_ff1", "hy_fb2", "hy_ff2")):
                    k.dma("sp", prm[:, i:i + 1], I[n][0].rearrange("(p o) -> p o", o=1), wb=[Buf("tmp")])
                k.barrier()
                h1 = k.tile(st2, "hyh1", [64, T])
                tr = k.tile(st2, "hytr", [64, 512])
                tn = k.tile(st2, "hytn", [64, 512])
                nb = min(T, 512)
                for blk in range(T // nb):
                    sl = slice(blk * nb, (blk + 1) * nb)
                    k.mm(PS[0][0:64, 0:nb], fw1[:], zT[:, sl])
                    sin_rr(k, h1[:, sl], PS[0][0:64, 0:nb], tr[:, 0:nb], tn[:, 0:nb], prm[:, 1:2], prm[:, 0:1])
                    k.mm(PS[1][0:64, 0:nb], fw2[:], h1[:, sl])
                    sin_rr(k, h2[:, sl], PS[1][0:64, 0:nb], tr[:, 0:nb], tn[:, 0:nb], prm[:, 3:4], prm[:, 2:3])
                if "DBG" in DBGF and T == LC:
                    k.dma("sp", S["DBG"][8, 0:64, 0:T], h1[:, :], wb=[Buf("x")])
                    k.dma("sp", S["DBG"][9, 0:64, 0:4], prm[:, :], wb=[Buf("x")])
                    k.dma("sp", S["DBG"][10, 0:33, 0:T], zT[:, :], wb=[Buf("x")])
                    k.dma("sp", S["DBG"][10, 64:97, 0:64], fw1[:, :], wb=[Buf("x")])
                k.barrier()
            fw3 = k.tile(st, "fw3", [64, 3072])
            k.dma("sp", fw3[:], I["hy_fw3"][0])
            trow = k.tile(st, "trow", [128, T])
            k.dma("sp", trow[:], I[tname][:, :])
            dec = k.tile(st, "dec", [128, T])
            kfb = k.tile(st, "kfb", [128, NB, T])
            raw = k.tile(st, "raw", [128, NB, T + 2])
            v = k.tile(st, "v", [128, NB, T])
            acc = k.tile(st, "acc", [128, NB, T])
            prm = k.tile(st, "cprm", [128, 3, 8])
            en = k.tile(st, "en", [128, 4])
            k.memset("dve", raw[:], 0.0)
            nb = min(T, 512)

            def load_conv(ct, blk, dst):
                col = blk * HY + ct * 128
                for b in range(NB):
                    k.dma("sp", raw[:, b, 1:T + 1], S["PT0"][b, col:col + 128, toff:toff + T])
                k.ts("dve", dst, raw[:, :, 0:T], prm[:, blk, 0:1], prm[:, blk, 3:4], ALU.mult, ALU.add)
                k.stt("dve", dst, raw[:, :, 1:T + 1], prm[:, blk, 1:2], dst, ALU.mult, ALU.add)
                k.stt("dve", dst, raw[:, :, 2:T + 2], prm[:, blk, 2:3], dst, ALU.mult, ALU.add)

            for ct in range(HY // 128):
                for blk in range(3):
                    col = blk * HY + ct * 128
                    k.dma("sp", prm[:, blk, 0:3], I["hy_conv_w"][0][:, col:col + 128].rearrange("j c -> c j"),
                          allow_slow_non_contiguous=True)
                    k.dma("sp", prm[:, blk, 3:4], I["hy_conv_b"][0][col:col + 128].rearrange("(p o) -> p o", o=1))
                for o in range(2):
                    k.dma("sp", prm[:, 0, 4 + o:5 + o], I["hy_bias"][0][o, ct * 128:(ct + 1) * 128].rearrange("(p o) -> p o", o=1))
                k.dma("sp", prm[:, 0, 6:7], I["hy_ndelta"][ct * 128:(ct + 1) * 128, :])
                k.act(dec[:], trow[:], AF.Exp, scale=prm[:, 0, 6:7])
                load_conv(ct, 2, v[:])
                for o in range(2):
                    for d_ in (0, 1):
                        col = o * 2 * HY + d_ * HY + ct * 128
                        for blk in range(T // nb):
                            sl = slice(blk * nb, (blk + 1) * nb)
                            ps = PS[blk % 4]
                            k.mm(ps[:, 0:nb], fw3[:, col:col + 128], h2[:, sl])
                            k.tt("dve", kfb[:, d_, sl], ps[:, 0:nb], dec[:, sl], ALU.mult)
                    k.memset("dve", kfb[:, 1, 0:1], 0.0)
                    k.act(acc[:, 0, :], kfb[:, 0, :], AF.Square, accum_out=en[:, 0:1])
                    k.act(acc[:, 0, :], kfb[:, 1, :], AF.Square, accum_out=en[:, 1:2])
                    k.tt("dve", en[:, 2:3], en[:, 0:1], en[:, 1:2], ALU.add)
                    k.act(en[:, 2:3], en[:, 2:3], AF.Sqrt)
                    k.recip(en[:, 3:4], en[:, 2:3])
                    k.ts("dve", kfb[:], kfb[:], en[:, 3:4], None, ALU.mult)
                    if "DBG" in DBGF and ct == 0 and T == LC:
                        k.dma("sp", S["DBG"][o * 4 + 0, :, 0:T], kfb[:, 0, :], wb=[Buf("x")])
                        k.dma("sp", S["DBG"][o * 4 + 1, :, 0:T], kfb[:, 1, :], wb=[Buf("x")])
                        k.dma("sp", S["DBG"][o * 4 + 2, :, 0:T], v[:, 0, :], wb=[Buf("x")])
                        k.dma("sp", S["DBG"][o * 4 + 3, 0:64, 0:T], h2[:, :], wb=[Buf("x")])
                    k.ts("dve", acc[:], v[:], kfb[:, 0, 0:1], None, ALU.mult)
                    for lag in range(1, T):
                        k.stt("dve", acc[:, :, lag:T], v[:, :, 0:T - lag], kfb[:, 0, lag:lag + 1], acc[:, :, lag:T],
                              ALU.mult, ALU.add)
                        k.stt("dve", acc[:, :, 0:T - lag], v[:, :, lag:T], kfb[:, 1, lag:lag + 1], acc[:, :, 0:T - lag],
                              ALU.mult, ALU.add)
                    k.stt("dve", acc[:], v[:], prm[:, 0, 4 + o:5 + o], acc[:], ALU.mult, ALU.add)
                    load_conv(ct, o, kfb[:])
                    if o == 0:
                        k.tt("dve", v[:], acc[:], kfb[:], ALU.mult)
                    else:
                        k.tt("dve", acc[:], acc[:], kfb[:], ALU.mult)
                        for b in range(NB):
                            k.dma("pool", S["YT0"][b, ct * 128:(ct + 1) * 128, toff:toff + T], acc[:, b, :],
                                  wb=[S["YT0"].reg((b, ct, toff))])
                    k.barrier()


CC = 64


def fwd_fft(k, C, I, S, st_tiles, Xin3, tab1, consumer):
    PS = C["PS"]
    A, A2, T2g = st_tiles["A"], st_tiles["A2"], st_tiles["T2g"]
    Xf = Xin3.rearrange("p n c -> p (n c)")
    ng = 64 * CC // 512
    for g in range(ng):
        pr, pi_ = PS[(g % 2) * 2], PS[(g % 2) * 2 + 1]
        k.mm(pr[:], tab1[:, 0:128], Xf[:, g * 512:(g + 1) * 512])
        k.mm(pi_[:], tab1[:, 128:256], Xf[:, g * 512:(g + 1) * 512])
        k.copy("act", A[:, 0].rearrange("p n c -> p (n c)")[:, g * 512:(g + 1) * 512], pr[:])
        k.copy("dve", A[:, 1].rearrange("p n c -> p (n c)")[:, g * 512:(g + 1) * 512], pi_[:])
    for r in range(2):
        k.dma("sp" if r == 0 else "pool", S["FS1"][r].rearrange("n k c -> k n c"), A[:, r])
    k.dma("sp", A2[:].rearrange("p k c -> p (k c)"), S["FS1"].a.rearrange("r n k c -> (r n) (k c)"))
    for k1g in range(16):
        tg = T2g[k1g % 2]
        k.dma("pool" if k1g % 2 else "sp", tg[:], I["fft_T2"][k1g * 8:(k1g + 1) * 8].rearrange("k r m -> r k m"))
        ps = PS[4 + k1g % 2]
        for j in range(8):
            k.mm(ps[:, j * CC:(j + 1) * CC], tg[:, j, :], A2[:, k1g * 8 + j, :])
        consumer(k1g, ps)


def phase_hyena_fft(k, I, S, C):
    PS = C["PS"]
    ident, antiI, ones = C["ident"], C["antiI"], C["ones"]
    T = L
    with contextlib.ExitStack() as st:
        h2 = k.tile(st, "fh2", [64, T])
        with contextlib.ExitStack() as st2:
            zT = k.tile(st2, "fzT", [33, T])
            k.dma("sp", zT[:], I["zT_lat"][:, :])
            fw1 = k.tile(st2, "ffw1", [33, 64])
            fw2 = k.tile(st2, "ffw2", [64, 64])
            k.dma("sp", fw1[:], I["hy_fw1"][0])
            k.dma("sp", fw2[:], I["hy_fw2"][0])
            prm = k.tile(st2, "fprm", [64, 4])
            for i, n in enumerate(("hy_fb1", "hy_ff1", "hy_fb2", "hy_ff2")):
                k.dma("sp", prm[:, i:i + 1], I[n][0].rearrange("(p o) -> p o", o=1), wb=[Buf("tmp")])
            k.barrier()
            h1 = k.tile(st2, "fh1", [64, T])
            tr = k.tile(st2, "ftr", [64, 512])
            tn = k.tile(st2, "ftn", [64, 512])
            for blk in range(T // 512):
                sl = slice(blk * 512, (blk + 1) * 512)
                k.mm(PS[0][0:64, :], fw1[:], zT[:, sl])
                sin_rr(k, h1[:, sl], PS[0][0:64, :], tr[:], tn[:], prm[:, 1:2], prm[:, 0:1])
                k.mm(PS[1][0:64, :], fw2[:], h1[:, sl])
                sin_rr(k, h2[:, sl], PS[1][0:64, :], tr[:], tn[:], prm[:, 3:4], prm[:, 2:3])
            k.barrier()
        fw3 = k.tile(st, "ffw3", [64, 3072])
        k.dma("sp", fw3[:], I["hy_fw3"][0])
        dlt = k.tile(st, "fdlt", [128, HY])
        load_bcast(k, "sp", dlt[:], I["ndelta_row"][0:1, :])
        tpos = k.tile(st, "ftpos", [128, 32])
        k.dma("sp", tpos[:], I["tneg_lat"][:, :])
        dec = k.tile(st, "fdec", [128, HY])
        hraw = [k.tile(st, f"fhraw{i}", [128, 3072]) for i in range(2)]
        accE = k.tile(st, "faccE", [128, 3072])
        sq = k.tile(st, "fsq", [128, 3072])
        for t in range(T // 128):
            hr = hraw[t % 2]
            k.act(dec[:], dlt[:], AF.Exp, scale=tpos[:, t:t + 1])
            for j in range(6):
                ps = PS[j % 4]
                k.mm(ps[:], h2[:, t * 128:(t + 1) * 128], fw3[:, j * 512:(j + 1) * 512])
                k.copy("act", hr[:, j * 512:(j + 1) * 512], ps[:])
            k.tt("dve", hr[:].rearrange("p (q c) -> p q c", c=HY), hr[:].rearrange("p (q c) -> p q c", c=HY),
                 dec[:].unsqueeze(1).to_broadcast([128, 4, HY]), ALU.mult)
            if t == 0:
                for o in range(2):
                    k.memset("dve", hr[0:1, o * 1536 + HY:o * 1536 + 2 * HY], 0.0)
            if t == 0:
                k.act(accE[:], hr[:], AF.Square)
            else:
                k.act(sq[:], hr[:], AF.Square)
                k.tt("pool", accE[:], accE[:], sq[:], ALU.add)
            for o in range(2):
                k.dma("sp", S["KT"][t * 128:(t + 1) * 128, o * HY:(o + 1) * HY], hr[:, o * 1536:o * 1536 + HY], wb=[Buf("x")])
                k.dma("pool", S["HB"][t * 128:(t + 1) * 128, o * HY:(o + 1) * HY], hr[:, o * 1536 + HY:(o + 1) * 1536],
                      wb=[Buf("x")])
        k.memset("dve", sq[:], 0.0)
        k.dma("sp", S["HB"][T:T + 128, :], sq[:, 0:1536], wb=[Buf("x")])
        en = k.tile(st, "fen", [1, 3072])
        for j in range(6):
            k.mm(PS[j % 4][0:1, :], ones[:, 0:1], accE[:, j * 512:(j + 1) * 512])
            k.copy("act", en[:, j * 512:(j + 1) * 512], PS[j % 4][0:1, :])
        es = k.tile(st, "fes", [1, 1536])
        for o in range(2):
            k.tt("dve", es[:, o * HY:(o + 1) * HY], en[:, o * 1536:o * 1536 + HY], en[:, o * 1536 + HY:(o + 1) * 1536], ALU.add)
        k.act(es[:], es[:], AF.Sqrt)
        k.recip(es[:], es[:])
        k.dma("sp", S["ESC"][0:1, :], es[:])
        k.barrier()
        for j in range(T // 128):
            g_ = hraw[j % 2]
            k.dma("sp", g_[:, 0:1536], S["HB"][128 * j + 1:128 * j + 129, :])
            fl = sq if j % 2 == 0 else accE
            for c3 in range(3):
                ps = PS[c3]
                k.mm(ps[:], antiI[:], g_[:, c3 * 512:(c3 + 1) * 512])
                k.copy("act" if c3 % 2 == 0 else "dve", fl[:, c3 * 512:(c3 + 1) * 512], ps[:])
            k.dma("pool", S["KT"][2 * T - 128 * (j + 1):2 * T - 128 * j, :], fl[:, 0:1536], wb=[Buf("x")])
        k.barrier()
    with contextlib.ExitStack() as st:
        tiles = dict(A=k.tile(st, "fA", [128, 2, 64, CC]), A2=k.tile(st, "fA2", [128, 128, CC]),
                     T2g=[k.tile(st, f"fT2g{i}", [128, 8, 128]) for i in range(2)])
        ff1 = k.tile(st, "fff1", [128, 256])
        f1d = k.tile(st, "ff1d", [128, 256])
        G1 = k.tile(st, "fG1", [128, 128])
        G2 = k.tile(st, "fG2", [128, 128])
        k.dma("sp", ff1[:], I["ff1"][:, :])
        k.dma("sp", f1d[:], I["f1d"][:, :])
        k.dma("sp", G1[:], I["fft_G1"][:, :])
        k.dma("sp", G2[:], I["fft_G2"][:, :])
        V = [k.tile(st, f"fV{i}", [128, 64, CC]) for i in range(2)]
        Xin = V[0]
        esc = k.tile(st, "fesc", [128, CC])
        okf = [k.tile(st, f"fokf{i}", [128, 8, CC]) for i in range(1)]
        KTv = S["KT"].a.rearrange("(a b) c -> a b c", b=64)
        for fc in range(24):
            o, c0 = fc // 12, (fc % 12) * CC
            k.dma("sp", Xin[:], KTv[:, :, fc * CC:(fc + 1) * CC])
            load_bcast(k, "sp", esc[:], S["ESC"][0:1, fc * CC:(fc + 1) * CC])

            def cons_f(k1g, ps, o=o, c0=c0):
                t_ = okf[0]
                k.tt("dve", t_[:], ps[:].rearrange("p (j c) -> p j c", c=CC), esc[:].unsqueeze(1).to_broadcast([128, 8, CC]), ALU.mult)
                k.dma("pool", S["KF"][o, :, k1g * 8:(k1g + 1) * 8, c0:c0 + CC], t_[:], wb=[Buf("x")])

            fwd_fft(k, C, I, S, tiles, Xin[:], ff1, cons_f)
        k.barrier()
        Bt = k.tile(st, "fB", [128, 128, CC])
        R = k.tile(st, "fR", [128, 66, CC])
        Gt = k.tile(st, "fGt", [128, 64, CC])
        wb_ = k.tile(st, "fwb", [128, 3, 4, CC])
        hb = k.tile(st, "fhb", [128, 2, CC])
        Kg = [k.tile(st, f"fKg{i}", [128, 2, 8, CC]) for i in range(2)]
        T12 = [k.tile(st, f"fT12{i}", [128, 2, 8 * CC]) for i in range(2)]
        T4g = [k.tile(st, f"fT4g{i}", [128, 4, 2, 128]) for i in range(2)]
        tmp = k.tile(st, "ftmp", [128, 4, CC])
        B2 = tiles["A"]

        def conv(blk, c0, dst):
            col = blk * HY + c0
            for b in range(NB):
                k.dma("sp", R[b * 64:(b + 1) * 64], AP(S["P0"].h, (b * (L + 2)) * 2304 + col, [[64 * 2304, 64], [2304, 66], [1, CC]]),
                      rb=[S["P0"]])
            w = lambda j: wb_[:, blk, j, :].unsqueeze(1).to_broadcast([128, 64, CC])
            k.tt("dve", dst, R[:, 0:64, :], w(0), ALU.mult)
            k.tt("pool", Gt_tmp[:, 0:64, :], R[:, 1:65, :], w(1), ALU.mult)
            k.tt("dve", dst, dst, Gt_tmp[:, 0:64, :], ALU.add)
            k.tt("pool", Gt_tmp[:, 0:64, :], R[:, 2:66, :], w(2), ALU.mult)
            k.tt("dve", dst, dst, Gt_tmp[:, 0:64, :], ALU.add)
            k.tt("dve", dst, dst, w(3), ALU.add)

        Gt_tmp = Bt
        for dc in range(HY // CC):
            c0 = dc * CC
            for blk in range(3):
                col = blk * HY + c0
                for j in range(3):
                    k.dma("sp", wb_[:, blk, j, :], I["hy_conv_w"][0][j:j + 1, col:col + CC].partition_broadcast(128), wb=[Buf("x")])
                k.dma("sp", wb_[:, blk, 3, :], I["hy_conv_b"][0:1, col:col + CC].partition_broadcast(128), wb=[Buf("x")])
            for o in range(2):
                k.dma("sp", hb[:, o, :], I["hy_bias"][0][o:o + 1, c0:c0 + CC].partition_broadcast(128), wb=[Buf("x")])
            k.barrier()
            conv(2, c0, V[0][:])
            for o in range(2):
                Vin, Vout = V[o], V[1 - o]

                def cons_d(k1g, ps, o=o, c0=c0):
                    kg = Kg[k1g % 2]
                    for r in range(2):
                        for hf in range(2):
                            k.dma("sp" if hf == 0 else "pool", kg[hf * 64:(hf + 1) * 64, r],
                                  S["KF"][o, r * 64:(r + 1) * 64, k1g * 8:(k1g + 1) * 8, c0:c0 + CC], rb=[S["KF"]], wb=[kg.sub[r * 2 + hf]])
                    t12 = T12[k1g % 2]
                    k.op("dve", lambda: k.nc.vector.tensor_tensor(t12[:, 0, :], ps[:], kg[:, 0].rearrange("p j c -> p (j c)"), ALU.mult),
                         [t12[:, 0, :]], [ps[:]], rb=[k.buf_of(ps[:])] + kg.sub, wb=[t12])
                    k.op("dve", lambda: k.nc.vector.tensor_tensor(t12[:, 1, :], ps[:], kg[:, 1].rearrange("p j c -> p (j c)"), ALU.mult),
                         [t12[:, 1, :]], [ps[:]], rb=[k.buf_of(ps[:])] + kg.sub, wb=[t12])
                    p2 = PS[6 + k1g % 2]
                    k.mm(p2[:], G1[:], t12[:, 0, :], start=True, stop=False)
                    k.mm(p2[:], G2[:], t12[:, 1, :], start=False, stop=True)
                    k.copy("act", Bt[:, k1g * 8:(k1g + 1) * 8, :], p2[:].rearrange("p (j c) -> p j c", c=CC))

                fwd_fft(k, C, I, S, tiles, Vin[:], f1d, cons_d)
                k.dma("sp", S["FS2"].a.rearrange("r n k c -> (r n) (k c)"), Bt[:].rearrange("p k c -> p (k c)"))
                for r in range(2):
                    k.dma("sp" if r == 0 else "pool", B2[:, r], S["FS2"][r].rearrange("n k c -> k n c"))
                conv(o, c0, Gt[:])
                for g in range(16):
                    t4 = T4g[g % 2]
                    for r in range(2):
                        k.dma("sp" if r == 0 else "pool", t4[:, :, r, :], I["fft_T4"][g * 4:(g + 1) * 4, r].rearrange("n k m -> k n m"),
                              wb=[t4.sub[r]])
                    ps = PS[g % 4]
                    for j in range(4):
                        n2 = g * 4 + j
                        k.op("pe", lambda: k.nc.tensor.matmul(ps[:, j * CC:(j + 1) * CC], t4[:, j, 0, :], B2[:, 0, n2, :], start=True, stop=False),
                             [ps[:]], [], rb=[B2] + t4.sub, wb=[k.buf_of(ps[:])])
                        k.op("pe", lambda: k.nc.tensor.matmul(ps[:, j * CC:(j + 1) * CC], t4[:, j, 1, :], B2[:, 1, n2, :], start=False, stop=True),
                             [ps[:]], [], rb=[B2] + t4.sub, wb=[k.buf_of(ps[:])])
                    gs = slice(g * 4, (g + 1) * 4)
                    k.tt("pool", tmp[:], Vin[:, gs, :], hb[:, o, :].unsqueeze(1).to_broadcast([128, 4, CC]), ALU.mult)
                    k.tt("dve", tmp[:], tmp[:], ps[:, 0:4 * CC].rearrange("p (j c) -> p j c", c=CC), ALU.add)
                    k.tt("dve", Vout[:, gs, :], tmp[:], Gt[:, gs, :], ALU.mult)
                if o == 1:
                    for b in range(NB):
                        k.dma("pool", AP(S["YH"].h, b * L * HY + c0, [[64 * HY, 64], [HY, 64], [1, CC]]), Vout[b * 64:(b + 1) * 64],
                              wb=[Buf("x")])
            k.barrier()


TC = 256


def phase_s5(k, I, S, C):
    PS = C["PS"]
    ident, antiI = C["ident"], C["antiI"]
    nblk = TT // 128
    for d in range(2):
        with contextlib.ExitStack() as st:
            BTr = [k.tile(st, f"BTr{j}", [128, 128]) for j in range(8)]
            BTi = [k.tile(st, f"BTi{j}", [128, 128]) for j in range(8)]
            Cr = [k.tile(st, f"Cr{j}", [128, 256]) for j in range(8)]
            nCi = [k.tile(st, f"nCi{j}", [128, 256]) for j in range(8)]
            ctab = [k.tile(st, f"ctab{j}", [128, TC]) for j in range(8)]
            stab = [k.tile(st, f"stab{j}", [128, TC]) for j in range(8)]
            rt = [k.tile(st, f"rt{j}", [128, TC]) for j in range(8)]
            carry = [k.tile(st, f"carry{j}", [128, 2]) for j in range(8)]
            iot = k.tile(st, "iot", [128, TC])
            k.dma("sp", iot[:], I["iota1"][:, :])
            with contextlib.ExitStack() as st2:
                sc = k.tile(st2, "s5sc", [128, 24])
                tr = k.tile(st2, "s5tr", [128, TC])
                tn = k.tile(st2, "s5tn", [128, TC])
                bre = k.tile(st2, "s5bre", [128, 16])
                bim = k.tile(st2, "s5bim", [128, 16])
                bb = k.tile(st2, "s5bb", [128, 2, 16])
                t16 = k.tile(st2, "s5t16", [128, 16])
                pad = k.tile(st2, "s5pad", [128, 128])
                for j in range(8):
                    g0 = 2 * j
                    k.dma("sp", sc[:, 0:1], I["s5_lam_re"][0, d, g0:g0 + 2, :].rearrange("g (p o) -> (g p) o", o=1))
                    k.dma("sp", sc[:, 1:2], I["s5_lam_im"][0, d, g0:g0 + 2, :].rearrange("g (p o) -> (g p) o", o=1))
                    for gl in range(2):
                        k.dma("sp", sc[gl * 64:(gl + 1) * 64, 2:3],
                              I["s5_log_step"][0, d:d + 1, g0 + gl:g0 + gl + 1].partition_broadcast(64))
                    k.dma("sp", bre[:], I["s5_b_re"][0, d, g0:g0 + 2].rearrange("g p n -> (g p) n"))
                    k.dma("sp", bim[:], I["s5_b_im"][0, d, g0:g0 + 2].rearrange("g p n -> (g p) n"))
                    k.act(sc[:, 3:4], sc[:, 2:3], AF.Exp)
                    k.tt("dve", sc[:, 4:5], sc[:, 0:1], sc[:, 3:4], ALU.mult)
                    k.tt("dve", sc[:, 5:6], sc[:, 1:2], sc[:, 3:4], ALU.mult)
                    k.act(sc[:, 6:7], sc[:, 4:5], AF.Exp)
                    sin_rr(k, sc[:, 7:8], sc[:, 5:6], tr[:, 0:1], tn[:, 0:1])
                    k.ts("dve", sc[:, 9:10], sc[:, 5:6], math.pi / 2, None, ALU.add)
                    sin_rr(k, sc[:, 8:9], sc[:, 9:10], tr[:, 0:1], tn[:, 0:1])
                    k.tt("dve", sc[:, 10:11], sc[:, 6:7], sc[:, 8:9], ALU.mult)
                    k.tt("dve", sc[:, 11:12], sc[:, 6:7], sc[:, 7:8], ALU.mult)
                    k.ts("dve", sc[:, 12:13], sc[:, 10:11], -1.0, None, ALU.add)
                    k.tt("dve", sc[:, 13:14], sc[:, 0:1], sc[:, 0:1], ALU.mult)
                    k.stt("dve", sc[:, 13:14], sc[:, 1:2], sc[:, 1:2], sc[:, 13:14], ALU.mult, ALU.add)
                    k.recip(sc[:, 14:15], sc[:, 13:14])
                    k.tt("dve", sc[:, 15:16], sc[:, 12:13], sc[:, 0:1], ALU.mult)
                    k.stt("dve", sc[:, 15:16], sc[:, 11:12], sc[:, 1:2], sc[:, 15:16], ALU.mult, ALU.add)
                    k.tt("dve", sc[:, 15:16], sc[:, 15:16], sc[:, 14:15], ALU.mult)
                    k.tt("dve", sc[:, 16:17], sc[:, 12:13], sc[:, 1:2], ALU.mult)
                    k.stt("dve", sc[:, 16:17], sc[:, 11:12], sc[:, 0:1], sc[:, 16:17], ALU.mult, ALU.subtract)
                    k.tt("dve", sc[:, 16:17], sc[:, 16:17], sc[:, 14:15], ALU.mult)
                    k.ts("dve", bb[:, 0, :], bre[:], sc[:, 15:16], None, ALU.mult)
                    k.ts("dve", t16[:], bim[:], sc[:, 16:17], None, ALU.mult)
                    k.tt("dve", bb[:, 0, :], bb[:, 0, :], t16[:], ALU.subtract)
                    k.ts("dve", bb[:, 1, :], bim[:], sc[:, 15:16], None, ALU.mult)
                    k.ts("dve", t16[:], bre[:], sc[:, 16:17], None, ALU.mult)
                    k.tt("dve", bb[:, 1, :], bb[:, 1, :], t16[:], ALU.add)
                    for ri, BT in ((0, BTr), (1, BTi)):
                        k.memset("dve", pad[:], 0.0)
                        for gl in range(2):
                            cg = ((g0 + gl) % 8) * 16
                            k.copy("dve", pad[gl * 64:(gl + 1) * 64, cg:cg + 16], bb[gl * 64:(gl + 1) * 64, ri, :])
                        k.mm(PS[0][:, 0:128], pad[:], ident[:])
                        k.copy("act", BT[j][:], PS[0][:, 0:128])
                    k.memset("dve", Cr[j][:], 0.0)
                    k.memset("dve", nCi[j][:], 0.0)
                    for gl in range(2):
                        g = g0 + gl
                        k.dma("sp", Cr[j][gl * 64:(gl + 1) * 64, g * 16:(g + 1) * 16],
                              I["s5_c_re"][0, d, g].rearrange("n p -> p n"), allow_slow_non_contiguous=True)
                        k.dma("sp", nCi[j][gl * 64:(gl + 1) * 64, g * 16:(g + 1) * 16],
                              I["s5_c_im"][0, d, g].rearrange("n p -> p n"), allow_slow_non_contiguous=True)
                    k.ts("dve", nCi[j][:], nCi[j][:], -1.0, None, ALU.mult)
                    k.ts("dve", tn[:], iot[:], sc[:, 5:6], None, ALU.mult)
                    sin_rr(k, stab[j][:], tn[:], tr[:], tn[:])
                    k.ts("dve", tn[:], iot[:], sc[:, 5:6], math.pi / 2, ALU.mult, ALU.add)
                    sin_rr(k, ctab[j][:], tn[:], tr[:], tn[:])
                    k.memset("dve", rt[j][:], 0.0)
                    k.ts("dve", rt[j][:], rt[j][:], sc[:, 6:7], None, ALU.add)
                k.barrier()
            ut = [k.tile(st, f"s5ut{i}", [128, 256]) for i in range(2)]
            uT = k.tile(st, "s5uT", [128, 2, TC])
            Sre = [k.tile(st, f"Sre{j}", [128, TC]) for j in range(8)]
            Sim = [k.tile(st, f"Sim{j}", [128, TC]) for j in range(8)]
            m1 = k.tile(st, "s5m1", [128, TC])
            m2 = k.tile(st, "s5m2", [128, TC])
            mre = k.tile(st, "s5mre", [128, TC])
            mim = k.tile(st, "s5mim", [128, TC])
            vre = k.tile(st, "s5vre", [128, TC])
            vim = k.tile(st, "s5vim", [128, TC])
            yt = [k.tile(st, f"s5yt{i}", [128, 256]) for i in range(2)]
            y2 = [k.tile(st, f"s5y2{i}", [128, 256]) for i in range(2)]
            revm = antiI if d == 1 else ident

            def row0(m):
                if d == 0:
                    return 128 * m
                return 128 * (1 - m) if m < 2 else 4480 - 128 * m

            it = 0
            for b in range(NB):
                for j in range(8):
                    k.memset("dve", carry[j][:], 0.0)
                for ch in range(TT // TC):
                    for bl in range(2):
                        m = ch * 2 + bl
                        u_ = ut[(it + bl) % 2]
                        k.dma("sp", u_[:], S["UT0"][b, row0(m):row0(m) + 128, :])
                        for hf in range(2):
                            k.mm(PS[0][:, (hf * 2 + bl) * 128:(hf * 2 + bl + 1) * 128], u_[:, hf * 128:(hf + 1) * 128], revm[:])
                    k.copy("act", uT[:], PS[0][:].rearrange("p (h t) -> p h t", h=2))
                    for j in range(8):
                        hf = j // 4
                        pr, pi_ = PS[1 + (j % 2) * 2], PS[2 + (j % 2) * 2]
                        k.mm(pr[:, 0:TC], BTr[j][:], uT[:, hf, :])
                        k.mm(pi_[:, 0:TC], BTi[j][:], uT[:, hf, :])
                        k.tt("dve", m1[:], pr[:, 0:TC], ctab[j][:], ALU.mult)
                        k.tt("dve", m2[:], pi_[:, 0:TC], stab[j][:], ALU.mult)
                        k.tt("pool", mre[:], m1[:], m2[:], ALU.add)
                        k.tt("dve", m1[:], pi_[:, 0:TC], ctab[j][:], ALU.mult)
                        k.tt("dve", m2[:], pr[:, 0:TC], stab[j][:], ALU.mult)
                        k.tt("pool", mim[:], m1[:], m2[:], ALU.subtract)
                        k.scan("dve", vre[:], rt[j][:], mre[:], carry[j][:, 0:1], ALU.mult, ALU.add)
                        k.scan("dve", vim[:], rt[j][:], mim[:], carry[j][:, 1:2], ALU.mult, ALU.add)
                        k.tt("dve", m1[:], vre[:], ctab[j][:], ALU.mult)
                        k.tt("dve", m2[:], vim[:], stab[j][:], ALU.mult)
                        k.tt("pool", Sre[j][:], m1[:], m2[:], ALU.subtract)
                        k.tt("dve", m1[:], vre[:], stab[j][:], ALU.mult)
                        k.tt("dve", m2[:], vim[:], ctab[j][:], ALU.mult)
                        k.tt("pool", Sim[j][:], m1[:], m2[:], ALU.add)
                        k.copy("pool", carry[j][:, 0:1], Sre[j][:, TC - 1:TC])
                        k.copy("pool", carry[j][:, 1:2], Sim[j][:, TC - 1:TC])
                    for bl in range(2):
                        m = ch * 2 + bl
                        py = PS[5 + bl]
                        for j in range(8):
                            k.mm(py[:, 0:256], Sre[j][:, bl * 128:(bl + 1) * 128], Cr[j][:], start=(j == 0), stop=False)
                            k.mm(py[:, 0:256], Sim[j][:, bl * 128:(bl + 1) * 128], nCi[j][:], start=False, stop=(j == 7))
                        y_ = yt[(it + bl) % 2]
                        k.copy("act", y_[:], py[:, 0:256])
                        if d == 1:
                            k.mm(PS[7][:, 0:256], antiI[:], y_[:])
                            y2_ = y2[(it + bl) % 2]
                            k.copy("act", y2_[:], PS[7][:, 0:256])
                            y_ = y2_
                        k.dma("pool", S["YS5"][d, b, row0(m):row0(m) + 128, :], y_[:], wb=[S["YS5"].reg((d, b, m))])
                    it += 1
        k.barrier()


def phase_s5_out(k, I, S, C):
    PS = C["PS"]
    ident = C["ident"]
    with contextlib.ExitStack() as st:
        Db = k.tile(st, "s5D", [128, 256])
        gb = k.tile(st, "s5gb", [128, 512])
        gw = k.tile(st, "s5gw", [128, 2, 512])
        load_bcast(k, "sp", Db[:], I["s5_d"][0:1, :])
        load_bcast(k, "sp", gb[:], I["s5_glu_b"][0:1, :])
        k.dma("sp", gw[:], I["s5_glu_w"][0].rearrange("(h p) n -> p h n", p=128))
        yf = [k.tile(st, f"o5yf{i}", [128, 256]) for i in range(2)]
        yb = [k.tile(st, f"o5yb{i}", [128, 256]) for i in range(2)]
        uu = [k.tile(st, f"o5u{i}", [128, 256]) for i in range(2)]
        y = k.tile(st, "o5y", [128, 256])
        t1 = k.tile(st, "o5t1", [128, 256])
        geT = k.tile(st, "o5geT", [128, 2, 128])
        a = k.tile(st, "o5a", [128, 512])
        o = k.tile(st, "o5o", [128, 256])
        oT = [k.tile(st, f"o5oT{i}", [128, 2, 128]) for i in range(2)]
        it = 0
        for b in range(NB):
            for m in range(TT // 128):
                r0 = m * 128
                i2 = it % 2
                k.dma("sp", yf[i2][:], S["YS5"][0, b, r0:r0 + 128, :])
                k.dma("sp", yb[i2][:], S["YS5"][1, b, r0:r0 + 128, :])
                k.dma("sp", uu[i2][:], S["UT0"][b, r0:r0 + 128, :])
                k.tt("dve", y[:], yf[i2][:], yb[i2][:], ALU.add)
                k.tt("dve", t1[:], uu[i2][:], Db[:], ALU.mult)
                k.tt("dve", y[:], y[:], t1[:], ALU.add)
                k.act(t1[:], y[:], AF.Square)
                k.ts("dve", t1[:], t1[:], 0.044715, 1.0, ALU.mult, ALU.add)
                k.tt("dve", t1[:], t1[:], y[:], ALU.mult)
                k.act(t1[:], t1[:], AF.Sigmoid, scale=2.0 * math.sqrt(2.0 / math.pi))
                k.tt("dve", y[:], y[:], t1[:], ALU.mult)
                for hf in range(2):
                    k.mm(PS[0][:, hf * 128:(hf + 1) * 128], y[:, hf * 128:(hf + 1) * 128], ident[:])
                k.copy("act", geT[:], PS[0][:, 0:256].rearrange("p (h t) -> p h t", h=2))
                for hf in range(2):
                    k.mm(PS[1][:], geT[:, hf, :], gw[:, hf, :], start=(hf == 0), stop=(hf == 1))
                k.tt("dve", a[:], PS[1][:], gb[:], ALU.add)
                k.act(a[:, 256:512], a[:, 256:512], AF.Sigmoid)
                k.tt("dve", o[:], a[:, 0:256], a[:, 256:512], ALU.mult)
                for hf in range(2):
                    k.mm(PS[2][:, hf * 128:(hf + 1) * 128], o[:, hf * 128:(hf + 1) * 128], ident[:])
                k.copy("act", oT[i2][:], PS[2][:, 0:256].rearrange("p (h t) -> p h t", h=2))
                k.dma("pool", S["YT0"][b, 768:1024, r0:r0 + 128].rearrange("(h p) t -> p h t", p=128), oT[i2][:],
                      wb=[S["YT0"].reg(("s5", b, m))])
                it += 1


def phase_moe(k, I, S, C, layer, yT_src, wout, tiles, final):
    PS = C["PS"]
    ident = C["ident"]
    with contextlib.ExitStack() as st:
        Wo = k.tile(st, "Wo", [128, 8, 1024])
        k.dma("sp", Wo[:], wout.rearrange("(k p) n -> p k n", p=128))
        Wr = k.tile(st, "Wr", [128, 8, 36])
        k.dma("sp", Wr[:, :, 0:4], I["moe_wg"][layer].rearrange("(k p) g -> p k g", p=128), allow_slow_non_contiguous=True,
              wb=[Buf("x")])
        k.dma("sp", Wr[:, :, 4:36], I["moe_we"][layer].rearrange("(k p) g -> p k g", p=128), allow_slow_non_contiguous=True,
              wb=[Buf("x")])
        rb = k.tile(st, "rbias", [128, 36])
        k.dma("sp", rb[:, 0:4], I["moe_bg"][layer:layer + 1, :].partition_broadcast(128), wb=[Buf("x")])
        k.dma("sp", rb[:, 4:36], I["moe_be"][layer:layer + 1, :].partition_broadcast(128), wb=[Buf("x")])
        fg = k.tile(st, "fg", [128, 1024])
        if final:
            load_bcast(k, "sp", fg[:], I["final_g"].a.rearrange("(o d) -> o d", o=1))
        k.barrier()
        mvt = [k.tile(st, f"mv{i}", [128, 1024]) for i in range(4)]
        yT = k.tile(st, "yT", [128, 8, 512])
        xa = [k.tile(st, f"xa{i}", [128, 1024]) for i in range(4)]
        acc = [k.tile(st, f"acc{i}", [128, 1024]) for i in range(4)]
        gate = [k.tile(st, f"gate{i}", [128, 32]) for i in range(4)]
        xin = k.tile(st, "xin", [128, 1024])
        h = k.tile(st, "hm", [128, 1024])
        hT = yT
        hTb = k.tile(st, "hTb", [128, 8, 512], BF16)
        sq = xin
        ss = k.tile(st, "ssm", [128, 1])
        rstd = k.tile(st, "rstdm", [128, 1])
        r_ = k.tile(st, "rt", [128, 64])
        lg = k.tile(st, "lg", [128, 36])
        Wg = [k.tile(st, f"Wg{i}", [128, 8, 256]) for i in range(2)]
        Wu = [k.tile(st, f"Wu{i}", [128, 8, 256]) for i in range(2)]
        Wd = [k.tile(st, f"Wd{i}", [128, 2, 1024]) for i in range(2)]
        Wgb = [k.tile(st, f"Wgb{i}", [128, 8, 256], BF16) for i in range(2)]
        Wub = [k.tile(st, f"Wub{i}", [128, 8, 256], BF16) for i in range(2)]
        Wdb = [k.tile(st, f"Wdb{i}", [128, 2, 1024], BF16) for i in range(2)]
        sl_ = [k.tile(st, f"sil{i}", [128, 512]) for i in range(2)]
        hid = [k.tile(st, f"hid{i}", [128, 512], BF16) for i in range(2)]
        cur_row = None
        ecount = 0
        for tl in tiles:
            if tl["row"] != cur_row:
                cur_row = tl["row"]
                for i, comp in enumerate((2, 3, 4, 5)):
                    load_bcast(k, "sp", mvt[i][:], S["MV"][layer, cur_row, comp:comp + 1, :])
            c0 = 0
            for (ap_, n) in tl["ysrc"]:
                if "yh" in tl:
                    k.dma("sp", yT[:, 6:8, c0:c0 + n], ap_.rearrange("(k p) t -> p k t", p=128))
                else:
                    k.dma("sp", yT[:, :, c0:c0 + n], ap_.rearrange("(k p) t -> p k t", p=128))
                c0 += n
            if "yh" in tl:
                for ts in range(4):
                    k.dma("sp", h[:, 0:HY], tl["yh"][ts])
                    for hf in range(2):
                        ps = PS[2 + hf]
                        for j in range(3):
                            kk = hf * 3 + j
                            k.mm(ps[:, j * 128:(j + 1) * 128], h[:, kk * 128:(kk + 1) * 128], ident[:])
                        k.copy("act", yT[:, hf * 3:(hf + 1) * 3, ts * 128:(ts + 1) * 128],
                               ps[:, 0:384].rearrange("p (j t) -> p j t", j=3))
            for ts in range(4):
                k.dma("sp", xin[:], tl["xsrc"][ts])
                for half in range(2):
                    ps = PS[half]
                    for kk in range(8):
                        k.mm(ps[:], yT[:, kk, ts * 128:(ts + 1) * 128], Wo[:, kk, half * 512:(half + 1) * 512],
                             start=(kk == 0), stop=(kk == 7))
                    k.tt("dve", xa[ts][:, half * 512:(half + 1) * 512], ps[:], mvt[0][:, half * 512:(half + 1) * 512], ALU.mult)
                k.tt("dve", xa[ts][:], xa[ts][:], xin[:], ALU.add)
                rms_mod(k, (sq, ss, rstd), xa[ts][:], mvt[2][:], mvt[1][:], h[:], "m")
                for hf in range(2):
                    ps = PS[2 + hf]
                    for j in range(4):
                        kk = hf * 4 + j
                        k.mm(ps[:, j * 128:(j + 1) * 128], h[:, kk * 128:(kk + 1) * 128], ident[:])
                    k.copy("act", hT[:, hf * 4:(hf + 1) * 4, ts * 128:(ts + 1) * 128], ps[:].rearrange("p (j t) -> p j t", j=4))
                    k.copy("dve", hTb[:, hf * 4:(hf + 1) * 4, ts * 128:(ts + 1) * 128], hT[:, hf * 4:(hf + 1) * 4, ts * 128:(ts + 1) * 128])
                ps = PS[4]
                for kk in range(8):
                    k.mm(ps[:, 0:36], hT[:, kk, ts * 128:(ts + 1) * 128], Wr[:, kk, :], start=(kk == 0), stop=(kk == 7))
                k.tt("dve", lg[:], ps[:, 0:36], rb[:], ALU.add)
                g_ = gate[ts]
                k.op("dve", lambda: k.nc.vector.reduce_max(r_[:, 0:1], lg[:, 0:4], AX.X), [r_[:, 0:1]], [lg[:, 0:4]])
                k.ts("dve", r_[:, 1:2], r_[:, 0:1], -1.0, None, ALU.mult)
                k.act(r_[:, 4:8], lg[:, 0:4], AF.Exp, bias=r_[:, 1:2], accum_out=r_[:, 2:3])
                k.recip(r_[:, 3:4], r_[:, 2:3])
                k.ts("dve", r_[:, 8:12], lg[:, 0:4], r_[:, 0:1], None, ALU.is_ge)
                k.ts("dve", r_[:, 16:24], lg[:, 4:12], r_[:, 8:9], None, ALU.mult)
                for g in range(1, 4):
                    k.stt("dve", r_[:, 16:24], lg[:, 4 + 8 * g:12 + 8 * g], r_[:, 8 + g:9 + g], r_[:, 16:24], ALU.mult, ALU.add)
                k.op("dve", lambda: k.nc.vector.reduce_max(r_[:, 12:13], r_[:, 16:24], AX.X), [r_[:, 12:13]], [r_[:, 16:24]])
                k.ts("dve", r_[:, 24:32], r_[:, 16:24], r_[:, 12:13], None, ALU.is_ge)
                k.stt("dve", r_[:, 32:40], r_[:, 24:32], -1e30, r_[:, 16:24], ALU.mult, ALU.add)
                k.op("dve", lambda: k.nc.vector.reduce_max(r_[:, 13:14], r_[:, 32:40], AX.X), [r_[:, 13:14]], [r_[:, 32:40]])
                k.ts("dve", r_[:, 40:48], r_[:, 32:40], r_[:, 13:14], None, ALU.is_ge)
                k.tt("dve", r_[:, 14:15], r_[:, 13:14], r_[:, 12:13], ALU.subtract)
                k.act(r_[:, 14:15], r_[:, 14:15], AF.Exp)
                k.ts("dve", r_[:, 14:15], r_[:, 14:15], 1.0, None, ALU.add)
                k.recip(r_[:, 15:16], r_[:, 14:15])
                k.tt("dve", r_[:, 48:49], r_[:, 15:16], r_[:, 3:4], ALU.mult)
                k.tt("dve", r_[:, 49:50], r_[:, 3:4], r_[:, 48:49], ALU.subtract)
                k.ts("dve", r_[:, 50:58], r_[:, 24:32], r_[:, 48:49], None, ALU.mult)
                k.stt("dve", r_[:, 50:58], r_[:, 40:48], r_[:, 49:50], r_[:, 50:58], ALU.mult, ALU.add)
                for g in range(4):
                    k.ts("dve", g_[:, g * 8:(g + 1) * 8], r_[:, 50:58], r_[:, 8 + g:9 + g], None, ALU.mult)
            for e in range(32):
                gi, ei = e // 8, e % 8
                i2 = ecount % 2
                ecount += 1
                k.dma("sp", Wg[i2][:], I["moe_w_gate"][layer, gi, ei].rearrange("(k p) n -> p k n", p=128))
                k.dma("pool", Wu[i2][:], I["moe_w_up"][layer, gi, ei].rearrange("(k p) n -> p k n", p=128))
                k.dma("sp", Wd[i2][:], I["moe_w_down"][layer, gi, ei].rearrange("(k p) n -> p k n", p=128))
                k.copy("act", Wgb[i2][:], Wg[i2][:])
                k.copy("act", Wub[i2][:], Wu[i2][:])
                k.copy("dve", Wdb[i2][:], Wd[i2][:])
                for hc in range(2):
                    pa, pu = PS[hc * 2], PS[hc * 2 + 1]
                    for kk in range(8):
                        k.mm(pa[:], Wgb[i2][:, kk, hc * 128:(hc + 1) * 128], hTb[:, kk, :], start=(kk == 0), stop=(kk == 7))
                    for kk in range(8):
                        k.mm(pu[:], Wub[i2][:, kk, hc * 128:(hc + 1) * 128], hTb[:, kk, :], start=(kk == 0), stop=(kk == 7))
                    k.act(sl_[hc][:], pa[:], AF.Silu)
                    k.tt("dve", hid[hc][:], sl_[hc][:], pu[:], ALU.mult)
                for ts in range(4):
                    for half in range(2):
                        po = PS[4 + (ts * 2 + half) % 4]
                        for hc in range(2):
                            k.mm(po[:], hid[hc][:, ts * 128:(ts + 1) * 128], Wdb[i2][:, hc, half * 512:(half + 1) * 512],
                                 start=(hc == 0), stop=(hc == 1))
                        eng = "dve" if half == 0 else "pool"
                        asl = acc[ts][:, half * 512:(half + 1) * 512]
                        if e == 0:
                            k.ts("dve", asl, po[:], gate[ts][:, e:e + 1], None, ALU.mult)
                        else:
                            k.stt("dve", asl, po[:], gate[ts][:, e:e + 1], asl, ALU.mult, ALU.add)
            for ts in range(4):
                k.tt("dve", acc[ts][:], acc[ts][:], mvt[3][:], ALU.mult)
                k.tt("dve", acc[ts][:], acc[ts][:], xa[ts][:], ALU.add)
                if final:
                    k.act(sq[:], acc[ts][:], AF.Square, accum_out=ss[:])
                    k.ts("dve", ss[:], ss[:], 1.0 / D, EPS, ALU.mult, ALU.add)
                    k.act(ss[:], ss[:], AF.Sqrt)
                    k.recip(rstd[:], ss[:])
                    k.stt("dve", acc[ts][:], acc[ts][:], rstd[:, 0:1], fg[:], ALU.mult, ALU.mult)
                k.dma("pool", tl["dst"][ts], acc[ts][:], wb=[Buf("x")])


def l0_moe_tiles(I, S):
    tiles = []
    tiles.append(dict(row=2, ysrc=[(S["YT0"][b, :, 0:LC], LC) for b in range(NB)],
                      xsrc=[I["ctx"][b, j * 128:(j + 1) * 128, :] for b in range(NB) for j in range(2)],
                      dst=[S["CTX1"][b, j * 128:(j + 1) * 128, :] for b in range(NB) for j in range(2)]))
    for b in range(NB):
        for i in range(L // 512):
            t0 = i * 512
            td = dict(row=b, ysrc=[(S["YT0"][b, :, LC + t0:LC + t0 + 512], 512)])
            if USE_FFT:
                td = dict(row=b, ysrc=[(S["YT0"][b, 768:1024, LC + t0:LC + t0 + 512], 512)],
                          yh=[S["YH"][b, t0 + j * 128:t0 + (j + 1) * 128, :] for j in range(4)])
            tiles.append(dict(td,
                              xsrc=[I["x"][b, t0 + j * 128:t0 + (j + 1) * 128, :] for j in range(4)],
                              dst=[S["X1"][b, t0 + j * 128:t0 + (j + 1) * 128, :] for j in range(4)]))
    return tiles


def phase_l1_norm(k, I, S, C):
    with contextlib.ExitStack() as st:
        A = k.tile(st, "A1b", [128, 1024])
        sh = k.tile(st, "sh1b", [128, 1024])
        xt = [k.tile(st, f"xtb{i}", [128, 1024]) for i in range(2)]
        h = k.tile(st, "hb", [128, 1024])
        hT = [k.tile(st, f"hTb{i}", [128, 8, 128], BF16) for i in range(2)]
        sq = k.tile(st, "sqb", [128, 1024])
        ss = k.tile(st, "ssb", [128, 1])
        rstd = k.tile(st, "rstdb", [128, 1])
        it = 0
        for b in range(NB):
            for (src, row, ntile, toff) in ((S["CTX1"], 2, LC // 128, 0), (S["X1"], b, L // 128, LC)):
                load_bcast(k, "sp", A[:], S["MV"][1, row, 1:2, :])
                load_bcast(k, "sp", sh[:], S["MV"][1, row, 0:1, :])
                for t in range(ntile):
                    x_ = xt[it % 2]
                    hT_ = hT[it % 2]
                    k.dma("sp", x_[:], src[b, t * 128:(t + 1) * 128, :])
                    rms_mod(k, (sq, ss, rstd), x_[:], A[:], sh[:], h[:], "l1")
                    transpose_1024(k, C, h, hT_)
                    t0 = toff + t * 128
                    k.dma("pool", S["HT1"][b].rearrange("(j p) t -> p j t", p=128)[:, :, t0:t0 + 128], hT_[:],
                          wb=[S["HT1"].reg((b, t0))])
                    it += 1


def phase_l1_inproj(k, I, S, C):
    PS = C["PS"]
    with contextlib.ExitStack() as st:
        W = k.tile(st, "W1g", [128, 8, 1024], BF16)
        wst = [k.tile(st, f"W1st{i}", [128, 1024]) for i in range(2)]
        hT = [k.tile(st, f"hT5{i}", [128, 8, 512], BF16) for i in range(2)]
        of = [k.tile(st, f"of{i}", [128, 8, 512]) for i in range(2)]
        ot = [k.tile(st, f"ot{i}", [128, 4, 1024]) for i in range(2)]
        wv = I["c_w_in"][0].rearrange("(k p) n -> p k n", p=128)
        it = 0
        for cg in range(5):
            k.barrier()
            for kk in range(8):
                k.dma("sp" if kk % 2 == 0 else "pool", wst[kk % 2][:], wv[:, kk, cg * 1024:(cg + 1) * 1024])
                k.copy("act" if kk % 2 == 0 else "dve", W[:, kk, :], wst[kk % 2][:])
            k.barrier()
            for b in range(NB):
                segs = [(LC + i * 512, 512) for i in range(L // 512)]
                if cg >= 2:
                    segs = [(0, LC)] + segs
                for (t0, n) in segs:
                    h_ = hT[it % 2]
                    k.dma("sp", h_[:, :, 0:n], S["HT1"][b].rearrange("(j p) t -> p j t", p=128)[:, :, t0:t0 + n])
                    if cg in (0, 3, 4):
                        o_ = of[it % 2]
                        for j in range(8):
                            ps = PS[j % 4]
                            for kk in range(8):
                                k.mm(ps[:, 0:n], W[:, kk, j * 128:(j + 1) * 128], h_[:, kk, 0:n], start=(kk == 0), stop=(kk == 7))
                            k.copy("act" if j % 2 == 0 else "dve", o_[:, j, 0:n], ps[:, 0:n])
                        if cg == 0:
                            dst = S["QT1"][b].rearrange("(j p) t -> p j t", p=128)[:, :, t0 - LC:t0 - LC + n]
                        else:
                            dst = S["ZF1" if cg == 3 else "ZB1"][b].rearrange("(j p) t -> p j t", p=128)[:, :, t0:t0 + n]
                        k.dma("pool", dst, o_[:, :, 0:n], wb=[Buf("x")])
                    else:
                        o_ = ot[it % 2]
                        for ts in range(n // 128):
                            for half in range(2):
                                ps = PS[4 + (ts * 2 + half) % 4]
                                for kk in range(8):
                                    k.mm(ps[:], h_[:, kk, ts * 128:(ts + 1) * 128], W[:, kk, half * 512:(half + 1) * 512],
                                         start=(kk == 0), stop=(kk == 7))
                                k.copy("act" if half == 0 else "dve", o_[:, ts, half * 512:(half + 1) * 512], ps[:])
                        if cg == 1:
                            dst = S["G1"][b, t0 - LC:t0 - LC + n, :].rearrange("(s p) d -> p s d", p=128)
                        else:
                            dst = S["V1"][b, t0:t0 + n, :].rearrange("(s p) d -> p s d", p=128)
                        k.dma("pool", dst, o_[:, 0:n // 128, :], wb=[Buf("x")])
                    it += 1
        k.barrier()


def phase_hgrn2(k, I, S, C):
    PS = C["PS"]
    ident = C["ident"]
    CH = 64
    with contextlib.ExitStack() as st:
        cmask = k.tile(st, "cmask", [128, 512])
        k.dma("sp", cmask[:], I["cmask"][:, :])
        tri = [k.tile(st, f"tri{d}", [64, 64]) for d in range(2)]
        k.dma("sp", tri[0][:], I["triu"][:, :])
        k.dma("sp", tri[1][:], I["tril"][:, :])
        lbt = k.tile(st, "lbt", [128, 8])
        names = ["z", "f", "lf", "kk", "bc", "bb", "eb", "qt", "kt", "kh", "tmp"]
        T_ = [{n: k.tile(st, f"g{d}{n}", [128, 512]) for n in names} for d in range(2)]
        ebe = [k.tile(st, f"ebe{d}", [128, 8]) for d in range(2)]
        vt = [k.tile(st, f"vt{d}", [64, 8, 128]) for d in range(2)]
        kht = [k.tile(st, f"kht{d}", [64, 8, 128]) for d in range(2)]
        ot = [k.tile(st, f"oo{d}", [64, 8, 128]) for d in range(2)]
        am = [k.tile(st, f"am{d}", [64, 64]) for d in range(2)]
        Sst = [k.tile(st, f"Sst{d}", [128, 128]) for d in range(2)]
        for b in range(NB):
            for hh in range(8):
                hs = slice(hh * 128, (hh + 1) * 128)
                for d in range(2):
                    for l_ in range(2):
                        k.dma("sp", lbt[:, d * 4 + l_:d * 4 + l_ + 1],
                              I["c_lower_bounds"][d, l_, hs].rearrange("(p o) -> p o", o=1))
                    k.tt("dve", lbt[:, d * 4 + 2:d * 4 + 3], lbt[:, d * 4:d * 4 + 1], lbt[:, d * 4 + 1:d * 4 + 2], ALU.subtract)
                    k.act(lbt[:, d * 4 + 2:d * 4 + 3], lbt[:, d * 4 + 2:d * 4 + 3], AF.Sigmoid)
                    k.ts("dve", lbt[:, d * 4 + 3:d * 4 + 4], lbt[:, d * 4 + 2:d * 4 + 3], -1.0, 1.0, ALU.mult, ALU.add)
                    k.memset("dve", Sst[d][:], 0.0)
                segs = {0: [(0, LC, False)] + [(LC + i * 512, 512, True) for i in range(L // 512)],
                        1: [(0, LC, False)] + [(LC + i * 512, 512, True) for i in reversed(range(L // 512))]}
                for si in range(len(segs[0])):
                    for d in range(2):
                        t0, n, is_lat = segs[d][si]
                        nchunk = n // CH
                        Td = T_[d]
                        zsrc = S["ZF1" if d == 0 else "ZB1"]
                        k.dma("sp", Td["z"][:, 0:n], zsrc[b, hs, t0:t0 + n])
                        k.dma("sp", vt[d][:, 0:nchunk, :], S["V1"][b, t0:t0 + n, hs].rearrange("(c s) e -> s c e", s=CH))
                        k.act(Td["f"][:, 0:n], Td["z"][:, 0:n], AF.Sigmoid)
                        k.ts("dve", Td["f"][:, 0:n], Td["f"][:, 0:n], lbt[:, d * 4 + 3:d * 4 + 4], lbt[:, d * 4 + 2:d * 4 + 3],
                             ALU.mult, ALU.add)
                        k.act(Td["lf"][:, 0:n], Td["f"][:, 0:n], AF.Ln)
                        k.ts("dve", Td["kk"][:, 0:n], Td["f"][:, 0:n], -1.0, 1.0, ALU.mult, ALU.add)
                        k.scan("dve", Td["bc"][:, 0:n], cmask[:, 0:n], Td["lf"][:, 0:n], 0.0, ALU.mult, ALU.add)
                        bc3 = Td["bc"][:, 0:n].rearrange("p (c s) -> p c s", s=CH)
                        bend = bc3[:, :, CH - 1:CH]
                        if d == 0:
                            bbv = Td["bc"]
                        else:
                            k.tt("dve", Td["bb"][:, 0:n].rearrange("p (c s) -> p c s", s=CH), bend.to_broadcast([128, nchunk, CH]),
                                 bc3, ALU.subtract)
                            k.tt("dve", Td["bb"][:, 0:n], Td["bb"][:, 0:n], Td["lf"][:, 0:n], ALU.add)
                            bbv = Td["bb"]
                        k.act(ebe[d][:, 0:nchunk], bend.rearrange("p c o -> p (c o)"), AF.Exp)
                        k.act(Td["tmp"][:, 0:n], bbv[:, 0:n], AF.Exp, scale=-1.0)
                        k.tt("dve", Td["kt"][:, 0:n], Td["kk"][:, 0:n], Td["tmp"][:, 0:n], ALU.mult)
                        k.tt("dve", Td["kh"][:, 0:n].rearrange("p (c s) -> p c s", s=CH),
                             Td["kt"][:, 0:n].rearrange("p (c s) -> p c s", s=CH),
                             ebe[d][:, 0:nchunk].unsqueeze(2).to_broadcast([128, nchunk, CH]), ALU.mult)
                        if is_lat:
                            k.dma("sp", Td["z"][:, 0:n], S["QT1"][b, hs, t0 - LC:t0 - LC + n])
                            k.act(Td["eb"][:, 0:n], bbv[:, 0:n], AF.Exp)
                            k.act(Td["qt"][:, 0:n], Td["z"][:, 0:n], AF.Silu)
                            k.tt("dve", Td["qt"][:, 0:n], Td["qt"][:, 0:n], Td["eb"][:, 0:n], ALU.mult)
                        for half in range((nchunk + 3) // 4):
                            ps = PS[d * 4 + 3]
                            nn = min(4, nchunk - half * 4)
                            for c in range(nn):
                                cc = half * 4 + c
                                k.mm(ps[0:64, c * 128:(c + 1) * 128], Td["kh"][:, cc * CH:(cc + 1) * CH], ident[:])
                            k.copy("act", kht[d][:, half * 4:half * 4 + nn, :],
                                   ps[0:64, 0:nn * 128].rearrange("p (c e) -> p c e", e=128))
                    nchunk = segs[0][si][1] // CH
                    is_lat = segs[0][si][2]
                    for ci in range(nchunk):
                        for d in range(2):
                            Td = T_[d]
                            c = ci if d == 0 else nchunk - 1 - ci
                            cs = slice(c * CH, (c + 1) * CH)
                            if is_lat:
                                pa = PS[d * 4 + 0]
                                k.mm(pa[0:64, 0:64], Td["kt"][:, cs], Td["qt"][:, cs])
                                k.tt("dve", am[d][:], pa[0:64, 0:64], tri[d][:], ALU.mult)
                                po = PS[d * 4 + 1]
                                k.mm(po[0:64, 0:128], am[d][:], vt[d][:, c, :], start=True, stop=False)
                                k.mm(po[0:64, 0:128], Td["qt"][:, cs], Sst[d][:], start=False, stop=True)
                                k.copy("act", ot[d][:, c, :], po[0:64, 0:128])
                            pd = PS[d * 4 + 2]
                            k.mm(pd[:, 0:128], kht[d][:, c, :], vt[d][:, c, :])
                            k.stt("dve", Sst[d][:], Sst[d][:], ebe[d][:, c:c + 1], pd[:, 0:128], ALU.mult, ALU.add)
                    if is_lat:
                        for d in range(2):
                            t0, n, _ = segs[d][si]
                            k.dma("pool", S["O1"][d, b, t0 - LC:t0 - LC + n, hs].rearrange("(c s) e -> s c e", s=CH),
                                  ot[d][:, 0:nchunk, :], wb=[Buf("x")])
                k.barrier()


def phase_hgrn2_out(k, I, S, C):
    PS = C["PS"]
    ident = C["ident"]
    with contextlib.ExitStack() as st:
        ng = k.tile(st, "ng", [128, 1024])
        load_bcast(k, "sp", ng[:], I["c_norm_g"][0:1, :])
        of_ = [k.tile(st, f"ro_f{i}", [128, 1024]) for i in range(2)]
        ob_ = [k.tile(st, f"ro_b{i}", [128, 1024]) for i in range(2)]
        gg = [k.tile(st, f"ro_g{i}", [128, 1024]) for i in range(2)]
        o = k.tile(st, "ro_o", [128, 1024])
        sq = k.tile(st, "ro_sq", [128, 1024])
        ss = k.tile(st, "ro_ss", [128, 8])
        oT = [k.tile(st, f"ro_oT{i}", [128, 8, 128]) for i in range(2)]
        it = 0
        for b in range(NB):
            for t in range(L // 128):
                i2 = it % 2
                r0 = t * 128
                k.dma("sp", of_[i2][:], S["O1"][0, b, r0:r0 + 128, :])
                k.dma("sp", ob_[i2][:], S["O1"][1, b, r0:r0 + 128, :])
                k.dma("sp", gg[i2][:], S["G1"][b, r0:r0 + 128, :])
                k.tt("dve", o[:], of_[i2][:], ob_[i2][:], ALU.add)
                k.act(sq[:], o[:], AF.Square)
                k.op("dve", lambda: k.nc.vector.reduce_sum(ss[:], sq[:].rearrange("p (h e) -> p h e", e=128), AX.X),
                     [ss[:]], [sq[:]])
                k.ts("dve", ss[:], ss[:], 1.0 / 128, EPS, ALU.mult, ALU.add)
                k.act(ss[:], ss[:], AF.Sqrt)
                k.recip(ss[:], ss[:])
                k.tt("dve", o[:].rearrange("p (h e) -> p h e", e=128), o[:].rearrange("p (h e) -> p h e", e=128),
                     ss[:].unsqueeze(2).to_broadcast([128, 8, 128]), ALU.mult)
                k.tt("dve", o[:], o[:], ng[:], ALU.mult)
                k.act(gg[i2][:], gg[i2][:], AF.Sigmoid)
                k.tt("dve", o[:], o[:], gg[i2][:], ALU.mult)
                transpose_1024(k, C, o, oT[i2])
                k.dma("pool", S["OT1"][b].rearrange("(j p) t -> p j t", p=128)[:, :, r0:r0 + 128], oT[i2][:], wb=[Buf("x")])
                it += 1


def l1_moe_tiles(I, S, OUT):
    tiles = []
    for b in range(NB):
        for i in range(L // 512):
            t0 = i * 512
            tiles.append(dict(row=b, ysrc=[(S["OT1"][b, :, t0:t0 + 512], 512)],
                              xsrc=[S["X1"][b, t0 + j * 128:t0 + (j + 1) * 128, :] for j in range(4)],
                              dst=[OUT[b, t0 + j * 128:t0 + (j + 1) * 128, :] for j in range(4)]))
    return tiles


_CACHE = {}


def kernel(**inputs):
    x = np.ascontiguousarray(inputs["x"], dtype=np.float32)
    c = np.asarray(inputs["c"], dtype=np.float32)
    ctx = np.ascontiguousarray(inputs["ctx"], dtype=np.float32)
    c_ctx = np.asarray(inputs["c_ctx"], dtype=np.float32)
    if "nc" not in _CACHE:
        _CACHE["nc"] = build()
    nc = _CACHE["nc"]
    consts = host_consts()
    shared = {n: np.ascontiguousarray(inputs[n], dtype=np.float32) for n in WEIGHT_SHAPES}
    shared.update(consts)
    in_maps = []
    for i in range(NCORES):
        m = dict(shared)
        m["x"] = x[i * NB:(i + 1) * NB]
        m["ctx"] = ctx[i * NB:(i + 1) * NB]
        m["cvec"] = np.concatenate([c[i * NB:(i + 1) * NB], c_ctx[None, :]], axis=0)
        in_maps.append(m)
    res = run_bass_kernel_spmd(nc, in_maps, core_ids=list(range(NCORES)))
    return np.concatenate([r["out"] for r in res.results], axis=0)
```
